# Optimizing a Trainium2 kernel written in Bass

```python
import math
import jax, jax.numpy as jnp
from jax import lax
import numpy as np

D_MODEL = 2048
BATCH = 4
SEQ = 8192
DEPTH = 1
DEC_BATCH = 8
DEC_SEQ = 4096
PAST_LEN = 128

MIX_WIDTH = D_MODEL
SSD_WIDTH = MIX_WIDTH // 2
S5_WIDTH = MIX_WIDTH - SSD_WIDTH
SSD_HEAD_DIM = 64
SSD_HEADS = SSD_WIDTH // SSD_HEAD_DIM
SSD_GROUPS = 4
SSD_STATE = 128
SSD_CONV = 3
SSD_CHUNK = 128
XBC_WIDTH = SSD_WIDTH + 2 * SSD_GROUPS * SSD_STATE
S5_GROUP_CH = 16
S5_GROUPS = S5_WIDTH // S5_GROUP_CH
S5_STATE = 64
IN_COLS = SSD_WIDTH + XBC_WIDTH + SSD_HEADS + S5_WIDTH
MEM_LEN = 256
MEM_HEADS = 4
MEM_HEAD_DIM = D_MODEL // MEM_HEADS
N_EXPERTS = 16
EXPERT_FF = D_MODEL
CAPACITY_FACTOR = 2
EPS = 1e-6

kernel_name = 'hybrid_ssd_s5_ec_encoder'


def _rmsnorm(x, g):
    xf = x.astype(jnp.float32)
    y = xf * lax.rsqrt(jnp.mean(xf * xf, axis=-1, keepdims=True) + EPS) * g.astype(jnp.float32)
    return y.astype(x.dtype)


def _depthwise_conv(x, w):
    return lax.conv_general_dilated(x, w[:, None, :].astype(x.dtype), (1,), 'SAME',
                                    dimension_numbers=('NWC', 'WIO', 'NWC'),
                                    feature_group_count=x.shape[-1])


def _ssd_chunked(x, dt, a, bm, cm):
    b, l, h, p = x.shape
    g, n = bm.shape[-2:]
    r = h // g
    q = SSD_CHUNK
    c = l // q
    xdt = (x * dt[..., None]).reshape(b, c, q, g, r, p)
    a_cs = jnp.cumsum((dt * a).reshape(b, c, q, g, r).transpose(0, 1, 3, 4, 2), axis=-1)
    lower = jnp.tril(jnp.ones((q, q), dtype=bool))
    seg = jnp.exp(jnp.where(lower, a_cs[..., :, None] - a_cs[..., None, :], -jnp.inf))
    bc = bm.reshape(b, c, q, g, n)
    cc = cm.reshape(b, c, q, g, n)
    cb = jnp.einsum('bclgn,bcsgn->bcgls', cc, bc)
    y_diag = jnp.einsum('bcgrls,bcsgrp->bclgrp', cb[:, :, :, None] * seg, xdt)
    decay_in = jnp.exp(a_cs[..., -1:] - a_cs).transpose(0, 1, 4, 2, 3)[..., None]
    chunk_states = jnp.einsum('bclgn,bclgrp->bcgrpn', bc, xdt * decay_in)
    chunk_decay = jnp.exp(a_cs[..., -1])

    def step(state, inp):
        dec, st = inp
        return state * dec[..., None, None] + st, state

    init = jnp.zeros((b, g, r, p, n), x.dtype)
    _, prev = lax.scan(step, init, (jnp.moveaxis(chunk_decay, 1, 0), jnp.moveaxis(chunk_states, 1, 0)))
    prev = jnp.moveaxis(prev, 0, 1)
    decay_out = jnp.exp(a_cs).transpose(0, 1, 4, 2, 3)[..., None]
    y_off = jnp.einsum('bclgn,bcgrpn->bclgrp', cc, prev) * decay_out
    return (y_diag + y_off).reshape(b, l, h, p)


def _ssd_mixer(z, xbc, dt_raw, conv_w, conv_b, dt_bias, a_log, d_skip, norm_g):
    b, l, _ = xbc.shape
    xbc = jax.nn.silu(_depthwise_conv(xbc, conv_w) + conv_b.astype(xbc.dtype))
    xs, bm, cm = jnp.split(xbc, [SSD_WIDTH, SSD_WIDTH + SSD_GROUPS * SSD_STATE], axis=-1)
    x32 = xs.reshape(b, l, SSD_HEADS, SSD_HEAD_DIM).astype(jnp.float32)
    bm = bm.reshape(b, l, SSD_GROUPS, SSD_STATE).astype(jnp.float32)
    cm = cm.reshape(b, l, SSD_GROUPS, SSD_STATE).astype(jnp.float32)
    dt32 = dt_raw.astype(jnp.float32)
    dt_f = jax.nn.softplus(dt32 + dt_bias[0].astype(jnp.float32))
    a_f = -jnp.exp(a_log[0].astype(jnp.float32))
    y = _ssd_chunked(x32, dt_f, a_f, bm, cm)
    dt_b = jax.nn.softplus(dt32 + dt_bias[1].astype(jnp.float32))
    a_b = -jnp.exp(a_log[1].astype(jnp.float32))
    flip = lambda t: jnp.flip(t, axis=1)
    y = y + flip(_ssd_chunked(flip(x32), flip(dt_b), a_b, flip(bm), flip(cm)))
    y = y + d_skip.astype(jnp.float32)[:, None] * x32
    y = y.reshape(b, l, SSD_WIDTH) * jax.nn.silu(z.astype(jnp.float32))
    yg = y.reshape(b, l, SSD_GROUPS, SSD_WIDTH // SSD_GROUPS)
    yg = yg * lax.rsqrt(jnp.mean(yg * yg, axis=-1, keepdims=True) + EPS)
    y = yg.reshape(b, l, SSD_WIDTH) * norm_g.astype(jnp.float32)
    return y.astype(z.dtype)


def _linear_combine(e1, e2):
    a1, b1 = e1
    a2, b2 = e2
    return a2 * a1, a2 * b1 + b2


def _s5_mixer(u, a_re, a_im, log_step, b_re, b_im, c_re, c_im, d_skip, w_glu, norm_g):
    b, l, _ = u.shape
    u32 = u.astype(jnp.float32)
    ug = u32.reshape(b, l, S5_GROUPS, S5_GROUP_CH)
    f32 = jnp.float32
    lam = lax.complex(a_re.astype(f32), a_im.astype(f32))
    delta = jnp.exp(log_step.astype(f32))[..., None]
    abar = jnp.exp(lam * delta)
    bbar = ((abar - 1.0) / lam)[..., None] * lax.complex(b_re.astype(f32), b_im.astype(f32))
    cmat = lax.complex(c_re.astype(f32), c_im.astype(f32))

    def one_seq(us):
        uc = us.astype(jnp.complex64)
        y = jnp.zeros(us.shape, f32)
        for direction in range(2):
            bu = jnp.einsum('gpc,lgc->lgp', bbar[direction], uc)
            a_seq = jnp.broadcast_to(abar[direction], bu.shape)
            _, states = lax.associative_scan(_linear_combine, (a_seq, bu), reverse=(direction == 1))
            y = y + jnp.real(jnp.einsum('gcp,lgp->lgc', cmat[direction], states))
        return y

    y = lax.map(one_seq, ug).reshape(b, l, S5_WIDTH) + d_skip.astype(f32) * u32
    y = jax.nn.gelu(y)
    y = y * jax.nn.sigmoid(y @ w_glu.astype(f32))
    y = _rmsnorm(y, norm_g)
    return y.astype(u.dtype)


def _mem_attention(hn, mem_n, w_q, w_k, w_v, w_o):
    b, l, _ = hn.shape
    m = mem_n.shape[1]
    q = (hn @ w_q).reshape(b, l, MEM_HEADS, MEM_HEAD_DIM)
    k = (mem_n @ w_k).reshape(b, m, MEM_HEADS, MEM_HEAD_DIM)
    v = (mem_n @ w_v).reshape(b, m, MEM_HEADS, MEM_HEAD_DIM)
    s = jnp.einsum('blhd,bmhd->bhlm', q, k).astype(jnp.float32) * (MEM_HEAD_DIM ** -0.5)
    pr = jax.nn.softmax(s, axis=-1).astype(v.dtype)
    o = jnp.einsum('bhlm,bmhd->blhd', pr, v).reshape(b, l, D_MODEL)
    return o @ w_o


def _expert_choice_moe(hn, w_router, w_gate, w_up, w_down):
    b, l, d = hn.shape
    n_tok = b * l
    cap = CAPACITY_FACTOR * n_tok // N_EXPERTS
    hf = hn.reshape(n_tok, d)
    probs = jax.nn.softmax((hf @ w_router).astype(jnp.float32), axis=-1)
    gates, idx = lax.top_k(probs.T, cap)

    def expert(args):
        wg, wu, wd, ix, gt = args
        xe = hf[ix]
        he = jax.nn.silu(xe @ wg) * (xe @ wu)
        return ((he @ wd) * gt[:, None]).astype(hf.dtype)

    ye = lax.map(expert, (w_gate, w_up, w_down, idx, gates))
    out = jnp.zeros_like(hf).at[idx.reshape(-1)].add(ye.reshape(-1, d))
    return out.reshape(b, l, d)


def _trunk(x, mem, w):
    for i in range(DEPTH):
        h = _rmsnorm(x, w['norm_mix'][i])
        proj = h @ w['w_in'][i]
        z, xbc, dt_raw, u = jnp.split(
            proj, [SSD_WIDTH, SSD_WIDTH + XBC_WIDTH, SSD_WIDTH + XBC_WIDTH + SSD_HEADS], axis=-1)
        y_ssd = _ssd_mixer(z, xbc, dt_raw, w['conv_w'][i], w['conv_b'][i], w['ssd_dt_bias'][i],
                           w['ssd_a_log'][i], w['ssd_d'][i], w['ssd_norm'][i])
        y_s5 = _s5_mixer(u, w['s5_a_re'][i], w['s5_a_im'][i], w['s5_log_step'][i], w['s5_b_re'][i],
                         w['s5_b_im'][i], w['s5_c_re'][i], w['s5_c_im'][i], w['s5_d'][i],
                         w['s5_w_glu'][i], w['s5_norm'][i])
        x = x + jnp.concatenate([y_ssd, y_s5], axis=-1) @ w['w_out'][i]
        x = x + _mem_attention(_rmsnorm(x, w['norm_attn'][i]), _rmsnorm(mem, w['norm_mem'][i]),
                               w['w_q'][i], w['w_k'][i], w['w_v'][i], w['w_o'][i])
        x = x + _expert_choice_moe(_rmsnorm(x, w['norm_ffn'][i]), w['w_router'][i],
                                   w['w_gate'][i], w['w_up'][i], w['w_down'][i])
    return _rmsnorm(x, w['norm_final'])


def setup_inputs(seed: int = 0) -> dict:
    key = jax.random.key(seed)
    ks = jax.random.split(key, 40)
    f32 = jnp.float32

    def nrm(k, shape, scale):
        return jax.random.normal(k, shape, f32) * scale

    def gain(k, shape):
        return 1.0 + 0.02 * jax.random.normal(k, shape, f32)

    dt0 = jnp.exp(jax.random.uniform(ks[8], (DEPTH, 2, SSD_HEADS), f32, math.log(1e-3), math.log(1e-1)))
    n_idx = jnp.arange(S5_STATE, dtype=f32)
    return {
        'x_prompt': nrm(ks[0], (BATCH, SEQ, D_MODEL), 1.0),
        'x_sample': nrm(ks[1], (DEC_BATCH, DEC_SEQ, D_MODEL), 1.0),
        'mem_prompt': nrm(ks[2], (BATCH, MEM_LEN, D_MODEL), 1.0),
        'mem_sample': nrm(ks[3], (DEC_BATCH, MEM_LEN, D_MODEL), 1.0),
        'norm_mix': gain(ks[4], (DEPTH, D_MODEL)),
        'w_in': nrm(ks[5], (DEPTH, D_MODEL, IN_COLS), D_MODEL ** -0.5),
        'conv_w': nrm(ks[6], (DEPTH, SSD_CONV, XBC_WIDTH), SSD_CONV ** -0.5),
        'conv_b': nrm(ks[7], (DEPTH, XBC_WIDTH), 0.01),
        'ssd_dt_bias': dt0 + jnp.log(-jnp.expm1(-dt0)),
        'ssd_a_log': jnp.log(jax.random.uniform(ks[9], (DEPTH, 2, SSD_HEADS), f32, 1.0, 16.0)),
        'ssd_d': gain(ks[10], (DEPTH, SSD_HEADS)),
        'ssd_norm': gain(ks[11], (DEPTH, SSD_WIDTH)),
        's5_a_re': -0.5 + 0.01 * jax.random.normal(ks[12], (DEPTH, 2, S5_GROUPS, S5_STATE), f32),
        's5_a_im': math.pi * n_idx + 0.01 * jax.random.normal(ks[13], (DEPTH, 2, S5_GROUPS, S5_STATE), f32),
        's5_log_step': jax.random.uniform(ks[14], (DEPTH, 2, S5_GROUPS), f32, math.log(1e-3), math.log(1e-1)),
        's5_b_re': nrm(ks[15], (DEPTH, 2, S5_GROUPS, S5_STATE, S5_GROUP_CH), (2 * S5_GROUP_CH) ** -0.5),
        's5_b_im': nrm(ks[16], (DEPTH, 2, S5_GROUPS, S5_STATE, S5_GROUP_CH), (2 * S5_GROUP_CH) ** -0.5),
        's5_c_re': nrm(ks[17], (DEPTH, 2, S5_GROUPS, S5_GROUP_CH, S5_STATE), (2 * S5_STATE) ** -0.5),
        's5_c_im': nrm(ks[18], (DEPTH, 2, S5_GROUPS, S5_GROUP_CH, S5_STATE), (2 * S5_STATE) ** -0.5),
        's5_d': nrm(ks[19], (DEPTH, S5_WIDTH), 1.0),
        's5_w_glu': nrm(ks[20], (DEPTH, S5_WIDTH, S5_WIDTH), S5_WIDTH ** -0.5),
        's5_norm': gain(ks[21], (DEPTH, S5_WIDTH)),
        'w_out': nrm(ks[22], (DEPTH, MIX_WIDTH, D_MODEL), MIX_WIDTH ** -0.5),
        'norm_attn': gain(ks[23], (DEPTH, D_MODEL)),
        'norm_mem': gain(ks[24], (DEPTH, D_MODEL)),
        'w_q': nrm(ks[25], (DEPTH, D_MODEL, D_MODEL), D_MODEL ** -0.5),
        'w_k': nrm(ks[26], (DEPTH, D_MODEL, D_MODEL), D_MODEL ** -0.5),
        'w_v': nrm(ks[27], (DEPTH, D_MODEL, D_MODEL), D_MODEL ** -0.5),
        'w_o': nrm(ks[28], (DEPTH, D_MODEL, D_MODEL), D_MODEL ** -0.5),
        'norm_ffn': gain(ks[29], (DEPTH, D_MODEL)),
        'w_router': nrm(ks[30], (DEPTH, D_MODEL, N_EXPERTS), D_MODEL ** -0.5),
        'w_gate': nrm(ks[31], (DEPTH, N_EXPERTS, D_MODEL, EXPERT_FF), D_MODEL ** -0.5),
        'w_up': nrm(ks[32], (DEPTH, N_EXPERTS, D_MODEL, EXPERT_FF), D_MODEL ** -0.5),
        'w_down': nrm(ks[33], (DEPTH, N_EXPERTS, EXPERT_FF, D_MODEL), EXPERT_FF ** -0.5),
        'norm_final': gain(ks[34], (D_MODEL,)),
    }


def reference(x_prompt, x_sample, mem_prompt, mem_sample, norm_mix, w_in, conv_w, conv_b,
              ssd_dt_bias, ssd_a_log, ssd_d, ssd_norm, s5_a_re, s5_a_im, s5_log_step, s5_b_re,
              s5_b_im, s5_c_re, s5_c_im, s5_d, s5_w_glu, s5_norm, w_out, norm_attn, norm_mem,
              w_q, w_k, w_v, w_o, norm_ffn, w_router, w_gate, w_up, w_down, norm_final):
    weights = dict(norm_mix=norm_mix, w_in=w_in, conv_w=conv_w, conv_b=conv_b,
                   ssd_dt_bias=ssd_dt_bias, ssd_a_log=ssd_a_log, ssd_d=ssd_d, ssd_norm=ssd_norm,
                   s5_a_re=s5_a_re, s5_a_im=s5_a_im, s5_log_step=s5_log_step, s5_b_re=s5_b_re,
                   s5_b_im=s5_b_im, s5_c_re=s5_c_re, s5_c_im=s5_c_im, s5_d=s5_d, s5_w_glu=s5_w_glu,
                   s5_norm=s5_norm, w_out=w_out, norm_attn=norm_attn, norm_mem=norm_mem,
                   w_q=w_q, w_k=w_k, w_v=w_v, w_o=w_o, norm_ffn=norm_ffn, w_router=w_router,
                   w_gate=w_gate, w_up=w_up, w_down=w_down, norm_final=norm_final)
    y_prompt = _trunk(x_prompt, mem_prompt, weights)
    y_sample = _trunk(x_sample, mem_sample, weights)
    return (y_prompt, y_sample)
```

```python
import numpy as np
from contextlib import ExitStack
import concourse.bass as bass
import concourse.mybir as mybir
from concourse.bass_utils import run_bass_kernel_spmd

F32 = mybir.dt.float32
BF16 = mybir.dt.bfloat16
I32 = mybir.dt.int32
U32 = mybir.dt.uint32
ALU = mybir.AluOpType
AF = mybir.ActivationFunctionType
AX = mybir.AxisListType

ENGS = ("pe", "act", "dve", "pool", "sp")
NDS = 24

D = 2048
EPS = 1e-6
INC = 4112


class Op:
    __slots__ = ("eng", "fn", "reads", "writes", "dma", "sem", "val", "waits", "desc", "inc")

    def __init__(self, eng, fn, reads, writes, dma):
        self.eng = eng
        self.fn = fn
        self.reads = tuple(reads)
        self.writes = tuple(writes)
        self.dma = dma
        self.waits = {}


class Prog:
    def __init__(self, nc, stack):
        self.nc = nc
        self.ops = []
        self.last_writer = {}
        self.readers = {}
        self.cnt = {e: 0 for e in ENGS}
        self.dcnt = {e: 0 for e in ENGS}
        self.sem = {e: stack.enter_context(nc.semaphore("s_" + e)) for e in ENGS if e != "sp"}
        self.dsem = {
            e: [stack.enter_context(nc.semaphore("d_%s_%d" % (e, i))) for i in range(NDS)]
            for e in ("sp", "act", "pool")
        }
        self.waited = {e: {} for e in ENGS}
        self.semobj = {}
        self.limit = None
        self.bank_last = {}
        self.csem = stack.enter_context(nc.semaphore("s_coll"))

    def add(self, eng, fn, reads=(), writes=(), dma=False, banks=(), own_sem=None):
        if self.limit is not None and len(self.ops) >= self.limit:
            return None
        op = Op(eng, fn, reads, writes, dma)
        deps = set()
        for b in op.reads:
            w = self.last_writer.get(b)
            if w is not None:
                deps.add(w)
        for b in op.writes:
            w = self.last_writer.get(b)
            if w is not None:
                deps.add(w)
            for r in self.readers.get(b, ()):
                if r.eng == eng and not r.dma and not dma:
                    continue
                deps.add(r)
        for tok in banks:
            pd = self.bank_last.get(tok)
            if pd is not None and pd.eng != eng:
                deps.add(pd)
            self.bank_last[tok] = op
        for b in op.reads:
            self.readers.setdefault(b, []).append(op)
        for b in op.writes:
            self.last_writer[b] = op
            self.readers[b] = []
        prewait = None
        op.inc = 16 if dma else 1
        if own_sem is not None:
            op.sem = own_sem
            op.val = 1
            op.inc = 1
        elif dma:
            i = self.dcnt[eng]
            self.dcnt[eng] += 1
            op.sem = self.dsem[eng][i % NDS]
            op.val = 16 * (i // NDS + 1)
            if i >= NDS:
                prewait = (op.sem, 16 * (i // NDS))
        else:
            self.cnt[eng] += 1
            op.sem = self.sem[eng]
            op.val = self.cnt[eng]
        for d in deps:
            if d is op:
                continue
            if d.eng == "pe" and eng == "pe" and not d.dma and not dma:
                continue
            k = id(d.sem)
            self.semobj[k] = d.sem
            if op.waits.get(k, 0) < d.val:
                op.waits[k] = d.val
        if prewait is not None:
            k = id(prewait[0])
            self.semobj[k] = prewait[0]
            if op.waits.get(k, 0) < prewait[1]:
                op.waits[k] = prewait[1]
        self.ops.append(op)
        return op

    @staticmethod
    def _banks(aps):
        toks = []
        for a in aps:
            sp = getattr(a, "space", None)
            if sp is not None and str(sp).endswith("PSUM"):
                toks.append(a.tensor.name)
        return toks

    def op(self, eng, method, reads, writes, *args, **kw):
        o = self.add(eng, lambda e: getattr(e, method)(*args, **kw), reads, writes,
                     banks=self._banks(list(args) + list(kw.values())))
        if o is not None:
            o.desc = method
        return o

    def dma(self, eng, out, in_, reads, writes, **kw):
        return self.add(eng, lambda e: e.dma_start(out=out, in_=in_, **kw), reads, writes, dma=True)

    def mm(self, out, lhsT, rhs, start, stop, reads, writes):
        return self.add("pe", lambda e: e.matmul(out, lhsT=lhsT, rhs=rhs, start=start, stop=stop), reads, writes,
                        banks=self._banks([out]))

    def tr(self, out, in_, ident, reads, writes):
        return self.add("pe", lambda e: e.transpose(out=out, in_=in_, identity=ident), reads, writes,
                        banks=self._banks([out]))

    def emit(self):
        nc = self.nc
        ops = self.ops
        self.ops = []
        per = {e: [o for o in ops if o.eng == e] for e in ENGS}
        finals = []
        for e in ENGS:
            if e != "sp" and self.cnt[e] > 0:
                finals.append((self.sem[e], self.cnt[e]))
        for e in ("sp", "act", "pool"):
            n = self.dcnt[e]
            for j in range(min(n, NDS)):
                uses = (n - 1 - j) // NDS + 1
                finals.append((self.dsem[e][j], 16 * uses))
        waited = self.waited
        semobj = self.semobj

        def run(engname, eng):
            wd = waited[engname]
            for o in per[engname]:
                for k, v in o.waits.items():
                    if wd.get(k, 0) < v:
                        eng.wait_ge(semobj[k], v)
                        wd[k] = v
                ins = o.fn(eng)
                ins.then_inc(o.sem, o.inc)
            for s, v in finals:
                k = id(s)
                if wd.get(k, 0) < v:
                    eng.wait_ge(s, v)
                    wd[k] = v

        with nc.Block() as block:
            @block.tensor
            def _(eng):
                run("pe", eng)

            @block.scalar
            def _(eng):
                run("act", eng)

            @block.vector
            def _(eng):
                run("dve", eng)

            @block.gpsimd
            def _(eng):
                run("pool", eng)

            @block.sync
            def _(eng):
                run("sp", eng)


class K:
    pass


def _rr(lst):
    state = {"i": 0}

    def nxt():
        v = lst[state["i"] % len(lst)]
        state["i"] += 1
        return v
    return nxt


def build(cfg):
    LSEG = cfg["LSEG"]
    LT = 2 * LSEG
    NST = LT // 512
    upto = cfg.get("upto", "all")
    nc = bass.Bass("TRN2", target_bir_lowering=False)
    k = K()
    k.nc = nc
    k.cfg = cfg

    def din(name, shape, dt=F32):
        return nc.dram_tensor(name, list(shape), dt, kind="ExternalInput").ap()

    def dscr(name, shape, dt=F32):
        return nc.dram_tensor(name, list(shape), dt, kind="Internal").ap()

    def dout(name, shape, dt=F32):
        return nc.dram_tensor(name, list(shape), dt, kind="ExternalOutput").ap()

    x_in = din("x", [LT, D])
    w_in = din("w_in", [D, INC])
    norm_mix = din("norm_mix", [128, 16])
    flag = din("flag", [128, 1])

    w_in_bf = dscr("w_in_bf", [9, 128, 16, 512], BF16)
    xbcT = dscr("xbcT", [2048, LT])
    uT = dscr("uT", [1024, LT], BF16)
    z_tok = dscr("z_tok", [LT, 1024])
    dt_tok = dscr("dt_tok", [LT, 16])

    conv_w = din("conv_w", [128, 16, 3])
    conv_b = din("conv_b", [128, 16])
    dtb_rep = din("dtb_rep", [128, 32])
    alog_rep = din("alog_rep", [128, 32])
    ssdd_rep = din("ssdd_rep", [128, 16])
    ssdn_rep = din("ssdn_rep", [128, 1024])
    xs_tok = dscr("xs_tok", [LT, 1024], BF16)
    B_tok = dscr("B_tok", [LT, 512], BF16)
    BT = dscr("BT", [512, LT], BF16)
    CT = dscr("CT", [512, LT], BF16)
    yf = dscr("yf", [LT, 1024])
    yssd = dscr("yssd", [LT, 1024], BF16)
    s5_small = din("s5_small", [2, 8, 128, 3, 8])
    s5_big = din("s5_big", [2, 8, 128, 4, 128])
    s5_dcol = din("s5_dcol", [128, 8])
    y5gT = dscr("y5gT", [1024, LT], BF16)
    norm_attn = din("norm_attn", [128, 16])
    norm_mem = din("norm_mem", [128, 16])
    w_glu = din("w_glu", [1024, 1024])
    w_out = din("w_out", [2048, 2048])
    w_q = din("w_q", [2048, 2048])
    w_k = din("w_k", [2048, 2048])
    w_v = din("w_v", [2048, 2048])
    w_o = din("w_o", [2048, 2048])
    mem_in = din("mem", [2, 256, 2048])
    s5_ncol = din("s5_ncol", [128, 8])
    w_glu_bf = dscr("w_glu_bf", [2, 128, 8, 512], BF16)
    w_out_bf = dscr("w_out_bf", [4, 128, 16, 512], BF16)
    w_q_bf = dscr("w_q_bf", [4, 128, 16, 512], BF16)
    w_k_bf = dscr("w_k_bf", [4, 128, 16, 512], BF16)
    w_v_bf = dscr("w_v_bf", [4, 128, 16, 512], BF16)
    w_o_bf = dscr("w_o_bf", [4, 128, 16, 512], BF16)
    kT_d = dscr("kT_d", [2, 128, 16, 256], BF16)
    v_d = dscr("v_d", [2, 128, 2, 2048], BF16)
    y5nT = dscr("y5nT", [1024, LT], BF16)
    nffn_rep = din("nffn_rep", [128, 2048])
    w_router_l = din("w_router_l", [128, 16, 16])
    CAPL = cfg["CAPL"]
    acc_l = [dscr("accd%d" % i, [LT + CAPL, 256]) for i in range(8)]
    hn3_bf = dscr("hn3_bf", [LT + CAPL, 2048], BF16)
    idx_d = dscr("idx_d", [16, 128, CAPL // 128], I32)
    gate_d = dscr("gate_d", [16, 128, CAPL // 128])
    probs_all = dscr("probs_all", [cfg.get("GSZ", 1) * LT, 16])
    w_gate = din("w_gate", [cfg.get("nexp", 16), 2048, 2048])
    w_up = din("w_up", [cfg.get("nexp", 16), 2048, 2048])
    w_down = din("w_down", [cfg.get("nexp", 16), 2048, 2048])
    nfin_rep = din("nfin_rep", [128, 2048])
    y_out = dout("y", [LT, 2048])
    probs_loc = dscr("probs_loc", [LT, 16])
    dbg = {}
    if upto == "F":
        dbg["o_acc"] = dout("o_acc", [LT + CAPL, 256])
        dbg["o_idx"] = dout("o_idx", [16, 128, CAPL // 128], I32)
        dbg["o_gate"] = dout("o_gate", [16, 128, CAPL // 128])
    if upto == "E2":
        dbg["o_acc"] = dout("o_acc", [LT + CAPL, 256])
        dbg["o_probs"] = dout("o_probs", [LT, 16])
        dbg["o_hn3"] = dout("o_hn3", [LT + CAPL, 2048], BF16)
    if upto == "E1":
        dbg["o_y5nT"] = dout("o_y5nT", [1024, LT], BF16)
        dbg["o_kT"] = dout("o_kT", [2, 128, 16, 256], BF16)
        dbg["o_v"] = dout("o_v", [2, 128, 2, 2048], BF16)
    if upto == "D":
        dbg["o_y5gT"] = dout("o_y5gT", [1024, LT], BF16)
    if upto == "C":
        dbg["o_yssd"] = dout("o_yssd", [LT, 1024], BF16)
        dbg["o_yf"] = dout("o_yf", [LT, 1024])
    if upto == "A":
        dbg["o_xbcT"] = dout("o_xbcT", [2048, LT])
        dbg["o_uT"] = dout("o_uT", [1024, LT], BF16)
        dbg["o_z"] = dout("o_z", [LT, 1024])
        dbg["o_dt"] = dout("o_dt", [LT, 16])

    with ExitStack() as gst:
        P = Prog(nc, gst)
        k.P = P
        with ExitStack() as st:
            sb = lambda n, s, d=F32: st.enter_context(nc.sbuf_tensor(n, list(s), d))
            gcol = sb("gcol", [128, 16])
            P.add("sp", lambda e: e.dma_start(out=gcol[:], in_=norm_mix),
                  writes=["gcol"], dma=True)
            wst = [sb("wst%d" % i, [128, INC]) for i in range(2)]
            wbf = [sb("wbf%d" % i, [128, 9 * 512], BF16) for i in range(2)]
            for i in range(2):
                (lambda i: P.add("pool", lambda e: e.memset(wbf[i][:], 0.0), writes=["wbf%d" % i]))(i)
            engs = _rr(["dve", "act", "pool"])
            for kc in range(16):
                b = kc % 2
                P.add("sp", (lambda b, kc: lambda e: e.dma_start(out=wst[b][:], in_=w_in[kc * 128:(kc + 1) * 128, :]))(b, kc),
                      writes=["wst%d" % b], dma=True)
                def cast(b=b, kc=kc):
                    srcs = [(0, 3072, 0), (3088, 4112, 3072), (3072, 3088, 4096)]
                    for (s0, s1, d0) in srcs:
                        def f(e, s0=s0, s1=s1, d0=d0):
                            return e.tensor_scalar(out=wbf[b][:, d0:d0 + (s1 - s0)], in0=wst[b][:, s0:s1],
                                                   scalar1=gcol[:, kc:kc + 1], scalar2=None, op0=ALU.mult)
                        P.add("dve", f, reads=["wst%d" % b, "gcol"], writes=["wbf%d" % b])
                cast()
                P.add("sp", (lambda b, kc: lambda e: e.dma_start(
                    out=w_in_bf[:, :, kc, :].rearrange("g p n -> p g n"),
                    in_=wbf[b][:].rearrange("p (g n) -> p g n", n=512)))(b, kc),
                    reads=["wbf%d" % b], writes=["w_in_bf"], dma=True)
        P.emit()

        with ExitStack() as st:
            sb = lambda n, s, d=F32: st.enter_context(nc.sbuf_tensor(n, list(s), d))
            ps = lambda n, s, d=F32: st.enter_context(nc.psum_tensor(n, list(s), d))
            ident = sb("ident", [128, 128], BF16)
            P.add("pool", lambda e: e.memset(ident[:], 1.0), writes=["ident"])
            P.add("pool", lambda e: e.affine_select(out=ident[:], in_=ident[:], pattern=[[-1, 128]],
                                                    compare_op=ALU.is_equal, fill=0.0, base=0, channel_multiplier=1),
                  reads=["ident"], writes=["ident"])
            xt = [sb("xt%d" % i, [128, D]) for i in range(2)]
            junk = sb("junk", [128, D], BF16)
            ss = [sb("ss%d" % i, [128, 1]) for i in range(2)]
            xn = [sb("xn%d" % i, [128, D], BF16) for i in range(2)]
            hT = sb("hT", [128, 16, 512], BF16)
            wg = [sb("wg%d" % i, [128, 16, 512], BF16) for i in range(2)]
            ostg = [sb("ostg%d" % i, [128, 512]) for i in range(3)]
            ostb = [sb("ostb%d" % i, [128, 512], BF16) for i in range(2)]
            dstg = [sb("dstg%d" % i, [128, 16]) for i in range(2)]
            ptr = [ps("ptr%d" % i, [128, 1024], BF16) for i in range(2)]
            pmm = [ps("pmm%d" % i, [128, 512]) for i in range(4)]
            wq = _rr([0, 1])
            oq = _rr([0, 1, 2])
            obq = _rr([0, 1])
            pq = _rr([0, 1, 2, 3])
            tq = _rr([0, 1])
            cpq = _rr(["act", "dve"])
            dq = _rr([0, 1])
            it = 0
            for s in range(NST):
                t0 = s * 512
                for tt in range(4):
                    b = it % 2
                    it += 1
                    r0 = t0 + tt * 128
                    P.add("sp", (lambda b, r0: lambda e: e.dma_start(out=xt[b][:], in_=x_in[r0:r0 + 128, :]))(b, r0),
                          writes=["xt%d" % b], dma=True)
                    P.add("act", (lambda b: lambda e: e.activation(out=junk[:], in_=xt[b][:], func=AF.Square,
                                                                   accum_out=ss[b][:]))(b),
                          reads=["xt%d" % b], writes=["junk", "ss%d" % b])
                    P.add("dve", (lambda b: lambda e: e.tensor_scalar(out=ss[b][:], in0=ss[b][:], scalar1=1.0 / D,
                                                                      scalar2=EPS, op0=ALU.mult, op1=ALU.add))(b),
                          reads=["ss%d" % b], writes=["ss%d" % b])
                    P.add("act", (lambda b: lambda e: e.activation(out=ss[b][:], in_=ss[b][:], func=AF.Sqrt))(b),
                          reads=["ss%d" % b], writes=["ss%d" % b])
                    P.add("dve", (lambda b: lambda e: e.reciprocal(out=ss[b][:], in_=ss[b][:]))(b),
                          reads=["ss%d" % b], writes=["ss%d" % b])
                    P.add("dve", (lambda b: lambda e: e.tensor_scalar(out=xn[b][:], in0=xt[b][:], scalar1=ss[b][:, 0:1],
                                                                      scalar2=None, op0=ALU.mult))(b),
                          reads=["xt%d" % b, "ss%d" % b], writes=["xn%d" % b])
                    for q in range(4):
                        pb = tq()
                        for j in range(4):
                            kc = q * 4 + j
                            P.add("pe", (lambda pb, j, b, kc: lambda e: e.transpose(
                                out=ptr[pb][:, j * 128:(j + 1) * 128], in_=xn[b][:, kc * 128:(kc + 1) * 128],
                                identity=ident[:]))(pb, j, b, kc),
                                reads=["xn%d" % b, "ident"], writes=["ptr%d" % pb])
                        ce = cpq()
                        if ce == "act":
                            P.add("act", (lambda pb, q, tt: lambda e: e.activation(
                                out=hT[:, q * 4:(q + 1) * 4, tt * 128:(tt + 1) * 128],
                                in_=ptr[pb][:, 0:512].rearrange("p (j n) -> p j n", n=128), func=AF.Copy))(pb, q, tt),
                                reads=["ptr%d" % pb], writes=["hT"])
                        else:
                            P.add("dve", (lambda pb, q, tt: lambda e: e.tensor_copy(
                                out=hT[:, q * 4:(q + 1) * 4, tt * 128:(tt + 1) * 128],
                                in_=ptr[pb][:, 0:512].rearrange("p (j n) -> p j n", n=128)))(pb, q, tt),
                                reads=["ptr%d" % pb], writes=["hT"])
                for g in range(9):
                    wb = wq()
                    P.add("sp", (lambda wb, g: lambda e: e.dma_start(out=wg[wb][:], in_=w_in_bf[g]))(wb, g),
                          reads=["w_in_bf"], writes=["wg%d" % wb], dma=True)
                    if g < 2:
                        for tt in range(4):
                            pb = pq()
                            for kc in range(16):
                                P.add("pe", (lambda pb, wb, kc, tt: lambda e: e.matmul(
                                    pmm[pb][:], lhsT=hT[:, kc, tt * 128:(tt + 1) * 128], rhs=wg[wb][:, kc, :],
                                    start=(kc == 0), stop=(kc == 15)))(pb, wb, kc, tt),
                                    reads=["hT", "wg%d" % wb], writes=["pmm%d" % pb])
                            ob = oq()
                            P.add("act", (lambda ob, pb: lambda e: e.activation(out=ostg[ob][:], in_=pmm[pb][:], func=AF.Copy))(ob, pb),
                                  reads=["pmm%d" % pb], writes=["ostg%d" % ob])
                            r0 = t0 + tt * 128
                            P.add("sp", (lambda ob, r0, g: lambda e: e.dma_start(
                                out=z_tok[r0:r0 + 128, g * 512:(g + 1) * 512], in_=ostg[ob][:]))(ob, r0, g),
                                reads=["ostg%d" % ob], writes=["z_tok"], dma=True)
                    elif g < 8:
                        for mc in range(4):
                            pb = pq()
                            for kc in range(16):
                                P.add("pe", (lambda pb, wb, kc, mc: lambda e: e.matmul(
                                    pmm[pb][:], lhsT=wg[wb][:, kc, mc * 128:(mc + 1) * 128], rhs=hT[:, kc, :],
                                    start=(kc == 0), stop=(kc == 15)))(pb, wb, kc, mc),
                                    reads=["hT", "wg%d" % wb], writes=["pmm%d" % pb])
                            if g < 6:
                                ob = oq()
                                ch0 = (g - 2) * 512 + mc * 128
                                P.add("act", (lambda ob, pb: lambda e: e.activation(out=ostg[ob][:], in_=pmm[pb][:], func=AF.Copy))(ob, pb),
                                      reads=["pmm%d" % pb], writes=["ostg%d" % ob])
                                P.add("sp", (lambda ob, ch0, t0: lambda e: e.dma_start(
                                    out=xbcT[ch0:ch0 + 128, t0:t0 + 512], in_=ostg[ob][:]))(ob, ch0, t0),
                                    reads=["ostg%d" % ob], writes=["xbcT"], dma=True)
                            else:
                                ob = obq()
                                ch0 = (g - 6) * 512 + mc * 128
                                P.add("dve", (lambda ob, pb: lambda e: e.tensor_copy(out=ostb[ob][:], in_=pmm[pb][:]))(ob, pb),
                                      reads=["pmm%d" % pb], writes=["ostb%d" % ob])
                                P.add("sp", (lambda ob, ch0, t0: lambda e: e.dma_start(
                                    out=uT[ch0:ch0 + 128, t0:t0 + 512], in_=ostb[ob][:]))(ob, ch0, t0),
                                    reads=["ostb%d" % ob], writes=["uT"], dma=True)
                    else:
                        for tt in range(4):
                            pb = pq()
                            for kc in range(16):
                                P.add("pe", (lambda pb, wb, kc, tt: lambda e: e.matmul(
                                    pmm[pb][:, 0:16], lhsT=hT[:, kc, tt * 128:(tt + 1) * 128], rhs=wg[wb][:, kc, 0:16],
                                    start=(kc == 0), stop=(kc == 15)))(pb, wb, kc, tt),
                                    reads=["hT", "wg%d" % wb], writes=["pmm%d" % pb])
                            ob = dq()
                            P.add("dve", (lambda ob, pb: lambda e: e.tensor_copy(out=dstg[ob][:], in_=pmm[pb][:, 0:16]))(ob, pb),
                                  reads=["pmm%d" % pb], writes=["dstg%d" % ob])
                            r0 = t0 + tt * 128
                            P.add("sp", (lambda ob, r0: lambda e: e.dma_start(out=dt_tok[r0:r0 + 128, :], in_=dstg[ob][:]))(ob, r0),
                                  reads=["dstg%d" % ob], writes=["dt_tok"], dma=True)
        P.emit()


        with ExitStack() as st:
            sb = lambda n, s, d=F32: st.enter_context(nc.sbuf_tensor(n, list(s), d))
            ps = lambda n, s, d=F32: st.enter_context(nc.psum_tensor(n, list(s), d))
            ident = sb("identB", [128, 128], BF16)
            P.op("pool", "memset", [], ["identB"], ident[:], 1.0)
            P.op("pool", "affine_select", ["identB"], ["identB"], out=ident[:], in_=ident[:], pattern=[[-1, 128]],
                 compare_op=ALU.is_equal, fill=0.0, base=0, channel_multiplier=1)
            cw = sb("cw", [128, 16, 3])
            cb = sb("cb", [128, 16])
            flg = sb("flg", [128, 1])
            P.dma("sp", cw[:], conv_w, [], ["cw"])
            P.dma("sp", cb[:], conv_b, [], ["cb"])
            P.dma("sp", flg[:], flag, [], ["flg"])
            NB = 3
            xw = [sb("xw%d" % i, [128, 514]) for i in range(NB)]
            acc = [sb("acc%d" % i, [128, 512]) for i in range(NB)]
            sl = [sb("sl%d" % i, [128, 512], BF16) for i in range(NB)]
            xs_stg = sb("xs_stg", [128, 4, 1024], BF16)
            b_stg = sb("b_stg", [128, 4, 512], BF16)
            ptb = [ps("ptb%d" % i, [128, 1024], BF16) for i in range(2)]
            it = 0
            tq = 0
            for s in range(NST):
                t0 = s * 512
                for c in range(16):
                    b = it % NB
                    it += 1
                    lo = max(t0 - 1, 0)
                    hi = min(t0 + 513, LT)
                    if t0 == 0:
                        P.op("pool", "memset", [], ["xw%d" % b], xw[b][:, 0:1], 0.0)
                    if t0 + 512 == LT:
                        P.op("pool", "memset", [], ["xw%d" % b], xw[b][:, 513:514], 0.0)
                    P.dma("sp", xw[b][:, (lo - (t0 - 1)):(hi - (t0 - 1))], xbcT[c * 128:(c + 1) * 128, lo:hi],
                          ["xbcT"], ["xw%d" % b])
                    if t0 == LSEG:
                        P.op("dve", "tensor_scalar", ["xw%d" % b, "flg"], ["xw%d" % b], out=xw[b][:, 0:1], in0=xw[b][:, 0:1],
                             scalar1=flg[:, 0:1], scalar2=None, op0=ALU.mult)
                    if t0 + 512 == LSEG:
                        P.op("dve", "tensor_scalar", ["xw%d" % b, "flg"], ["xw%d" % b], out=xw[b][:, 513:514], in0=xw[b][:, 513:514],
                             scalar1=flg[:, 0:1], scalar2=None, op0=ALU.mult)
                    P.op("dve", "tensor_scalar", ["xw%d" % b, "cw"], ["acc%d" % b], out=acc[b][:], in0=xw[b][:, 1:513],
                         scalar1=cw[:, c, 1:2], scalar2=None, op0=ALU.mult)
                    P.op("dve", "scalar_tensor_tensor", ["xw%d" % b, "cw", "acc%d" % b], ["acc%d" % b], out=acc[b][:],
                         in0=xw[b][:, 0:512], scalar=cw[:, c, 0:1], in1=acc[b][:], op0=ALU.mult, op1=ALU.add)
                    P.op("dve", "scalar_tensor_tensor", ["xw%d" % b, "cw", "acc%d" % b], ["acc%d" % b], out=acc[b][:],
                         in0=xw[b][:, 2:514], scalar=cw[:, c, 2:3], in1=acc[b][:], op0=ALU.mult, op1=ALU.add)
                    P.op("act", "activation", ["acc%d" % b, "cb"], ["sl%d" % b], out=sl[b][:], in_=acc[b][:], func=AF.Silu,
                         bias=cb[:, c:c + 1])
                    if c >= 8:
                        dst = BT if c < 12 else CT
                        r0 = (c - 8) * 128 if c < 12 else (c - 12) * 128
                        P.dma("sp", dst[r0:r0 + 128, t0:t0 + 512], sl[b][:], ["sl%d" % b], ["BT" if c < 12 else "CT"])
                    if c < 12:
                        pb = tq % 2
                        tq += 1
                        for tt in range(4):
                            P.tr(ptb[pb][:, tt * 128:(tt + 1) * 128], sl[b][:, tt * 128:(tt + 1) * 128], ident[:],
                                 ["sl%d" % b, "identB"], ["ptb%d" % pb])
                        if c < 8:
                            P.op("act", "activation", ["ptb%d" % pb], ["xs_stg"], out=xs_stg[:, :, c * 128:(c + 1) * 128],
                                 in_=ptb[pb][:, 0:512].rearrange("p (t n) -> p t n", n=128), func=AF.Copy)
                        else:
                            P.op("act", "activation", ["ptb%d" % pb], ["b_stg"], out=b_stg[:, :, (c - 8) * 128:(c - 7) * 128],
                                 in_=ptb[pb][:, 0:512].rearrange("p (t n) -> p t n", n=128), func=AF.Copy)
                    if c == 7:
                        P.dma("sp", xs_tok[t0:t0 + 512, :].rearrange("(t p) c -> p t c", p=128), xs_stg[:], ["xs_stg"], ["xs_tok"])
                    if c == 11:
                        P.dma("sp", B_tok[t0:t0 + 512, :].rearrange("(t p) c -> p t c", p=128), b_stg[:], ["b_stg"], ["B_tok"])
        P.emit()

        NCH = LT // 128
        CPS = LSEG // 128
        with ExitStack() as st:
            sb = lambda n, s, d=F32: st.enter_context(nc.sbuf_tensor(n, list(s), d))
            ps = lambda n, s, d=F32: st.enter_context(nc.psum_tensor(n, list(s), d))
            tri = [sb("triF", [128, 128]), sb("triB", [128, 128])]
            nmk = [sb("nmkF", [128, 128]), sb("nmkB", [128, 128])]
            ones = sb("onesC", [128, 128])
            P.op("pool", "memset", [], ["onesC"], ones[:], 1.0)
            for d_ in range(2):
                pat = [[1, 128]] if d_ == 0 else [[-1, 128]]
                cm = -1 if d_ == 0 else 1
                nm = "FB"[d_]
                P.op("pool", "memset", [], ["tri" + nm], tri[d_][:], 1.0)
                P.op("pool", "affine_select", ["tri" + nm], ["tri" + nm], out=tri[d_][:], in_=tri[d_][:], pattern=pat,
                     compare_op=ALU.is_ge, fill=0.0, base=0, channel_multiplier=cm)
                P.op("pool", "memset", [], ["nmk" + nm], nmk[d_][:], 0.0)
                P.op("pool", "affine_select", ["nmk" + nm], ["nmk" + nm], out=nmk[d_][:], in_=nmk[d_][:], pattern=pat,
                     compare_op=ALU.is_ge, fill=-30000.0, base=0, channel_multiplier=cm)
            dtb = sb("dtb", [128, 32])
            aneg = sb("aneg", [128, 32])
            dsk = sb("dsk", [128, 16])
            gn = sb("gn", [128, 1024])
            flg = sb("flgC", [128, 1])
            P.dma("sp", dtb[:], dtb_rep, [], ["dtb"])
            P.dma("sp", aneg[:], alog_rep, [], ["aneg"])
            P.dma("sp", dsk[:], ssdd_rep, [], ["dsk"])
            P.dma("sp", gn[:], ssdn_rep, [], ["gn"])
            P.dma("sp", flg[:], flag, [], ["flgC"])
            P.op("act", "activation", ["aneg"], ["aneg"], out=aneg[:], in_=aneg[:], func=AF.Exp)
            P.op("dve", "tensor_scalar", ["aneg"], ["aneg"], out=aneg[:], in0=aneg[:], scalar1=-1.0, scalar2=None, op0=ALU.mult)
            NBUF = 2
            xs = [sb("xs%d" % i, [128, 16, 64], BF16) for i in range(NBUF)]
            bt_ = [sb("btok%d" % i, [128, 512], BF16) for i in range(NBUF)]
            bT = [sb("bT%d" % i, [128, 4, 128], BF16) for i in range(NBUF)]
            cT = [sb("cT%d" % i, [128, 4, 128], BF16) for i in range(NBUF)]
            dtr = [sb("dtr%d" % i, [128, 16]) for i in range(NBUF)]
            dt_ = sb("dt_", [128, 16])
            dtA = sb("dtA", [128, 16])
            acs = sb("acs", [128, 16])
            dout_ = sb("dout_", [128, 16])
            cd = sb("cd", [128, 16])
            rhsb = [sb("rhsb%d" % i, [128, 4, 128]) for i in range(2)]
            dif = [sb("dif%d" % i, [128, 4, 128]) for i in range(2)]
            seg = [sb("seg%d" % i, [128, 4, 128]) for i in range(2)]
            mT = [sb("mT%d" % i, [128, 4, 128], BF16) for i in range(2)]
            xdt = sb("xdt", [128, 16, 64], BF16)
            xdd = [sb("xdd%d" % i, [128, 4, 64], BF16) for i in range(2)]
            prev = sb("prev", [128, 16, 64])
            prevb = sb("prevb", [128, 16, 64], BF16)
            tmp = [sb("tmpc%d" % i, [128, 4, 64]) for i in range(2)]
            ybuf = sb("ybuf", [128, 16, 64])
            yfl = sb("yfl", [128, 16, 64])
            zt = sb("zt", [128, 1024])
            sq = sb("sqc", [128, 1024])
            gss = sb("gss", [128, 4])
            yo = sb("yo", [128, 1024], BF16)
            pA = [ps("pA%d" % i, [128, 4, 128]) for i in range(2)]
            pB = [ps("pB%d" % i, [128, 512]) for i in range(2)]
            pC = [ps("pC%d" % i, [128, 512]) for i in range(2)]
            pD = ps("pD", [128, 512])
            it = 0
            for d_ in range(2):
                nm = "FB"[d_]
                last = 127 if d_ == 0 else 0
                order = list(range(NCH)) if d_ == 0 else list(range(NCH - 1, -1, -1))
                P.op("dve", "memset", [], ["prev"], prev[:], 0.0)
                P.op("pool", "memset", [], ["prevb0", "prevb1", "prevb2", "prevb3"], prevb[:], 0.0)
                for ci, c in enumerate(order):
                    t0 = c * 128
                    b = it % NBUF
                    it += 1
                    if ci == CPS:
                        P.op("dve", "tensor_scalar", ["prev", "flgC"], ["prev"], out=prev[:], in0=prev[:], scalar1=flg[:, 0:1],
                             scalar2=None, op0=ALU.mult)
                        P.op("dve", "tensor_scalar", ["prevb0", "prevb1", "prevb2", "prevb3", "flgC"], ["prevb0", "prevb1", "prevb2", "prevb3"], out=prevb[:], in0=prevb[:], scalar1=flg[:, 0:1],
                             scalar2=None, op0=ALU.mult)
                    P.dma("sp", xs[b][:], xs_tok[t0:t0 + 128, :].rearrange("p (h q) -> p h q", q=64), ["xs_tok"], ["xs%d" % b])
                    P.dma("sp", bt_[b][:], B_tok[t0:t0 + 128, :], ["B_tok"], ["btok%d" % b])
                    P.dma("sp", bT[b][:], BT[:, t0:t0 + 128].rearrange("(g n) l -> n g l", n=128), ["BT"], ["bT%d" % b])
                    P.dma("sp", cT[b][:], CT[:, t0:t0 + 128].rearrange("(g n) l -> n g l", n=128), ["CT"], ["cT%d" % b])
                    P.dma("sp", dtr[b][:], dt_tok[t0:t0 + 128, :], ["dt_tok"], ["dtr%d" % b])
                    P.op("dve", "tensor_tensor", ["dtr%d" % b, "dtb"], ["dt_"], out=dt_[:], in0=dtr[b][:], in1=dtb[:, d_ * 16:(d_ + 1) * 16], op=ALU.add)
                    P.op("act", "activation", ["dt_"], ["dt_"], out=dt_[:], in_=dt_[:], func=AF.Exp)
                    P.op("act", "activation", ["dt_"], ["dt_"], out=dt_[:], in_=dt_[:], func=AF.Ln, bias=1.0)
                    P.op("dve", "tensor_tensor", ["dt_", "aneg"], ["dtA"], out=dtA[:], in0=dt_[:], in1=aneg[:, d_ * 16:(d_ + 1) * 16], op=ALU.mult)
                    P.mm(pD[:, 0:16], tri[d_][:], dtA[:], True, True, ["tri" + nm, "dtA"], ["pD"])
                    P.op("dve", "tensor_copy", ["pD"], ["acs"], out=acs[:], in_=pD[:, 0:16])
                    P.op("act", "activation", ["acs"], ["dout_"], out=dout_[:], in_=acs[:], func=AF.Exp)
                    P.op("pool", "tensor_tensor", ["xs%d" % b, "dt_"], ["xdt"], out=xdt[:], in0=xs[b][:],
                         in1=dt_[:].unsqueeze(2).to_broadcast([128, 16, 64]), op=ALU.mult)
                    for g in range(4):
                        q = (it * 4 + g) % 2
                        h0 = g * 4
                        P.op("dve", "tensor_tensor", ["tri" + nm, "dtA"], ["rhsb%d" % q], out=rhsb[q][:],
                             in0=tri[d_][:].unsqueeze(1).to_broadcast([128, 4, 128]),
                             in1=dtA[:, h0:h0 + 4].unsqueeze(2).to_broadcast([128, 4, 128]), op=ALU.mult)
                        P.mm(pA[q][:].rearrange("p r l -> p (r l)"), ones[:], rhsb[q][:].rearrange("p r l -> p (r l)"), True, True,
                             ["onesC", "rhsb%d" % q], ["pA%d" % q])
                        P.op("dve", "tensor_tensor", ["pA%d" % q, "acs"], ["dif%d" % q], out=dif[q][:], in0=pA[q][:],
                             in1=acs[:, h0:h0 + 4].unsqueeze(2).to_broadcast([128, 4, 128]), op=ALU.subtract)
                        P.op("dve", "tensor_tensor", ["dif%d" % q, "nmk" + nm], ["dif%d" % q], out=dif[q][:], in0=dif[q][:],
                             in1=nmk[d_][:].unsqueeze(1).to_broadcast([128, 4, 128]), op=ALU.min)
                        P.op("act", "activation", ["dif%d" % q], ["seg%d" % q], out=seg[q][:], in_=dif[q][:], func=AF.Exp)
                        P.op("act", "activation", ["pA%d" % q], ["cd"], out=cd[:, h0:h0 + 4], in_=pA[q][:, :, last], func=AF.Exp)
                        P.mm(pB[q][:, 0:128], bT[b][:, g, :], cT[b][:, g, :], True, True, ["bT%d" % b, "cT%d" % b], ["pB%dcb" % q])
                        P.op("dve", "tensor_tensor", ["seg%d" % q, "pB%dcb" % q], ["mT%d" % q], out=mT[q][:], in0=seg[q][:],
                             in1=pB[q][:, 0:128].unsqueeze(1).to_broadcast([128, 4, 128]), op=ALU.mult)
                        for r in range(4):
                            P.mm(pB[q][:, 128 + r * 64:128 + (r + 1) * 64], mT[q][:, r, :], xdt[:, h0 + r, :], True, True,
                                 ["mT%d" % q, "xdt"], ["pB%dy" % q])
                        P.op("pool", "tensor_tensor", ["xdt", "seg%d" % q], ["xdd%d" % q], out=xdd[q][:], in0=xdt[:, h0:h0 + 4, :],
                             in1=seg[q][:, :, last:last + 1].to_broadcast([128, 4, 64]), op=ALU.mult)
                        P.mm(pC[q][:, 0:256], bt_[b][:, g * 128:(g + 1) * 128], xdd[q][:].rearrange("p r q -> p (r q)"), True, True,
                             ["btok%d" % b, "xdd%d" % q], ["pC%ds" % q])
                        P.mm(pC[q][:, 256:512], cT[b][:, g, :], prevb[:, h0:h0 + 4, :].rearrange("p r q -> p (r q)"), True, True,
                             ["cT%d" % b, "prevb%d" % g], ["pC%do" % q])
                        P.op("dve", "tensor_tensor", ["pC%do" % q, "dout_"], ["tmpc%d" % q], out=tmp[q][:],
                             in0=pC[q][:, 256:512].rearrange("p (r q) -> p r q", q=64),
                             in1=dout_[:, h0:h0 + 4].unsqueeze(2).to_broadcast([128, 4, 64]), op=ALU.mult)
                        P.op("dve", "tensor_tensor", ["tmpc%d" % q, "pB%dy" % q], ["ybuf"], out=ybuf[:, h0:h0 + 4, :], in0=tmp[q][:],
                             in1=pB[q][:, 128:384].rearrange("p (r q) -> p r q", q=64), op=ALU.add)
                        P.op("dve", "tensor_tensor", ["prev", "cd"], ["prev"], out=prev[:, h0:h0 + 4, :], in0=prev[:, h0:h0 + 4, :],
                             in1=cd[:, h0:h0 + 4].unsqueeze(2).to_broadcast([128, 4, 64]), op=ALU.mult)
                        P.op("dve", "tensor_tensor", ["prev", "pC%ds" % q], ["prev"], out=prev[:, h0:h0 + 4, :], in0=prev[:, h0:h0 + 4, :],
                             in1=pC[q][:, 0:256].rearrange("p (r q) -> p r q", q=64), op=ALU.add)
                        P.op("act", "activation", ["prev"], ["prevb%d" % g], out=prevb[:, h0:h0 + 4, :], in_=prev[:, h0:h0 + 4, :], func=AF.Copy)
                    if d_ == 0:
                        P.dma("sp", yf[t0:t0 + 128, :], ybuf[:].rearrange("p h q -> p (h q)"), ["ybuf"], ["yf"])
                    else:
                        P.dma("sp", yfl[:].rearrange("p h q -> p (h q)"), yf[t0:t0 + 128, :], ["yf"], ["yfl"])
                        P.dma("sp", zt[:], z_tok[t0:t0 + 128, :], ["z_tok"], ["zt"])
                        P.op("dve", "tensor_tensor", ["ybuf", "yfl"], ["ybuf"], out=ybuf[:], in0=ybuf[:], in1=yfl[:], op=ALU.add)
                        P.op("pool", "tensor_tensor", ["xs%d" % b, "dsk"], ["yfl"], out=yfl[:], in0=xs[b][:],
                             in1=dsk[:].unsqueeze(2).to_broadcast([128, 16, 64]), op=ALU.mult)
                        P.op("dve", "tensor_tensor", ["ybuf", "yfl"], ["ybuf"], out=ybuf[:], in0=ybuf[:], in1=yfl[:], op=ALU.add)
                        P.op("act", "activation", ["zt"], ["zt"], out=zt[:], in_=zt[:], func=AF.Silu)
                        yb2 = ybuf[:].rearrange("p h q -> p (h q)")
                        P.op("dve", "tensor_tensor", ["ybuf", "zt"], ["ybuf"], out=yb2, in0=yb2, in1=zt[:], op=ALU.mult)
                        P.op("pool", "tensor_tensor", ["ybuf"], ["sqc"], out=sq[:], in0=yb2, in1=yb2, op=ALU.mult)
                        P.op("dve", "tensor_reduce", ["sqc"], ["gss"], out=gss[:], in_=sq[:].rearrange("p (g c) -> p g c", c=256),
                             op=ALU.add, axis=AX.X)
                        P.op("dve", "tensor_scalar", ["gss"], ["gss"], out=gss[:], in0=gss[:], scalar1=1.0 / 256, scalar2=EPS,
                             op0=ALU.mult, op1=ALU.add)
                        P.op("act", "activation", ["gss"], ["gss"], out=gss[:], in_=gss[:], func=AF.Sqrt)
                        P.op("dve", "reciprocal", ["gss"], ["gss"], out=gss[:], in_=gss[:])
                        P.op("dve", "tensor_tensor", ["ybuf", "gss"], ["ybuf"], out=ybuf[:].rearrange("p (g h) q -> p g (h q)", g=4),
                             in0=ybuf[:].rearrange("p (g h) q -> p g (h q)", g=4),
                             in1=gss[:].unsqueeze(2).to_broadcast([128, 4, 256]), op=ALU.mult)
                        P.op("dve", "tensor_tensor", ["ybuf", "gn"], ["yo"], out=yo[:], in0=yb2, in1=gn[:], op=ALU.mult)
                        P.dma("sp", yssd[t0:t0 + 128, :], yo[:], ["yo"], ["yssd"])
        P.emit()

        NB = LT // 8
        NBS = LSEG // 8
        P.limit = cfg.get("dmax")
        TWO_PI = 6.283185307179586
        with ExitStack() as st:
            sb = lambda n, s, d=F32: st.enter_context(nc.sbuf_tensor(n, list(s), d))
            ps = lambda n, s, d=F32: st.enter_context(nc.psum_tensor(n, list(s), d))
            identF = sb("identF", [128, 128])
            P.op("pool", "memset", [], ["identF"], identF[:], 1.0)
            P.op("pool", "affine_select", ["identF"], ["identF"], out=identF[:], in_=identF[:], pattern=[[-1, 128]],
                 compare_op=ALU.is_equal, fill=0.0, base=0, channel_multiplier=1)
            bm = sb("bmD", [128, 8, 16])
            P.op("pool", "memset", [], ["bmD"], bm[:], 1.0)
            P.op("pool", "affine_select", ["bmD"], ["bmD"], out=bm[:], in_=bm[:], pattern=[[-16, 8], [0, 16]],
                 compare_op=ALU.is_ge, fill=0.0, base=0, channel_multiplier=1)
            P.op("pool", "affine_select", ["bmD"], ["bmD"], out=bm[:], in_=bm[:], pattern=[[16, 8], [0, 16]],
                 compare_op=ALU.is_ge, fill=0.0, base=15, channel_multiplier=-1)
            PM = [sb("PM%d" % i, [128, 2, 64]) for i in range(4)]
            CM = [sb("CM%d" % i, [128, 8, 16]) for i in range(4)]
            for pr_ in range(4):
                P.op("pool", "memset", [], ["PM%d" % pr_], PM[pr_][:], 1.0)
                P.op("pool", "affine_select", ["PM%d" % pr_], ["PM%d" % pr_], out=PM[pr_][:], in_=PM[pr_][:],
                     pattern=[[-16, 2], [0, 64]], compare_op=ALU.is_ge, fill=0.0, base=-32 * pr_, channel_multiplier=1)
                P.op("pool", "affine_select", ["PM%d" % pr_], ["PM%d" % pr_], out=PM[pr_][:], in_=PM[pr_][:],
                     pattern=[[16, 2], [0, 64]], compare_op=ALU.is_ge, fill=0.0, base=32 * pr_ + 15, channel_multiplier=-1)
                P.op("pool", "memset", [], ["CM%d" % pr_], CM[pr_][:], 1.0)
                for h in range(2):
                    P.op("pool", "affine_select", ["CM%d" % pr_], ["CM%d" % pr_], out=CM[pr_][h * 64:(h + 1) * 64],
                         in_=CM[pr_][h * 64:(h + 1) * 64], pattern=[[1, 8], [0, 16]], compare_op=ALU.is_equal, fill=0.0,
                         base=-(2 * pr_ + h), channel_multiplier=0)
            KV = sb("KV", [128, 8, 9])
            P.op("pool", "iota", [], ["KV"], KV[:], pattern=[[0, 8], [1, 9]], base=0, channel_multiplier=0,
                 allow_small_or_imprecise_dtypes=True)
            BL = sb("BL", [128, NBS])
            BHs = [sb("BH%d" % i, [128, NBS]) for i in range(2)]
            P.op("pool", "iota", [], ["BL"], BL[:], pattern=[[0, NBS // 32], [1, 32]], base=0, channel_multiplier=0,
                 allow_small_or_imprecise_dtypes=True)
            for s_ in range(2):
                P.op("pool", "iota", [], ["BH%d" % s_], BHs[s_][:], pattern=[[1, NBS // 32], [0, 32]], base=s_ * (NBS // 32),
                     channel_multiplier=0, allow_small_or_imprecise_dtypes=True)
            dcol = sb("dcolD", [128, 8])
            flg = sb("flgD", [128, 1])
            P.dma("sp", dcol[:], s5_dcol, [], ["dcolD"])
            P.dma("sp", flg[:], flag, [], ["flgD"])
            sm = sb("smD", [128, 3, 8])
            bg = sb("bgD", [128, 4, 128])
            ncim = sb("ncim", [128, 128])
            sml = {n_: sb("sm_" + n_, [128, 8]) for n_ in ("dl", "xr", "xi", "numr", "den", "t1", "t2", "qr", "qi", "tb", "t0", "t32")}
            smi = sb("sm_i", [128, 8], I32)
            k9 = {n_: sb("k9_" + n_, [128, 8, 9]) for n_ in ("er", "tn", "tf", "cs", "sn", "pr", "pi")}
            k9i = sb("k9_i", [128, 8, 9], I32)
            Bb = {n_: sb("Bb_" + n_, [128, 8, 16]) for n_ in ("r", "i", "a", "b")}
            colsT = sb("colsT", [128, 3, 4])
            scr = sb("scrD", [128, 11, 512])
            RN = lambda a, b_: ["R%d" % i for i in range(a, b_)]
            X_r = scr[:, 0:2, :].rearrange("p a (b c) -> p (a b) c", c=128)
            X_i = scr[:, 2:4, :].rearrange("p a (b c) -> p (a b) c", c=128)
            Zr_t = sb("Zr_t", [128, 9, 128])
            Zi_t = sb("Zi_t", [128, 9, 128])
            xtmp = sb("xtmp", [128, 8, 128])
            W1 = sb("W1", [128, 8, 2, 4, 128], BF16)
            W2 = sb("W2", [128, 2, 9, 2, 4, 128], BF16)
            Kbd = sb("Kbd", [128, 2, 8, 128], BF16)
            ktmp = sb("ktmp", [128, 128])
            uft = sb("uft", [128, LT], BF16)
            u3 = uft[:].rearrange("p (b j) -> p j b", j=8)
            S = sb("S_D", [128, 4, 2, NBS])
            HB = sb("HB_D", [128, 2, 4, 2, NB + 1], BF16)
            carry = sb("carryD", [128, 4, 2])
            stg = sb("stgD", [128, NBS, 8], BF16)
            gl = [sb("glD%d" % i, [128, NBS]) for i in range(3)]
            pk = ps("pkD", [128, 4, 128])
            ptl = [ps("ptD%d" % i, [128, 4, 128]) for i in range(2)]
            pS = [ps("pSD%d" % i, [128, 512]) for i in range(2)]
            po = [ps("poD%d" % i, [128, 512]) for i in range(2)]
            mskq = _rr(["dve", "pool"])
            psq = 0
            poq = 0
            T = lambda i: scr[:, i, 0:NBS]
            DST = cfg.get("dstage", 9)
            for ft in range(cfg.get("nft", 8)):
                P.dma("sp", uft[:], uT[ft * 128:(ft + 1) * 128, :], ["uT"], ["uft"])
                for d_ in range(2):
                    P.dma("sp", sm[:], s5_small[d_, ft], [], ["smD"])
                    P.dma("sp", bg[:], s5_big[d_, ft], [], ["bgD"])
                    are, aim, lst = sm[:, 0, :], sm[:, 1, :], sm[:, 2, :]
                    bre, bim, cre, cim = bg[:, 0, :], bg[:, 1, :], bg[:, 2, :], bg[:, 3, :]
                    V = lambda n_: sml[n_][:]
                    P.op("act", "activation", ["smD"], ["sm_dl"], out=V("dl"), in_=lst, func=AF.Exp)
                    P.op("dve", "tensor_tensor", ["smD", "sm_dl"], ["sm_xr"], out=V("xr"), in0=are, in1=V("dl"), op=ALU.mult)
                    P.op("dve", "tensor_tensor", ["smD", "sm_dl"], ["sm_xi"], out=V("xi"), in0=aim, in1=V("dl"), op=ALU.mult)
                    b9 = lambda ap: ap.unsqueeze(2).to_broadcast([128, 8, 9])
                    P.op("dve", "tensor_tensor", ["KV", "sm_xr"], ["k9_er"], out=k9["er"][:], in0=KV[:], in1=b9(V("xr")), op=ALU.mult)
                    P.op("act", "activation", ["k9_er"], ["k9_er"], out=k9["er"][:], in_=k9["er"][:], func=AF.Exp)
                    P.op("dve", "tensor_tensor", ["KV", "sm_xi"], ["k9_tn"], out=k9["tn"][:], in0=KV[:], in1=b9(V("xi")), op=ALU.mult)
                    P.op("dve", "tensor_scalar", ["k9_tn"], ["k9_tn"], out=k9["tn"][:], in0=k9["tn"][:], scalar1=1.0 / TWO_PI, scalar2=None, op0=ALU.mult)
                    for (dst, off) in (("sn", 0.0), ("cs", 0.25)):
                        if off:
                            P.op("dve", "tensor_scalar", ["k9_tn"], ["k9_tn"], out=k9["tn"][:], in0=k9["tn"][:], scalar1=off, scalar2=None, op0=ALU.add)
                        P.op("dve", "tensor_copy", ["k9_tn"], ["k9_i"], out=k9i[:], in_=k9["tn"][:])
                        P.op("dve", "tensor_copy", ["k9_i"], ["k9_tf"], out=k9["tf"][:], in_=k9i[:])
                        P.op("dve", "tensor_tensor", ["k9_tn", "k9_tf"], ["k9_tf"], out=k9["tf"][:], in0=k9["tn"][:], in1=k9["tf"][:], op=ALU.subtract)
                        P.op("act", "activation", ["k9_tf"], ["k9_" + dst], out=k9[dst][:], in_=k9["tf"][:], func=AF.Sin, scale=TWO_PI)
                    P.op("dve", "tensor_tensor", ["k9_er", "k9_cs"], ["k9_pr"], out=k9["pr"][:], in0=k9["er"][:], in1=k9["cs"][:], op=ALU.mult)
                    P.op("dve", "tensor_tensor", ["k9_er", "k9_sn"], ["k9_pi"], out=k9["pi"][:], in0=k9["er"][:], in1=k9["sn"][:], op=ALU.mult)
                    pr1, pi1 = k9["pr"][:, :, 1], k9["pi"][:, :, 1]
                    P.op("dve", "tensor_scalar", ["k9_pr"], ["sm_numr"], out=V("numr"), in0=pr1, scalar1=-1.0, scalar2=None, op0=ALU.add)
                    P.op("dve", "tensor_tensor", ["smD"], ["sm_den"], out=V("den"), in0=are, in1=are, op=ALU.mult)
                    P.op("dve", "tensor_tensor", ["smD"], ["sm_t1"], out=V("t1"), in0=aim, in1=aim, op=ALU.mult)
                    P.op("dve", "tensor_tensor", ["sm_den", "sm_t1"], ["sm_den"], out=V("den"), in0=V("den"), in1=V("t1"), op=ALU.add)
                    P.op("dve", "reciprocal", ["sm_den"], ["sm_den"], out=V("den"), in_=V("den"))
                    P.op("dve", "tensor_tensor", ["sm_numr", "smD"], ["sm_t1"], out=V("t1"), in0=V("numr"), in1=are, op=ALU.mult)
                    P.op("dve", "tensor_tensor", ["k9_pi", "smD"], ["sm_t2"], out=V("t2"), in0=pi1, in1=aim, op=ALU.mult)
                    P.op("dve", "tensor_tensor", ["sm_t1", "sm_t2"], ["sm_t1"], out=V("t1"), in0=V("t1"), in1=V("t2"), op=ALU.add)
                    P.op("dve", "tensor_tensor", ["sm_t1", "sm_den"], ["sm_qr"], out=V("qr"), in0=V("t1"), in1=V("den"), op=ALU.mult)
                    P.op("dve", "tensor_tensor", ["k9_pi", "smD"], ["sm_t1"], out=V("t1"), in0=pi1, in1=are, op=ALU.mult)
                    P.op("dve", "tensor_tensor", ["sm_numr", "smD"], ["sm_t2"], out=V("t2"), in0=V("numr"), in1=aim, op=ALU.mult)
                    P.op("dve", "tensor_tensor", ["sm_t1", "sm_t2"], ["sm_t1"], out=V("t1"), in0=V("t1"), in1=V("t2"), op=ALU.subtract)
                    P.op("dve", "tensor_tensor", ["sm_t1", "sm_den"], ["sm_qi"], out=V("qi"), in0=V("t1"), in1=V("den"), op=ALU.mult)
                    b16 = lambda ap: ap.unsqueeze(2).to_broadcast([128, 8, 16])
                    g16 = lambda ap: ap.rearrange("p (g c) -> p g c", c=16)
                    TT = lambda rd, wr, out, in0, in1, op, eng="dve": P.op(eng, "tensor_tensor", rd, wr, out=out, in0=in0, in1=in1, op=op)
                    TT(["bgD", "sm_qr"], ["Bb_a"], Bb["a"][:], g16(bre), b16(V("qr")), ALU.mult)
                    TT(["bgD", "sm_qi"], ["Bb_b"], Bb["b"][:], g16(bim), b16(V("qi")), ALU.mult)
                    TT(["Bb_a", "Bb_b"], ["Bb_r"], Bb["r"][:], Bb["a"][:], Bb["b"][:], ALU.subtract)
                    TT(["bgD", "sm_qr"], ["Bb_a"], Bb["a"][:], g16(bim), b16(V("qr")), ALU.mult)
                    TT(["bgD", "sm_qi"], ["Bb_b"], Bb["b"][:], g16(bre), b16(V("qi")), ALU.mult)
                    TT(["Bb_a", "Bb_b"], ["Bb_i"], Bb["i"][:], Bb["a"][:], Bb["b"][:], ALU.add)
                    for kk in range(8):
                        prk = b16(k9["pr"][:, :, kk])
                        pik = b16(k9["pi"][:, :, kk])
                        xr_k = g16(X_r[:, kk, :])
                        xi_k = g16(X_i[:, kk, :])
                        xt_k = g16(xtmp[:, kk, :])
                        TT(["k9_pr", "Bb_r"], RN(0, 2), xr_k, Bb["r"][:], prk, ALU.mult)
                        TT(["k9_pi", "Bb_i"], ["xtmp"], xt_k, Bb["i"][:], pik, ALU.mult, "pool")
                        TT(RN(0, 2) + ["xtmp"], RN(0, 2), xr_k, xr_k, xt_k, ALU.subtract)
                        TT(["k9_pr", "Bb_i"], RN(2, 4), xi_k, Bb["i"][:], prk, ALU.mult)
                        TT(["k9_pi", "Bb_r"], ["xtmp"], xt_k, Bb["r"][:], pik, ALU.mult, "pool")
                        TT(RN(2, 4) + ["xtmp"], RN(2, 4), xi_k, xi_k, xt_k, ALU.add)
                    for kk in range(9):
                        prk = b16(k9["pr"][:, :, kk])
                        pik = b16(k9["pi"][:, :, kk])
                        zr_k = g16(Zr_t[:, kk, :])
                        zi_k = g16(Zi_t[:, kk, :])
                        xt_k = g16(xtmp[:, kk % 8, :])
                        TT(["k9_pr", "bgD"], ["Zr_t"], zr_k, g16(cre), prk, ALU.mult)
                        TT(["k9_pi", "bgD"], ["xtmp"], xt_k, g16(cim), pik, ALU.mult, "pool")
                        TT(["Zr_t", "xtmp"], ["Zr_t"], zr_k, zr_k, xt_k, ALU.subtract)
                        TT(["k9_pi", "bgD"], ["Zi_t"], zi_k, g16(cre), pik, ALU.mult)
                        TT(["k9_pr", "bgD"], ["xtmp"], xt_k, g16(cim), prk, ALU.mult, "pool")
                        TT(["Zi_t", "xtmp"], ["Zi_t"], zi_k, zi_k, xt_k, ALU.add)
                    P.op("dve", "tensor_scalar", ["bgD"], ["ncim"], out=ncim[:], in0=cim, scalar1=-1.0, scalar2=None, op0=ALU.mult)
                    for half in range(2):
                        for tl in range(4):
                            tau = half * 4 + tl
                            P.mm(pk[:, tl, :], X_r[0:64, tau, :], cre[0:64, :], True, False, RN(0, 2) + ["bgD"], ["pkD"])
                            P.mm(pk[:, tl, :], X_i[0:64, tau, :], ncim[0:64, :], False, True, RN(2, 4) + ["ncim"], ["pkD"])
                        for tl in range(4):
                            tau = half * 4 + tl
                            if tau == 0 and d_ == 0:
                                P.op("dve", "scalar_tensor_tensor", ["identF", "dcolD", "pkD"], ["ktmp"], out=ktmp[:], in0=identF[:],
                                     scalar=dcol[:, ft:ft + 1], in1=pk[:, tl, :], op0=ALU.mult, op1=ALU.add)
                                P.op("dve", "tensor_tensor", ["ktmp", "bmD"], ["Kbd"], out=Kbd[:, d_, tau, :], in0=ktmp[:],
                                     in1=bm[:].rearrange("p g c -> p (g c)"), op=ALU.mult)
                            else:
                                P.op("dve", "tensor_tensor", ["pkD", "bmD"], ["Kbd"], out=Kbd[:, d_, tau, :], in0=pk[:, tl, :],
                                     in1=bm[:].rearrange("p g c -> p (g c)"), op=ALU.mult)
                    for kk in range(8):
                        for ri in range(2):
                            src = (X_r if ri == 0 else X_i)[:, kk, :]
                            slot = (kk * 2 + ri) % 2
                            pt = ptl[slot]
                            P.tr(pt[:, 0, :], src, identF[:], (RN(0, 2) if ri == 0 else RN(2, 4)) + ["identF"], ["ptD%d" % slot])
                            for pr_ in range(4):
                                P.op("dve", "tensor_tensor", ["ptD%d" % slot, "PM%d" % pr_], ["W1"], out=W1[:, kk, ri, pr_, :],
                                     in0=pt[:, 0, :], in1=PM[pr_][:].rearrange("p a b -> p (a b)"), op=ALU.mult)
                    for kk in range(1, 9):
                        for ri in range(2):
                            for pr_ in range(4):
                                cmv = CM[pr_][:].rearrange("p g c -> p (g c)")
                                if ri == 0:
                                    P.op("pool", "tensor_tensor", ["Zr_t", "CM%d" % pr_], ["W2"], out=W2[:, d_, kk, ri, pr_, :],
                                         in0=Zr_t[:, kk, :], in1=cmv, op=ALU.mult)
                                else:
                                    P.op("dve", "scalar_tensor_tensor", ["Zi_t", "CM%d" % pr_], ["W2"], out=W2[:, d_, kk, ri, pr_, :],
                                         in0=Zi_t[:, kk, :], scalar=-1.0, in1=cmv, op0=ALU.mult, op1=ALU.mult)
                    P.op("dve", "tensor_scalar", ["sm_xi"], ["sm_tb"], out=V("tb"), in0=V("xi"), scalar1=8.0 / TWO_PI, scalar2=None, op0=ALU.mult)
                    P.op("dve", "tensor_copy", ["sm_tb"], ["sm_i"], out=smi[:], in_=V("tb"))
                    P.op("dve", "tensor_copy", ["sm_i"], ["sm_t0"], out=V("t0"), in_=smi[:])
                    P.op("dve", "tensor_tensor", ["sm_tb", "sm_t0"], ["sm_t0"], out=V("t0"), in0=V("tb"), in1=V("t0"), op=ALU.subtract)
                    P.op("dve", "tensor_scalar", ["sm_t0"], ["sm_tb"], out=V("tb"), in0=V("t0"), scalar1=32.0, scalar2=None, op0=ALU.mult)
                    P.op("dve", "tensor_copy", ["sm_tb"], ["sm_i"], out=smi[:], in_=V("tb"))
                    P.op("dve", "tensor_copy", ["sm_i"], ["sm_t32"], out=V("t32"), in_=smi[:])
                    P.op("dve", "tensor_tensor", ["sm_tb", "sm_t32"], ["sm_t32"], out=V("t32"), in0=V("tb"), in1=V("t32"), op=ALU.subtract)
                    for h in range(2):
                        hs = slice(h * 64, (h + 1) * 64)
                        P.op("dve", "tensor_copy", ["sm_t0"], ["colsT"], out=colsT[hs, 0, :], in_=sml["t0"][hs, h::2])
                        P.op("dve", "tensor_copy", ["sm_t32"], ["colsT"], out=colsT[hs, 1, :], in_=sml["t32"][hs, h::2])
                        P.op("dve", "tensor_copy", ["k9_er"], ["colsT"], out=colsT[hs, 2, :], in_=k9["er"][hs, h::2, 8])
                    if DST < 2:
                        continue
                    for ui, s_ in enumerate((0, 1) if d_ == 0 else (1, 0)):
                        blk0 = s_ * NBS
                        for pr_ in range(4):
                            for ri in range(2):
                                pb = psq % 2
                                psq += 1
                                for j in range(8):
                                    kk = (7 - j) if d_ == 0 else j
                                    P.mm(pS[pb][:, 0:NBS], W1[:, kk, ri, pr_, :], u3[:, j, blk0:blk0 + NBS], j == 0, j == 7,
                                         ["W1", "uft"], ["pSD%d" % pb])
                                P.op("act", "activation", ["pSD%d" % pb], ["S_D%d" % pr_], out=S[:, pr_, ri, :], in_=pS[pb][:, 0:NBS], func=AF.Copy)
                        if DST < 3:
                            continue
                        for pr_ in range(4):
                            tu, ti_, tf, cs, sn, T_r, T_i, g_r, g_i, m1, m2 = [T(i) for i in range(11)]
                            tii = ti_.bitcast(I32)
                            t0c, t32c, decc = colsT[:, 0, pr_:pr_ + 1], colsT[:, 1, pr_:pr_ + 1], colsT[:, 2, pr_:pr_ + 1]
                            P.op("pool", "tensor_scalar", ["BH%d" % s_, "colsT"], ["R0"], out=tu, in0=BHs[s_][:], scalar1=t32c, scalar2=None, op0=ALU.mult)
                            P.op("dve", "scalar_tensor_tensor", ["BL", "colsT", "R0"], ["R0"], out=tu, in0=BL[:], scalar=t0c, in1=tu, op0=ALU.mult, op1=ALU.add)
                            for (dst, dn, off) in ((sn, "R4", 0.0), (cs, "R3", 0.25)):
                                if off:
                                    P.op("pool", "tensor_scalar", ["R0"], ["R0"], out=tu, in0=tu, scalar1=off, scalar2=None, op0=ALU.add)
                                P.op("dve", "tensor_copy", ["R0"], ["R1"], out=tii, in_=tu)
                                P.op("dve", "tensor_copy", ["R1"], ["R2"], out=tf, in_=tii)
                                P.op("pool", "tensor_tensor", ["R0", "R2"], ["R2"], out=tf, in0=tu, in1=tf, op=ALU.subtract)
                                P.op("act", "activation", ["R2"], [dn], out=dst, in_=tf, func=AF.Sin, scale=TWO_PI)
                            S_r, S_i = S[:, pr_, 0, :], S[:, pr_, 1, :]
                            sg = 1.0 if d_ == 0 else -1.0
                            sop = ALU.add if d_ == 0 else ALU.subtract
                            sop2 = ALU.subtract if d_ == 0 else ALU.add
                            sn_ = "S_D%d" % pr_
                            TT([sn_, "R3"], ["R5"], T_r, S_r, cs, ALU.mult)
                            TT([sn_, "R4"], ["R9"], m1, S_i, sn, ALU.mult, "pool")
                            TT(["R5", "R9"], ["R5"], T_r, T_r, m1, sop)
                            TT([sn_, "R3"], ["R6"], T_i, S_i, cs, ALU.mult)
                            TT([sn_, "R4"], ["R10"], m2, S_r, sn, ALU.mult, "pool")
                            TT(["R6", "R10"], ["R6"], T_i, T_i, m2, sop2)
                            rv = (lambda ap: ap) if d_ == 0 else (lambda ap: ap[:, ::-1])
                            dbc = decc.to_broadcast([128, NBS])
                            for (src, sname, dst, dname, ri) in ((T_r, "R5", g_r, "R7", 0), (T_i, "R6", g_i, "R8", 1)):
                                init = 0.0 if ui == 0 else carry[:, pr_, ri:ri + 1]
                                P.op("dve", "tensor_tensor_scan", [sname, "colsT", "carryD"], [dname], out=rv(dst), data0=dbc, data1=rv(src),
                                     initial=init, op0=ALU.mult, op1=ALU.add)
                                if ui == 0:
                                    lastc = (NBS - 1) if d_ == 0 else 0
                                    P.op("dve", "tensor_scalar", [dname, "flgD"], ["carryD"], out=carry[:, pr_, ri:ri + 1], in0=dst[:, lastc:lastc + 1],
                                         scalar1=flg[:, 0:1], scalar2=None, op0=ALU.mult)
                            if d_ == 0:
                                o_r = HB[:, 0, pr_, 0, blk0 + 1:blk0 + NBS + 1]
                                o_i = HB[:, 0, pr_, 1, blk0 + 1:blk0 + NBS + 1]
                                sl_ = slice(0, NBS)
                            elif s_ == 1:
                                o_r = HB[:, 1, pr_, 0, NBS - 1:NB - 1]
                                o_i = HB[:, 1, pr_, 1, NBS - 1:NB - 1]
                                sl_ = slice(0, NBS)
                            else:
                                o_r = HB[:, 1, pr_, 0, 0:NBS - 1]
                                o_i = HB[:, 1, pr_, 1, 0:NBS - 1]
                                sl_ = slice(1, NBS)
                            hn = "HB%d_%d" % (d_, pr_)
                            TT(["R7", "R3"], ["R9"], m1, g_r, cs, ALU.mult)
                            TT(["R8", "R4"], ["R10"], m2, g_i, sn, ALU.mult, "pool")
                            TT(["R9", "R10"], [hn], o_r, m1[:, sl_], m2[:, sl_], sop2)
                            TT(["R8", "R3"], ["R9"], m1, g_i, cs, ALU.mult)
                            TT(["R7", "R4"], ["R10"], m2, g_r, sn, ALU.mult, "pool")
                            TT(["R9", "R10"], [hn], o_i, m1[:, sl_], m2[:, sl_], sop)
                            if ui == 1:
                                zc = 0 if d_ == 0 else NB - 1
                                fc = NBS if d_ == 0 else NBS - 1
                                P.op("dve", "memset", [], [hn], HB[:, d_, pr_, :, zc:zc + 1], 0.0)
                                P.op("dve", "tensor_scalar", [hn, "flgD"], [hn], out=HB[:, d_, pr_, :, fc:fc + 1], in0=HB[:, d_, pr_, :, fc:fc + 1],
                                     scalar1=flg[:, 0:1], scalar2=None, op0=ALU.mult)
                if DST < 4:
                    continue
                hall = ["HB%d_%d" % (a, b_) for a in range(2) for b_ in range(4)]
                for s_ in range(2):
                    blk0 = s_ * NBS
                    for i in range(8):
                        pb = poq % 2
                        poq += 1
                        mms = []
                        for j in range(0, i + 1):
                            mms.append((Kbd[:, 0, i - j, :], u3[:, j, blk0:blk0 + NBS]))
                        for j in range(i, 8):
                            mms.append((Kbd[:, 1, j - i, :], u3[:, j, blk0:blk0 + NBS]))
                        for pr_ in range(4):
                            for ri in range(2):
                                mms.append((W2[:, 0, i + 1, ri, pr_, :], HB[:, 0, pr_, ri, blk0:blk0 + NBS]))
                                mms.append((W2[:, 1, 8 - i, ri, pr_, :], HB[:, 1, pr_, ri, blk0:blk0 + NBS]))
                        for mi, (l_, r_) in enumerate(mms):
                            P.mm(po[pb][:, 0:NBS], l_, r_, mi == 0, mi == len(mms) - 1, ["Kbd", "W2", "uft"] + hall, ["poD%d" % pb])
                        v = po[pb][:, 0:NBS]
                        P.op("act", "activation", ["poD%d" % pb], ["glD0"], out=gl[0][:], in_=v, func=AF.Square)
                        P.op("dve", "tensor_scalar", ["glD0"], ["glD0"], out=gl[0][:], in0=gl[0][:], scalar1=0.044715, scalar2=1.0, op0=ALU.mult, op1=ALU.add)
                        P.op("dve", "tensor_tensor", ["glD0", "poD%d" % pb], ["glD1"], out=gl[1][:], in0=gl[0][:], in1=v, op=ALU.mult)
                        P.op("act", "activation", ["glD1"], ["glD2"], out=gl[2][:], in_=gl[1][:], func=AF.Sigmoid, scale=1.5957691216057308)
                        P.op("dve", "tensor_tensor", ["glD2", "poD%d" % pb], ["stgD"], out=stg[:, :, i], in0=gl[2][:], in1=v, op=ALU.mult)
                    P.dma("sp", y5gT[ft * 128:(ft + 1) * 128, s_ * LSEG:(s_ + 1) * LSEG], stg[:].rearrange("p b i -> p (b i)"), ["stgD"], ["y5gT"])
        if cfg.get("dump"):
            for i_, o_ in enumerate(P.ops):
                print(i_, o_.eng, getattr(o_, 'desc', 'mm/dma') if hasattr(o_, 'desc') else '-', o_.reads, o_.writes)
        P.emit()
        P.limit = None

        with ExitStack() as st:
            sb = lambda n, s, d=F32: st.enter_context(nc.sbuf_tensor(n, list(s), d))
            gA = sb("gA", [128, 16])
            gM = sb("gM", [128, 16])
            P.dma("sp", gA[:], norm_attn, [], ["gA"])
            P.dma("sp", gM[:], norm_mem, [], ["gM"])
            wst2 = [sb("wst2_%d" % i, [128, 2048]) for i in range(2)]
            wbf2 = [sb("wbf2_%d" % i, [128, 2048], BF16) for i in range(2)]
            ci = 0
            ceng = _rr(["dve", "pool"])
            for (src, dst, gn_, gname, kdim, ncol) in ((w_glu, w_glu_bf, None, None, 1024, 1024), (w_out, w_out_bf, None, None, 2048, 2048),
                                                       (w_q, w_q_bf, gA, "gA", 2048, 2048), (w_k, w_k_bf, gM, "gM", 2048, 2048),
                                                       (w_v, w_v_bf, gM, "gM", 2048, 2048), (w_o, w_o_bf, None, None, 2048, 2048)):
                for kc in range(kdim // 128):
                    b = ci % 2
                    ci += 1
                    P.dma("sp", wst2[b][:, 0:ncol], src[kc * 128:(kc + 1) * 128, :], [], ["wst2_%d" % b])
                    if gn_ is None:
                        P.op(ceng(), "tensor_copy", ["wst2_%d" % b], ["wbf2_%d" % b], out=wbf2[b][:, 0:ncol], in_=wst2[b][:, 0:ncol])
                    else:
                        P.op(ceng(), "tensor_scalar", ["wst2_%d" % b, gname], ["wbf2_%d" % b], out=wbf2[b][:, 0:ncol], in0=wst2[b][:, 0:ncol],
                             scalar1=gn_[:, kc:kc + 1], scalar2=None, op0=ALU.mult)
                    P.dma("sp", dst[:, :, kc, :].rearrange("g p n -> p g n"), wbf2[b][:, 0:ncol].rearrange("p (g n) -> p g n", n=512),
                          ["wbf2_%d" % b], [dst.tensor.name])
        P.emit()

        with ExitStack() as st:
            sb = lambda n, s, d=F32: st.enter_context(nc.sbuf_tensor(n, list(s), d))
            ps = lambda n, s, d=F32: st.enter_context(nc.psum_tensor(n, list(s), d))
            ident = sb("identM", [128, 128], BF16)
            P.op("pool", "memset", [], ["identM"], ident[:], 1.0)
            P.op("pool", "affine_select", ["identM"], ["identM"], out=ident[:], in_=ident[:], pattern=[[-1, 128]],
                 compare_op=ALU.is_equal, fill=0.0, base=0, channel_multiplier=1)
            mt = sb("mtM", [128, 2048])
            junk = sb("junkM", [128, 2048], BF16)
            ssm = sb("ssM", [128, 1])
            mn = sb("mnM", [128, 2048], BF16)
            memT = sb("memT", [128, 16, 256], BF16)
            wck = [sb("wckM%d" % i, [128, 16, 512], BF16) for i in range(2)]
            kTs = sb("kTs", [128, 16, 256], BF16)
            vs = sb("vs", [128, 2, 2048], BF16)
            ptm = [ps("ptM%d" % i, [128, 1024], BF16) for i in range(2)]
            pmm_ = [ps("pmM%d" % i, [128, 512]) for i in range(2)]
            tq = 0
            pq = 0
            wq_ = 0
            for sg in range(2):
                for mc in range(2):
                    P.dma("sp", mt[:], mem_in[sg, mc * 128:(mc + 1) * 128, :], [], ["mtM"])
                    P.op("act", "activation", ["mtM"], ["junkM", "ssM"], out=junk[:], in_=mt[:], func=AF.Square, accum_out=ssm[:])
                    P.op("dve", "tensor_scalar", ["ssM"], ["ssM"], out=ssm[:], in0=ssm[:], scalar1=1.0 / D, scalar2=EPS, op0=ALU.mult, op1=ALU.add)
                    P.op("act", "activation", ["ssM"], ["ssM"], out=ssm[:], in_=ssm[:], func=AF.Sqrt)
                    P.op("dve", "reciprocal", ["ssM"], ["ssM"], out=ssm[:], in_=ssm[:])
                    P.op("dve", "tensor_scalar", ["mtM", "ssM"], ["mnM"], out=mn[:], in0=mt[:], scalar1=ssm[:, 0:1], scalar2=None, op0=ALU.mult)
                    for q4 in range(4):
                        pb = tq % 2
                        tq += 1
                        for j in range(4):
                            kc = q4 * 4 + j
                            P.tr(ptm[pb][:, j * 128:(j + 1) * 128], mn[:, kc * 128:(kc + 1) * 128], ident[:], ["mnM", "identM"], ["ptM%d" % pb])
                        P.op("act", "activation", ["ptM%d" % pb], ["memT"], out=memT[:, q4 * 4:(q4 + 1) * 4, mc * 128:(mc + 1) * 128],
                             in_=ptm[pb][:, 0:512].rearrange("p (j n) -> p j n", n=128), func=AF.Copy)
                for g in range(4):
                    wb = wq_ % 2
                    wq_ += 1
                    P.dma("sp", wck[wb][:], w_k_bf[g], ["w_k_bf"], ["wckM%d" % wb])
                    for mcc in range(4):
                        pb = pq % 2
                        pq += 1
                        for kc in range(16):
                            P.mm(pmm_[pb][:, 0:256], wck[wb][:, kc, mcc * 128:(mcc + 1) * 128], memT[:, kc, :], kc == 0, kc == 15,
                                 ["wckM%d" % wb, "memT"], ["pmM%d" % pb])
                        P.op("act", "activation", ["pmM%d" % pb], ["kTs"], out=kTs[:, g * 4 + mcc, :], in_=pmm_[pb][:, 0:256], func=AF.Copy)
                for g in range(4):
                    wb = wq_ % 2
                    wq_ += 1
                    P.dma("sp", wck[wb][:], w_v_bf[g], ["w_v_bf"], ["wckM%d" % wb])
                    for mc in range(2):
                        pb = pq % 2
                        pq += 1
                        for kc in range(16):
                            P.mm(pmm_[pb][:], memT[:, kc, mc * 128:(mc + 1) * 128], wck[wb][:, kc, :], kc == 0, kc == 15,
                                 ["wckM%d" % wb, "memT"], ["pmM%d" % pb])
                        P.op("dve", "tensor_copy", ["pmM%d" % pb], ["vs"], out=vs[:, mc, g * 512:(g + 1) * 512], in_=pmm_[pb][:])
                P.dma("sp", kT_d[sg], kTs[:], ["kTs"], ["kT_d"])
                P.dma("sp", v_d[sg], vs[:], ["vs"], ["v_d"])
        P.emit()

        with ExitStack() as st:
            sb = lambda n, s, d=F32: st.enter_context(nc.sbuf_tensor(n, list(s), d))
            ps = lambda n, s, d=F32: st.enter_context(nc.psum_tensor(n, list(s), d))
            wgl = sb("wgl", [128, 8, 1024], BF16)
            for g_ in range(2):
                P.dma("sp", wgl[:, :, g_ * 512:(g_ + 1) * 512], w_glu_bf[g_], ["w_glu_bf"], ["wgl"])
            onesb = sb("onesbE", [128, 128], BF16)
            P.op("pool", "memset", [], ["onesbE"], onesb[:], 1.0)
            g5 = sb("g5", [128, 8])
            P.dma("sp", g5[:], s5_ncol, [], ["g5"])
            y5 = [sb("y5E%d" % i, [128, 8, 512], BF16) for i in range(2)]
            y2 = sb("y2E", [128, 8, 512])
            sig = [sb("sigE%d" % i, [128, 512]) for i in range(2)]
            sqb = [sb("sqE%d" % i, [128, 512], BF16) for i in range(2)]
            rs = sb("rsE", [128, 512])
            pg = [ps("pgE%d" % i, [128, 512]) for i in range(2)]
            pss = ps("pssE", [128, 512])
            for s in range(NST):
                t0 = s * 512
                b = s % 2
                P.dma("sp", y5[b][:], y5gT[:, t0:t0 + 512].rearrange("(f p) t -> p f t", p=128), ["y5gT"], ["y5E%d" % b])
                for n_ in range(8):
                    q = n_ % 2
                    for kc in range(8):
                        P.mm(pg[q][:], wgl[:, kc, n_ * 128:(n_ + 1) * 128], y5[b][:, kc, :], kc == 0, kc == 7, ["wgl", "y5E%d" % b], ["pgE%d" % q])
                    P.op("act", "activation", ["pgE%d" % q], ["sigE%d" % q], out=sig[q][:], in_=pg[q][:], func=AF.Sigmoid)
                    P.op("dve", "tensor_tensor", ["y5E%d" % b, "sigE%d" % q], ["y2E"], out=y2[:, n_, :], in0=y5[b][:, n_, :], in1=sig[q][:], op=ALU.mult)
                    P.op("act", "activation", ["y2E"], ["sqE%d" % q], out=sqb[q][:], in_=y2[:, n_, :], func=AF.Square)
                    P.mm(pss[:], onesb[:], sqb[q][:], n_ == 0, n_ == 7, ["onesbE", "sqE%d" % q], ["pssE"])
                P.op("dve", "tensor_scalar", ["pssE"], ["rsE"], out=rs[:], in0=pss[:], scalar1=1.0 / 1024, scalar2=EPS, op0=ALU.mult, op1=ALU.add)
                P.op("act", "activation", ["rsE"], ["rsE"], out=rs[:], in_=rs[:], func=AF.Sqrt)
                P.op("dve", "reciprocal", ["rsE"], ["rsE"], out=rs[:], in_=rs[:])
                for n_ in range(8):
                    P.op("dve", "scalar_tensor_tensor", ["y2E", "g5", "rsE"], ["y5E%d" % b], out=y5[b][:, n_, :], in0=y2[:, n_, :],
                         scalar=g5[:, n_:n_ + 1], in1=rs[:], op0=ALU.mult, op1=ALU.mult)
                P.dma("sp", y5nT[:, t0:t0 + 512].rearrange("(f p) t -> p f t", p=128), y5[b][:], ["y5E%d" % b], ["y5nT"])
        P.emit()

        NTT = LT // 128
        with ExitStack() as st:
            sb = lambda n, s, d=F32: st.enter_context(nc.sbuf_tensor(n, list(s), d))
            ps = lambda n, s, d=F32: st.enter_context(nc.psum_tensor(n, list(s), d))
            ident = sb("identE", [128, 128], BF16)
            P.op("pool", "memset", [], ["identE"], ident[:], 1.0)
            P.op("pool", "affine_select", ["identE"], ["identE"], out=ident[:], in_=ident[:], pattern=[[-1, 128]],
                 compare_op=ALU.is_equal, fill=0.0, base=0, channel_multiplier=1)
            onesb = sb("onesb2", [128, 128], BF16)
            P.op("pool", "memset", [], ["onesb2"], onesb[:], 1.0)
            gff = sb("gff", [128, 2048])
            P.dma("sp", gff[:], nffn_rep, [], ["gff"])
            wrs = sb("wrs", [128, 16, 16])
            wrb = sb("wrb", [128, 16, 16], BF16)
            P.dma("sp", wrs[:], w_router_l, [], ["wrs"])
            P.op("dve", "tensor_copy", ["wrs"], ["wrb"], out=wrb[:], in_=wrs[:])
            zrow = sb("zrow", [128, 2048])
            zrowb = sb("zrowb", [128, 2048], BF16)
            P.op("pool", "memset", [], ["zrow"], zrow[:], 0.0)
            P.op("pool", "memset", [], ["zrowb"], zrowb[:], 0.0)
            for r_ in range(0, CAPL, 128):
                for g_ in range(8):
                    P.dma("sp", acc_l[g_][LT + r_:LT + r_ + 128, :], zrow[:, 0:256], ["zrow"], ["acc"])
                P.dma("sp", hn3_bf[LT + r_:LT + r_ + 128, :], zrowb[:], ["zrowb"], ["hn3_bf"])
            wch = [sb("wch%d" % i, [128, 16, 512], BF16) for i in range(2)]
            x1 = sb("x1", [128, 4, 2048])
            ysd = sb("ysd", [128, 4, 1024], BF16)
            yT = sb("yT", [128, 16, 512], BF16)
            hn = sb("hnE", [128, 2048], BF16)
            junk = sb("junkE", [128, 2048], BF16)
            ss = sb("ssE", [128, 1])
            hnT = sb("hnT", [128, 16, 512], BF16)
            qT = sb("qT", [128, 16, 512], BF16)
            kTs = sb("kTs2", [128, 16, 256], BF16)
            vs = sb("vs2", [128, 2, 2048], BF16)
            eT = [sb("eT%d" % i, [128, 512], BF16) for i in range(2)]
            rden = sb("rden", [128, 512])
            hT3 = sb("hT3", [128, 16, 128], BF16)
            pe_ = sb("peE", [128, 16])
            psum_ = sb("psumE", [128, 1])
            ptr = [ps("ptrE%d" % i, [128, 1024], BF16) for i in range(2)]
            pmm = [ps("pmmE%d" % i, [128, 512]) for i in range(4)]
            prt = ps("prtE", [128, 512])
            tq = [0]
            pq = [0]
            wq_ = [0]
            cpe = _rr(["act", "dve"])

            def nxt(c, n):
                v = c[0] % n
                c[0] += 1
                return v

            def rms_to(tt, out_ap, names_w, extra=None):
                P.op("act", "activation", ["x1_%d" % tt], ["junkE", "ssE"], out=junk[:], in_=x1[:, tt, :], func=AF.Square, accum_out=ss[:])
                P.op("dve", "tensor_scalar", ["ssE"], ["ssE"], out=ss[:], in0=ss[:], scalar1=1.0 / D, scalar2=EPS, op0=ALU.mult, op1=ALU.add)
                P.op("act", "activation", ["ssE"], ["ssE"], out=ss[:], in_=ss[:], func=AF.Sqrt)
                P.op("dve", "reciprocal", ["ssE"], ["ssE"], out=ss[:], in_=ss[:])
                if extra is None:
                    P.op("dve", "tensor_scalar", ["x1_%d" % tt, "ssE"], names_w, out=out_ap, in0=x1[:, tt, :], scalar1=ss[:, 0:1], scalar2=None, op0=ALU.mult)
                else:
                    P.op("dve", "scalar_tensor_tensor", ["x1_%d" % tt, "ssE", "gff"], names_w, out=out_ap, in0=x1[:, tt, :], scalar=ss[:, 0:1],
                         in1=extra, op0=ALU.mult, op1=ALU.mult)

            def transposes16(src_tile, src_name, dst_fn, dst_name):
                for q4 in range(4):
                    pb = nxt(tq, 2)
                    for j in range(4):
                        kc = q4 * 4 + j
                        P.tr(ptr[pb][:, j * 128:(j + 1) * 128], src_tile[:, kc * 128:(kc + 1) * 128], ident[:], [src_name, "identE"], ["ptrE%d" % pb])
                    ce = cpe()
                    src = ptr[pb][:, 0:512].rearrange("p (j n) -> p j n", n=128)
                    if ce == "act":
                        P.op("act", "activation", ["ptrE%d" % pb], [dst_name], out=dst_fn(q4), in_=src, func=AF.Copy)
                    else:
                        P.op("dve", "tensor_copy", ["ptrE%d" % pb], [dst_name], out=dst_fn(q4), in_=src)

            def proj_tok(w_d, wname, lhs_tile, lhs_name):
                for dch in range(4):
                    wb = nxt(wq_, 2)
                    P.dma("sp", wch[wb][:], w_d[dch], [wname], ["wch%d" % wb])
                    for tt in range(4):
                        pb = nxt(pq, 4)
                        for kc in range(16):
                            P.mm(pmm[pb][:], lhs_tile[:, kc, tt * 128:(tt + 1) * 128], wch[wb][:, kc, :], kc == 0, kc == 15,
                                 [lhs_name, "wch%d" % wb], ["pmmE%d" % pb])
                        P.op("dve", "tensor_tensor", ["x1_%d" % tt, "pmmE%d" % pb], ["x1_%d" % tt], out=x1[:, tt, dch * 512:(dch + 1) * 512],
                             in0=x1[:, tt, dch * 512:(dch + 1) * 512], in1=pmm[pb][:], op=ALU.add)

            cur_seg = -1
            for s in range(NST):
                t0 = s * 512
                sg = t0 // LSEG
                if sg != cur_seg:
                    cur_seg = sg
                    P.dma("sp", kTs[:], kT_d[sg], ["kT_d"], ["kTs2"])
                    P.dma("sp", vs[:], v_d[sg], ["v_d"], ["vs2"])
                for tt in range(4):
                    P.dma("sp", x1[:, tt, :], x_in[t0 + tt * 128:t0 + (tt + 1) * 128, :], [], ["x1_%d" % tt])
                P.dma("sp", ysd[:], yssd[t0:t0 + 512, :].rearrange("(t p) c -> p t c", p=128), ["yssd"], ["ysd"])
                P.dma("sp", yT[:, 8:16, :], y5nT[:, t0:t0 + 512].rearrange("(f p) t -> p f t", p=128), ["y5nT"], ["yT"])
                for tt in range(4):
                    for q2 in range(2):
                        pb = nxt(tq, 2)
                        for j in range(4):
                            kc = q2 * 4 + j
                            P.tr(ptr[pb][:, j * 128:(j + 1) * 128], ysd[:, tt, kc * 128:(kc + 1) * 128], ident[:], ["ysd", "identE"], ["ptrE%d" % pb])
                        P.op("act", "activation", ["ptrE%d" % pb], ["yT"], out=yT[:, q2 * 4:(q2 + 1) * 4, tt * 128:(tt + 1) * 128],
                             in_=ptr[pb][:, 0:512].rearrange("p (j n) -> p j n", n=128), func=AF.Copy)
                proj_tok(w_out_bf, "w_out_bf", yT, "yT")
                for tt in range(4):
                    rms_to(tt, hn[:], ["hnE"])
                    transposes16(hn, "hnE", lambda q4, tt=tt: hnT[:, q4 * 4:(q4 + 1) * 4, tt * 128:(tt + 1) * 128], "hnT")
                for g in range(4):
                    wb = nxt(wq_, 2)
                    P.dma("sp", wch[wb][:], w_q_bf[g], ["w_q_bf"], ["wch%d" % wb])
                    for mcc in range(4):
                        pb = nxt(pq, 4)
                        for kc in range(16):
                            P.mm(pmm[pb][:], wch[wb][:, kc, mcc * 128:(mcc + 1) * 128], hnT[:, kc, :], kc == 0, kc == 15,
                                 ["wch%d" % wb, "hnT"], ["pmmE%d" % pb])
                        P.op("act", "activation", ["pmmE%d" % pb], ["qT"], out=qT[:, g * 4 + mcc, :], in_=pmm[pb][:], func=AF.Copy)
                for h in range(4):
                    for mck in range(2):
                        pb = nxt(pq, 4)
                        for dc in range(4):
                            P.mm(pmm[pb][:], kTs[:, h * 4 + dc, mck * 128:(mck + 1) * 128], qT[:, h * 4 + dc, :], dc == 0, dc == 3,
                                 ["kTs2", "qT"], ["pmmE%d" % pb])
                        P.op("act", "activation", ["pmmE%d" % pb], ["eT%d" % mck], out=eT[mck][:], in_=pmm[pb][:], func=AF.Exp, scale=512 ** -0.5)
                    pb = nxt(pq, 4)
                    for mck in range(2):
                        P.mm(pmm[pb][:], onesb[:], eT[mck][:], mck == 0, mck == 1, ["onesb2", "eT%d" % mck], ["pmmE%d" % pb])
                    P.op("dve", "reciprocal", ["pmmE%d" % pb], ["rden"], out=rden[:], in_=pmm[pb][:])
                    for dvc in range(4):
                        pb = nxt(pq, 4)
                        for mck in range(2):
                            P.mm(pmm[pb][:], vs[:, mck, h * 512 + dvc * 128:h * 512 + (dvc + 1) * 128], eT[mck][:], mck == 0, mck == 1,
                                 ["vs2", "eT%d" % mck], ["pmmE%d" % pb])
                        P.op("dve", "tensor_tensor", ["pmmE%d" % pb, "rden"], ["hnT"], out=hnT[:, h * 4 + dvc, :], in0=pmm[pb][:], in1=rden[:], op=ALU.mult)
                proj_tok(w_o_bf, "w_o_bf", hnT, "hnT")
                for tt in range(4):
                    r0 = t0 + tt * 128
                    for g_ in range(8):
                        P.dma("sp", acc_l[g_][r0:r0 + 128, :], x1[:, tt, g_ * 256:(g_ + 1) * 256], ["x1_%d" % tt], ["acc"])
                    rms_to(tt, hn[:], ["hnE"], extra=gff[:])
                    P.dma("sp", hn3_bf[r0:r0 + 128, :], hn[:], ["hnE"], ["hn3_bf"])
                    transposes16(hn, "hnE", lambda q4: hT3[:, q4 * 4:(q4 + 1) * 4, :], "hT3")
                    for kc in range(16):
                        P.mm(prt[:, 0:16], hT3[:, kc, :], wrb[:, kc, :], kc == 0, kc == 15, ["hT3", "wrb"], ["prtE"])
                    P.op("act", "activation", ["prtE"], ["peE", "psumE"], out=pe_[:], in_=prt[:, 0:16], func=AF.Exp, accum_out=psum_[:])
                    P.op("dve", "reciprocal", ["psumE"], ["psumE"], out=psum_[:], in_=psum_[:])
                    P.op("dve", "tensor_scalar", ["peE", "psumE"], ["peE"], out=pe_[:], in0=pe_[:], scalar1=psum_[:, 0:1], scalar2=None, op0=ALU.mult)
                    P.dma("sp", probs_loc[r0:r0 + 128, :], pe_[:], ["peE"], ["probs_loc"])
        P.emit()

        GSZ = cfg.get("GSZ", 1)
        NG = GSZ * LT
        JG = NG // 128
        CAPG = NG // 8
        NCHK = CAPL // 128
        if GSZ > 1:
            P.add("pool", lambda e: e.collective_compute("AllGather", ALU.bypass, replica_groups=cfg["groups"], ins=[probs_loc], outs=[probs_all]),
                  reads=["probs_loc"], writes=["probs_all"], own_sem=P.csem)
            pall = probs_all
        else:
            pall = probs_loc
        with ExitStack() as st:
            sb = lambda n, s, d=F32: st.enter_context(nc.sbuf_tensor(n, list(s), d))
            ps = lambda n, s, d=F32: st.enter_context(nc.psum_tensor(n, list(s), d))
            PA = sb("PA", [128, JG, 16])
            cmpt = sb("cmpt", [128, JG, 16])
            P.dma("sp", PA[:], pall.rearrange("(p j) e -> p j e", j=JG), ["probs_all", "probs_loc"], ["PA"])
            onesF = sb("onesFF", [128, 128])
            P.op("pool", "memset", [], ["onesFF"], onesF[:], 1.0)
            lo, hi, mid, ge, dd, cntp = [sb("bs_" + n_, [128, 16]) for n_ in ("lo", "hi", "mid", "ge", "dd", "cntp")]
            P.op("dve", "memset", [], ["bs_lo"], lo[:], 0.0)
            P.op("dve", "memset", [], ["bs_hi"], hi[:], 1.0)
            ptot = ps("ptot", [128, 512])
            for it_ in range(32):
                P.op("dve", "tensor_tensor", ["bs_lo", "bs_hi"], ["bs_mid"], out=mid[:], in0=lo[:], in1=hi[:], op=ALU.add)
                P.op("dve", "tensor_scalar", ["bs_mid"], ["bs_mid"], out=mid[:], in0=mid[:], scalar1=0.5, scalar2=None, op0=ALU.mult)
                P.op("dve", "tensor_tensor", ["PA", "bs_mid"], ["cmpt"], out=cmpt[:], in0=PA[:], in1=mid[:].unsqueeze(1).to_broadcast([128, JG, 16]), op=ALU.is_ge)
                P.op("dve", "tensor_reduce", ["cmpt"], ["bs_cntp"], out=cntp[:], in_=cmpt[:].rearrange("p j e -> p e j"), op=ALU.add, axis=AX.X)
                P.mm(ptot[:, 0:16], onesF[:], cntp[:], True, True, ["onesFF", "bs_cntp"], ["ptot"])
                P.op("dve", "tensor_scalar", ["ptot"], ["bs_ge"], out=ge[:], in0=ptot[:, 0:16], scalar1=float(CAPG), scalar2=None, op0=ALU.is_ge)
                P.op("dve", "tensor_tensor", ["bs_mid", "bs_lo"], ["bs_dd"], out=dd[:], in0=mid[:], in1=lo[:], op=ALU.subtract)
                P.op("dve", "tensor_tensor", ["bs_dd", "bs_ge"], ["bs_dd"], out=dd[:], in0=dd[:], in1=ge[:], op=ALU.mult)
                P.op("dve", "tensor_tensor", ["bs_lo", "bs_dd"], ["bs_lo"], out=lo[:], in0=lo[:], in1=dd[:], op=ALU.add)
                P.op("dve", "tensor_tensor", ["bs_hi", "bs_mid"], ["bs_dd"], out=dd[:], in0=hi[:], in1=mid[:], op=ALU.subtract)
                P.op("dve", "tensor_tensor", ["bs_dd", "bs_ge"], ["bs_dd"], out=dd[:], in0=dd[:], in1=ge[:], op=ALU.mult)
                P.op("dve", "tensor_tensor", ["bs_mid", "bs_dd"], ["bs_hi"], out=hi[:], in0=mid[:], in1=dd[:], op=ALU.add)
            PL = sb("PL", [128, NTT, 16])
            msk = sb("msk", [128, NTT, 16])
            gat = sb("gat", [128, NTT, 16])
            mskb = sb("mskb", [128, NTT, 16], BF16)
            P.dma("sp", PL[:], probs_loc.rearrange("(t p) e -> p t e", p=128), ["probs_loc"], ["PL"])
            P.op("dve", "tensor_tensor", ["PL", "bs_lo"], ["msk"], out=msk[:], in0=PL[:], in1=lo[:].unsqueeze(1).to_broadcast([128, NTT, 16]), op=ALU.is_ge)
            P.op("dve", "tensor_tensor", ["PL", "msk"], ["gat"], out=gat[:], in0=PL[:], in1=msk[:], op=ALU.mult)
            P.op("dve", "tensor_copy", ["msk"], ["mskb"], out=mskb[:], in_=msk[:])
            triS = sb("triS", [128, 128], BF16)
            onesb = sb("onesbF", [128, 128], BF16)
            P.op("pool", "memset", [], ["onesbF"], onesb[:], 1.0)
            P.op("pool", "memset", [], ["triS"], triS[:], 1.0)
            P.op("pool", "affine_select", ["triS"], ["triS"], out=triS[:], in_=triS[:], pattern=[[1, 128]], compare_op=ALU.is_ge, fill=0.0,
                 base=-1, channel_multiplier=-1)
            posi = sb("posi", [128, NTT, 16])
            tots = sb("tots", [128, NTT, 16])
            cum = sb("cum", [128, NTT, 16])
            pps = [ps("ppsF%d" % i, [128, 512]) for i in range(2)]
            mflat = mskb[:].rearrange("p t e -> p (t e)")
            NTE = NTT * 16
            for c0 in range(0, NTE, 512):
                c1 = min(c0 + 512, NTE)
                P.mm(pps[0][:, 0:c1 - c0], triS[:], mflat[:, c0:c1], True, True, ["triS", "mskb"], ["ppsF0"])
                P.op("dve", "tensor_copy", ["ppsF0"], ["posi"], out=posi[:].rearrange("p t e -> p (t e)")[:, c0:c1], in_=pps[0][:, 0:c1 - c0])
                P.mm(pps[1][:, 0:c1 - c0], onesb[:], mflat[:, c0:c1], True, True, ["onesbF", "mskb"], ["ppsF1"])
                P.op("dve", "tensor_copy", ["ppsF1"], ["tots"], out=tots[:].rearrange("p t e -> p (t e)")[:, c0:c1], in_=pps[1][:, 0:c1 - c0])
            onecol = sb("onecol", [128, 1])
            P.op("dve", "memset", [], ["onecol"], onecol[:], 1.0)
            for e_ in range(16):
                P.op("dve", "tensor_tensor_scan", ["tots", "onecol"], ["cum"], out=cum[:, :, e_], data0=onecol[:, 0:1].to_broadcast([128, NTT]),
                     data1=tots[:, :, e_], initial=0.0, op0=ALU.mult, op1=ALU.add)
            P.op("dve", "tensor_tensor", ["cum", "tots"], ["cum"], out=cum[:], in0=cum[:], in1=tots[:], op=ALU.subtract)
            P.op("dve", "tensor_tensor", ["cum", "posi"], ["posi"], out=posi[:], in0=posi[:], in1=cum[:], op=ALU.add)
            P.op("dve", "tensor_scalar", ["posi"], ["posi"], out=posi[:], in0=posi[:], scalar1=1.0, scalar2=None, op0=ALU.add)
            P.op("dve", "tensor_tensor", ["posi", "msk"], ["posi"], out=posi[:], in0=posi[:], in1=msk[:], op=ALU.mult)
            P.op("dve", "tensor_scalar", ["posi"], ["posi"], out=posi[:], in0=posi[:], scalar1=-1.0, scalar2=None, op0=ALU.add)
            G5 = sb("G5", [128, NTT, 16, 8], BF16)
            P.op("pool", "memset", [], ["G5"], G5[:], 0.0)
            tvf = sb("tvf", [128, NTT, 16])
            P.op("pool", "iota", [], ["tvf"], tvf[:], pattern=[[0, NTT], [0, 16]], base=0, channel_multiplier=1, allow_small_or_imprecise_dtypes=True)
            P.op("dve", "tensor_copy", ["tvf", "G5"], ["G5"], out=G5[:, :, :, 0], in_=tvf[:])
            P.op("pool", "iota", ["tvf"], ["tvf"], tvf[:], pattern=[[128, NTT], [0, 16]], base=0, channel_multiplier=0, allow_small_or_imprecise_dtypes=True)
            P.op("dve", "tensor_copy", ["tvf", "G5"], ["G5"], out=G5[:, :, :, 1], in_=tvf[:])
            P.op("dve", "memset", ["G5"], ["G5"], G5[:, :, :, 5], 1.0)
            gres = sb("gres", [128, NTT, 16])
            P.op("dve", "tensor_copy", ["gat", "G5"], ["G5"], out=G5[:, :, :, 2], in_=gat[:])
            P.op("dve", "tensor_tensor", ["gat", "G5"], ["gres"], out=gres[:], in0=gat[:], in1=G5[:, :, :, 2], op=ALU.subtract)
            P.op("dve", "tensor_copy", ["gres", "G5"], ["G5"], out=G5[:, :, :, 3], in_=gres[:])
            P.op("dve", "tensor_tensor", ["gres", "G5"], ["gres"], out=gres[:], in0=gres[:], in1=G5[:, :, :, 3], op=ALU.subtract)
            P.op("dve", "tensor_copy", ["gres", "G5"], ["G5"], out=G5[:, :, :, 4], in_=gres[:])
            iotaS = sb("iotaS", [128, CAPL])
            P.op("pool", "iota", [], ["iotaS"], iotaS[:], pattern=[[1, CAPL]], base=0, channel_multiplier=0, allow_small_or_imprecise_dtypes=True)
            dmy = sb("dmy", [128, NCHK])
            P.op("pool", "iota", [], ["dmy"], dmy[:], pattern=[[128, NCHK]], base=LT, channel_multiplier=1, allow_small_or_imprecise_dtypes=True)
            oh = [sb("oh%d" % i, [128, CAPL], BF16) for i in range(2)]
            pI = [ps("pI%d" % i, [128, 64, 8]) for i in range(2)]
            rr_ = sb("rrF", [128, NCHK, 8])
            ia = sb("iaF", [128, NCHK])
            ii = sb("iiF", [128, NCHK], I32)
            gs = sb("gsF", [128, NCHK])
            ohq = 0
            for e_ in range(16):
                pb = e_ % 2
                first = True
                for t in range(NTT):
                    ob = ohq % 2
                    ohq += 1
                    P.op("dve", "tensor_scalar", ["iotaS", "posi"], ["oh%d" % ob], out=oh[ob][:], in0=iotaS[:], scalar1=posi[:, t, e_:e_ + 1],
                         scalar2=None, op0=ALU.is_equal)
                    for ch in range(NCHK):
                        P.add("pe", (lambda pb, ch, ob, t, e_, first, lastf: lambda e: e.matmul(
                            pI[pb][:, ch, :], lhsT=oh[ob][:, ch * 128:(ch + 1) * 128], rhs=G5[:, t, e_, :], start=first, stop=lastf,
                            skip_group_check=True))(pb, ch, ob, t, e_, first, (t == NTT - 1 and ch == NCHK - 1)),
                            reads=["oh%d" % ob, "G5"], writes=["pI%d" % pb], banks=["pI%d" % pb])
                        first = False
                P.op("dve", "tensor_copy", ["pI%d" % pb], ["rrF"], out=rr_[:], in_=pI[pb][:, 0:NCHK, :])
                P.op("dve", "tensor_tensor", ["rrF"], ["iaF"], out=ia[:], in0=rr_[:, :, 0], in1=rr_[:, :, 1], op=ALU.add)
                P.op("dve", "tensor_tensor", ["iaF", "dmy"], ["iaF"], out=ia[:], in0=ia[:], in1=dmy[:], op=ALU.subtract)
                P.op("dve", "tensor_tensor", ["iaF", "rrF"], ["iaF"], out=ia[:], in0=ia[:], in1=rr_[:, :, 5], op=ALU.mult)
                P.op("dve", "tensor_tensor", ["iaF", "dmy"], ["iaF"], out=ia[:], in0=ia[:], in1=dmy[:], op=ALU.add)
                P.op("dve", "tensor_copy", ["iaF"], ["iiF"], out=ii[:], in_=ia[:])
                P.op("dve", "tensor_tensor", ["rrF"], ["gsF"], out=gs[:], in0=rr_[:, :, 2], in1=rr_[:, :, 3], op=ALU.add)
                P.op("dve", "tensor_tensor", ["rrF", "gsF"], ["gsF"], out=gs[:], in0=gs[:], in1=rr_[:, :, 4], op=ALU.add)
                P.dma("sp", idx_d[e_], ii[:], ["iiF"], ["idx_d"])
                P.dma("sp", gate_d[e_], gs[:], ["gsF"], ["gate_d"])
        P.emit()

        with ExitStack() as st:
            sb = lambda n, s, d=F32: st.enter_context(nc.sbuf_tensor(n, list(s), d))
            ps = lambda n, s, d=F32: st.enter_context(nc.psum_tensor(n, list(s), d))
            ident = sb("identX", [128, 128], BF16)
            P.op("pool", "memset", [], ["identX"], ident[:], 1.0)
            P.op("pool", "affine_select", ["identX"], ["identX"], out=ident[:], in_=ident[:], pattern=[[-1, 128]],
                 compare_op=ALU.is_equal, fill=0.0, base=0, channel_multiplier=1)
            idxt = [sb("idxt%d" % i, [128, NCHK], I32) for i in range(2)]
            gtt = [sb("gtt%d" % i, [128, NCHK]) for i in range(2)]
            xe = [sb("xe%d" % i, [128, 2048], BF16) for i in range(2)]
            xeT = sb("xeT", [128, 16, CAPL], BF16)
            heT = sb("heT", [128, 16, CAPL], BF16)
            stw = sb("stw", [128, 16, 256])
            wb_ = [sb("wbX%d" % i, [128, 16, 256], BF16) for i in range(4)]
            sgl = [sb("sglX%d" % i, [128, 512]) for i in range(2)]
            yst = [sb("ystX%d" % i, [128, 256], BF16) for i in range(3)]
            ptr = [ps("ptrX%d" % i, [128, 1024], BF16) for i in range(2)]
            pg = [ps("pgX%d" % i, [128, 512]) for i in range(2)]
            pu = [ps("puX%d" % i, [128, 512]) for i in range(2)]
            py = [ps("pyX%d" % i, [128, 512]) for i in range(2)]
            cgs = [(c0, min(c0 + 512, CAPL)) for c0 in range(0, CAPL, 512)]
            cnt = {"tq": 0, "wq": 0, "gq": 0, "yq": 0, "sq": 0, "ysq": 0, "cast": 0, "xq": 0}

            def nx(k_, n):
                v = cnt[k_] % n
                cnt[k_] += 1
                return v

            def load_w(src_ap, name):
                wi = nx("wq", 4)
                P.dma("sp", stw[:], src_ap, [], ["stw"])
                ce = ("act", "dve")[nx("cast", 2)]
                if ce == "act":
                    P.op("act", "activation", ["stw"], ["wbX%d" % wi], out=wb_[wi][:], in_=stw[:], func=AF.Copy)
                else:
                    P.op("dve", "tensor_copy", ["stw"], ["wbX%d" % wi], out=wb_[wi][:], in_=stw[:])
                return wi

            prev_sc = {}
            for e_ in range(cfg.get("nexp", 16)):
                ib = e_ % 2
                P.dma("sp", idxt[ib][:], idx_d[e_], ["idx_d"], ["idxt%d" % ib])
                P.dma("sp", gtt[ib][:], gate_d[e_], ["gate_d"], ["gtt%d" % ib])
                for ch in range(NCHK):
                    xb = nx("xq", 2)
                    P.add("pool", (lambda xb, ib, ch: lambda e: e.indirect_dma_start(
                        out=xe[xb][:], out_offset=None, in_=hn3_bf[:, :],
                        in_offset=bass.IndirectOffsetOnAxis(ap=idxt[ib][:, ch:ch + 1], axis=0)))(xb, ib, ch),
                        reads=["idxt%d" % ib, "hn3_bf"], writes=["xe%d" % xb], dma=True)
                    for q4 in range(4):
                        pb = nx("tq", 2)
                        for j in range(4):
                            kc = q4 * 4 + j
                            P.tr(ptr[pb][:, j * 128:(j + 1) * 128], xe[xb][:, kc * 128:(kc + 1) * 128], ident[:], ["xe%d" % xb, "identX"], ["ptrX%d" % pb])
                        o_ap = xeT[:, q4 * 4:(q4 + 1) * 4, ch * 128:(ch + 1) * 128]
                        i_ap = ptr[pb][:, 0:512].rearrange("p (j n) -> p j n", n=128)
                        if q4 % 2 == 0:
                            P.op("act", "activation", ["ptrX%d" % pb], ["xeT"], out=o_ap, in_=i_ap, func=AF.Copy)
                        else:
                            P.op("dve", "tensor_copy", ["ptrX%d" % pb], ["xeT"], out=o_ap, in_=i_ap)
                for fg in range(8):
                    wg_i = load_w(w_gate[e_].rearrange("(kc p) f -> p kc f", p=128)[:, :, fg * 256:(fg + 1) * 256], "g")
                    wu_i = load_w(w_up[e_].rearrange("(kc p) f -> p kc f", p=128)[:, :, fg * 256:(fg + 1) * 256], "u")
                    for fc2 in range(2):
                        for (c0, c1) in cgs:
                            gb = nx("gq", 2)
                            for kc in range(16):
                                P.mm(pg[gb][:, 0:c1 - c0], wb_[wg_i][:, kc, fc2 * 128:(fc2 + 1) * 128], xeT[:, kc, c0:c1], kc == 0, kc == 15,
                                     ["wbX%d" % wg_i, "xeT"], ["pgX%d" % gb])
                            for kc in range(16):
                                P.mm(pu[gb][:, 0:c1 - c0], wb_[wu_i][:, kc, fc2 * 128:(fc2 + 1) * 128], xeT[:, kc, c0:c1], kc == 0, kc == 15,
                                     ["wbX%d" % wu_i, "xeT"], ["puX%d" % gb])
                            sb_i = nx("sq", 2)
                            P.op("act", "activation", ["pgX%d" % gb], ["sglX%d" % sb_i], out=sgl[sb_i][:, 0:c1 - c0], in_=pg[gb][:, 0:c1 - c0], func=AF.Silu)
                            P.op("dve", "tensor_tensor", ["sglX%d" % sb_i, "puX%d" % gb], ["heT"], out=heT[:, fg * 2 + fc2, c0:c1], in0=sgl[sb_i][:, 0:c1 - c0],
                                 in1=pu[gb][:, 0:c1 - c0], op=ALU.mult)
                new_sc = {}
                for dg in range(8):
                    wd_i = load_w(w_down[e_].rearrange("(kc p) f -> p kc f", p=128)[:, :, dg * 256:(dg + 1) * 256], "d")
                    for st_ in range(NCHK):
                        yb = nx("yq", 2)
                        for fc in range(16):
                            P.mm(py[yb][:, 0:256], heT[:, fc, st_ * 128:(st_ + 1) * 128], wb_[wd_i][:, fc, :], fc == 0, fc == 15,
                                 ["heT", "wbX%d" % wd_i], ["pyX%d" % yb])
                        ys = nx("ysq", 3)
                        P.op("dve", "tensor_scalar", ["pyX%d" % yb, "gtt%d" % ib], ["ystX%d" % ys], out=yst[ys][:], in0=py[yb][:, 0:256],
                             scalar1=gtt[ib][:, st_:st_ + 1], scalar2=None, op0=ALU.mult)
                        nm_ = "sc_%d_%d_%d" % (e_, dg, st_)
                        new_sc.setdefault(dg, []).append(nm_)
                        P.add("pool", (lambda ys, ib, st_, dg: lambda e: e.indirect_dma_start(
                            out=acc_l[dg][:, :], out_offset=bass.IndirectOffsetOnAxis(ap=idxt[ib][:, st_:st_ + 1], axis=0),
                            in_=yst[ys][:], in_offset=None, compute_op=ALU.add))(ys, ib, st_, dg),
                            reads=["ystX%d" % ys, "idxt%d" % ib, "acc_d"] + prev_sc.get(dg, []), writes=[nm_], dma=True)
                prev_sc = new_sc
        P.emit()

        with ExitStack() as st:
            sb = lambda n, s, d=F32: st.enter_context(nc.sbuf_tensor(n, list(s), d))
            gfin = sb("gfin", [128, 2048])
            P.dma("sp", gfin[:], nfin_rep, [], ["gfin"])
            xa = [sb("xaG%d" % i, [128, 8, 256]) for i in range(2)]
            junk = sb("junkG", [128, 2048], BF16)
            ss = [sb("ssG%d" % i, [128, 1]) for i in range(2)]
            yo_ = [sb("yoG%d" % i, [128, 2048]) for i in range(2)]
            for t in range(NTT):
                b = t % 2
                r0 = t * 128
                for g_ in range(8):
                    P.dma("sp", xa[b][:, g_, :], acc_l[g_][r0:r0 + 128, :], [], ["xaG%d" % b])
                xf = xa[b][:].rearrange("p g c -> p (g c)")
                P.op("act", "activation", ["xaG%d" % b], ["junkG", "ssG%d" % b], out=junk[:], in_=xf, func=AF.Square, accum_out=ss[b][:])
                P.op("dve", "tensor_scalar", ["ssG%d" % b], ["ssG%d" % b], out=ss[b][:], in0=ss[b][:], scalar1=1.0 / D, scalar2=EPS, op0=ALU.mult, op1=ALU.add)
                P.op("act", "activation", ["ssG%d" % b], ["ssG%d" % b], out=ss[b][:], in_=ss[b][:], func=AF.Sqrt)
                P.op("dve", "reciprocal", ["ssG%d" % b], ["ssG%d" % b], out=ss[b][:], in_=ss[b][:])
                P.op("dve", "scalar_tensor_tensor", ["xaG%d" % b, "ssG%d" % b, "gfin"], ["yoG%d" % b], out=yo_[b][:], in0=xf, scalar=ss[b][:, 0:1],
                     in1=gfin[:], op0=ALU.mult, op1=ALU.mult)
                P.dma("sp", y_out[r0:r0 + 128, :], yo_[b][:], ["yoG%d" % b], ["y_out"])
        P.emit()

        if dbg and upto not in ("all",):
            for nm_, ap_ in dbg.items():
                src = {"o_xbcT": xbcT, "o_uT": uT, "o_z": z_tok, "o_dt": dt_tok, "o_yssd": yssd, "o_yf": yf, "o_y5gT": y5gT, "o_y5nT": y5nT, "o_kT": kT_d, "o_v": v_d, "o_acc": acc_l[0], "o_probs": probs_loc, "o_hn3": hn3_bf, "o_idx": idx_d, "o_gate": gate_d}[nm_]
                P.dma("sp", ap_, src, [], [nm_])
            P.emit()
    return nc


def _rep(v):
    v = np.asarray(v, np.float32)
    return np.ascontiguousarray(np.broadcast_to(v.reshape(1, -1), (128, v.size)))


def _colpk(v):
    v = np.asarray(v, np.float32)
    return np.ascontiguousarray(v.reshape(-1, 128).T)


def _prep_weights(W):
    o = {}
    o["w_in"] = np.ascontiguousarray(W["w_in"][0])
    o["norm_mix"] = _colpk(W["norm_mix"][0])
    o["conv_w"] = np.ascontiguousarray(W["conv_w"][0].reshape(3, 16, 128).transpose(2, 1, 0))
    o["conv_b"] = _colpk(W["conv_b"][0])
    o["dtb_rep"] = _rep(W["ssd_dt_bias"][0])
    o["alog_rep"] = _rep(W["ssd_a_log"][0])
    o["ssdd_rep"] = _rep(W["ssd_d"][0])
    o["ssdn_rep"] = _rep(W["ssd_norm"][0])

    def gp(a):
        a = a.reshape(2, 8, 8, 64).transpose(0, 1, 3, 2)
        return np.concatenate([a, a], axis=2)
    are = gp(W["s5_a_re"][0])
    aim = gp(W["s5_a_im"][0])
    lst = gp(np.broadcast_to(W["s5_log_step"][0][:, :, None], (2, 64, 64)))
    o["s5_small"] = np.ascontiguousarray(np.stack([are, aim, lst], axis=3)).astype(np.float32)

    def bb(a):
        a = a.reshape(2, 8, 8, 64, 16).transpose(0, 1, 3, 2, 4).reshape(2, 8, 64, 128)
        return np.concatenate([a, a], axis=2)

    def cc(a):
        a = a.reshape(2, 8, 8, 16, 64).transpose(0, 1, 4, 2, 3).reshape(2, 8, 64, 128)
        return np.concatenate([a, a], axis=2)
    o["s5_big"] = np.ascontiguousarray(np.stack([bb(W["s5_b_re"][0]), bb(W["s5_b_im"][0]), cc(W["s5_c_re"][0]),
                                                 cc(W["s5_c_im"][0])], axis=3)).astype(np.float32)
    o["s5_dcol"] = _colpk(W["s5_d"][0])
    o["s5_ncol"] = _colpk(W["s5_norm"][0])
    o["norm_attn"] = _colpk(W["norm_attn"][0])
    o["norm_mem"] = _colpk(W["norm_mem"][0])
    for k_ in ("w_out", "w_q", "w_k", "w_v", "w_o"):
        o[k_] = np.ascontiguousarray(W[k_][0])
    o["nffn_rep"] = _rep(W["norm_ffn"][0])
    o["w_router_l"] = np.ascontiguousarray(W["w_router"][0].reshape(16, 128, 16).transpose(1, 0, 2))
    o["nfin_rep"] = _rep(W["norm_final"])
    o["w_glu"] = np.ascontiguousarray(W["s5_w_glu"][0])
    o["w_gate"] = np.ascontiguousarray(W["w_gate"][0])
    o["w_up"] = np.ascontiguousarray(W["w_up"][0])
    o["w_down"] = np.ascontiguousarray(W["w_down"][0])
    return o


_NC_CACHE = {}


def kernel(**inputs):
    W = {k_: np.asarray(v, np.float32) for k_, v in inputs.items()}
    xp, xs = W.pop("x_prompt"), W.pop("x_sample")
    mp, ms = W.pop("mem_prompt"), W.pop("mem_sample")
    LSEG = 4096
    cfg = {"LSEG": LSEG, "upto": "all", "CAPL": 1536, "GSZ": 4, "groups": [[0, 1, 2, 3], [4, 5, 6, 7]]}
    if "nc" not in _NC_CACHE:
        _NC_CACHE["nc"] = build(cfg)
    nc = _NC_CACHE["nc"]
    wl = _prep_weights(W)
    in_maps = []
    for c in range(8):
        m = dict(wl)
        if c < 4:
            m["x"] = np.ascontiguousarray(xp[c])
            m["mem"] = np.ascontiguousarray(np.stack([mp[c], mp[c]]))
            m["flag"] = np.ones((128, 1), np.float32)
        else:
            j = c - 4
            m["x"] = np.ascontiguousarray(xs[2 * j:2 * j + 2].reshape(2 * LSEG, D))
            m["mem"] = np.ascontiguousarray(ms[2 * j:2 * j + 2])
            m["flag"] = np.zeros((128, 1), np.float32)
        in_maps.append(m)
    res = run_bass_kernel_spmd(nc, in_maps, core_ids=list(range(8)))
    ys = [np.asarray(r["y"], np.float32) for r in res.results]
    y_prompt = np.stack(ys[0:4]).reshape(4, 8192, D)
    y_sample = np.concatenate([y.reshape(2, LSEG, D) for y in ys[4:8]], axis=0)
    return (y_prompt, y_sample)
```

```python
import numpy as np
from contextlib import ExitStack
import concourse.bass as bass
import concourse.mybir as mybir
from concourse.bass_utils import run_bass_kernel_spmd

F32 = mybir.dt.float32
BF16 = mybir.dt.bfloat16
I32 = mybir.dt.int32
U32 = mybir.dt.uint32
ALU = mybir.AluOpType
AF = mybir.ActivationFunctionType
AX = mybir.AxisListType

ENGS = ("pe", "act", "dve", "pool", "sp")
NDS = 24

D = 2048
EPS = 1e-6
INC = 4112


class Op:
    __slots__ = ("eng", "fn", "reads", "writes", "dma", "sem", "val", "waits", "desc", "inc")

    def __init__(self, eng, fn, reads, writes, dma):
        self.eng = eng
        self.fn = fn
        self.reads = tuple(reads)
        self.writes = tuple(writes)
        self.dma = dma
        self.waits = {}


class Prog:
    def __init__(self, nc, stack):
        self.nc = nc
        self.ops = []
        self.last_writer = {}
        self.readers = {}
        self.cnt = {e: 0 for e in ENGS}
        self.dcnt = {e: 0 for e in ENGS}
        self.sem = {e: stack.enter_context(nc.semaphore("s_" + e)) for e in ENGS if e != "sp"}
        self.dsem = {
            e: [stack.enter_context(nc.semaphore("d_%s_%d" % (e, i))) for i in range(NDS)]
            for e in ("sp", "act", "pool")
        }
        self.waited = {e: {} for e in ENGS}
        self.semobj = {}
        self.limit = None
        self.bank_last = {}
        self.csem = stack.enter_context(nc.semaphore("s_coll"))
        self.capture = None

    def add(self, eng, fn, reads=(), writes=(), dma=False, banks=(), own_sem=None):
        if self.capture is not None:
            self.capture.append((eng, fn, tuple(reads), tuple(writes), dma, tuple(banks), own_sem))
            return None
        if self.limit is not None and len(self.ops) >= self.limit:
            return None
        op = Op(eng, fn, reads, writes, dma)
        deps = set()
        for b in op.reads:
            w = self.last_writer.get(b)
            if w is not None:
                deps.add(w)
        for b in op.writes:
            w = self.last_writer.get(b)
            if w is not None:
                deps.add(w)
            for r in self.readers.get(b, ()):
                if r.eng == eng and not r.dma and not dma:
                    continue
                deps.add(r)
        for tok in banks:
            pd = self.bank_last.get(tok)
            if pd is not None and pd.eng != eng:
                deps.add(pd)
            self.bank_last[tok] = op
        for b in op.reads:
            self.readers.setdefault(b, []).append(op)
        for b in op.writes:
            self.last_writer[b] = op
            self.readers[b] = []
        prewait = None
        op.inc = 16 if dma else 1
        if own_sem is not None:
            op.sem = own_sem
            op.val = 1
            op.inc = 1
        elif dma:
            i = self.dcnt[eng]
            self.dcnt[eng] += 1
            op.sem = self.dsem[eng][i % NDS]
            op.val = 16 * (i // NDS + 1)
            if i >= NDS:
                prewait = (op.sem, 16 * (i // NDS))
        else:
            self.cnt[eng] += 1
            op.sem = self.sem[eng]
            op.val = self.cnt[eng]
        for d in deps:
            if d is op:
                continue
            if d.eng == "pe" and eng == "pe" and not d.dma and not dma:
                continue
            k = id(d.sem)
            self.semobj[k] = d.sem
            if op.waits.get(k, 0) < d.val:
                op.waits[k] = d.val
        if prewait is not None:
            k = id(prewait[0])
            self.semobj[k] = prewait[0]
            if op.waits.get(k, 0) < prewait[1]:
                op.waits[k] = prewait[1]
        self.ops.append(op)
        return op

    @staticmethod
    def _banks(aps):
        toks = []
        for a in aps:
            sp = getattr(a, "space", None)
            if sp is not None and str(sp).endswith("PSUM"):
                toks.append(a.tensor.name)
        return toks

    def op(self, eng, method, reads, writes, *args, **kw):
        o = self.add(eng, lambda e: getattr(e, method)(*args, **kw), reads, writes,
                     banks=self._banks(list(args) + list(kw.values())))
        return o

    def dma(self, eng, out, in_, reads, writes, **kw):
        return self.add(eng, lambda e: e.dma_start(out=out, in_=in_, **kw), reads, writes, dma=True)

    def mm(self, out, lhsT, rhs, start, stop, reads, writes):
        return self.add("pe", lambda e: e.matmul(out, lhsT=lhsT, rhs=rhs, start=start, stop=stop), reads, writes,
                        banks=self._banks([out]))

    def tr(self, out, in_, ident, reads, writes):
        return self.add("pe", lambda e: e.transpose(out=out, in_=in_, identity=ident), reads, writes,
                        banks=self._banks([out]))

    def threads(self, fns):
        lists = []
        for f in fns:
            self.capture = []
            f()
            lists.append(self.capture)
        self.capture = None
        n = max(len(l) for l in lists)
        for i in range(n):
            for l in lists:
                if i < len(l):
                    self.add(*l[i])

    def emit(self):
        nc = self.nc
        ops = self.ops
        self.ops = []
        per = {e: [o for o in ops if o.eng == e] for e in ENGS}
        finals = []
        for e in ENGS:
            if e != "sp" and self.cnt[e] > 0:
                finals.append((self.sem[e], self.cnt[e]))
        for e in ("sp", "act", "pool"):
            n = self.dcnt[e]
            for j in range(min(n, NDS)):
                uses = (n - 1 - j) // NDS + 1
                finals.append((self.dsem[e][j], 16 * uses))
        waited = self.waited
        semobj = self.semobj

        def run(engname, eng):
            wd = waited[engname]
            for o in per[engname]:
                for k, v in o.waits.items():
                    if wd.get(k, 0) < v:
                        eng.wait_ge(semobj[k], v)
                        wd[k] = v
                ins = o.fn(eng)
                ins.then_inc(o.sem, o.inc)
            for s, v in finals:
                k = id(s)
                if wd.get(k, 0) < v:
                    eng.wait_ge(s, v)
                    wd[k] = v

        with nc.Block() as block:
            @block.tensor
            def _(eng):
                run("pe", eng)

            @block.scalar
            def _(eng):
                run("act", eng)

            @block.vector
            def _(eng):
                run("dve", eng)

            @block.gpsimd
            def _(eng):
                run("pool", eng)

            @block.sync
            def _(eng):
                run("sp", eng)


class K:
    pass


def _rr(lst):
    state = {"i": 0}

    def nxt():
        v = lst[state["i"] % len(lst)]
        state["i"] += 1
        return v
    return nxt


def build(cfg):
    LSEG = cfg["LSEG"]
    LT = 2 * LSEG
    NST = LT // 512
    upto = cfg.get("upto", "all")
    nc = bass.Bass("TRN2", target_bir_lowering=False)
    k = K()
    k.nc = nc
    k.cfg = cfg

    def din(name, shape, dt=F32):
        return nc.dram_tensor(name, list(shape), dt, kind="ExternalInput").ap()

    def dscr(name, shape, dt=F32):
        return nc.dram_tensor(name, list(shape), dt, kind="Internal").ap()

    def dout(name, shape, dt=F32):
        return nc.dram_tensor(name, list(shape), dt, kind="ExternalOutput").ap()

    x_in = din("x", [LT, D])
    w_in = din("w_in", [D, INC])
    norm_mix = din("norm_mix", [128, 16])
    flag = din("flag", [128, 1])

    w_in_bf = dscr("w_in_bf", [9, 128, 16, 512], BF16)
    xbcT = dscr("xbcT", [2048, LT])
    uT = dscr("uT", [1024, LT], BF16)
    z_tok = dscr("z_tok", [LT, 1024])
    dt_tok = dscr("dt_tok", [LT, 16])

    conv_w = din("conv_w", [128, 16, 3])
    conv_b = din("conv_b", [128, 16])
    dtb_rep = din("dtb_rep", [128, 32])
    alog_rep = din("alog_rep", [128, 32])
    ssdd_rep = din("ssdd_rep", [128, 16])
    ssdn_rep = din("ssdn_rep", [128, 1024])
    xs_tok = dscr("xs_tok", [LT, 1024], BF16)
    B_tok = dscr("B_tok", [LT, 512], BF16)
    BT = dscr("BT", [512, LT], BF16)
    CT = dscr("CT", [512, LT], BF16)
    yf = dscr("yf", [LT, 1024])
    yb_d = dscr("yb_d", [LT, 1024])
    yssd = dscr("yssd", [LT, 1024], BF16)
    s5_small = din("s5_small", [2, 8, 128, 3, 8])
    s5_big = din("s5_big", [2, 8, 128, 4, 128])
    s5_dcol = din("s5_dcol", [128, 8])
    y5gT = dscr("y5gT", [1024, LT], BF16)
    norm_attn = din("norm_attn", [128, 16])
    norm_mem = din("norm_mem", [128, 16])
    w_glu = din("w_glu", [1024, 1024])
    w_out = din("w_out", [2048, 2048])
    w_q = din("w_q", [2048, 2048])
    w_k = din("w_k", [2048, 2048])
    w_v = din("w_v", [2048, 2048])
    w_o = din("w_o", [2048, 2048])
    mem_in = din("mem", [2, 256, 2048])
    s5_ncol = din("s5_ncol", [128, 8])
    w_glu_bf = dscr("w_glu_bf", [2, 128, 8, 512], BF16)
    w_out_bf = dscr("w_out_bf", [4, 128, 16, 512], BF16)
    w_q_bf = dscr("w_q_bf", [4, 128, 16, 512], BF16)
    w_k_bf = dscr("w_k_bf", [4, 128, 16, 512], BF16)
    w_v_bf = dscr("w_v_bf", [4, 128, 16, 512], BF16)
    w_o_bf = dscr("w_o_bf", [4, 128, 16, 512], BF16)
    kT_d = dscr("kT_d", [2, 128, 16, 256], BF16)
    v_d = dscr("v_d", [2, 128, 2, 2048], BF16)
    y5nT = dscr("y5nT", [1024, LT], BF16)
    nffn_rep = din("nffn_rep", [128, 2048])
    w_router_l = din("w_router_l", [128, 16, 16])
    CAPL = cfg["CAPL"]
    acc_l = [dscr("accd%d" % i, [LT + CAPL, 256]) for i in range(8)]
    hn3_bf = dscr("hn3_bf", [LT + CAPL, 2048], BF16)
    idx_d = dscr("idx_d", [16, 128, CAPL // 128], I32)
    gate_d = dscr("gate_d", [16, 128, CAPL // 128])
    probs_all = dscr("probs_all", [cfg.get("GSZ", 1) * LT, 16])
    w_gate = din("w_gate", [cfg.get("nexp", 16), 2048, 2048])
    w_up = din("w_up", [cfg.get("nexp", 16), 2048, 2048])
    w_down = din("w_down", [cfg.get("nexp", 16), 2048, 2048])
    nfin_rep = din("nfin_rep", [128, 2048])
    y_out = dout("y", [LT, 2048])
    cnt_out = dout("slot_idx", [16, 128, CAPL // 128], I32)
    probs_loc = dscr("probs_loc", [LT, 16])
    dbg = {}
    if upto == "F":
        dbg["o_acc"] = dout("o_acc", [LT + CAPL, 256])
        dbg["o_idx"] = dout("o_idx", [16, 128, CAPL // 128], I32)
        dbg["o_gate"] = dout("o_gate", [16, 128, CAPL // 128])
    if upto == "E2":
        dbg["o_acc"] = dout("o_acc", [LT + CAPL, 256])
        dbg["o_probs"] = dout("o_probs", [LT, 16])
        dbg["o_hn3"] = dout("o_hn3", [LT + CAPL, 2048], BF16)
    if upto == "E1":
        dbg["o_y5nT"] = dout("o_y5nT", [1024, LT], BF16)
        dbg["o_kT"] = dout("o_kT", [2, 128, 16, 256], BF16)
        dbg["o_v"] = dout("o_v", [2, 128, 2, 2048], BF16)
    if upto == "D":
        dbg["o_y5gT"] = dout("o_y5gT", [1024, LT], BF16)
    if upto == "C":
        dbg["o_yssd"] = dout("o_yssd", [LT, 1024], BF16)
        dbg["o_yf"] = dout("o_yf", [LT, 1024])
    if upto == "A":
        dbg["o_xbcT"] = dout("o_xbcT", [2048, LT])
        dbg["o_uT"] = dout("o_uT", [1024, LT], BF16)
        dbg["o_z"] = dout("o_z", [LT, 1024])
        dbg["o_dt"] = dout("o_dt", [LT, 16])

    with ExitStack() as gst:
        P = Prog(nc, gst)
        k.P = P
        with ExitStack() as st:
            sb = lambda n, s, d=F32: st.enter_context(nc.sbuf_tensor(n, list(s), d))
            gcol = sb("gcol", [128, 16])
            P.add("sp", lambda e: e.dma_start(out=gcol[:], in_=norm_mix),
                  writes=["gcol"], dma=True)
            wst = [sb("wst%d" % i, [128, INC]) for i in range(2)]
            wbf = [sb("wbf%d" % i, [128, 9 * 512], BF16) for i in range(2)]
            for i in range(2):
                (lambda i: P.add("pool", lambda e: e.memset(wbf[i][:], 0.0), writes=["wbf%d" % i]))(i)
            engs = _rr(["dve", "act", "pool"])
            for kc in range(16):
                b = kc % 2
                P.add("sp", (lambda b, kc: lambda e: e.dma_start(out=wst[b][:], in_=w_in[kc * 128:(kc + 1) * 128, :]))(b, kc),
                      writes=["wst%d" % b], dma=True)
                def cast(b=b, kc=kc):
                    srcs = [(0, 3072, 0), (3088, 4112, 3072), (3072, 3088, 4096)]
                    for (s0, s1, d0) in srcs:
                        def f(e, s0=s0, s1=s1, d0=d0):
                            return e.tensor_scalar(out=wbf[b][:, d0:d0 + (s1 - s0)], in0=wst[b][:, s0:s1],
                                                   scalar1=gcol[:, kc:kc + 1], scalar2=None, op0=ALU.mult)
                        P.add("dve", f, reads=["wst%d" % b, "gcol"], writes=["wbf%d" % b])
                cast()
                P.add("sp", (lambda b, kc: lambda e: e.dma_start(
                    out=w_in_bf[:, :, kc, :].rearrange("g p n -> p g n"),
                    in_=wbf[b][:].rearrange("p (g n) -> p g n", n=512)))(b, kc),
                    reads=["wbf%d" % b], writes=["w_in_bf"], dma=True)
        P.emit()

        with ExitStack() as st:
            sb = lambda n, s, d=F32: st.enter_context(nc.sbuf_tensor(n, list(s), d))
            ps = lambda n, s, d=F32: st.enter_context(nc.psum_tensor(n, list(s), d))
            ident = sb("ident", [128, 128], BF16)
            P.add("pool", lambda e: e.memset(ident[:], 1.0), writes=["ident"])
            P.add("pool", lambda e: e.affine_select(out=ident[:], in_=ident[:], pattern=[[-1, 128]],
                                                    compare_op=ALU.is_equal, fill=0.0, base=0, channel_multiplier=1),
                  reads=["ident"], writes=["ident"])
            xt = [sb("xt%d" % i, [128, D]) for i in range(2)]
            junk = sb("junk", [128, D], BF16)
            ss = [sb("ss%d" % i, [128, 1]) for i in range(2)]
            xn = [sb("xn%d" % i, [128, D], BF16) for i in range(2)]
            hT = sb("hT", [128, 16, 512], BF16)
            wg = [sb("wg%d" % i, [128, 16, 512], BF16) for i in range(2)]
            ostg = [sb("ostg%d" % i, [128, 512]) for i in range(3)]
            ostb = [sb("ostb%d" % i, [128, 512], BF16) for i in range(2)]
            dstg = [sb("dstg%d" % i, [128, 16]) for i in range(2)]
            ptr = [ps("ptr%d" % i, [128, 1024], BF16) for i in range(2)]
            pmm = [ps("pmm%d" % i, [128, 512]) for i in range(4)]
            wq = _rr([0, 1])
            oq = _rr([0, 1, 2])
            obq = _rr([0, 1])
            pq = _rr([0, 1, 2, 3])
            tq = _rr([0, 1])
            cpq = _rr(["act", "dve"])
            dq = _rr([0, 1])
            it = 0
            for s in range(NST):
                t0 = s * 512
                for tt in range(4):
                    b = it % 2
                    it += 1
                    r0 = t0 + tt * 128
                    P.add("sp", (lambda b, r0: lambda e: e.dma_start(out=xt[b][:], in_=x_in[r0:r0 + 128, :]))(b, r0),
                          writes=["xt%d" % b], dma=True)
                    P.add("act", (lambda b: lambda e: e.activation(out=junk[:], in_=xt[b][:], func=AF.Square,
                                                                   accum_out=ss[b][:]))(b),
                          reads=["xt%d" % b], writes=["junk", "ss%d" % b])
                    P.add("dve", (lambda b: lambda e: e.tensor_scalar(out=ss[b][:], in0=ss[b][:], scalar1=1.0 / D,
                                                                      scalar2=EPS, op0=ALU.mult, op1=ALU.add))(b),
                          reads=["ss%d" % b], writes=["ss%d" % b])
                    P.add("act", (lambda b: lambda e: e.activation(out=ss[b][:], in_=ss[b][:], func=AF.Sqrt))(b),
                          reads=["ss%d" % b], writes=["ss%d" % b])
                    P.add("dve", (lambda b: lambda e: e.reciprocal(out=ss[b][:], in_=ss[b][:]))(b),
                          reads=["ss%d" % b], writes=["ss%d" % b])
                    P.add("dve", (lambda b: lambda e: e.tensor_scalar(out=xn[b][:], in0=xt[b][:], scalar1=ss[b][:, 0:1],
                                                                      scalar2=None, op0=ALU.mult))(b),
                          reads=["xt%d" % b, "ss%d" % b], writes=["xn%d" % b])
                    for q in range(4):
                        pb = tq()
                        for j in range(4):
                            kc = q * 4 + j
                            P.add("pe", (lambda pb, j, b, kc: lambda e: e.transpose(
                                out=ptr[pb][:, j * 128:(j + 1) * 128], in_=xn[b][:, kc * 128:(kc + 1) * 128],
                                identity=ident[:]))(pb, j, b, kc),
                                reads=["xn%d" % b, "ident"], writes=["ptr%d" % pb])
                        ce = cpq()
                        if ce == "act":
                            P.add("act", (lambda pb, q, tt: lambda e: e.activation(
                                out=hT[:, q * 4:(q + 1) * 4, tt * 128:(tt + 1) * 128],
                                in_=ptr[pb][:, 0:512].rearrange("p (j n) -> p j n", n=128), func=AF.Copy))(pb, q, tt),
                                reads=["ptr%d" % pb], writes=["hT"])
                        else:
                            P.add("dve", (lambda pb, q, tt: lambda e: e.tensor_copy(
                                out=hT[:, q * 4:(q + 1) * 4, tt * 128:(tt + 1) * 128],
                                in_=ptr[pb][:, 0:512].rearrange("p (j n) -> p j n", n=128)))(pb, q, tt),
                                reads=["ptr%d" % pb], writes=["hT"])
                for g in range(9):
                    wb = wq()
                    P.add("sp", (lambda wb, g: lambda e: e.dma_start(out=wg[wb][:], in_=w_in_bf[g]))(wb, g),
                          reads=["w_in_bf"], writes=["wg%d" % wb], dma=True)
                    if g < 2:
                        for tt in range(4):
                            pb = pq()
                            for kc in range(16):
                                P.add("pe", (lambda pb, wb, kc, tt: lambda e: e.matmul(
                                    pmm[pb][:], lhsT=hT[:, kc, tt * 128:(tt + 1) * 128], rhs=wg[wb][:, kc, :],
                                    start=(kc == 0), stop=(kc == 15)))(pb, wb, kc, tt),
                                    reads=["hT", "wg%d" % wb], writes=["pmm%d" % pb])
                            ob = oq()
                            P.add("act", (lambda ob, pb: lambda e: e.activation(out=ostg[ob][:], in_=pmm[pb][:], func=AF.Copy))(ob, pb),
                                  reads=["pmm%d" % pb], writes=["ostg%d" % ob])
                            r0 = t0 + tt * 128
                            P.add("sp", (lambda ob, r0, g: lambda e: e.dma_start(
                                out=z_tok[r0:r0 + 128, g * 512:(g + 1) * 512], in_=ostg[ob][:]))(ob, r0, g),
                                reads=["ostg%d" % ob], writes=["z_tok"], dma=True)
                    elif g < 8:
                        for mc in range(4):
                            pb = pq()
                            for kc in range(16):
                                P.add("pe", (lambda pb, wb, kc, mc: lambda e: e.matmul(
                                    pmm[pb][:], lhsT=wg[wb][:, kc, mc * 128:(mc + 1) * 128], rhs=hT[:, kc, :],
                                    start=(kc == 0), stop=(kc == 15)))(pb, wb, kc, mc),
                                    reads=["hT", "wg%d" % wb], writes=["pmm%d" % pb])
                            if g < 6:
                                ob = oq()
                                ch0 = (g - 2) * 512 + mc * 128
                                P.add("act", (lambda ob, pb: lambda e: e.activation(out=ostg[ob][:], in_=pmm[pb][:], func=AF.Copy))(ob, pb),
                                      reads=["pmm%d" % pb], writes=["ostg%d" % ob])
                                P.add("sp", (lambda ob, ch0, t0: lambda e: e.dma_start(
                                    out=xbcT[ch0:ch0 + 128, t0:t0 + 512], in_=ostg[ob][:]))(ob, ch0, t0),
                                    reads=["ostg%d" % ob], writes=["xbcT"], dma=True)
                            else:
                                ob = obq()
                                ch0 = (g - 6) * 512 + mc * 128
                                P.add("dve", (lambda ob, pb: lambda e: e.tensor_copy(out=ostb[ob][:], in_=pmm[pb][:]))(ob, pb),
                                      reads=["pmm%d" % pb], writes=["ostb%d" % ob])
                                P.add("sp", (lambda ob, ch0, t0: lambda e: e.dma_start(
                                    out=uT[ch0:ch0 + 128, t0:t0 + 512], in_=ostb[ob][:]))(ob, ch0, t0),
                                    reads=["ostb%d" % ob], writes=["uT"], dma=True)
                    else:
                        for tt in range(4):
                            pb = pq()
                            for kc in range(16):
                                P.add("pe", (lambda pb, wb, kc, tt: lambda e: e.matmul(
                                    pmm[pb][:, 0:16], lhsT=hT[:, kc, tt * 128:(tt + 1) * 128], rhs=wg[wb][:, kc, 0:16],
                                    start=(kc == 0), stop=(kc == 15)))(pb, wb, kc, tt),
                                    reads=["hT", "wg%d" % wb], writes=["pmm%d" % pb])
                            ob = dq()
                            P.add("dve", (lambda ob, pb: lambda e: e.tensor_copy(out=dstg[ob][:], in_=pmm[pb][:, 0:16]))(ob, pb),
                                  reads=["pmm%d" % pb], writes=["dstg%d" % ob])
                            r0 = t0 + tt * 128
                            P.add("sp", (lambda ob, r0: lambda e: e.dma_start(out=dt_tok[r0:r0 + 128, :], in_=dstg[ob][:]))(ob, r0),
                                  reads=["dstg%d" % ob], writes=["dt_tok"], dma=True)
        P.emit()


        with ExitStack() as st:
            sb = lambda n, s, d=F32: st.enter_context(nc.sbuf_tensor(n, list(s), d))
            ps = lambda n, s, d=F32: st.enter_context(nc.psum_tensor(n, list(s), d))
            ident = sb("identB", [128, 128], BF16)
            P.op("pool", "memset", [], ["identB"], ident[:], 1.0)
            P.op("pool", "affine_select", ["identB"], ["identB"], out=ident[:], in_=ident[:], pattern=[[-1, 128]],
                 compare_op=ALU.is_equal, fill=0.0, base=0, channel_multiplier=1)
            cw = sb("cw", [128, 16, 3])
            cb = sb("cb", [128, 16])
            flg = sb("flg", [128, 1])
            P.dma("sp", cw[:], conv_w, [], ["cw"])
            P.dma("sp", cb[:], conv_b, [], ["cb"])
            P.dma("sp", flg[:], flag, [], ["flg"])
            NB = 3
            xw = [sb("xw%d" % i, [128, 514]) for i in range(NB)]
            acc = [sb("acc%d" % i, [128, 512]) for i in range(NB)]
            sl = [sb("sl%d" % i, [128, 512], BF16) for i in range(NB)]
            xs_stg = sb("xs_stg", [128, 4, 1024], BF16)
            b_stg = sb("b_stg", [128, 4, 512], BF16)
            ptb = [ps("ptb%d" % i, [128, 1024], BF16) for i in range(2)]
            it = 0
            tq = 0
            for s in range(NST):
                t0 = s * 512
                for c in range(16):
                    b = it % NB
                    it += 1
                    lo = max(t0 - 1, 0)
                    hi = min(t0 + 513, LT)
                    if t0 == 0:
                        P.op("pool", "memset", [], ["xw%d" % b], xw[b][:, 0:1], 0.0)
                    if t0 + 512 == LT:
                        P.op("pool", "memset", [], ["xw%d" % b], xw[b][:, 513:514], 0.0)
                    P.dma("sp", xw[b][:, (lo - (t0 - 1)):(hi - (t0 - 1))], xbcT[c * 128:(c + 1) * 128, lo:hi],
                          ["xbcT"], ["xw%d" % b])
                    if t0 == LSEG:
                        P.op("dve", "tensor_scalar", ["xw%d" % b, "flg"], ["xw%d" % b], out=xw[b][:, 0:1], in0=xw[b][:, 0:1],
                             scalar1=flg[:, 0:1], scalar2=None, op0=ALU.mult)
                    if t0 + 512 == LSEG:
                        P.op("dve", "tensor_scalar", ["xw%d" % b, "flg"], ["xw%d" % b], out=xw[b][:, 513:514], in0=xw[b][:, 513:514],
                             scalar1=flg[:, 0:1], scalar2=None, op0=ALU.mult)
                    P.op("dve", "tensor_scalar", ["xw%d" % b, "cw"], ["acc%d" % b], out=acc[b][:], in0=xw[b][:, 1:513],
                         scalar1=cw[:, c, 1:2], scalar2=None, op0=ALU.mult)
                    P.op("dve", "scalar_tensor_tensor", ["xw%d" % b, "cw", "acc%d" % b], ["acc%d" % b], out=acc[b][:],
                         in0=xw[b][:, 0:512], scalar=cw[:, c, 0:1], in1=acc[b][:], op0=ALU.mult, op1=ALU.add)
                    P.op("dve", "scalar_tensor_tensor", ["xw%d" % b, "cw", "acc%d" % b], ["acc%d" % b], out=acc[b][:],
                         in0=xw[b][:, 2:514], scalar=cw[:, c, 2:3], in1=acc[b][:], op0=ALU.mult, op1=ALU.add)
                    P.op("act", "activation", ["acc%d" % b, "cb"], ["sl%d" % b], out=sl[b][:], in_=acc[b][:], func=AF.Silu,
                         bias=cb[:, c:c + 1])
                    if c >= 8:
                        dst = BT if c < 12 else CT
                        r0 = (c - 8) * 128 if c < 12 else (c - 12) * 128
                        P.dma("sp", dst[r0:r0 + 128, t0:t0 + 512], sl[b][:], ["sl%d" % b], ["BT" if c < 12 else "CT"])
                    if c < 12:
                        pb = tq % 2
                        tq += 1
                        for tt in range(4):
                            P.tr(ptb[pb][:, tt * 128:(tt + 1) * 128], sl[b][:, tt * 128:(tt + 1) * 128], ident[:],
                                 ["sl%d" % b, "identB"], ["ptb%d" % pb])
                        if c < 8:
                            P.op("act", "activation", ["ptb%d" % pb], ["xs_stg"], out=xs_stg[:, :, c * 128:(c + 1) * 128],
                                 in_=ptb[pb][:, 0:512].rearrange("p (t n) -> p t n", n=128), func=AF.Copy)
                        else:
                            P.op("act", "activation", ["ptb%d" % pb], ["b_stg"], out=b_stg[:, :, (c - 8) * 128:(c - 7) * 128],
                                 in_=ptb[pb][:, 0:512].rearrange("p (t n) -> p t n", n=128), func=AF.Copy)
                    if c == 7:
                        P.dma("sp", xs_tok[t0:t0 + 512, :].rearrange("(t p) c -> p t c", p=128), xs_stg[:], ["xs_stg"], ["xs_tok"])
                    if c == 11:
                        P.dma("sp", B_tok[t0:t0 + 512, :].rearrange("(t p) c -> p t c", p=128), b_stg[:], ["b_stg"], ["B_tok"])
        P.emit()

        NCH = LT // 128
        CPS = LSEG // 128
        with ExitStack() as st:
            sb = lambda n, s, d=F32: st.enter_context(nc.sbuf_tensor(n, list(s), d))
            ps = lambda n, s, d=F32: st.enter_context(nc.psum_tensor(n, list(s), d))
            tri = [sb("triF", [128, 128]), sb("triB", [128, 128])]
            nmk = [sb("nmkF", [128, 128]), sb("nmkB", [128, 128])]
            ones = sb("onesC", [128, 128])
            P.op("pool", "memset", [], ["onesC"], ones[:], 1.0)
            for d_ in range(2):
                pat = [[1, 128]] if d_ == 0 else [[-1, 128]]
                cm = -1 if d_ == 0 else 1
                nm = "FB"[d_]
                P.op("pool", "memset", [], ["tri" + nm], tri[d_][:], 1.0)
                P.op("pool", "affine_select", ["tri" + nm], ["tri" + nm], out=tri[d_][:], in_=tri[d_][:], pattern=pat,
                     compare_op=ALU.is_ge, fill=0.0, base=0, channel_multiplier=cm)
                P.op("pool", "memset", [], ["nmk" + nm], nmk[d_][:], 0.0)
                P.op("pool", "affine_select", ["nmk" + nm], ["nmk" + nm], out=nmk[d_][:], in_=nmk[d_][:], pattern=pat,
                     compare_op=ALU.is_ge, fill=-30000.0, base=0, channel_multiplier=cm)
            dtb = sb("dtb", [128, 32])
            aneg = sb("aneg", [128, 32])
            flg = sb("flgC", [128, 1])
            P.dma("sp", dtb[:], dtb_rep, [], ["dtb"])
            P.dma("sp", aneg[:], alog_rep, [], ["aneg"])
            P.dma("sp", flg[:], flag, [], ["flgC"])
            P.op("act", "activation", ["aneg"], ["aneg"], out=aneg[:], in_=aneg[:], func=AF.Exp)
            P.op("dve", "tensor_scalar", ["aneg"], ["aneg"], out=aneg[:], in0=aneg[:], scalar1=-1.0, scalar2=None, op0=ALU.mult)

            def ssd_dir(d_):
                nm = "FB"[d_]
                X = lambda s_: s_ + nm
                NBUF = 2
                xs = [sb(X("xs%d" % i), [128, 16, 64], BF16) for i in range(NBUF)]
                bt_ = [sb(X("btok%d" % i), [128, 512], BF16) for i in range(NBUF)]
                bT = [sb(X("bT%d" % i), [128, 4, 128], BF16) for i in range(NBUF)]
                cT = [sb(X("cT%d" % i), [128, 4, 128], BF16) for i in range(NBUF)]
                dtr = [sb(X("dtr%d" % i), [128, 16]) for i in range(NBUF)]
                dt_ = sb(X("dt_"), [128, 16])
                dtA = sb(X("dtA"), [128, 16])
                acs = sb(X("acs"), [128, 16])
                dout_ = sb(X("dout_"), [128, 16])
                cd = sb(X("cd"), [128, 16])
                rhsb = [sb(X("rhsb%d" % i), [128, 4, 128]) for i in range(2)]
                dif = [sb(X("dif%d" % i), [128, 4, 128]) for i in range(2)]
                seg = [sb(X("seg%d" % i), [128, 4, 128]) for i in range(2)]
                mT = [sb(X("mT%d" % i), [128, 4, 128], BF16) for i in range(2)]
                xdt = sb(X("xdt"), [128, 16, 64], BF16)
                xdd = [sb(X("xdd%d" % i), [128, 4, 64], BF16) for i in range(2)]
                prev = sb(X("prev"), [128, 16, 64])
                prevb = sb(X("prevb"), [128, 16, 64], BF16)
                tmp = [sb(X("tmpc%d" % i), [128, 4, 64]) for i in range(2)]
                ybuf = [sb(X("ybuf%d" % i), [128, 16, 64]) for i in range(2)]
                pA = ps(X("pA"), [128, 4, 128])
                pB = ps(X("pB"), [128, 512])
                pC = ps(X("pC"), [128, 512])
                pD = ps(X("pD"), [128, 512])
                ydst = yf if d_ == 0 else yb_d

                def body():
                    it = 0
                    last = 127 if d_ == 0 else 0
                    order = list(range(NCH)) if d_ == 0 else list(range(NCH - 1, -1, -1))
                    pbn = [X("prevb%d" % g) for g in range(4)]
                    P.op("dve", "memset", [], [X("prev")], prev[:], 0.0)
                    P.op("pool", "memset", [], pbn, prevb[:], 0.0)
                    for ci, c in enumerate(order):
                        t0 = c * 128
                        b = it % NBUF
                        it += 1
                        if ci == CPS:
                            P.op("dve", "tensor_scalar", [X("prev"), "flgC"], [X("prev")], out=prev[:], in0=prev[:], scalar1=flg[:, 0:1],
                                 scalar2=None, op0=ALU.mult)
                            P.op("dve", "tensor_scalar", pbn + ["flgC"], pbn, out=prevb[:], in0=prevb[:], scalar1=flg[:, 0:1],
                                 scalar2=None, op0=ALU.mult)
                        P.dma("sp", xs[b][:], xs_tok[t0:t0 + 128, :].rearrange("p (h q) -> p h q", q=64), ["xs_tok"], [X("xs%d" % b)])
                        P.dma("sp", bt_[b][:], B_tok[t0:t0 + 128, :], ["B_tok"], [X("btok%d" % b)])
                        P.dma("sp", bT[b][:], BT[:, t0:t0 + 128].rearrange("(g n) l -> n g l", n=128), ["BT"], [X("bT%d" % b)])
                        P.dma("sp", cT[b][:], CT[:, t0:t0 + 128].rearrange("(g n) l -> n g l", n=128), ["CT"], [X("cT%d" % b)])
                        P.dma("sp", dtr[b][:], dt_tok[t0:t0 + 128, :], ["dt_tok"], [X("dtr%d" % b)])
                        P.op("dve", "tensor_tensor", [X("dtr%d" % b), "dtb"], [X("dt_")], out=dt_[:], in0=dtr[b][:], in1=dtb[:, d_ * 16:(d_ + 1) * 16], op=ALU.add)
                        P.op("act", "activation", [X("dt_")], [X("dt_")], out=dt_[:], in_=dt_[:], func=AF.Exp)
                        P.op("act", "activation", [X("dt_")], [X("dt_")], out=dt_[:], in_=dt_[:], func=AF.Ln, bias=1.0)
                        P.op("dve", "tensor_tensor", [X("dt_"), "aneg"], [X("dtA")], out=dtA[:], in0=dt_[:], in1=aneg[:, d_ * 16:(d_ + 1) * 16], op=ALU.mult)
                        P.mm(pD[:, 0:16], tri[d_][:], dtA[:], True, True, ["tri" + nm, X("dtA")], [X("pD")])
                        P.op("dve", "tensor_copy", [X("pD")], [X("acs")], out=acs[:], in_=pD[:, 0:16])
                        P.op("act", "activation", [X("acs")], [X("dout_")], out=dout_[:], in_=acs[:], func=AF.Exp)
                        P.op("pool", "tensor_tensor", [X("xs%d" % b), X("dt_")], [X("xdt")], out=xdt[:], in0=xs[b][:],
                             in1=dt_[:].unsqueeze(2).to_broadcast([128, 16, 64]), op=ALU.mult)
                        yb_i = ci % 2
                        yn = X("ybuf%d" % yb_i)
                        for g in range(4):
                            q = g % 2
                            h0 = g * 4
                            P.op("dve", "tensor_tensor", ["tri" + nm, X("dtA")], [X("rhsb%d" % q)], out=rhsb[q][:],
                                 in0=tri[d_][:].unsqueeze(1).to_broadcast([128, 4, 128]),
                                 in1=dtA[:, h0:h0 + 4].unsqueeze(2).to_broadcast([128, 4, 128]), op=ALU.mult)
                            P.mm(pA[:].rearrange("p r l -> p (r l)"), ones[:], rhsb[q][:].rearrange("p r l -> p (r l)"), True, True,
                                 ["onesC", X("rhsb%d" % q)], [X("pA")])
                            P.op("dve", "tensor_tensor", [X("pA"), X("acs")], [X("dif%d" % q)], out=dif[q][:], in0=pA[:],
                                 in1=acs[:, h0:h0 + 4].unsqueeze(2).to_broadcast([128, 4, 128]), op=ALU.subtract)
                            P.op("act", "activation", [X("pA")], [X("cd")], out=cd[:, h0:h0 + 4], in_=pA[:, :, last], func=AF.Exp)
                            P.op("dve", "tensor_tensor", [X("dif%d" % q), "nmk" + nm], [X("dif%d" % q)], out=dif[q][:], in0=dif[q][:],
                                 in1=nmk[d_][:].unsqueeze(1).to_broadcast([128, 4, 128]), op=ALU.min)
                            P.op("act", "activation", [X("dif%d" % q)], [X("seg%d" % q)], out=seg[q][:], in_=dif[q][:], func=AF.Exp)
                            P.mm(pB[:, 0:128], bT[b][:, g, :], cT[b][:, g, :], True, True, [X("bT%d" % b), X("cT%d" % b)], [X("pBcb")])
                            P.op("dve", "tensor_tensor", [X("seg%d" % q), X("pBcb")], [X("mT%d" % q)], out=mT[q][:], in0=seg[q][:],
                                 in1=pB[:, 0:128].unsqueeze(1).to_broadcast([128, 4, 128]), op=ALU.mult)
                            for r in range(4):
                                P.mm(pB[:, 128 + r * 64:128 + (r + 1) * 64], mT[q][:, r, :], xdt[:, h0 + r, :], True, True,
                                     [X("mT%d" % q), X("xdt")], [X("pBy")])
                            P.op("pool", "tensor_tensor", [X("xdt"), X("seg%d" % q)], [X("xdd%d" % q)], out=xdd[q][:], in0=xdt[:, h0:h0 + 4, :],
                                 in1=seg[q][:, :, last:last + 1].to_broadcast([128, 4, 64]), op=ALU.mult)
                            P.mm(pC[:, 0:256], bt_[b][:, g * 128:(g + 1) * 128], xdd[q][:].rearrange("p r q -> p (r q)"), True, True,
                                 [X("btok%d" % b), X("xdd%d" % q)], [X("pCs")])
                            P.mm(pC[:, 256:512], cT[b][:, g, :], prevb[:, h0:h0 + 4, :].rearrange("p r q -> p (r q)"), True, True,
                                 [X("cT%d" % b), X("prevb%d" % g)], [X("pCo")])
                            P.op("dve", "tensor_tensor", [X("pCo"), X("dout_")], [X("tmpc%d" % q)], out=tmp[q][:],
                                 in0=pC[:, 256:512].rearrange("p (r q) -> p r q", q=64),
                                 in1=dout_[:, h0:h0 + 4].unsqueeze(2).to_broadcast([128, 4, 64]), op=ALU.mult)
                            P.op("dve", "tensor_tensor", [X("tmpc%d" % q), X("pBy")], [yn], out=ybuf[yb_i][:, h0:h0 + 4, :], in0=tmp[q][:],
                                 in1=pB[:, 128:384].rearrange("p (r q) -> p r q", q=64), op=ALU.add)
                            P.op("dve", "tensor_tensor", [X("prev"), X("cd")], [X("prev")], out=prev[:, h0:h0 + 4, :], in0=prev[:, h0:h0 + 4, :],
                                 in1=cd[:, h0:h0 + 4].unsqueeze(2).to_broadcast([128, 4, 64]), op=ALU.mult)
                            P.op("dve", "tensor_tensor", [X("prev"), X("pCs")], [X("prev")], out=prev[:, h0:h0 + 4, :], in0=prev[:, h0:h0 + 4, :],
                                 in1=pC[:, 0:256].rearrange("p (r q) -> p r q", q=64), op=ALU.add)
                            P.op("act", "activation", [X("prev")], [X("prevb%d" % g)], out=prevb[:, h0:h0 + 4, :], in_=prev[:, h0:h0 + 4, :], func=AF.Copy)
                        P.dma("act", ydst[t0:t0 + 128, :], ybuf[yb_i][:].rearrange("p h q -> p (h q)"), [yn], [X("ydst%d" % (ci % 8))])
                return body

            P.threads([ssd_dir(0), ssd_dir(1)])
        P.emit()

        with ExitStack() as st:
            sb = lambda n, s, d=F32: st.enter_context(nc.sbuf_tensor(n, list(s), d))
            dsk = sb("dsk", [128, 16])
            gn = sb("gn", [128, 1024])
            P.dma("sp", dsk[:], ssdd_rep, [], ["dsk"])
            P.dma("sp", gn[:], ssdn_rep, [], ["gn"])

            def fin(par):
                X = lambda s_: s_ + str(par)
                ya = sb(X("yaC"), [128, 16, 64])
                ybb = sb(X("ybC"), [128, 16, 64])
                xs_ = sb(X("xsC"), [128, 16, 64], BF16)
                zt = sb(X("ztC"), [128, 1024])
                sq = sb(X("sqC"), [128, 1024])
                gss = sb(X("gssC"), [128, 4])
                yo = sb(X("yoC"), [128, 1024], BF16)

                def body():
                    for c in range(par, NCH, 2):
                        t0 = c * 128
                        P.dma("sp", ya[:].rearrange("p h q -> p (h q)"), yf[t0:t0 + 128, :], [], [X("yaC")])
                        P.dma("sp", ybb[:].rearrange("p h q -> p (h q)"), yb_d[t0:t0 + 128, :], [], [X("ybC")])
                        P.dma("sp", xs_[:], xs_tok[t0:t0 + 128, :].rearrange("p (h q) -> p h q", q=64), [], [X("xsC")])
                        P.dma("sp", zt[:], z_tok[t0:t0 + 128, :], [], [X("ztC")])
                        P.op("dve", "tensor_tensor", [X("yaC"), X("ybC")], [X("yaC")], out=ya[:], in0=ya[:], in1=ybb[:], op=ALU.add)
                        P.op("pool", "tensor_tensor", [X("xsC"), "dsk"], [X("ybC")], out=ybb[:], in0=xs_[:],
                             in1=dsk[:].unsqueeze(2).to_broadcast([128, 16, 64]), op=ALU.mult)
                        P.op("dve", "tensor_tensor", [X("yaC"), X("ybC")], [X("yaC")], out=ya[:], in0=ya[:], in1=ybb[:], op=ALU.add)
                        P.op("act", "activation", [X("ztC")], [X("ztC")], out=zt[:], in_=zt[:], func=AF.Silu)
                        y2 = ya[:].rearrange("p h q -> p (h q)")
                        P.op("dve", "tensor_tensor", [X("yaC"), X("ztC")], [X("yaC")], out=y2, in0=y2, in1=zt[:], op=ALU.mult)
                        P.op("pool", "tensor_tensor", [X("yaC")], [X("sqC")], out=sq[:], in0=y2, in1=y2, op=ALU.mult)
                        P.op("dve", "tensor_reduce", [X("sqC")], [X("gssC")], out=gss[:], in_=sq[:].rearrange("p (g c) -> p g c", c=256),
                             op=ALU.add, axis=AX.X)
                        P.op("dve", "tensor_scalar", [X("gssC")], [X("gssC")], out=gss[:], in0=gss[:], scalar1=1.0 / 256, scalar2=EPS,
                             op0=ALU.mult, op1=ALU.add)
                        P.op("act", "activation", [X("gssC")], [X("gssC")], out=gss[:], in_=gss[:], func=AF.Sqrt)
                        P.op("dve", "reciprocal", [X("gssC")], [X("gssC")], out=gss[:], in_=gss[:])
                        P.op("dve", "tensor_tensor", [X("yaC"), X("gssC")], [X("yaC")], out=ya[:].rearrange("p (g h) q -> p g (h q)", g=4),
                             in0=ya[:].rearrange("p (g h) q -> p g (h q)", g=4),
                             in1=gss[:].unsqueeze(2).to_broadcast([128, 4, 256]), op=ALU.mult)
                        P.op("dve", "tensor_tensor", [X("yaC"), "gn"], [X("yoC")], out=yo[:], in0=y2, in1=gn[:], op=ALU.mult)
                        P.dma("act", yssd[t0:t0 + 128, :], yo[:], [X("yoC")], [X("yssdw%d" % (c % 8))])
                return body

            P.threads([fin(0), fin(1)])
        P.emit()

        NB = LT // 8
        NBS = LSEG // 8
        P.limit = cfg.get("dmax")
        TWO_PI = 6.283185307179586
        with ExitStack() as st:
            sb = lambda n, s, d=F32: st.enter_context(nc.sbuf_tensor(n, list(s), d))
            ps = lambda n, s, d=F32: st.enter_context(nc.psum_tensor(n, list(s), d))
            identF = sb("identF", [128, 128])
            P.op("pool", "memset", [], ["identF"], identF[:], 1.0)
            P.op("pool", "affine_select", ["identF"], ["identF"], out=identF[:], in_=identF[:], pattern=[[-1, 128]],
                 compare_op=ALU.is_equal, fill=0.0, base=0, channel_multiplier=1)
            bm = sb("bmD", [128, 8, 16])
            P.op("pool", "memset", [], ["bmD"], bm[:], 1.0)
            P.op("pool", "affine_select", ["bmD"], ["bmD"], out=bm[:], in_=bm[:], pattern=[[-16, 8], [0, 16]],
                 compare_op=ALU.is_ge, fill=0.0, base=0, channel_multiplier=1)
            P.op("pool", "affine_select", ["bmD"], ["bmD"], out=bm[:], in_=bm[:], pattern=[[16, 8], [0, 16]],
                 compare_op=ALU.is_ge, fill=0.0, base=15, channel_multiplier=-1)
            PM = [sb("PM%d" % i, [128, 2, 64]) for i in range(4)]
            CM = [sb("CM%d" % i, [128, 8, 16]) for i in range(4)]
            for pr_ in range(4):
                P.op("pool", "memset", [], ["PM%d" % pr_], PM[pr_][:], 1.0)
                P.op("pool", "affine_select", ["PM%d" % pr_], ["PM%d" % pr_], out=PM[pr_][:], in_=PM[pr_][:],
                     pattern=[[-16, 2], [0, 64]], compare_op=ALU.is_ge, fill=0.0, base=-32 * pr_, channel_multiplier=1)
                P.op("pool", "affine_select", ["PM%d" % pr_], ["PM%d" % pr_], out=PM[pr_][:], in_=PM[pr_][:],
                     pattern=[[16, 2], [0, 64]], compare_op=ALU.is_ge, fill=0.0, base=32 * pr_ + 15, channel_multiplier=-1)
                P.op("pool", "memset", [], ["CM%d" % pr_], CM[pr_][:], 1.0)
                for h in range(2):
                    P.op("pool", "affine_select", ["CM%d" % pr_], ["CM%d" % pr_], out=CM[pr_][h * 64:(h + 1) * 64],
                         in_=CM[pr_][h * 64:(h + 1) * 64], pattern=[[1, 8], [0, 16]], compare_op=ALU.is_equal, fill=0.0,
                         base=-(2 * pr_ + h), channel_multiplier=0)
            KV = sb("KV", [128, 8, 9])
            P.op("pool", "iota", [], ["KV"], KV[:], pattern=[[0, 8], [1, 9]], base=0, channel_multiplier=0,
                 allow_small_or_imprecise_dtypes=True)
            BL = sb("BL", [128, NBS])
            BHs = [sb("BH%d" % i, [128, NBS]) for i in range(2)]
            P.op("pool", "iota", [], ["BL"], BL[:], pattern=[[0, NBS // 32], [1, 32]], base=0, channel_multiplier=0,
                 allow_small_or_imprecise_dtypes=True)
            for s_ in range(2):
                P.op("pool", "iota", [], ["BH%d" % s_], BHs[s_][:], pattern=[[1, NBS // 32], [0, 32]], base=s_ * (NBS // 32),
                     channel_multiplier=0, allow_small_or_imprecise_dtypes=True)
            dcol = sb("dcolD", [128, 8])
            flg = sb("flgD", [128, 1])
            P.dma("sp", dcol[:], s5_dcol, [], ["dcolD"])
            P.dma("sp", flg[:], flag, [], ["flgD"])
            sm = sb("smD", [128, 3, 8])
            bg = sb("bgD", [128, 4, 128])
            ncim = sb("ncim", [128, 128])
            sml = {n_: sb("sm_" + n_, [128, 8]) for n_ in ("dl", "xr", "xi", "numr", "den", "t1", "t2", "qr", "qi", "tb", "t0", "t32")}
            smi = sb("sm_i", [128, 8], I32)
            k9 = {n_: sb("k9_" + n_, [128, 8, 9]) for n_ in ("er", "tn", "tf", "cs", "sn", "pr", "pi")}
            k9i = sb("k9_i", [128, 8, 9], I32)
            Bb = {n_: sb("Bb_" + n_, [128, 8, 16]) for n_ in ("r", "i", "a", "b")}
            colsT = sb("colsT", [128, 3, 4])
            scr = sb("scrD", [128, 11, 512])
            RN = lambda a, b_: ["R%d" % i for i in range(a, b_)]
            X_r = scr[:, 0:2, :].rearrange("p a (b c) -> p (a b) c", c=128)
            X_i = scr[:, 2:4, :].rearrange("p a (b c) -> p (a b) c", c=128)
            Zr_t = sb("Zr_t", [128, 9, 128])
            Zi_t = sb("Zi_t", [128, 9, 128])
            xtmp = sb("xtmp", [128, 8, 128])
            W1 = sb("W1", [128, 8, 2, 4, 128], BF16)
            W2 = sb("W2", [128, 2, 9, 2, 4, 128], BF16)
            Kbd = sb("Kbd", [128, 2, 8, 128], BF16)
            ktmp = sb("ktmp", [128, 128])
            uft = sb("uft", [128, LT], BF16)
            u3 = uft[:].rearrange("p (b j) -> p j b", j=8)
            S = sb("S_D", [128, 4, 2, NBS])
            HB = sb("HB_D", [128, 2, 4, 2, NB + 1], BF16)
            carry = sb("carryD", [128, 4, 2])
            stg = sb("stgD", [128, NBS, 8], BF16)
            gl = [sb("glD%d" % i, [128, NBS]) for i in range(3)]
            pk = ps("pkD", [128, 4, 128])
            ptl = [ps("ptD%d" % i, [128, 4, 128]) for i in range(2)]
            pS = [ps("pSD%d" % i, [128, 512]) for i in range(2)]
            po = [ps("poD%d" % i, [128, 512]) for i in range(2)]
            mskq = _rr(["dve", "pool"])
            psq = 0
            poq = 0
            T = lambda i: scr[:, i, 0:NBS]
            DST = cfg.get("dstage", 9)
            for ft in range(cfg.get("nft", 8)):
                P.dma("sp", uft[:], uT[ft * 128:(ft + 1) * 128, :], ["uT"], ["uft"])
                for d_ in range(2):
                    P.dma("sp", sm[:], s5_small[d_, ft], [], ["smD"])
                    P.dma("sp", bg[:], s5_big[d_, ft], [], ["bgD"])
                    are, aim, lst = sm[:, 0, :], sm[:, 1, :], sm[:, 2, :]
                    bre, bim, cre, cim = bg[:, 0, :], bg[:, 1, :], bg[:, 2, :], bg[:, 3, :]
                    V = lambda n_: sml[n_][:]
                    P.op("act", "activation", ["smD"], ["sm_dl"], out=V("dl"), in_=lst, func=AF.Exp)
                    P.op("dve", "tensor_tensor", ["smD", "sm_dl"], ["sm_xr"], out=V("xr"), in0=are, in1=V("dl"), op=ALU.mult)
                    P.op("dve", "tensor_tensor", ["smD", "sm_dl"], ["sm_xi"], out=V("xi"), in0=aim, in1=V("dl"), op=ALU.mult)
                    b9 = lambda ap: ap.unsqueeze(2).to_broadcast([128, 8, 9])
                    P.op("dve", "tensor_tensor", ["KV", "sm_xr"], ["k9_er"], out=k9["er"][:], in0=KV[:], in1=b9(V("xr")), op=ALU.mult)
                    P.op("act", "activation", ["k9_er"], ["k9_er"], out=k9["er"][:], in_=k9["er"][:], func=AF.Exp)
                    P.op("dve", "tensor_tensor", ["KV", "sm_xi"], ["k9_tn"], out=k9["tn"][:], in0=KV[:], in1=b9(V("xi")), op=ALU.mult)
                    P.op("dve", "tensor_scalar", ["k9_tn"], ["k9_tn"], out=k9["tn"][:], in0=k9["tn"][:], scalar1=1.0 / TWO_PI, scalar2=None, op0=ALU.mult)
                    for (dst, off) in (("sn", 0.0), ("cs", 0.25)):
                        if off:
                            P.op("dve", "tensor_scalar", ["k9_tn"], ["k9_tn"], out=k9["tn"][:], in0=k9["tn"][:], scalar1=off, scalar2=None, op0=ALU.add)
                        P.op("dve", "tensor_copy", ["k9_tn"], ["k9_i"], out=k9i[:], in_=k9["tn"][:])
                        P.op("dve", "tensor_copy", ["k9_i"], ["k9_tf"], out=k9["tf"][:], in_=k9i[:])
                        P.op("dve", "tensor_tensor", ["k9_tn", "k9_tf"], ["k9_tf"], out=k9["tf"][:], in0=k9["tn"][:], in1=k9["tf"][:], op=ALU.subtract)
                        P.op("act", "activation", ["k9_tf"], ["k9_" + dst], out=k9[dst][:], in_=k9["tf"][:], func=AF.Sin, scale=TWO_PI)
                    P.op("dve", "tensor_tensor", ["k9_er", "k9_cs"], ["k9_pr"], out=k9["pr"][:], in0=k9["er"][:], in1=k9["cs"][:], op=ALU.mult)
                    P.op("dve", "tensor_tensor", ["k9_er", "k9_sn"], ["k9_pi"], out=k9["pi"][:], in0=k9["er"][:], in1=k9["sn"][:], op=ALU.mult)
                    pr1, pi1 = k9["pr"][:, :, 1], k9["pi"][:, :, 1]
                    P.op("dve", "tensor_scalar", ["k9_pr"], ["sm_numr"], out=V("numr"), in0=pr1, scalar1=-1.0, scalar2=None, op0=ALU.add)
                    P.op("dve", "tensor_tensor", ["smD"], ["sm_den"], out=V("den"), in0=are, in1=are, op=ALU.mult)
                    P.op("dve", "tensor_tensor", ["smD"], ["sm_t1"], out=V("t1"), in0=aim, in1=aim, op=ALU.mult)
                    P.op("dve", "tensor_tensor", ["sm_den", "sm_t1"], ["sm_den"], out=V("den"), in0=V("den"), in1=V("t1"), op=ALU.add)
                    P.op("dve", "reciprocal", ["sm_den"], ["sm_den"], out=V("den"), in_=V("den"))
                    P.op("dve", "tensor_tensor", ["sm_numr", "smD"], ["sm_t1"], out=V("t1"), in0=V("numr"), in1=are, op=ALU.mult)
                    P.op("dve", "tensor_tensor", ["k9_pi", "smD"], ["sm_t2"], out=V("t2"), in0=pi1, in1=aim, op=ALU.mult)
                    P.op("dve", "tensor_tensor", ["sm_t1", "sm_t2"], ["sm_t1"], out=V("t1"), in0=V("t1"), in1=V("t2"), op=ALU.add)
                    P.op("dve", "tensor_tensor", ["sm_t1", "sm_den"], ["sm_qr"], out=V("qr"), in0=V("t1"), in1=V("den"), op=ALU.mult)
                    P.op("dve", "tensor_tensor", ["k9_pi", "smD"], ["sm_t1"], out=V("t1"), in0=pi1, in1=are, op=ALU.mult)
                    P.op("dve", "tensor_tensor", ["sm_numr", "smD"], ["sm_t2"], out=V("t2"), in0=V("numr"), in1=aim, op=ALU.mult)
                    P.op("dve", "tensor_tensor", ["sm_t1", "sm_t2"], ["sm_t1"], out=V("t1"), in0=V("t1"), in1=V("t2"), op=ALU.subtract)
                    P.op("dve", "tensor_tensor", ["sm_t1", "sm_den"], ["sm_qi"], out=V("qi"), in0=V("t1"), in1=V("den"), op=ALU.mult)
                    b16 = lambda ap: ap.unsqueeze(2).to_broadcast([128, 8, 16])
                    g16 = lambda ap: ap.rearrange("p (g c) -> p g c", c=16)
                    TT = lambda rd, wr, out, in0, in1, op, eng="dve": P.op(eng, "tensor_tensor", rd, wr, out=out, in0=in0, in1=in1, op=op)
                    TT(["bgD", "sm_qr"], ["Bb_a"], Bb["a"][:], g16(bre), b16(V("qr")), ALU.mult)
                    TT(["bgD", "sm_qi"], ["Bb_b"], Bb["b"][:], g16(bim), b16(V("qi")), ALU.mult)
                    TT(["Bb_a", "Bb_b"], ["Bb_r"], Bb["r"][:], Bb["a"][:], Bb["b"][:], ALU.subtract)
                    TT(["bgD", "sm_qr"], ["Bb_a"], Bb["a"][:], g16(bim), b16(V("qr")), ALU.mult)
                    TT(["bgD", "sm_qi"], ["Bb_b"], Bb["b"][:], g16(bre), b16(V("qi")), ALU.mult)
                    TT(["Bb_a", "Bb_b"], ["Bb_i"], Bb["i"][:], Bb["a"][:], Bb["b"][:], ALU.add)
                    for kk in range(8):
                        prk = b16(k9["pr"][:, :, kk])
                        pik = b16(k9["pi"][:, :, kk])
                        xr_k = g16(X_r[:, kk, :])
                        xi_k = g16(X_i[:, kk, :])
                        xt_k = g16(xtmp[:, kk, :])
                        TT(["k9_pr", "Bb_r"], RN(0, 2), xr_k, Bb["r"][:], prk, ALU.mult)
                        TT(["k9_pi", "Bb_i"], ["xtmp"], xt_k, Bb["i"][:], pik, ALU.mult, "pool")
                        TT(RN(0, 2) + ["xtmp"], RN(0, 2), xr_k, xr_k, xt_k, ALU.subtract)
                        TT(["k9_pr", "Bb_i"], RN(2, 4), xi_k, Bb["i"][:], prk, ALU.mult)
                        TT(["k9_pi", "Bb_r"], ["xtmp"], xt_k, Bb["r"][:], pik, ALU.mult, "pool")
                        TT(RN(2, 4) + ["xtmp"], RN(2, 4), xi_k, xi_k, xt_k, ALU.add)
                    for kk in range(9):
                        prk = b16(k9["pr"][:, :, kk])
                        pik = b16(k9["pi"][:, :, kk])
                        zr_k = g16(Zr_t[:, kk, :])
                        zi_k = g16(Zi_t[:, kk, :])
                        xt_k = g16(xtmp[:, kk % 8, :])
                        TT(["k9_pr", "bgD"], ["Zr_t"], zr_k, g16(cre), prk, ALU.mult)
                        TT(["k9_pi", "bgD"], ["xtmp"], xt_k, g16(cim), pik, ALU.mult, "pool")
                        TT(["Zr_t", "xtmp"], ["Zr_t"], zr_k, zr_k, xt_k, ALU.subtract)
                        TT(["k9_pi", "bgD"], ["Zi_t"], zi_k, g16(cre), pik, ALU.mult)
                        TT(["k9_pr", "bgD"], ["xtmp"], xt_k, g16(cim), prk, ALU.mult, "pool")
                        TT(["Zi_t", "xtmp"], ["Zi_t"], zi_k, zi_k, xt_k, ALU.add)
                    P.op("dve", "tensor_scalar", ["bgD"], ["ncim"], out=ncim[:], in0=cim, scalar1=-1.0, scalar2=None, op0=ALU.mult)
                    for half in range(2):
                        for tl in range(4):
                            tau = half * 4 + tl
                            P.mm(pk[:, tl, :], X_r[0:64, tau, :], cre[0:64, :], True, False, RN(0, 2) + ["bgD"], ["pkD"])
                            P.mm(pk[:, tl, :], X_i[0:64, tau, :], ncim[0:64, :], False, True, RN(2, 4) + ["ncim"], ["pkD"])
                        for tl in range(4):
                            tau = half * 4 + tl
                            if tau == 0 and d_ == 0:
                                P.op("dve", "scalar_tensor_tensor", ["identF", "dcolD", "pkD"], ["ktmp"], out=ktmp[:], in0=identF[:],
                                     scalar=dcol[:, ft:ft + 1], in1=pk[:, tl, :], op0=ALU.mult, op1=ALU.add)
                                P.op("dve", "tensor_tensor", ["ktmp", "bmD"], ["Kbd"], out=Kbd[:, d_, tau, :], in0=ktmp[:],
                                     in1=bm[:].rearrange("p g c -> p (g c)"), op=ALU.mult)
                            else:
                                P.op("dve", "tensor_tensor", ["pkD", "bmD"], ["Kbd"], out=Kbd[:, d_, tau, :], in0=pk[:, tl, :],
                                     in1=bm[:].rearrange("p g c -> p (g c)"), op=ALU.mult)
                    for kk in range(8):
                        for ri in range(2):
                            src = (X_r if ri == 0 else X_i)[:, kk, :]
                            slot = (kk * 2 + ri) % 2
                            pt = ptl[slot]
                            P.tr(pt[:, 0, :], src, identF[:], (RN(0, 2) if ri == 0 else RN(2, 4)) + ["identF"], ["ptD%d" % slot])
                            for pr_ in range(4):
                                P.op("dve", "tensor_tensor", ["ptD%d" % slot, "PM%d" % pr_], ["W1"], out=W1[:, kk, ri, pr_, :],
                                     in0=pt[:, 0, :], in1=PM[pr_][:].rearrange("p a b -> p (a b)"), op=ALU.mult)
                    for kk in range(1, 9):
                        for ri in range(2):
                            for pr_ in range(4):
                                cmv = CM[pr_][:].rearrange("p g c -> p (g c)")
                                if ri == 0:
                                    P.op("pool", "tensor_tensor", ["Zr_t", "CM%d" % pr_], ["W2"], out=W2[:, d_, kk, ri, pr_, :],
                                         in0=Zr_t[:, kk, :], in1=cmv, op=ALU.mult)
                                else:
                                    P.op("dve", "scalar_tensor_tensor", ["Zi_t", "CM%d" % pr_], ["W2"], out=W2[:, d_, kk, ri, pr_, :],
                                         in0=Zi_t[:, kk, :], scalar=-1.0, in1=cmv, op0=ALU.mult, op1=ALU.mult)
                    P.op("dve", "tensor_scalar", ["sm_xi"], ["sm_tb"], out=V("tb"), in0=V("xi"), scalar1=8.0 / TWO_PI, scalar2=None, op0=ALU.mult)
                    P.op("dve", "tensor_copy", ["sm_tb"], ["sm_i"], out=smi[:], in_=V("tb"))
                    P.op("dve", "tensor_copy", ["sm_i"], ["sm_t0"], out=V("t0"), in_=smi[:])
                    P.op("dve", "tensor_tensor", ["sm_tb", "sm_t0"], ["sm_t0"], out=V("t0"), in0=V("tb"), in1=V("t0"), op=ALU.subtract)
                    P.op("dve", "tensor_scalar", ["sm_t0"], ["sm_tb"], out=V("tb"), in0=V("t0"), scalar1=32.0, scalar2=None, op0=ALU.mult)
                    P.op("dve", "tensor_copy", ["sm_tb"], ["sm_i"], out=smi[:], in_=V("tb"))
                    P.op("dve", "tensor_copy", ["sm_i"], ["sm_t32"], out=V("t32"), in_=smi[:])
                    P.op("dve", "tensor_tensor", ["sm_tb", "sm_t32"], ["sm_t32"], out=V("t32"), in0=V("tb"), in1=V("t32"), op=ALU.subtract)
                    for h in range(2):
                        hs = slice(h * 64, (h + 1) * 64)
                        P.op("dve", "tensor_copy", ["sm_t0"], ["colsT"], out=colsT[hs, 0, :], in_=sml["t0"][hs, h::2])
                        P.op("dve", "tensor_copy", ["sm_t32"], ["colsT"], out=colsT[hs, 1, :], in_=sml["t32"][hs, h::2])
                        P.op("dve", "tensor_copy", ["k9_er"], ["colsT"], out=colsT[hs, 2, :], in_=k9["er"][hs, h::2, 8])
                    if DST < 2:
                        continue
                    for ui, s_ in enumerate((0, 1) if d_ == 0 else (1, 0)):
                        blk0 = s_ * NBS
                        for pr_ in range(4):
                            for ri in range(2):
                                pb = psq % 2
                                psq += 1
                                for j in range(8):
                                    kk = (7 - j) if d_ == 0 else j
                                    P.mm(pS[pb][:, 0:NBS], W1[:, kk, ri, pr_, :], u3[:, j, blk0:blk0 + NBS], j == 0, j == 7,
                                         ["W1", "uft"], ["pSD%d" % pb])
                                P.op("act", "activation", ["pSD%d" % pb], ["S_D%d" % pr_], out=S[:, pr_, ri, :], in_=pS[pb][:, 0:NBS], func=AF.Copy)
                        if DST < 3:
                            continue
                        for pr_ in range(4):
                            tu, ti_, tf, cs, sn, T_r, T_i, g_r, g_i, m1, m2 = [T(i) for i in range(11)]
                            tii = ti_.bitcast(I32)
                            t0c, t32c, decc = colsT[:, 0, pr_:pr_ + 1], colsT[:, 1, pr_:pr_ + 1], colsT[:, 2, pr_:pr_ + 1]
                            P.op("pool", "tensor_scalar", ["BH%d" % s_, "colsT"], ["R0"], out=tu, in0=BHs[s_][:], scalar1=t32c, scalar2=None, op0=ALU.mult)
                            P.op("dve", "scalar_tensor_tensor", ["BL", "colsT", "R0"], ["R0"], out=tu, in0=BL[:], scalar=t0c, in1=tu, op0=ALU.mult, op1=ALU.add)
                            for (dst, dn, off) in ((sn, "R4", 0.0), (cs, "R3", 0.25)):
                                if off:
                                    P.op("pool", "tensor_scalar", ["R0"], ["R0"], out=tu, in0=tu, scalar1=off, scalar2=None, op0=ALU.add)
                                P.op("dve", "tensor_copy", ["R0"], ["R1"], out=tii, in_=tu)
                                P.op("dve", "tensor_copy", ["R1"], ["R2"], out=tf, in_=tii)
                                P.op("pool", "tensor_tensor", ["R0", "R2"], ["R2"], out=tf, in0=tu, in1=tf, op=ALU.subtract)
                                P.op("act", "activation", ["R2"], [dn], out=dst, in_=tf, func=AF.Sin, scale=TWO_PI)
                            S_r, S_i = S[:, pr_, 0, :], S[:, pr_, 1, :]
                            sg = 1.0 if d_ == 0 else -1.0
                            sop = ALU.add if d_ == 0 else ALU.subtract
                            sop2 = ALU.subtract if d_ == 0 else ALU.add
                            sn_ = "S_D%d" % pr_
                            TT([sn_, "R3"], ["R5"], T_r, S_r, cs, ALU.mult)
                            TT([sn_, "R4"], ["R9"], m1, S_i, sn, ALU.mult, "pool")
                            TT(["R5", "R9"], ["R5"], T_r, T_r, m1, sop)
                            TT([sn_, "R3"], ["R6"], T_i, S_i, cs, ALU.mult)
                            TT([sn_, "R4"], ["R10"], m2, S_r, sn, ALU.mult, "pool")
                            TT(["R6", "R10"], ["R6"], T_i, T_i, m2, sop2)
                            rv = (lambda ap: ap) if d_ == 0 else (lambda ap: ap[:, ::-1])
                            dbc = decc.to_broadcast([128, NBS])
                            for (src, sname, dst, dname, ri) in ((T_r, "R5", g_r, "R7", 0), (T_i, "R6", g_i, "R8", 1)):
                                init = 0.0 if ui == 0 else carry[:, pr_, ri:ri + 1]
                                P.op("dve", "tensor_tensor_scan", [sname, "colsT", "carryD"], [dname], out=rv(dst), data0=dbc, data1=rv(src),
                                     initial=init, op0=ALU.mult, op1=ALU.add)
                                if ui == 0:
                                    lastc = (NBS - 1) if d_ == 0 else 0
                                    P.op("dve", "tensor_scalar", [dname, "flgD"], ["carryD"], out=carry[:, pr_, ri:ri + 1], in0=dst[:, lastc:lastc + 1],
                                         scalar1=flg[:, 0:1], scalar2=None, op0=ALU.mult)
                            if d_ == 0:
                                o_r = HB[:, 0, pr_, 0, blk0 + 1:blk0 + NBS + 1]
                                o_i = HB[:, 0, pr_, 1, blk0 + 1:blk0 + NBS + 1]
                                sl_ = slice(0, NBS)
                            elif s_ == 1:
                                o_r = HB[:, 1, pr_, 0, NBS - 1:NB - 1]
                                o_i = HB[:, 1, pr_, 1, NBS - 1:NB - 1]
                                sl_ = slice(0, NBS)
                            else:
                                o_r = HB[:, 1, pr_, 0, 0:NBS - 1]
                                o_i = HB[:, 1, pr_, 1, 0:NBS - 1]
                                sl_ = slice(1, NBS)
                            hn = "HB%d_%d" % (d_, pr_)
                            TT(["R7", "R3"], ["R9"], m1, g_r, cs, ALU.mult)
                            TT(["R8", "R4"], ["R10"], m2, g_i, sn, ALU.mult, "pool")
                            TT(["R9", "R10"], [hn], o_r, m1[:, sl_], m2[:, sl_], sop2)
                            TT(["R8", "R3"], ["R9"], m1, g_i, cs, ALU.mult)
                            TT(["R7", "R4"], ["R10"], m2, g_r, sn, ALU.mult, "pool")
                            TT(["R9", "R10"], [hn], o_i, m1[:, sl_], m2[:, sl_], sop)
                            if ui == 1:
                                zc = 0 if d_ == 0 else NB - 1
                                fc = NBS if d_ == 0 else NBS - 1
                                P.op("dve", "memset", [], [hn], HB[:, d_, pr_, :, zc:zc + 1], 0.0)
                                P.op("dve", "tensor_scalar", [hn, "flgD"], [hn], out=HB[:, d_, pr_, :, fc:fc + 1], in0=HB[:, d_, pr_, :, fc:fc + 1],
                                     scalar1=flg[:, 0:1], scalar2=None, op0=ALU.mult)
                if DST < 4:
                    continue
                hall = ["HB%d_%d" % (a, b_) for a in range(2) for b_ in range(4)]
                for s_ in range(2):
                    blk0 = s_ * NBS
                    for i in range(8):
                        pb = poq % 2
                        poq += 1
                        mms = []
                        for j in range(0, i + 1):
                            mms.append((Kbd[:, 0, i - j, :], u3[:, j, blk0:blk0 + NBS]))
                        for j in range(i, 8):
                            mms.append((Kbd[:, 1, j - i, :], u3[:, j, blk0:blk0 + NBS]))
                        for pr_ in range(4):
                            for ri in range(2):
                                mms.append((W2[:, 0, i + 1, ri, pr_, :], HB[:, 0, pr_, ri, blk0:blk0 + NBS]))
                                mms.append((W2[:, 1, 8 - i, ri, pr_, :], HB[:, 1, pr_, ri, blk0:blk0 + NBS]))
                        for mi, (l_, r_) in enumerate(mms):
                            P.mm(po[pb][:, 0:NBS], l_, r_, mi == 0, mi == len(mms) - 1, ["Kbd", "W2", "uft"] + hall, ["poD%d" % pb])
                        v = po[pb][:, 0:NBS]
                        P.op("act", "activation", ["poD%d" % pb], ["glD0"], out=gl[0][:], in_=v, func=AF.Square)
                        P.op("dve", "tensor_scalar", ["glD0"], ["glD0"], out=gl[0][:], in0=gl[0][:], scalar1=0.044715, scalar2=1.0, op0=ALU.mult, op1=ALU.add)
                        P.op("dve", "tensor_tensor", ["glD0", "poD%d" % pb], ["glD1"], out=gl[1][:], in0=gl[0][:], in1=v, op=ALU.mult)
                        P.op("act", "activation", ["glD1"], ["glD2"], out=gl[2][:], in_=gl[1][:], func=AF.Sigmoid, scale=1.5957691216057308)
                        P.op("dve", "tensor_tensor", ["glD2", "poD%d" % pb], ["stgD"], out=stg[:, :, i], in0=gl[2][:], in1=v, op=ALU.mult)
                    P.dma("sp", y5gT[ft * 128:(ft + 1) * 128, s_ * LSEG:(s_ + 1) * LSEG], stg[:].rearrange("p b i -> p (b i)"), ["stgD"], ["y5gT"])
        if cfg.get("dump"):
            for i_, o_ in enumerate(P.ops):
                print(i_, o_.eng, getattr(o_, 'desc', 'mm/dma') if hasattr(o_, 'desc') else '-', o_.reads, o_.writes)
        P.emit()
        P.limit = None

        with ExitStack() as st:
            sb = lambda n, s, d=F32: st.enter_context(nc.sbuf_tensor(n, list(s), d))
            gA = sb("gA", [128, 16])
            gM = sb("gM", [128, 16])
            P.dma("sp", gA[:], norm_attn, [], ["gA"])
            P.dma("sp", gM[:], norm_mem, [], ["gM"])
            wst2 = [sb("wst2_%d" % i, [128, 2048]) for i in range(2)]
            wbf2 = [sb("wbf2_%d" % i, [128, 2048], BF16) for i in range(2)]
            ci = 0
            ceng = _rr(["dve", "pool"])
            for (src, dst, gn_, gname, kdim, ncol) in ((w_glu, w_glu_bf, None, None, 1024, 1024), (w_out, w_out_bf, None, None, 2048, 2048),
                                                       (w_q, w_q_bf, gA, "gA", 2048, 2048), (w_k, w_k_bf, gM, "gM", 2048, 2048),
                                                       (w_v, w_v_bf, gM, "gM", 2048, 2048), (w_o, w_o_bf, None, None, 2048, 2048)):
                for kc in range(kdim // 128):
                    b = ci % 2
                    ci += 1
                    P.dma("sp", wst2[b][:, 0:ncol], src[kc * 128:(kc + 1) * 128, :], [], ["wst2_%d" % b])
                    if gn_ is None:
                        P.op(ceng(), "tensor_copy", ["wst2_%d" % b], ["wbf2_%d" % b], out=wbf2[b][:, 0:ncol], in_=wst2[b][:, 0:ncol])
                    else:
                        P.op(ceng(), "tensor_scalar", ["wst2_%d" % b, gname], ["wbf2_%d" % b], out=wbf2[b][:, 0:ncol], in0=wst2[b][:, 0:ncol],
                             scalar1=gn_[:, kc:kc + 1], scalar2=None, op0=ALU.mult)
                    P.dma("sp", dst[:, :, kc, :].rearrange("g p n -> p g n"), wbf2[b][:, 0:ncol].rearrange("p (g n) -> p g n", n=512),
                          ["wbf2_%d" % b], [dst.tensor.name])
        P.emit()

        with ExitStack() as st:
            sb = lambda n, s, d=F32: st.enter_context(nc.sbuf_tensor(n, list(s), d))
            ps = lambda n, s, d=F32: st.enter_context(nc.psum_tensor(n, list(s), d))
            ident = sb("identM", [128, 128], BF16)
            P.op("pool", "memset", [], ["identM"], ident[:], 1.0)
            P.op("pool", "affine_select", ["identM"], ["identM"], out=ident[:], in_=ident[:], pattern=[[-1, 128]],
                 compare_op=ALU.is_equal, fill=0.0, base=0, channel_multiplier=1)
            mt = sb("mtM", [128, 2048])
            junk = sb("junkM", [128, 2048], BF16)
            ssm = sb("ssM", [128, 1])
            mn = sb("mnM", [128, 2048], BF16)
            memT = sb("memT", [128, 16, 256], BF16)
            wck = [sb("wckM%d" % i, [128, 16, 512], BF16) for i in range(2)]
            kTs = sb("kTs", [128, 16, 256], BF16)
            vs = sb("vs", [128, 2, 2048], BF16)
            ptm = [ps("ptM%d" % i, [128, 1024], BF16) for i in range(2)]
            pmm_ = [ps("pmM%d" % i, [128, 512]) for i in range(2)]
            tq = 0
            pq = 0
            wq_ = 0
            for sg in range(2):
                for mc in range(2):
                    P.dma("sp", mt[:], mem_in[sg, mc * 128:(mc + 1) * 128, :], [], ["mtM"])
                    P.op("act", "activation", ["mtM"], ["junkM", "ssM"], out=junk[:], in_=mt[:], func=AF.Square, accum_out=ssm[:])
                    P.op("dve", "tensor_scalar", ["ssM"], ["ssM"], out=ssm[:], in0=ssm[:], scalar1=1.0 / D, scalar2=EPS, op0=ALU.mult, op1=ALU.add)
                    P.op("act", "activation", ["ssM"], ["ssM"], out=ssm[:], in_=ssm[:], func=AF.Sqrt)
                    P.op("dve", "reciprocal", ["ssM"], ["ssM"], out=ssm[:], in_=ssm[:])
                    P.op("dve", "tensor_scalar", ["mtM", "ssM"], ["mnM"], out=mn[:], in0=mt[:], scalar1=ssm[:, 0:1], scalar2=None, op0=ALU.mult)
                    for q4 in range(4):
                        pb = tq % 2
                        tq += 1
                        for j in range(4):
                            kc = q4 * 4 + j
                            P.tr(ptm[pb][:, j * 128:(j + 1) * 128], mn[:, kc * 128:(kc + 1) * 128], ident[:], ["mnM", "identM"], ["ptM%d" % pb])
                        P.op("act", "activation", ["ptM%d" % pb], ["memT"], out=memT[:, q4 * 4:(q4 + 1) * 4, mc * 128:(mc + 1) * 128],
                             in_=ptm[pb][:, 0:512].rearrange("p (j n) -> p j n", n=128), func=AF.Copy)
                for g in range(4):
                    wb = wq_ % 2
                    wq_ += 1
                    P.dma("sp", wck[wb][:], w_k_bf[g], ["w_k_bf"], ["wckM%d" % wb])
                    for mcc in range(4):
                        pb = pq % 2
                        pq += 1
                        for kc in range(16):
                            P.mm(pmm_[pb][:, 0:256], wck[wb][:, kc, mcc * 128:(mcc + 1) * 128], memT[:, kc, :], kc == 0, kc == 15,
                                 ["wckM%d" % wb, "memT"], ["pmM%d" % pb])
                        P.op("act", "activation", ["pmM%d" % pb], ["kTs"], out=kTs[:, g * 4 + mcc, :], in_=pmm_[pb][:, 0:256], func=AF.Copy)
                for g in range(4):
                    wb = wq_ % 2
                    wq_ += 1
                    P.dma("sp", wck[wb][:], w_v_bf[g], ["w_v_bf"], ["wckM%d" % wb])
                    for mc in range(2):
                        pb = pq % 2
                        pq += 1
                        for kc in range(16):
                            P.mm(pmm_[pb][:], memT[:, kc, mc * 128:(mc + 1) * 128], wck[wb][:, kc, :], kc == 0, kc == 15,
                                 ["wckM%d" % wb, "memT"], ["pmM%d" % pb])
                        P.op("dve", "tensor_copy", ["pmM%d" % pb], ["vs"], out=vs[:, mc, g * 512:(g + 1) * 512], in_=pmm_[pb][:])
                P.dma("sp", kT_d[sg], kTs[:], ["kTs"], ["kT_d"])
                P.dma("sp", v_d[sg], vs[:], ["vs"], ["v_d"])
        P.emit()

        with ExitStack() as st:
            sb = lambda n, s, d=F32: st.enter_context(nc.sbuf_tensor(n, list(s), d))
            ps = lambda n, s, d=F32: st.enter_context(nc.psum_tensor(n, list(s), d))
            wgl = sb("wgl", [128, 8, 1024], BF16)
            for g_ in range(2):
                P.dma("sp", wgl[:, :, g_ * 512:(g_ + 1) * 512], w_glu_bf[g_], ["w_glu_bf"], ["wgl"])
            onesb = sb("onesbE", [128, 128], BF16)
            P.op("pool", "memset", [], ["onesbE"], onesb[:], 1.0)
            g5 = sb("g5", [128, 8])
            P.dma("sp", g5[:], s5_ncol, [], ["g5"])
            y5 = [sb("y5E%d" % i, [128, 8, 512], BF16) for i in range(2)]
            y2 = sb("y2E", [128, 8, 512])
            sig = [sb("sigE%d" % i, [128, 512]) for i in range(2)]
            sqb = [sb("sqE%d" % i, [128, 512], BF16) for i in range(2)]
            rs = sb("rsE", [128, 512])
            pg = [ps("pgE%d" % i, [128, 512]) for i in range(2)]
            pss = ps("pssE", [128, 512])
            for s in range(NST):
                t0 = s * 512
                b = s % 2
                P.dma("sp", y5[b][:], y5gT[:, t0:t0 + 512].rearrange("(f p) t -> p f t", p=128), ["y5gT"], ["y5E%d" % b])
                for n_ in range(8):
                    q = n_ % 2
                    for kc in range(8):
                        P.mm(pg[q][:], wgl[:, kc, n_ * 128:(n_ + 1) * 128], y5[b][:, kc, :], kc == 0, kc == 7, ["wgl", "y5E%d" % b], ["pgE%d" % q])
                    P.op("act", "activation", ["pgE%d" % q], ["sigE%d" % q], out=sig[q][:], in_=pg[q][:], func=AF.Sigmoid)
                    P.op("dve", "tensor_tensor", ["y5E%d" % b, "sigE%d" % q], ["y2E"], out=y2[:, n_, :], in0=y5[b][:, n_, :], in1=sig[q][:], op=ALU.mult)
                    P.op("act", "activation", ["y2E"], ["sqE%d" % q], out=sqb[q][:], in_=y2[:, n_, :], func=AF.Square)
                    P.mm(pss[:], onesb[:], sqb[q][:], n_ == 0, n_ == 7, ["onesbE", "sqE%d" % q], ["pssE"])
                P.op("dve", "tensor_scalar", ["pssE"], ["rsE"], out=rs[:], in0=pss[:], scalar1=1.0 / 1024, scalar2=EPS, op0=ALU.mult, op1=ALU.add)
                P.op("act", "activation", ["rsE"], ["rsE"], out=rs[:], in_=rs[:], func=AF.Sqrt)
                P.op("dve", "reciprocal", ["rsE"], ["rsE"], out=rs[:], in_=rs[:])
                for n_ in range(8):
                    P.op("dve", "scalar_tensor_tensor", ["y2E", "g5", "rsE"], ["y5E%d" % b], out=y5[b][:, n_, :], in0=y2[:, n_, :],
                         scalar=g5[:, n_:n_ + 1], in1=rs[:], op0=ALU.mult, op1=ALU.mult)
                P.dma("sp", y5nT[:, t0:t0 + 512].rearrange("(f p) t -> p f t", p=128), y5[b][:], ["y5E%d" % b], ["y5nT"])
        P.emit()

        NTT = LT // 128
        with ExitStack() as st:
            sb = lambda n, s, d=F32: st.enter_context(nc.sbuf_tensor(n, list(s), d))
            ps = lambda n, s, d=F32: st.enter_context(nc.psum_tensor(n, list(s), d))
            ident = sb("identE", [128, 128], BF16)
            P.op("pool", "memset", [], ["identE"], ident[:], 1.0)
            P.op("pool", "affine_select", ["identE"], ["identE"], out=ident[:], in_=ident[:], pattern=[[-1, 128]],
                 compare_op=ALU.is_equal, fill=0.0, base=0, channel_multiplier=1)
            onesb = sb("onesb2", [128, 128], BF16)
            P.op("pool", "memset", [], ["onesb2"], onesb[:], 1.0)
            gff = sb("gff", [128, 2048])
            P.dma("sp", gff[:], nffn_rep, [], ["gff"])
            wrs = sb("wrs", [128, 16, 16])
            wrb = sb("wrb", [128, 16, 16], BF16)
            P.dma("sp", wrs[:], w_router_l, [], ["wrs"])
            P.op("dve", "tensor_copy", ["wrs"], ["wrb"], out=wrb[:], in_=wrs[:])
            zrow = sb("zrow", [128, 2048])
            zrowb = sb("zrowb", [128, 2048], BF16)
            P.op("pool", "memset", [], ["zrow"], zrow[:], 0.0)
            P.op("pool", "memset", [], ["zrowb"], zrowb[:], 0.0)
            for r_ in range(0, CAPL, 128):
                for g_ in range(8):
                    P.dma("sp", acc_l[g_][LT + r_:LT + r_ + 128, :], zrow[:, 0:256], ["zrow"], ["acc"])
                P.dma("sp", hn3_bf[LT + r_:LT + r_ + 128, :], zrowb[:], ["zrowb"], ["hn3_bf"])
            wch = [sb("wch%d" % i, [128, 16, 512], BF16) for i in range(2)]
            x1 = sb("x1", [128, 4, 2048])
            ysd = sb("ysd", [128, 4, 1024], BF16)
            yT = sb("yT", [128, 16, 512], BF16)
            hn = sb("hnE", [128, 2048], BF16)
            junk = sb("junkE", [128, 2048], BF16)
            ss = sb("ssE", [128, 1])
            hnT = sb("hnT", [128, 16, 512], BF16)
            qT = sb("qT", [128, 16, 512], BF16)
            kTs = sb("kTs2", [128, 16, 256], BF16)
            vs = sb("vs2", [128, 2, 2048], BF16)
            eT = [sb("eT%d" % i, [128, 512], BF16) for i in range(2)]
            rden = sb("rden", [128, 512])
            hT3 = sb("hT3", [128, 16, 128], BF16)
            pe_ = sb("peE", [128, 16])
            psum_ = sb("psumE", [128, 1])
            ptr = [ps("ptrE%d" % i, [128, 1024], BF16) for i in range(2)]
            pmm = [ps("pmmE%d" % i, [128, 512]) for i in range(4)]
            prt = ps("prtE", [128, 512])
            tq = [0]
            pq = [0]
            wq_ = [0]
            cpe = _rr(["act", "dve"])

            def nxt(c, n):
                v = c[0] % n
                c[0] += 1
                return v

            def rms_to(tt, out_ap, names_w, extra=None):
                P.op("act", "activation", ["x1_%d" % tt], ["junkE", "ssE"], out=junk[:], in_=x1[:, tt, :], func=AF.Square, accum_out=ss[:])
                P.op("dve", "tensor_scalar", ["ssE"], ["ssE"], out=ss[:], in0=ss[:], scalar1=1.0 / D, scalar2=EPS, op0=ALU.mult, op1=ALU.add)
                P.op("act", "activation", ["ssE"], ["ssE"], out=ss[:], in_=ss[:], func=AF.Sqrt)
                P.op("dve", "reciprocal", ["ssE"], ["ssE"], out=ss[:], in_=ss[:])
                if extra is None:
                    P.op("dve", "tensor_scalar", ["x1_%d" % tt, "ssE"], names_w, out=out_ap, in0=x1[:, tt, :], scalar1=ss[:, 0:1], scalar2=None, op0=ALU.mult)
                else:
                    P.op("dve", "scalar_tensor_tensor", ["x1_%d" % tt, "ssE", "gff"], names_w, out=out_ap, in0=x1[:, tt, :], scalar=ss[:, 0:1],
                         in1=extra, op0=ALU.mult, op1=ALU.mult)

            def transposes16(src_tile, src_name, dst_fn, dst_name):
                for q4 in range(4):
                    pb = nxt(tq, 2)
                    for j in range(4):
                        kc = q4 * 4 + j
                        P.tr(ptr[pb][:, j * 128:(j + 1) * 128], src_tile[:, kc * 128:(kc + 1) * 128], ident[:], [src_name, "identE"], ["ptrE%d" % pb])
                    ce = cpe()
                    src = ptr[pb][:, 0:512].rearrange("p (j n) -> p j n", n=128)
                    if ce == "act":
                        P.op("act", "activation", ["ptrE%d" % pb], [dst_name], out=dst_fn(q4), in_=src, func=AF.Copy)
                    else:
                        P.op("dve", "tensor_copy", ["ptrE%d" % pb], [dst_name], out=dst_fn(q4), in_=src)

            def proj_tok(w_d, wname, lhs_tile, lhs_name):
                for dch in range(4):
                    wb = nxt(wq_, 2)
                    P.dma("sp", wch[wb][:], w_d[dch], [wname], ["wch%d" % wb])
                    for tt in range(4):
                        pb = nxt(pq, 4)
                        for kc in range(16):
                            P.mm(pmm[pb][:], lhs_tile[:, kc, tt * 128:(tt + 1) * 128], wch[wb][:, kc, :], kc == 0, kc == 15,
                                 [lhs_name, "wch%d" % wb], ["pmmE%d" % pb])
                        P.op("dve", "tensor_tensor", ["x1_%d" % tt, "pmmE%d" % pb], ["x1_%d" % tt], out=x1[:, tt, dch * 512:(dch + 1) * 512],
                             in0=x1[:, tt, dch * 512:(dch + 1) * 512], in1=pmm[pb][:], op=ALU.add)

            cur_seg = -1
            for s in range(NST):
                t0 = s * 512
                sg = t0 // LSEG
                if sg != cur_seg:
                    cur_seg = sg
                    P.dma("sp", kTs[:], kT_d[sg], ["kT_d"], ["kTs2"])
                    P.dma("sp", vs[:], v_d[sg], ["v_d"], ["vs2"])
                for tt in range(4):
                    P.dma("sp", x1[:, tt, :], x_in[t0 + tt * 128:t0 + (tt + 1) * 128, :], [], ["x1_%d" % tt])
                P.dma("sp", ysd[:], yssd[t0:t0 + 512, :].rearrange("(t p) c -> p t c", p=128), ["yssd"], ["ysd"])
                P.dma("sp", yT[:, 8:16, :], y5nT[:, t0:t0 + 512].rearrange("(f p) t -> p f t", p=128), ["y5nT"], ["yT"])
                for tt in range(4):
                    for q2 in range(2):
                        pb = nxt(tq, 2)
                        for j in range(4):
                            kc = q2 * 4 + j
                            P.tr(ptr[pb][:, j * 128:(j + 1) * 128], ysd[:, tt, kc * 128:(kc + 1) * 128], ident[:], ["ysd", "identE"], ["ptrE%d" % pb])
                        P.op("act", "activation", ["ptrE%d" % pb], ["yT"], out=yT[:, q2 * 4:(q2 + 1) * 4, tt * 128:(tt + 1) * 128],
                             in_=ptr[pb][:, 0:512].rearrange("p (j n) -> p j n", n=128), func=AF.Copy)
                proj_tok(w_out_bf, "w_out_bf", yT, "yT")
                for tt in range(4):
                    rms_to(tt, hn[:], ["hnE"])
                    transposes16(hn, "hnE", lambda q4, tt=tt: hnT[:, q4 * 4:(q4 + 1) * 4, tt * 128:(tt + 1) * 128], "hnT")
                for g in range(4):
                    wb = nxt(wq_, 2)
                    P.dma("sp", wch[wb][:], w_q_bf[g], ["w_q_bf"], ["wch%d" % wb])
                    for mcc in range(4):
                        pb = nxt(pq, 4)
                        for kc in range(16):
                            P.mm(pmm[pb][:], wch[wb][:, kc, mcc * 128:(mcc + 1) * 128], hnT[:, kc, :], kc == 0, kc == 15,
                                 ["wch%d" % wb, "hnT"], ["pmmE%d" % pb])
                        P.op("act", "activation", ["pmmE%d" % pb], ["qT"], out=qT[:, g * 4 + mcc, :], in_=pmm[pb][:], func=AF.Copy)
                for h in range(4):
                    for mck in range(2):
                        pb = nxt(pq, 4)
                        for dc in range(4):
                            P.mm(pmm[pb][:], kTs[:, h * 4 + dc, mck * 128:(mck + 1) * 128], qT[:, h * 4 + dc, :], dc == 0, dc == 3,
                                 ["kTs2", "qT"], ["pmmE%d" % pb])
                        P.op("act", "activation", ["pmmE%d" % pb], ["eT%d" % mck], out=eT[mck][:], in_=pmm[pb][:], func=AF.Exp, scale=512 ** -0.5)
                    pb = nxt(pq, 4)
                    for mck in range(2):
                        P.mm(pmm[pb][:], onesb[:], eT[mck][:], mck == 0, mck == 1, ["onesb2", "eT%d" % mck], ["pmmE%d" % pb])
                    P.op("dve", "reciprocal", ["pmmE%d" % pb], ["rden"], out=rden[:], in_=pmm[pb][:])
                    for dvc in range(4):
                        pb = nxt(pq, 4)
                        for mck in range(2):
                            P.mm(pmm[pb][:], vs[:, mck, h * 512 + dvc * 128:h * 512 + (dvc + 1) * 128], eT[mck][:], mck == 0, mck == 1,
                                 ["vs2", "eT%d" % mck], ["pmmE%d" % pb])
                        P.op("dve", "tensor_tensor", ["pmmE%d" % pb, "rden"], ["hnT"], out=hnT[:, h * 4 + dvc, :], in0=pmm[pb][:], in1=rden[:], op=ALU.mult)
                proj_tok(w_o_bf, "w_o_bf", hnT, "hnT")
                for tt in range(4):
                    r0 = t0 + tt * 128
                    for g_ in range(8):
                        P.dma("sp", acc_l[g_][r0:r0 + 128, :], x1[:, tt, g_ * 256:(g_ + 1) * 256], ["x1_%d" % tt], ["acc"])
                    rms_to(tt, hn[:], ["hnE"], extra=gff[:])
                    P.dma("sp", hn3_bf[r0:r0 + 128, :], hn[:], ["hnE"], ["hn3_bf"])
                    transposes16(hn, "hnE", lambda q4: hT3[:, q4 * 4:(q4 + 1) * 4, :], "hT3")
                    for kc in range(16):
                        P.mm(prt[:, 0:16], hT3[:, kc, :], wrb[:, kc, :], kc == 0, kc == 15, ["hT3", "wrb"], ["prtE"])
                    P.op("act", "activation", ["prtE"], ["peE", "psumE"], out=pe_[:], in_=prt[:, 0:16], func=AF.Exp, accum_out=psum_[:])
                    P.op("dve", "reciprocal", ["psumE"], ["psumE"], out=psum_[:], in_=psum_[:])
                    P.op("dve", "tensor_scalar", ["peE", "psumE"], ["peE"], out=pe_[:], in0=pe_[:], scalar1=psum_[:, 0:1], scalar2=None, op0=ALU.mult)
                    P.dma("sp", probs_loc[r0:r0 + 128, :], pe_[:], ["peE"], ["probs_loc"])
        P.emit()

        GSZ = cfg.get("GSZ", 1)
        NG = GSZ * LT
        JG = NG // 128
        CAPG = NG // 8
        NCHK = CAPL // 128
        if GSZ > 1:
            P.add("pool", lambda e: e.collective_compute("AllGather", ALU.bypass, replica_groups=cfg["groups"], ins=[probs_loc], outs=[probs_all]),
                  reads=["probs_loc"], writes=["probs_all"], own_sem=P.csem)
            pall = probs_all
        else:
            pall = probs_loc
        with ExitStack() as st:
            sb = lambda n, s, d=F32: st.enter_context(nc.sbuf_tensor(n, list(s), d))
            ps = lambda n, s, d=F32: st.enter_context(nc.psum_tensor(n, list(s), d))
            PA = sb("PA", [128, JG, 16])
            cmpt = sb("cmpt", [128, JG, 16])
            P.dma("sp", PA[:], pall.rearrange("(p j) e -> p j e", j=JG), ["probs_all", "probs_loc"], ["PA"])
            onesF = sb("onesFF", [128, 128])
            P.op("pool", "memset", [], ["onesFF"], onesF[:], 1.0)
            lo, hi, mid, ge, dd, cntp = [sb("bs_" + n_, [128, 16]) for n_ in ("lo", "hi", "mid", "ge", "dd", "cntp")]
            P.op("dve", "memset", [], ["bs_lo"], lo[:], 0.0)
            P.op("dve", "memset", [], ["bs_hi"], hi[:], 1.0)
            ptot = ps("ptot", [128, 512])
            for it_ in range(32):
                P.op("dve", "tensor_tensor", ["bs_lo", "bs_hi"], ["bs_mid"], out=mid[:], in0=lo[:], in1=hi[:], op=ALU.add)
                P.op("dve", "tensor_scalar", ["bs_mid"], ["bs_mid"], out=mid[:], in0=mid[:], scalar1=0.5, scalar2=None, op0=ALU.mult)
                P.op("dve", "tensor_tensor", ["PA", "bs_mid"], ["cmpt"], out=cmpt[:], in0=PA[:], in1=mid[:].unsqueeze(1).to_broadcast([128, JG, 16]), op=ALU.is_ge)
                P.op("dve", "tensor_reduce", ["cmpt"], ["bs_cntp"], out=cntp[:], in_=cmpt[:].rearrange("p j e -> p e j"), op=ALU.add, axis=AX.X)
                P.mm(ptot[:, 0:16], onesF[:], cntp[:], True, True, ["onesFF", "bs_cntp"], ["ptot"])
                P.op("dve", "tensor_scalar", ["ptot"], ["bs_ge"], out=ge[:], in0=ptot[:, 0:16], scalar1=float(CAPG), scalar2=None, op0=ALU.is_ge)
                P.op("dve", "tensor_tensor", ["bs_mid", "bs_lo"], ["bs_dd"], out=dd[:], in0=mid[:], in1=lo[:], op=ALU.subtract)
                P.op("dve", "tensor_tensor", ["bs_dd", "bs_ge"], ["bs_dd"], out=dd[:], in0=dd[:], in1=ge[:], op=ALU.mult)
                P.op("dve", "tensor_tensor", ["bs_lo", "bs_dd"], ["bs_lo"], out=lo[:], in0=lo[:], in1=dd[:], op=ALU.add)
                P.op("dve", "tensor_tensor", ["bs_hi", "bs_mid"], ["bs_dd"], out=dd[:], in0=hi[:], in1=mid[:], op=ALU.subtract)
                P.op("dve", "tensor_tensor", ["bs_dd", "bs_ge"], ["bs_dd"], out=dd[:], in0=dd[:], in1=ge[:], op=ALU.mult)
                P.op("dve", "tensor_tensor", ["bs_mid", "bs_dd"], ["bs_hi"], out=hi[:], in0=mid[:], in1=dd[:], op=ALU.add)
            PL = sb("PL", [128, NTT, 16])
            msk = sb("msk", [128, NTT, 16])
            gat = sb("gat", [128, NTT, 16])
            mskb = sb("mskb", [128, NTT, 16], BF16)
            P.dma("sp", PL[:], probs_loc.rearrange("(t p) e -> p t e", p=128), ["probs_loc"], ["PL"])
            P.op("dve", "tensor_tensor", ["PL", "bs_lo"], ["msk"], out=msk[:], in0=PL[:], in1=lo[:].unsqueeze(1).to_broadcast([128, NTT, 16]), op=ALU.is_ge)
            P.op("dve", "tensor_tensor", ["PL", "msk"], ["gat"], out=gat[:], in0=PL[:], in1=msk[:], op=ALU.mult)
            P.op("dve", "tensor_copy", ["msk"], ["mskb"], out=mskb[:], in_=msk[:])
            triS = sb("triS", [128, 128], BF16)
            onesb = sb("onesbF", [128, 128], BF16)
            P.op("pool", "memset", [], ["onesbF"], onesb[:], 1.0)
            P.op("pool", "memset", [], ["triS"], triS[:], 1.0)
            P.op("pool", "affine_select", ["triS"], ["triS"], out=triS[:], in_=triS[:], pattern=[[1, 128]], compare_op=ALU.is_ge, fill=0.0,
                 base=-1, channel_multiplier=-1)
            posi = sb("posi", [128, NTT, 16])
            tots = sb("tots", [128, NTT, 16])
            cum = sb("cum", [128, NTT, 16])
            pps = [ps("ppsF%d" % i, [128, 512]) for i in range(2)]
            mflat = mskb[:].rearrange("p t e -> p (t e)")
            NTE = NTT * 16
            for c0 in range(0, NTE, 512):
                c1 = min(c0 + 512, NTE)
                P.mm(pps[0][:, 0:c1 - c0], triS[:], mflat[:, c0:c1], True, True, ["triS", "mskb"], ["ppsF0"])
                P.op("dve", "tensor_copy", ["ppsF0"], ["posi"], out=posi[:].rearrange("p t e -> p (t e)")[:, c0:c1], in_=pps[0][:, 0:c1 - c0])
                P.mm(pps[1][:, 0:c1 - c0], onesb[:], mflat[:, c0:c1], True, True, ["onesbF", "mskb"], ["ppsF1"])
                P.op("dve", "tensor_copy", ["ppsF1"], ["tots"], out=tots[:].rearrange("p t e -> p (t e)")[:, c0:c1], in_=pps[1][:, 0:c1 - c0])
            onecol = sb("onecol", [128, 1])
            P.op("dve", "memset", [], ["onecol"], onecol[:], 1.0)
            for e_ in range(16):
                P.op("dve", "tensor_tensor_scan", ["tots", "onecol"], ["cum"], out=cum[:, :, e_], data0=onecol[:, 0:1].to_broadcast([128, NTT]),
                     data1=tots[:, :, e_], initial=0.0, op0=ALU.mult, op1=ALU.add)
            P.op("dve", "tensor_tensor", ["cum", "tots"], ["cum"], out=cum[:], in0=cum[:], in1=tots[:], op=ALU.subtract)
            P.op("dve", "tensor_tensor", ["cum", "posi"], ["posi"], out=posi[:], in0=posi[:], in1=cum[:], op=ALU.add)
            P.op("dve", "tensor_scalar", ["posi"], ["posi"], out=posi[:], in0=posi[:], scalar1=1.0, scalar2=None, op0=ALU.add)
            P.op("dve", "tensor_tensor", ["posi", "msk"], ["posi"], out=posi[:], in0=posi[:], in1=msk[:], op=ALU.mult)
            P.op("dve", "tensor_scalar", ["posi"], ["posi"], out=posi[:], in0=posi[:], scalar1=-1.0, scalar2=None, op0=ALU.add)
            G5 = sb("G5", [128, NTT, 16, 8], BF16)
            P.op("pool", "memset", [], ["G5"], G5[:], 0.0)
            tvf = sb("tvf", [128, NTT, 16])
            P.op("pool", "iota", [], ["tvf"], tvf[:], pattern=[[0, NTT], [0, 16]], base=0, channel_multiplier=1, allow_small_or_imprecise_dtypes=True)
            P.op("dve", "tensor_copy", ["tvf", "G5"], ["G5"], out=G5[:, :, :, 0], in_=tvf[:])
            P.op("pool", "iota", ["tvf"], ["tvf"], tvf[:], pattern=[[128, NTT], [0, 16]], base=0, channel_multiplier=0, allow_small_or_imprecise_dtypes=True)
            P.op("dve", "tensor_copy", ["tvf", "G5"], ["G5"], out=G5[:, :, :, 1], in_=tvf[:])
            P.op("dve", "memset", ["G5"], ["G5"], G5[:, :, :, 5], 1.0)
            gres = sb("gres", [128, NTT, 16])
            P.op("dve", "tensor_copy", ["gat", "G5"], ["G5"], out=G5[:, :, :, 2], in_=gat[:])
            P.op("dve", "tensor_tensor", ["gat", "G5"], ["gres"], out=gres[:], in0=gat[:], in1=G5[:, :, :, 2], op=ALU.subtract)
            P.op("dve", "tensor_copy", ["gres", "G5"], ["G5"], out=G5[:, :, :, 3], in_=gres[:])
            P.op("dve", "tensor_tensor", ["gres", "G5"], ["gres"], out=gres[:], in0=gres[:], in1=G5[:, :, :, 3], op=ALU.subtract)
            P.op("dve", "tensor_copy", ["gres", "G5"], ["G5"], out=G5[:, :, :, 4], in_=gres[:])
            iotaS = sb("iotaS", [128, CAPL])
            P.op("pool", "iota", [], ["iotaS"], iotaS[:], pattern=[[1, CAPL]], base=0, channel_multiplier=0, allow_small_or_imprecise_dtypes=True)
            dmy = sb("dmy", [128, NCHK])
            P.op("pool", "iota", [], ["dmy"], dmy[:], pattern=[[128, NCHK]], base=LT, channel_multiplier=1, allow_small_or_imprecise_dtypes=True)
            oh = [sb("oh%d" % i, [128, CAPL], BF16) for i in range(2)]
            pI = [ps("pI%d" % i, [128, 64, 8]) for i in range(2)]
            rr_ = sb("rrF", [128, NCHK, 8])
            ia = sb("iaF", [128, NCHK])
            ii = sb("iiF", [128, NCHK], I32)
            gs = sb("gsF", [128, NCHK])
            ohq = 0
            for e_ in range(16):
                pb = e_ % 2
                first = True
                for t in range(NTT):
                    ob = ohq % 2
                    ohq += 1
                    P.op("dve", "tensor_scalar", ["iotaS", "posi"], ["oh%d" % ob], out=oh[ob][:], in0=iotaS[:], scalar1=posi[:, t, e_:e_ + 1],
                         scalar2=None, op0=ALU.is_equal)
                    for ch in range(NCHK):
                        P.add("pe", (lambda pb, ch, ob, t, e_, first, lastf: lambda e: e.matmul(
                            pI[pb][:, ch, :], lhsT=oh[ob][:, ch * 128:(ch + 1) * 128], rhs=G5[:, t, e_, :], start=first, stop=lastf,
                            skip_group_check=True))(pb, ch, ob, t, e_, first, (t == NTT - 1 and ch == NCHK - 1)),
                            reads=["oh%d" % ob, "G5"], writes=["pI%d" % pb], banks=["pI%d" % pb])
                        first = False
                P.op("dve", "tensor_copy", ["pI%d" % pb], ["rrF"], out=rr_[:], in_=pI[pb][:, 0:NCHK, :])
                P.op("dve", "tensor_tensor", ["rrF"], ["iaF"], out=ia[:], in0=rr_[:, :, 0], in1=rr_[:, :, 1], op=ALU.add)
                P.op("dve", "tensor_tensor", ["iaF", "dmy"], ["iaF"], out=ia[:], in0=ia[:], in1=dmy[:], op=ALU.subtract)
                P.op("dve", "tensor_tensor", ["iaF", "rrF"], ["iaF"], out=ia[:], in0=ia[:], in1=rr_[:, :, 5], op=ALU.mult)
                P.op("dve", "tensor_tensor", ["iaF", "dmy"], ["iaF"], out=ia[:], in0=ia[:], in1=dmy[:], op=ALU.add)
                P.op("dve", "tensor_copy", ["iaF"], ["iiF"], out=ii[:], in_=ia[:])
                P.op("dve", "tensor_tensor", ["rrF"], ["gsF"], out=gs[:], in0=rr_[:, :, 2], in1=rr_[:, :, 3], op=ALU.add)
                P.op("dve", "tensor_tensor", ["rrF", "gsF"], ["gsF"], out=gs[:], in0=gs[:], in1=rr_[:, :, 4], op=ALU.add)
                P.dma("sp", idx_d[e_], ii[:], ["iiF"], ["idx_d"])
                P.dma("sp", cnt_out[e_], ii[:], ["iiF"], ["cnt_out"])
                P.dma("sp", gate_d[e_], gs[:], ["gsF"], ["gate_d"])
        P.emit()

        with ExitStack() as st:
            sb = lambda n, s, d=F32: st.enter_context(nc.sbuf_tensor(n, list(s), d))
            ps = lambda n, s, d=F32: st.enter_context(nc.psum_tensor(n, list(s), d))
            ident = sb("identX", [128, 128], BF16)
            P.op("pool", "memset", [], ["identX"], ident[:], 1.0)
            P.op("pool", "affine_select", ["identX"], ["identX"], out=ident[:], in_=ident[:], pattern=[[-1, 128]],
                 compare_op=ALU.is_equal, fill=0.0, base=0, channel_multiplier=1)
            idxt = [sb("idxt%d" % i, [128, NCHK], I32) for i in range(2)]
            gtt = [sb("gtt%d" % i, [128, NCHK]) for i in range(2)]
            xe = [sb("xe%d" % i, [128, 2048], BF16) for i in range(2)]
            xeT = sb("xeT", [128, 16, CAPL], BF16)
            heT = sb("heT", [128, 16, CAPL], BF16)
            stw = sb("stw", [128, 16, 256])
            wb_ = [sb("wbX%d" % i, [128, 16, 256], BF16) for i in range(4)]
            sgl = [sb("sglX%d" % i, [128, 512]) for i in range(2)]
            yst = [sb("ystX%d" % i, [128, 256], BF16) for i in range(3)]
            ptr = [ps("ptrX%d" % i, [128, 1024], BF16) for i in range(2)]
            pg = [ps("pgX%d" % i, [128, 512]) for i in range(2)]
            pu = [ps("puX%d" % i, [128, 512]) for i in range(2)]
            py = [ps("pyX%d" % i, [128, 512]) for i in range(2)]
            cgs = [(c0, min(c0 + 512, CAPL)) for c0 in range(0, CAPL, 512)]
            cnt = {"tq": 0, "wq": 0, "gq": 0, "yq": 0, "sq": 0, "ysq": 0, "cast": 0, "xq": 0}

            def nx(k_, n):
                v = cnt[k_] % n
                cnt[k_] += 1
                return v

            def load_w(src_ap, name):
                wi = nx("wq", 4)
                P.dma("sp", stw[:], src_ap, [], ["stw"])
                ce = ("act", "dve")[nx("cast", 2)]
                if ce == "act":
                    P.op("act", "activation", ["stw"], ["wbX%d" % wi], out=wb_[wi][:], in_=stw[:], func=AF.Copy)
                else:
                    P.op("dve", "tensor_copy", ["stw"], ["wbX%d" % wi], out=wb_[wi][:], in_=stw[:])
                return wi

            prev_sc = {}
            for e_ in range(cfg.get("nexp", 16)):
                ib = e_ % 2
                P.dma("sp", idxt[ib][:], idx_d[e_], ["idx_d"], ["idxt%d" % ib])
                P.dma("sp", gtt[ib][:], gate_d[e_], ["gate_d"], ["gtt%d" % ib])
                for ch in range(NCHK):
                    xb = nx("xq", 2)
                    P.add("pool", (lambda xb, ib, ch: lambda e: e.indirect_dma_start(
                        out=xe[xb][:], out_offset=None, in_=hn3_bf[:, :],
                        in_offset=bass.IndirectOffsetOnAxis(ap=idxt[ib][:, ch:ch + 1], axis=0)))(xb, ib, ch),
                        reads=["idxt%d" % ib, "hn3_bf"], writes=["xe%d" % xb], dma=True)
                    for q4 in range(4):
                        pb = nx("tq", 2)
                        for j in range(4):
                            kc = q4 * 4 + j
                            P.tr(ptr[pb][:, j * 128:(j + 1) * 128], xe[xb][:, kc * 128:(kc + 1) * 128], ident[:], ["xe%d" % xb, "identX"], ["ptrX%d" % pb])
                        o_ap = xeT[:, q4 * 4:(q4 + 1) * 4, ch * 128:(ch + 1) * 128]
                        i_ap = ptr[pb][:, 0:512].rearrange("p (j n) -> p j n", n=128)
                        if q4 % 2 == 0:
                            P.op("act", "activation", ["ptrX%d" % pb], ["xeT"], out=o_ap, in_=i_ap, func=AF.Copy)
                        else:
                            P.op("dve", "tensor_copy", ["ptrX%d" % pb], ["xeT"], out=o_ap, in_=i_ap)
                for fg in range(8):
                    wg_i = load_w(w_gate[e_].rearrange("(kc p) f -> p kc f", p=128)[:, :, fg * 256:(fg + 1) * 256], "g")
                    wu_i = load_w(w_up[e_].rearrange("(kc p) f -> p kc f", p=128)[:, :, fg * 256:(fg + 1) * 256], "u")
                    for fc2 in range(2):
                        for (c0, c1) in cgs:
                            gb = nx("gq", 2)
                            for kc in range(16):
                                P.mm(pg[gb][:, 0:c1 - c0], wb_[wg_i][:, kc, fc2 * 128:(fc2 + 1) * 128], xeT[:, kc, c0:c1], kc == 0, kc == 15,
                                     ["wbX%d" % wg_i, "xeT"], ["pgX%d" % gb])
                            for kc in range(16):
                                P.mm(pu[gb][:, 0:c1 - c0], wb_[wu_i][:, kc, fc2 * 128:(fc2 + 1) * 128], xeT[:, kc, c0:c1], kc == 0, kc == 15,
                                     ["wbX%d" % wu_i, "xeT"], ["puX%d" % gb])
                            sb_i = nx("sq", 2)
                            P.op("act", "activation", ["pgX%d" % gb], ["sglX%d" % sb_i], out=sgl[sb_i][:, 0:c1 - c0], in_=pg[gb][:, 0:c1 - c0], func=AF.Silu)
                            P.op("dve", "tensor_tensor", ["sglX%d" % sb_i, "puX%d" % gb], ["heT"], out=heT[:, fg * 2 + fc2, c0:c1], in0=sgl[sb_i][:, 0:c1 - c0],
                                 in1=pu[gb][:, 0:c1 - c0], op=ALU.mult)
                new_sc = {}
                for dg in range(8):
                    wd_i = load_w(w_down[e_].rearrange("(kc p) f -> p kc f", p=128)[:, :, dg * 256:(dg + 1) * 256], "d")
                    for st_ in range(NCHK):
                        yb = nx("yq", 2)
                        for fc in range(16):
                            P.mm(py[yb][:, 0:256], heT[:, fc, st_ * 128:(st_ + 1) * 128], wb_[wd_i][:, fc, :], fc == 0, fc == 15,
                                 ["heT", "wbX%d" % wd_i], ["pyX%d" % yb])
                        ys = nx("ysq", 3)
                        P.op("dve", "tensor_scalar", ["pyX%d" % yb, "gtt%d" % ib], ["ystX%d" % ys], out=yst[ys][:], in0=py[yb][:, 0:256],
                             scalar1=gtt[ib][:, st_:st_ + 1], scalar2=None, op0=ALU.mult)
                        nm_ = "sc_%d_%d_%d" % (e_, dg, st_)
                        new_sc.setdefault(dg, []).append(nm_)
                        P.add("pool", (lambda ys, ib, st_, dg: lambda e: e.indirect_dma_start(
                            out=acc_l[dg][:, :], out_offset=bass.IndirectOffsetOnAxis(ap=idxt[ib][:, st_:st_ + 1], axis=0),
                            in_=yst[ys][:], in_offset=None, compute_op=ALU.add))(ys, ib, st_, dg),
                            reads=["ystX%d" % ys, "idxt%d" % ib, "acc_d"] + prev_sc.get(dg, []), writes=[nm_], dma=True)
                prev_sc = new_sc
        P.emit()

        with ExitStack() as st:
            sb = lambda n, s, d=F32: st.enter_context(nc.sbuf_tensor(n, list(s), d))
            gfin = sb("gfin", [128, 2048])
            P.dma("sp", gfin[:], nfin_rep, [], ["gfin"])
            xa = [sb("xaG%d" % i, [128, 8, 256]) for i in range(2)]
            junk = sb("junkG", [128, 2048], BF16)
            ss = [sb("ssG%d" % i, [128, 1]) for i in range(2)]
            yo_ = [sb("yoG%d" % i, [128, 2048]) for i in range(2)]
            for t in range(NTT):
                b = t % 2
                r0 = t * 128
                for g_ in range(8):
                    P.dma("sp", xa[b][:, g_, :], acc_l[g_][r0:r0 + 128, :], [], ["xaG%d" % b])
                xf = xa[b][:].rearrange("p g c -> p (g c)")
                P.op("act", "activation", ["xaG%d" % b], ["junkG", "ssG%d" % b], out=junk[:], in_=xf, func=AF.Square, accum_out=ss[b][:])
                P.op("dve", "tensor_scalar", ["ssG%d" % b], ["ssG%d" % b], out=ss[b][:], in0=ss[b][:], scalar1=1.0 / D, scalar2=EPS, op0=ALU.mult, op1=ALU.add)
                P.op("act", "activation", ["ssG%d" % b], ["ssG%d" % b], out=ss[b][:], in_=ss[b][:], func=AF.Sqrt)
                P.op("dve", "reciprocal", ["ssG%d" % b], ["ssG%d" % b], out=ss[b][:], in_=ss[b][:])
                P.op("dve", "scalar_tensor_tensor", ["xaG%d" % b, "ssG%d" % b, "gfin"], ["yoG%d" % b], out=yo_[b][:], in0=xf, scalar=ss[b][:, 0:1],
                     in1=gfin[:], op0=ALU.mult, op1=ALU.mult)
                P.dma("sp", y_out[r0:r0 + 128, :], yo_[b][:], ["yoG%d" % b], ["y_out"])
        P.emit()

        if dbg and upto not in ("all",):
            for nm_, ap_ in dbg.items():
                src = {"o_xbcT": xbcT, "o_uT": uT, "o_z": z_tok, "o_dt": dt_tok, "o_yssd": yssd, "o_yf": yf, "o_y5gT": y5gT, "o_y5nT": y5nT, "o_kT": kT_d, "o_v": v_d, "o_acc": acc_l[0], "o_probs": probs_loc, "o_hn3": hn3_bf, "o_idx": idx_d, "o_gate": gate_d}[nm_]
                P.dma("sp", ap_, src, [], [nm_])
            P.emit()
    return nc


def _rep(v):
    v = np.asarray(v, np.float32)
    return np.ascontiguousarray(np.broadcast_to(v.reshape(1, -1), (128, v.size)))


def _colpk(v):
    v = np.asarray(v, np.float32)
    return np.ascontiguousarray(v.reshape(-1, 128).T)


def _prep_weights(W):
    o = {}
    o["w_in"] = np.ascontiguousarray(W["w_in"][0])
    o["norm_mix"] = _colpk(W["norm_mix"][0])
    o["conv_w"] = np.ascontiguousarray(W["conv_w"][0].reshape(3, 16, 128).transpose(2, 1, 0))
    o["conv_b"] = _colpk(W["conv_b"][0])
    o["dtb_rep"] = _rep(W["ssd_dt_bias"][0])
    o["alog_rep"] = _rep(W["ssd_a_log"][0])
    o["ssdd_rep"] = _rep(W["ssd_d"][0])
    o["ssdn_rep"] = _rep(W["ssd_norm"][0])

    def gp(a):
        a = a.reshape(2, 8, 8, 64).transpose(0, 1, 3, 2)
        return np.concatenate([a, a], axis=2)
    are = gp(W["s5_a_re"][0])
    aim = gp(W["s5_a_im"][0])
    lst = gp(np.broadcast_to(W["s5_log_step"][0][:, :, None], (2, 64, 64)))
    o["s5_small"] = np.ascontiguousarray(np.stack([are, aim, lst], axis=3)).astype(np.float32)

    def bb(a):
        a = a.reshape(2, 8, 8, 64, 16).transpose(0, 1, 3, 2, 4).reshape(2, 8, 64, 128)
        return np.concatenate([a, a], axis=2)

    def cc(a):
        a = a.reshape(2, 8, 8, 16, 64).transpose(0, 1, 4, 2, 3).reshape(2, 8, 64, 128)
        return np.concatenate([a, a], axis=2)
    o["s5_big"] = np.ascontiguousarray(np.stack([bb(W["s5_b_re"][0]), bb(W["s5_b_im"][0]), cc(W["s5_c_re"][0]),
                                                 cc(W["s5_c_im"][0])], axis=3)).astype(np.float32)
    o["s5_dcol"] = _colpk(W["s5_d"][0])
    o["s5_ncol"] = _colpk(W["s5_norm"][0])
    o["norm_attn"] = _colpk(W["norm_attn"][0])
    o["norm_mem"] = _colpk(W["norm_mem"][0])
    for k_ in ("w_out", "w_q", "w_k", "w_v", "w_o"):
        o[k_] = np.ascontiguousarray(W[k_][0])
    o["nffn_rep"] = _rep(W["norm_ffn"][0])
    o["w_router_l"] = np.ascontiguousarray(W["w_router"][0].reshape(16, 128, 16).transpose(1, 0, 2))
    o["nfin_rep"] = _rep(W["norm_final"])
    o["w_glu"] = np.ascontiguousarray(W["s5_w_glu"][0])
    o["w_gate"] = np.ascontiguousarray(W["w_gate"][0])
    o["w_up"] = np.ascontiguousarray(W["w_up"][0])
    o["w_down"] = np.ascontiguousarray(W["w_down"][0])
    return o


_NC_CACHE = {}


def kernel(**inputs):
    W = {k_: np.asarray(v, np.float32) for k_, v in inputs.items()}
    xp, xs = W.pop("x_prompt"), W.pop("x_sample")
    mp, ms = W.pop("mem_prompt"), W.pop("mem_sample")
    LSEG = 4096
    cfg = {"LSEG": LSEG, "upto": "all", "CAPL": 1536, "GSZ": 4, "groups": [[0, 1, 2, 3], [4, 5, 6, 7]]}
    if "nc" not in _NC_CACHE:
        _NC_CACHE["nc"] = build(cfg)
    nc = _NC_CACHE["nc"]
    wl = _prep_weights(W)
    in_maps = []
    for c in range(8):
        m = dict(wl)
        if c < 4:
            m["x"] = np.ascontiguousarray(xp[c])
            m["mem"] = np.ascontiguousarray(np.stack([mp[c], mp[c]]))
            m["flag"] = np.ones((128, 1), np.float32)
        else:
            j = c - 4
            m["x"] = np.ascontiguousarray(xs[2 * j:2 * j + 2].reshape(2 * LSEG, D))
            m["mem"] = np.ascontiguousarray(ms[2 * j:2 * j + 2])
            m["flag"] = np.zeros((128, 1), np.float32)
        in_maps.append(m)
    res = run_bass_kernel_spmd(nc, in_maps, core_ids=list(range(8)))
    ys = [np.asarray(r["y"], np.float32) for r in res.results]
    try:
        import sys
        cn = np.array([[(np.asarray(r["slot_idx"])[e] < 2 * LSEG).sum() for e in range(16)] for r in res.results])
        print("[kernel] per-(core,expert) slot counts: mean %.1f std %.1f max %d (cap %d)" % (cn.mean(), cn.std(), cn.max(), cfg["CAPL"]), file=sys.stderr)
    except Exception:
        pass
    y_prompt = np.stack(ys[0:4]).reshape(4, 8192, D)
    y_sample = np.concatenate([y.reshape(2, LSEG, D) for y in ys[4:8]], axis=0)
    return (y_prompt, y_sample)
```

```python
import numpy as np
from contextlib import ExitStack
import concourse.bass as bass
import concourse.mybir as mybir
from concourse.bass_utils import run_bass_kernel_spmd

F32 = mybir.dt.float32
BF16 = mybir.dt.bfloat16
I32 = mybir.dt.int32
U32 = mybir.dt.uint32
ALU = mybir.AluOpType
AF = mybir.ActivationFunctionType
AX = mybir.AxisListType

ENGS = ("pe", "act", "dve", "pool", "sp")
NDS = 24

D = 2048
EPS = 1e-6
INC = 4112


class Op:
    __slots__ = ("eng", "fn", "reads", "writes", "dma", "sem", "val", "waits", "desc", "inc")

    def __init__(self, eng, fn, reads, writes, dma):
        self.eng = eng
        self.fn = fn
        self.reads = tuple(reads)
        self.writes = tuple(writes)
        self.dma = dma
        self.waits = {}


class Prog:
    def __init__(self, nc, stack):
        self.nc = nc
        self.ops = []
        self.last_writer = {}
        self.readers = {}
        self.cnt = {e: 0 for e in ENGS}
        self.dcnt = {e: 0 for e in ENGS}
        self.sem = {e: stack.enter_context(nc.semaphore("s_" + e)) for e in ENGS if e != "sp"}
        self.dsem = {
            e: [stack.enter_context(nc.semaphore("d_%s_%d" % (e, i))) for i in range(NDS)]
            for e in ("sp", "act", "pool")
        }
        self.waited = {e: {} for e in ENGS}
        self.semobj = {}
        self.limit = None
        self.bank_last = {}
        self.csem = stack.enter_context(nc.semaphore("s_coll"))
        self.capture = None

    def add(self, eng, fn, reads=(), writes=(), dma=False, banks=(), own_sem=None):
        if self.capture is not None:
            self.capture.append((eng, fn, tuple(reads), tuple(writes), dma, tuple(banks), own_sem))
            return None
        if self.limit is not None and len(self.ops) >= self.limit:
            return None
        op = Op(eng, fn, reads, writes, dma)
        deps = set()
        for b in op.reads:
            w = self.last_writer.get(b)
            if w is not None:
                deps.add(w)
        for b in op.writes:
            w = self.last_writer.get(b)
            if w is not None:
                deps.add(w)
            for r in self.readers.get(b, ()):
                if r.eng == eng and not r.dma and not dma:
                    continue
                deps.add(r)
        for tok in banks:
            pd = self.bank_last.get(tok)
            if pd is not None and pd.eng != eng:
                deps.add(pd)
            self.bank_last[tok] = op
        for b in op.reads:
            self.readers.setdefault(b, []).append(op)
        for b in op.writes:
            self.last_writer[b] = op
            self.readers[b] = []
        prewait = None
        op.inc = 16 if dma else 1
        if own_sem is not None:
            op.sem = own_sem
            op.val = 1
            op.inc = 1
        elif dma:
            i = self.dcnt[eng]
            self.dcnt[eng] += 1
            op.sem = self.dsem[eng][i % NDS]
            op.val = 16 * (i // NDS + 1)
            if i >= NDS:
                prewait = (op.sem, 16 * (i // NDS))
        else:
            self.cnt[eng] += 1
            op.sem = self.sem[eng]
            op.val = self.cnt[eng]
        for d in deps:
            if d is op:
                continue
            if d.eng == "pe" and eng == "pe" and not d.dma and not dma:
                continue
            k = id(d.sem)
            self.semobj[k] = d.sem
            if op.waits.get(k, 0) < d.val:
                op.waits[k] = d.val
        if prewait is not None:
            k = id(prewait[0])
            self.semobj[k] = prewait[0]
            if op.waits.get(k, 0) < prewait[1]:
                op.waits[k] = prewait[1]
        self.ops.append(op)
        return op

    @staticmethod
    def _banks(aps):
        toks = []
        for a in aps:
            sp = getattr(a, "space", None)
            if sp is not None and str(sp).endswith("PSUM"):
                toks.append(a.tensor.name)
        return toks

    def op(self, eng, method, reads, writes, *args, **kw):
        o = self.add(eng, lambda e: getattr(e, method)(*args, **kw), reads, writes,
                     banks=self._banks(list(args) + list(kw.values())))
        return o

    def dma(self, eng, out, in_, reads, writes, **kw):
        return self.add(eng, lambda e: e.dma_start(out=out, in_=in_, **kw), reads, writes, dma=True)

    def mm(self, out, lhsT, rhs, start, stop, reads, writes):
        return self.add("pe", lambda e: e.matmul(out, lhsT=lhsT, rhs=rhs, start=start, stop=stop), reads, writes,
                        banks=self._banks([out]))

    def tr(self, out, in_, ident, reads, writes):
        return self.add("pe", lambda e: e.transpose(out=out, in_=in_, identity=ident), reads, writes,
                        banks=self._banks([out]))

    def threads(self, fns):
        lists = []
        for f in fns:
            self.capture = []
            f()
            lists.append(self.capture)
        self.capture = None
        n = max(len(l) for l in lists)
        for i in range(n):
            for l in lists:
                if i < len(l):
                    self.add(*l[i])

    def emit(self):
        nc = self.nc
        ops = self.ops
        self.ops = []
        per = {e: [o for o in ops if o.eng == e] for e in ENGS}
        finals = []
        for e in ENGS:
            if e != "sp" and self.cnt[e] > 0:
                finals.append((self.sem[e], self.cnt[e]))
        for e in ("sp", "act", "pool"):
            n = self.dcnt[e]
            for j in range(min(n, NDS)):
                uses = (n - 1 - j) // NDS + 1
                finals.append((self.dsem[e][j], 16 * uses))
        waited = self.waited
        semobj = self.semobj

        def run(engname, eng):
            wd = waited[engname]
            for o in per[engname]:
                for k, v in o.waits.items():
                    if wd.get(k, 0) < v:
                        eng.wait_ge(semobj[k], v)
                        wd[k] = v
                ins = o.fn(eng)
                ins.then_inc(o.sem, o.inc)
            for s, v in finals:
                k = id(s)
                if wd.get(k, 0) < v:
                    eng.wait_ge(s, v)
                    wd[k] = v

        with nc.Block() as block:
            @block.tensor
            def _(eng):
                run("pe", eng)

            @block.scalar
            def _(eng):
                run("act", eng)

            @block.vector
            def _(eng):
                run("dve", eng)

            @block.gpsimd
            def _(eng):
                run("pool", eng)

            @block.sync
            def _(eng):
                run("sp", eng)


class K:
    pass


def _rr(lst):
    state = {"i": 0}

    def nxt():
        v = lst[state["i"] % len(lst)]
        state["i"] += 1
        return v
    return nxt


def build(cfg):
    LSEG = cfg["LSEG"]
    LT = 2 * LSEG
    NST = LT // 512
    upto = cfg.get("upto", "all")
    nc = bass.Bass("TRN2", target_bir_lowering=False)
    k = K()
    k.nc = nc
    k.cfg = cfg

    def din(name, shape, dt=F32):
        return nc.dram_tensor(name, list(shape), dt, kind="ExternalInput").ap()

    def dscr(name, shape, dt=F32):
        return nc.dram_tensor(name, list(shape), dt, kind="Internal").ap()

    def dout(name, shape, dt=F32):
        return nc.dram_tensor(name, list(shape), dt, kind="ExternalOutput").ap()

    x_in = din("x", [LT, D])
    w_in = din("w_in", [D, INC])
    norm_mix = din("norm_mix", [128, 16])
    flag = din("flag", [128, 1])

    w_in_bf = dscr("w_in_bf", [9, 128, 16, 512], BF16)
    xbcT = dscr("xbcT", [2048, LT])
    uT = dscr("uT", [1024, LT], BF16)
    z_tok = dscr("z_tok", [LT, 1024])
    dt_tok = dscr("dt_tok", [LT, 16])

    conv_w = din("conv_w", [128, 16, 3])
    conv_b = din("conv_b", [128, 16])
    dtb_rep = din("dtb_rep", [128, 32])
    alog_rep = din("alog_rep", [128, 32])
    ssdd_rep = din("ssdd_rep", [128, 16])
    ssdn_rep = din("ssdn_rep", [128, 1024])
    xs_tok = dscr("xs_tok", [LT, 1024], BF16)
    B_tok = dscr("B_tok", [LT, 512], BF16)
    BT = dscr("BT", [512, LT], BF16)
    CT = dscr("CT", [512, LT], BF16)
    yf = dscr("yf", [LT, 1024])
    yb_d = dscr("yb_d", [LT, 1024])
    yssd = dscr("yssd", [LT, 1024], BF16)
    s5_small = din("s5_small", [2, 8, 128, 3, 8])
    s5_big = din("s5_big", [2, 8, 128, 4, 128])
    s5_dcol = din("s5_dcol", [128, 8])
    y5gT = dscr("y5gT", [1024, LT], BF16)
    norm_attn = din("norm_attn", [128, 16])
    norm_mem = din("norm_mem", [128, 16])
    w_glu = din("w_glu", [1024, 1024])
    w_out = din("w_out", [2048, 2048])
    w_q = din("w_q", [2048, 2048])
    w_k = din("w_k", [2048, 2048])
    w_v = din("w_v", [2048, 2048])
    w_o = din("w_o", [2048, 2048])
    mem_in = din("mem", [2, 256, 2048])
    s5_ncol = din("s5_ncol", [128, 8])
    w_glu_bf = dscr("w_glu_bf", [2, 128, 8, 512], BF16)
    w_out_bf = dscr("w_out_bf", [4, 128, 16, 512], BF16)
    w_q_bf = dscr("w_q_bf", [4, 128, 16, 512], BF16)
    w_k_bf = dscr("w_k_bf", [4, 128, 16, 512], BF16)
    w_v_bf = dscr("w_v_bf", [4, 128, 16, 512], BF16)
    w_o_bf = dscr("w_o_bf", [4, 128, 16, 512], BF16)
    kT_d = dscr("kT_d", [2, 128, 16, 256], BF16)
    v_d = dscr("v_d", [2, 128, 2, 2048], BF16)
    y5nT = dscr("y5nT", [1024, LT], BF16)
    nffn_rep = din("nffn_rep", [128, 2048])
    w_router_l = din("w_router_l", [128, 16, 16])
    CAPL = cfg["CAPL"]
    acc_l = [dscr("accd%d" % i, [LT + CAPL, 256]) for i in range(8)]
    hn3_bf = dscr("hn3_bf", [LT + CAPL, 2048], BF16)
    idx_d = dscr("idx_d", [16, 128, CAPL // 128], I32)
    gate_d = dscr("gate_d", [16, 128, CAPL // 128])
    probs_all = dscr("probs_all", [cfg.get("GSZ", 1) * LT, 16])
    w_gate = din("w_gate", [cfg.get("nexp", 16), 2048, 2048])
    w_up = din("w_up", [cfg.get("nexp", 16), 2048, 2048])
    w_down = din("w_down", [cfg.get("nexp", 16), 2048, 2048])
    nfin_rep = din("nfin_rep", [128, 2048])
    y_out = dout("y", [LT, 2048])
    cnt_out = dout("slot_idx", [16, 128, CAPL // 128], I32)
    probs_loc = dscr("probs_loc", [LT, 16])
    dbg = {}
    if upto == "F":
        dbg["o_acc"] = dout("o_acc", [LT + CAPL, 256])
        dbg["o_idx"] = dout("o_idx", [16, 128, CAPL // 128], I32)
        dbg["o_gate"] = dout("o_gate", [16, 128, CAPL // 128])
    if upto == "E2":
        dbg["o_acc"] = dout("o_acc", [LT + CAPL, 256])
        dbg["o_probs"] = dout("o_probs", [LT, 16])
        dbg["o_hn3"] = dout("o_hn3", [LT + CAPL, 2048], BF16)
    if upto == "E1":
        dbg["o_y5nT"] = dout("o_y5nT", [1024, LT], BF16)
        dbg["o_kT"] = dout("o_kT", [2, 128, 16, 256], BF16)
        dbg["o_v"] = dout("o_v", [2, 128, 2, 2048], BF16)
    if upto == "D":
        dbg["o_y5gT"] = dout("o_y5gT", [1024, LT], BF16)
    if upto == "C":
        dbg["o_yssd"] = dout("o_yssd", [LT, 1024], BF16)
        dbg["o_yf"] = dout("o_yf", [LT, 1024])
    if upto == "A":
        dbg["o_xbcT"] = dout("o_xbcT", [2048, LT])
        dbg["o_uT"] = dout("o_uT", [1024, LT], BF16)
        dbg["o_z"] = dout("o_z", [LT, 1024])
        dbg["o_dt"] = dout("o_dt", [LT, 16])

    with ExitStack() as gst:
        P = Prog(nc, gst)
        k.P = P
        with ExitStack() as st:
            sb = lambda n, s, d=F32: st.enter_context(nc.sbuf_tensor(n, list(s), d))
            gcol = sb("gcol", [128, 16])
            P.add("sp", lambda e: e.dma_start(out=gcol[:], in_=norm_mix),
                  writes=["gcol"], dma=True)
            wst = [sb("wst%d" % i, [128, INC]) for i in range(2)]
            wbf = [sb("wbf%d" % i, [128, 9 * 512], BF16) for i in range(2)]
            for i in range(2):
                (lambda i: P.add("pool", lambda e: e.memset(wbf[i][:], 0.0), writes=["wbf%d" % i]))(i)
            engs = _rr(["dve", "act", "pool"])
            for kc in range(16):
                b = kc % 2
                P.add("sp", (lambda b, kc: lambda e: e.dma_start(out=wst[b][:], in_=w_in[kc * 128:(kc + 1) * 128, :]))(b, kc),
                      writes=["wst%d" % b], dma=True)
                def cast(b=b, kc=kc):
                    srcs = [(0, 3072, 0), (3088, 4112, 3072), (3072, 3088, 4096)]
                    for (s0, s1, d0) in srcs:
                        def f(e, s0=s0, s1=s1, d0=d0):
                            return e.tensor_scalar(out=wbf[b][:, d0:d0 + (s1 - s0)], in0=wst[b][:, s0:s1],
                                                   scalar1=gcol[:, kc:kc + 1], scalar2=None, op0=ALU.mult)
                        P.add("dve", f, reads=["wst%d" % b, "gcol"], writes=["wbf%d" % b])
                cast()
                P.add("sp", (lambda b, kc: lambda e: e.dma_start(
                    out=w_in_bf[:, :, kc, :].rearrange("g p n -> p g n"),
                    in_=wbf[b][:].rearrange("p (g n) -> p g n", n=512)))(b, kc),
                    reads=["wbf%d" % b], writes=["w_in_bf"], dma=True)
        P.emit()

        with ExitStack() as st:
            sb = lambda n, s, d=F32: st.enter_context(nc.sbuf_tensor(n, list(s), d))
            ps = lambda n, s, d=F32: st.enter_context(nc.psum_tensor(n, list(s), d))
            ident = sb("ident", [128, 128], BF16)
            P.add("pool", lambda e: e.memset(ident[:], 1.0), writes=["ident"])
            P.add("pool", lambda e: e.affine_select(out=ident[:], in_=ident[:], pattern=[[-1, 128]],
                                                    compare_op=ALU.is_equal, fill=0.0, base=0, channel_multiplier=1),
                  reads=["ident"], writes=["ident"])
            xt = [sb("xt%d" % i, [128, D]) for i in range(2)]
            junk = sb("junk", [128, D], BF16)
            ss = [sb("ss%d" % i, [128, 1]) for i in range(2)]
            xn = [sb("xn%d" % i, [128, D], BF16) for i in range(2)]
            hT = sb("hT", [128, 16, 512], BF16)
            wg = [sb("wg%d" % i, [128, 16, 512], BF16) for i in range(2)]
            ostg = [sb("ostg%d" % i, [128, 512]) for i in range(3)]
            ostb = [sb("ostb%d" % i, [128, 512], BF16) for i in range(2)]
            dstg = [sb("dstg%d" % i, [128, 16]) for i in range(2)]
            ptr = [ps("ptr%d" % i, [128, 1024], BF16) for i in range(2)]
            pmm = [ps("pmm%d" % i, [128, 512]) for i in range(4)]
            wq = _rr([0, 1])
            oq = _rr([0, 1, 2])
            obq = _rr([0, 1])
            pq = _rr([0, 1, 2, 3])
            tq = _rr([0, 1])
            cpq = _rr(["act", "dve"])
            dq = _rr([0, 1])
            it = 0
            for s in range(NST):
                t0 = s * 512
                for tt in range(4):
                    b = it % 2
                    it += 1
                    r0 = t0 + tt * 128
                    P.add("sp", (lambda b, r0: lambda e: e.dma_start(out=xt[b][:], in_=x_in[r0:r0 + 128, :]))(b, r0),
                          writes=["xt%d" % b], dma=True)
                    P.add("act", (lambda b: lambda e: e.activation(out=junk[:], in_=xt[b][:], func=AF.Square,
                                                                   accum_out=ss[b][:]))(b),
                          reads=["xt%d" % b], writes=["junk", "ss%d" % b])
                    P.add("dve", (lambda b: lambda e: e.tensor_scalar(out=ss[b][:], in0=ss[b][:], scalar1=1.0 / D,
                                                                      scalar2=EPS, op0=ALU.mult, op1=ALU.add))(b),
                          reads=["ss%d" % b], writes=["ss%d" % b])
                    P.add("act", (lambda b: lambda e: e.activation(out=ss[b][:], in_=ss[b][:], func=AF.Sqrt))(b),
                          reads=["ss%d" % b], writes=["ss%d" % b])
                    P.add("dve", (lambda b: lambda e: e.reciprocal(out=ss[b][:], in_=ss[b][:]))(b),
                          reads=["ss%d" % b], writes=["ss%d" % b])
                    P.add("dve", (lambda b: lambda e: e.tensor_scalar(out=xn[b][:], in0=xt[b][:], scalar1=ss[b][:, 0:1],
                                                                      scalar2=None, op0=ALU.mult))(b),
                          reads=["xt%d" % b, "ss%d" % b], writes=["xn%d" % b])
                    for q in range(4):
                        pb = tq()
                        for j in range(4):
                            kc = q * 4 + j
                            P.add("pe", (lambda pb, j, b, kc: lambda e: e.transpose(
                                out=ptr[pb][:, j * 128:(j + 1) * 128], in_=xn[b][:, kc * 128:(kc + 1) * 128],
                                identity=ident[:]))(pb, j, b, kc),
                                reads=["xn%d" % b, "ident"], writes=["ptr%d" % pb])
                        ce = cpq()
                        if ce == "act":
                            P.add("act", (lambda pb, q, tt: lambda e: e.activation(
                                out=hT[:, q * 4:(q + 1) * 4, tt * 128:(tt + 1) * 128],
                                in_=ptr[pb][:, 0:512].rearrange("p (j n) -> p j n", n=128), func=AF.Copy))(pb, q, tt),
                                reads=["ptr%d" % pb], writes=["hT"])
                        else:
                            P.add("dve", (lambda pb, q, tt: lambda e: e.tensor_copy(
                                out=hT[:, q * 4:(q + 1) * 4, tt * 128:(tt + 1) * 128],
                                in_=ptr[pb][:, 0:512].rearrange("p (j n) -> p j n", n=128)))(pb, q, tt),
                                reads=["ptr%d" % pb], writes=["hT"])
                for g in range(9):
                    wb = wq()
                    P.add("sp", (lambda wb, g: lambda e: e.dma_start(out=wg[wb][:], in_=w_in_bf[g]))(wb, g),
                          reads=["w_in_bf"], writes=["wg%d" % wb], dma=True)
                    if g < 2:
                        for tt in range(4):
                            pb = pq()
                            for kc in range(16):
                                P.add("pe", (lambda pb, wb, kc, tt: lambda e: e.matmul(
                                    pmm[pb][:], lhsT=hT[:, kc, tt * 128:(tt + 1) * 128], rhs=wg[wb][:, kc, :],
                                    start=(kc == 0), stop=(kc == 15)))(pb, wb, kc, tt),
                                    reads=["hT", "wg%d" % wb], writes=["pmm%d" % pb])
                            ob = oq()
                            P.add("act", (lambda ob, pb: lambda e: e.activation(out=ostg[ob][:], in_=pmm[pb][:], func=AF.Copy))(ob, pb),
                                  reads=["pmm%d" % pb], writes=["ostg%d" % ob])
                            r0 = t0 + tt * 128
                            P.add("sp", (lambda ob, r0, g: lambda e: e.dma_start(
                                out=z_tok[r0:r0 + 128, g * 512:(g + 1) * 512], in_=ostg[ob][:]))(ob, r0, g),
                                reads=["ostg%d" % ob], writes=["z_tok"], dma=True)
                    elif g < 8:
                        for mc in range(4):
                            pb = pq()
                            for kc in range(16):
                                P.add("pe", (lambda pb, wb, kc, mc: lambda e: e.matmul(
                                    pmm[pb][:], lhsT=wg[wb][:, kc, mc * 128:(mc + 1) * 128], rhs=hT[:, kc, :],
                                    start=(kc == 0), stop=(kc == 15)))(pb, wb, kc, mc),
                                    reads=["hT", "wg%d" % wb], writes=["pmm%d" % pb])
                            if g < 6:
                                ob = oq()
                                ch0 = (g - 2) * 512 + mc * 128
                                P.add("act", (lambda ob, pb: lambda e: e.activation(out=ostg[ob][:], in_=pmm[pb][:], func=AF.Copy))(ob, pb),
                                      reads=["pmm%d" % pb], writes=["ostg%d" % ob])
                                P.add("sp", (lambda ob, ch0, t0: lambda e: e.dma_start(
                                    out=xbcT[ch0:ch0 + 128, t0:t0 + 512], in_=ostg[ob][:]))(ob, ch0, t0),
                                    reads=["ostg%d" % ob], writes=["xbcT"], dma=True)
                            else:
                                ob = obq()
                                ch0 = (g - 6) * 512 + mc * 128
                                P.add("dve", (lambda ob, pb: lambda e: e.tensor_copy(out=ostb[ob][:], in_=pmm[pb][:]))(ob, pb),
                                      reads=["pmm%d" % pb], writes=["ostb%d" % ob])
                                P.add("sp", (lambda ob, ch0, t0: lambda e: e.dma_start(
                                    out=uT[ch0:ch0 + 128, t0:t0 + 512], in_=ostb[ob][:]))(ob, ch0, t0),
                                    reads=["ostb%d" % ob], writes=["uT"], dma=True)
                    else:
                        for tt in range(4):
                            pb = pq()
                            for kc in range(16):
                                P.add("pe", (lambda pb, wb, kc, tt: lambda e: e.matmul(
                                    pmm[pb][:, 0:16], lhsT=hT[:, kc, tt * 128:(tt + 1) * 128], rhs=wg[wb][:, kc, 0:16],
                                    start=(kc == 0), stop=(kc == 15)))(pb, wb, kc, tt),
                                    reads=["hT", "wg%d" % wb], writes=["pmm%d" % pb])
                            ob = dq()
                            P.add("dve", (lambda ob, pb: lambda e: e.tensor_copy(out=dstg[ob][:], in_=pmm[pb][:, 0:16]))(ob, pb),
                                  reads=["pmm%d" % pb], writes=["dstg%d" % ob])
                            r0 = t0 + tt * 128
                            P.add("sp", (lambda ob, r0: lambda e: e.dma_start(out=dt_tok[r0:r0 + 128, :], in_=dstg[ob][:]))(ob, r0),
                                  reads=["dstg%d" % ob], writes=["dt_tok"], dma=True)
        P.emit()


        with ExitStack() as st:
            sb = lambda n, s, d=F32: st.enter_context(nc.sbuf_tensor(n, list(s), d))
            ps = lambda n, s, d=F32: st.enter_context(nc.psum_tensor(n, list(s), d))
            ident = sb("identB", [128, 128], BF16)
            P.op("pool", "memset", [], ["identB"], ident[:], 1.0)
            P.op("pool", "affine_select", ["identB"], ["identB"], out=ident[:], in_=ident[:], pattern=[[-1, 128]],
                 compare_op=ALU.is_equal, fill=0.0, base=0, channel_multiplier=1)
            cw = sb("cw", [128, 16, 3])
            cb = sb("cb", [128, 16])
            flg = sb("flg", [128, 1])
            P.dma("sp", cw[:], conv_w, [], ["cw"])
            P.dma("sp", cb[:], conv_b, [], ["cb"])
            P.dma("sp", flg[:], flag, [], ["flg"])
            NB = 3
            xw = [sb("xw%d" % i, [128, 514]) for i in range(NB)]
            acc = [sb("acc%d" % i, [128, 512]) for i in range(NB)]
            sl = [sb("sl%d" % i, [128, 512], BF16) for i in range(NB)]
            xs_stg = sb("xs_stg", [128, 4, 1024], BF16)
            b_stg = sb("b_stg", [128, 4, 512], BF16)
            ptb = [ps("ptb%d" % i, [128, 1024], BF16) for i in range(2)]
            it = 0
            tq = 0
            for s in range(NST):
                t0 = s * 512
                for c in range(16):
                    b = it % NB
                    it += 1
                    lo = max(t0 - 1, 0)
                    hi = min(t0 + 513, LT)
                    if t0 == 0:
                        P.op("pool", "memset", [], ["xw%d" % b], xw[b][:, 0:1], 0.0)
                    if t0 + 512 == LT:
                        P.op("pool", "memset", [], ["xw%d" % b], xw[b][:, 513:514], 0.0)
                    P.dma("sp", xw[b][:, (lo - (t0 - 1)):(hi - (t0 - 1))], xbcT[c * 128:(c + 1) * 128, lo:hi],
                          ["xbcT"], ["xw%d" % b])
                    if t0 == LSEG:
                        P.op("dve", "tensor_scalar", ["xw%d" % b, "flg"], ["xw%d" % b], out=xw[b][:, 0:1], in0=xw[b][:, 0:1],
                             scalar1=flg[:, 0:1], scalar2=None, op0=ALU.mult)
                    if t0 + 512 == LSEG:
                        P.op("dve", "tensor_scalar", ["xw%d" % b, "flg"], ["xw%d" % b], out=xw[b][:, 513:514], in0=xw[b][:, 513:514],
                             scalar1=flg[:, 0:1], scalar2=None, op0=ALU.mult)
                    P.op("dve", "tensor_scalar", ["xw%d" % b, "cw"], ["acc%d" % b], out=acc[b][:], in0=xw[b][:, 1:513],
                         scalar1=cw[:, c, 1:2], scalar2=None, op0=ALU.mult)
                    P.op("dve", "scalar_tensor_tensor", ["xw%d" % b, "cw", "acc%d" % b], ["acc%d" % b], out=acc[b][:],
                         in0=xw[b][:, 0:512], scalar=cw[:, c, 0:1], in1=acc[b][:], op0=ALU.mult, op1=ALU.add)
                    P.op("dve", "scalar_tensor_tensor", ["xw%d" % b, "cw", "acc%d" % b], ["acc%d" % b], out=acc[b][:],
                         in0=xw[b][:, 2:514], scalar=cw[:, c, 2:3], in1=acc[b][:], op0=ALU.mult, op1=ALU.add)
                    P.op("act", "activation", ["acc%d" % b, "cb"], ["sl%d" % b], out=sl[b][:], in_=acc[b][:], func=AF.Silu,
                         bias=cb[:, c:c + 1])
                    if c >= 8:
                        dst = BT if c < 12 else CT
                        r0 = (c - 8) * 128 if c < 12 else (c - 12) * 128
                        P.dma("sp", dst[r0:r0 + 128, t0:t0 + 512], sl[b][:], ["sl%d" % b], ["BT" if c < 12 else "CT"])
                    if c < 12:
                        pb = tq % 2
                        tq += 1
                        for tt in range(4):
                            P.tr(ptb[pb][:, tt * 128:(tt + 1) * 128], sl[b][:, tt * 128:(tt + 1) * 128], ident[:],
                                 ["sl%d" % b, "identB"], ["ptb%d" % pb])
                        if c < 8:
                            P.op("act", "activation", ["ptb%d" % pb], ["xs_stg"], out=xs_stg[:, :, c * 128:(c + 1) * 128],
                                 in_=ptb[pb][:, 0:512].rearrange("p (t n) -> p t n", n=128), func=AF.Copy)
                        else:
                            P.op("act", "activation", ["ptb%d" % pb], ["b_stg"], out=b_stg[:, :, (c - 8) * 128:(c - 7) * 128],
                                 in_=ptb[pb][:, 0:512].rearrange("p (t n) -> p t n", n=128), func=AF.Copy)
                    if c == 7:
                        P.dma("sp", xs_tok[t0:t0 + 512, :].rearrange("(t p) c -> p t c", p=128), xs_stg[:], ["xs_stg"], ["xs_tok"])
                    if c == 11:
                        P.dma("sp", B_tok[t0:t0 + 512, :].rearrange("(t p) c -> p t c", p=128), b_stg[:], ["b_stg"], ["B_tok"])
        P.emit()

        NCH = LT // 128
        CPS = LSEG // 128
        with ExitStack() as st:
            sb = lambda n, s, d=F32: st.enter_context(nc.sbuf_tensor(n, list(s), d))
            ps = lambda n, s, d=F32: st.enter_context(nc.psum_tensor(n, list(s), d))
            tri = [sb("triF", [128, 128]), sb("triB", [128, 128])]
            nmk = [sb("nmkF", [128, 128]), sb("nmkB", [128, 128])]
            ones = sb("onesC", [128, 128])
            P.op("pool", "memset", [], ["onesC"], ones[:], 1.0)
            for d_ in range(2):
                pat = [[1, 128]] if d_ == 0 else [[-1, 128]]
                cm = -1 if d_ == 0 else 1
                nm = "FB"[d_]
                P.op("pool", "memset", [], ["tri" + nm], tri[d_][:], 1.0)
                P.op("pool", "affine_select", ["tri" + nm], ["tri" + nm], out=tri[d_][:], in_=tri[d_][:], pattern=pat,
                     compare_op=ALU.is_ge, fill=0.0, base=0, channel_multiplier=cm)
                P.op("pool", "memset", [], ["nmk" + nm], nmk[d_][:], 0.0)
                P.op("pool", "affine_select", ["nmk" + nm], ["nmk" + nm], out=nmk[d_][:], in_=nmk[d_][:], pattern=pat,
                     compare_op=ALU.is_ge, fill=-30000.0, base=0, channel_multiplier=cm)
            dtb = sb("dtb", [128, 32])
            aneg = sb("aneg", [128, 32])
            flg = sb("flgC", [128, 1])
            P.dma("sp", dtb[:], dtb_rep, [], ["dtb"])
            P.dma("sp", aneg[:], alog_rep, [], ["aneg"])
            P.dma("sp", flg[:], flag, [], ["flgC"])
            P.op("act", "activation", ["aneg"], ["aneg"], out=aneg[:], in_=aneg[:], func=AF.Exp)
            P.op("dve", "tensor_scalar", ["aneg"], ["aneg"], out=aneg[:], in0=aneg[:], scalar1=-1.0, scalar2=None, op0=ALU.mult)

            def ssd_dir(d_):
                nm = "FB"[d_]
                X = lambda s_: s_ + nm
                NBUF = 2
                xs = [sb(X("xs%d" % i), [128, 16, 64], BF16) for i in range(NBUF)]
                bt_ = [sb(X("btok%d" % i), [128, 512], BF16) for i in range(NBUF)]
                bT = [sb(X("bT%d" % i), [128, 4, 128], BF16) for i in range(NBUF)]
                cT = [sb(X("cT%d" % i), [128, 4, 128], BF16) for i in range(NBUF)]
                dtr = [sb(X("dtr%d" % i), [128, 16]) for i in range(NBUF)]
                dt_ = sb(X("dt_"), [128, 16])
                dtA = sb(X("dtA"), [128, 16])
                acs = sb(X("acs"), [128, 16])
                dout_ = sb(X("dout_"), [128, 16])
                cd = sb(X("cd"), [128, 16])
                rhsb = [sb(X("rhsb%d" % i), [128, 4, 128]) for i in range(2)]
                dif = [sb(X("dif%d" % i), [128, 4, 128]) for i in range(2)]
                seg = [sb(X("seg%d" % i), [128, 4, 128]) for i in range(2)]
                mT = [sb(X("mT%d" % i), [128, 4, 128], BF16) for i in range(2)]
                xdt = sb(X("xdt"), [128, 16, 64], BF16)
                xdd = [sb(X("xdd%d" % i), [128, 4, 64], BF16) for i in range(2)]
                prev = sb(X("prev"), [128, 16, 64])
                prevb = sb(X("prevb"), [128, 16, 64], BF16)
                tmp = [sb(X("tmpc%d" % i), [128, 4, 64]) for i in range(2)]
                ybuf = [sb(X("ybuf%d" % i), [128, 16, 64]) for i in range(2)]
                pA = ps(X("pA"), [128, 4, 128])
                pB = ps(X("pB"), [128, 512])
                pC = ps(X("pC"), [128, 512])
                pD = ps(X("pD"), [128, 512])
                ydst = yf if d_ == 0 else yb_d

                def body():
                    it = 0
                    last = 127 if d_ == 0 else 0
                    order = list(range(NCH)) if d_ == 0 else list(range(NCH - 1, -1, -1))
                    pbn = [X("prevb%d" % g) for g in range(4)]
                    P.op("dve", "memset", [], [X("prev")], prev[:], 0.0)
                    P.op("pool", "memset", [], pbn, prevb[:], 0.0)
                    for ci, c in enumerate(order):
                        t0 = c * 128
                        b = it % NBUF
                        it += 1
                        if ci == CPS:
                            P.op("dve", "tensor_scalar", [X("prev"), "flgC"], [X("prev")], out=prev[:], in0=prev[:], scalar1=flg[:, 0:1],
                                 scalar2=None, op0=ALU.mult)
                            P.op("dve", "tensor_scalar", pbn + ["flgC"], pbn, out=prevb[:], in0=prevb[:], scalar1=flg[:, 0:1],
                                 scalar2=None, op0=ALU.mult)
                        P.dma("sp", xs[b][:], xs_tok[t0:t0 + 128, :].rearrange("p (h q) -> p h q", q=64), ["xs_tok"], [X("xs%d" % b)])
                        P.dma("sp", bt_[b][:], B_tok[t0:t0 + 128, :], ["B_tok"], [X("btok%d" % b)])
                        P.dma("sp", bT[b][:], BT[:, t0:t0 + 128].rearrange("(g n) l -> n g l", n=128), ["BT"], [X("bT%d" % b)])
                        P.dma("sp", cT[b][:], CT[:, t0:t0 + 128].rearrange("(g n) l -> n g l", n=128), ["CT"], [X("cT%d" % b)])
                        P.dma("sp", dtr[b][:], dt_tok[t0:t0 + 128, :], ["dt_tok"], [X("dtr%d" % b)])
                        P.op("dve", "tensor_tensor", [X("dtr%d" % b), "dtb"], [X("dt_")], out=dt_[:], in0=dtr[b][:], in1=dtb[:, d_ * 16:(d_ + 1) * 16], op=ALU.add)
                        P.op("act", "activation", [X("dt_")], [X("dt_")], out=dt_[:], in_=dt_[:], func=AF.Exp)
                        P.op("act", "activation", [X("dt_")], [X("dt_")], out=dt_[:], in_=dt_[:], func=AF.Ln, bias=1.0)
                        P.op("dve", "tensor_tensor", [X("dt_"), "aneg"], [X("dtA")], out=dtA[:], in0=dt_[:], in1=aneg[:, d_ * 16:(d_ + 1) * 16], op=ALU.mult)
                        P.mm(pD[:, 0:16], tri[d_][:], dtA[:], True, True, ["tri" + nm, X("dtA")], [X("pD")])
                        P.op("dve", "tensor_copy", [X("pD")], [X("acs")], out=acs[:], in_=pD[:, 0:16])
                        P.op("act", "activation", [X("acs")], [X("dout_")], out=dout_[:], in_=acs[:], func=AF.Exp)
                        P.op("pool", "tensor_tensor", [X("xs%d" % b), X("dt_")], [X("xdt")], out=xdt[:], in0=xs[b][:],
                             in1=dt_[:].unsqueeze(2).to_broadcast([128, 16, 64]), op=ALU.mult)
                        yb_i = ci % 2
                        yn = X("ybuf%d" % yb_i)
                        for g in range(4):
                            q = g % 2
                            h0 = g * 4
                            P.op("dve", "tensor_tensor", ["tri" + nm, X("dtA")], [X("rhsb%d" % q)], out=rhsb[q][:],
                                 in0=tri[d_][:].unsqueeze(1).to_broadcast([128, 4, 128]),
                                 in1=dtA[:, h0:h0 + 4].unsqueeze(2).to_broadcast([128, 4, 128]), op=ALU.mult)
                            P.mm(pA[:].rearrange("p r l -> p (r l)"), ones[:], rhsb[q][:].rearrange("p r l -> p (r l)"), True, True,
                                 ["onesC", X("rhsb%d" % q)], [X("pA")])
                            P.op("dve", "tensor_tensor", [X("pA"), X("acs")], [X("dif%d" % q)], out=dif[q][:], in0=pA[:],
                                 in1=acs[:, h0:h0 + 4].unsqueeze(2).to_broadcast([128, 4, 128]), op=ALU.subtract)
                            P.op("act", "activation", [X("pA")], [X("cd")], out=cd[:, h0:h0 + 4], in_=pA[:, :, last], func=AF.Exp)
                            P.op("dve", "tensor_tensor", [X("dif%d" % q), "nmk" + nm], [X("dif%d" % q)], out=dif[q][:], in0=dif[q][:],
                                 in1=nmk[d_][:].unsqueeze(1).to_broadcast([128, 4, 128]), op=ALU.min)
                            P.op("act", "activation", [X("dif%d" % q)], [X("seg%d" % q)], out=seg[q][:], in_=dif[q][:], func=AF.Exp)
                            P.mm(pB[:, 0:128], bT[b][:, g, :], cT[b][:, g, :], True, True, [X("bT%d" % b), X("cT%d" % b)], [X("pBcb")])
                            P.op("dve", "tensor_tensor", [X("seg%d" % q), X("pBcb")], [X("mT%d" % q)], out=mT[q][:], in0=seg[q][:],
                                 in1=pB[:, 0:128].unsqueeze(1).to_broadcast([128, 4, 128]), op=ALU.mult)
                            for r in range(4):
                                P.mm(pB[:, 128 + r * 64:128 + (r + 1) * 64], mT[q][:, r, :], xdt[:, h0 + r, :], True, True,
                                     [X("mT%d" % q), X("xdt")], [X("pBy")])
                            P.op("pool", "tensor_tensor", [X("xdt"), X("seg%d" % q)], [X("xdd%d" % q)], out=xdd[q][:], in0=xdt[:, h0:h0 + 4, :],
                                 in1=seg[q][:, :, last:last + 1].to_broadcast([128, 4, 64]), op=ALU.mult)
                            P.mm(pC[:, 0:256], bt_[b][:, g * 128:(g + 1) * 128], xdd[q][:].rearrange("p r q -> p (r q)"), True, True,
                                 [X("btok%d" % b), X("xdd%d" % q)], [X("pCs")])
                            P.mm(pC[:, 256:512], cT[b][:, g, :], prevb[:, h0:h0 + 4, :].rearrange("p r q -> p (r q)"), True, True,
                                 [X("cT%d" % b), X("prevb%d" % g)], [X("pCo")])
                            P.op("dve", "tensor_tensor", [X("pCo"), X("dout_")], [X("tmpc%d" % q)], out=tmp[q][:],
                                 in0=pC[:, 256:512].rearrange("p (r q) -> p r q", q=64),
                                 in1=dout_[:, h0:h0 + 4].unsqueeze(2).to_broadcast([128, 4, 64]), op=ALU.mult)
                            P.op("dve", "tensor_tensor", [X("tmpc%d" % q), X("pBy")], [yn], out=ybuf[yb_i][:, h0:h0 + 4, :], in0=tmp[q][:],
                                 in1=pB[:, 128:384].rearrange("p (r q) -> p r q", q=64), op=ALU.add)
                            P.op("dve", "tensor_tensor", [X("prev"), X("cd")], [X("prev")], out=prev[:, h0:h0 + 4, :], in0=prev[:, h0:h0 + 4, :],
                                 in1=cd[:, h0:h0 + 4].unsqueeze(2).to_broadcast([128, 4, 64]), op=ALU.mult)
                            P.op("dve", "tensor_tensor", [X("prev"), X("pCs")], [X("prev")], out=prev[:, h0:h0 + 4, :], in0=prev[:, h0:h0 + 4, :],
                                 in1=pC[:, 0:256].rearrange("p (r q) -> p r q", q=64), op=ALU.add)
                            P.op("act", "activation", [X("prev")], [X("prevb%d" % g)], out=prevb[:, h0:h0 + 4, :], in_=prev[:, h0:h0 + 4, :], func=AF.Copy)
                        P.dma("act", ydst[t0:t0 + 128, :], ybuf[yb_i][:].rearrange("p h q -> p (h q)"), [yn], [X("ydst%d" % (ci % 8))])
                return body

            P.threads([ssd_dir(0), ssd_dir(1)])
        P.emit()

        with ExitStack() as st:
            sb = lambda n, s, d=F32: st.enter_context(nc.sbuf_tensor(n, list(s), d))
            dsk = sb("dsk", [128, 16])
            gn = sb("gn", [128, 1024])
            P.dma("sp", dsk[:], ssdd_rep, [], ["dsk"])
            P.dma("sp", gn[:], ssdn_rep, [], ["gn"])

            def fin(par):
                X = lambda s_: s_ + str(par)
                ya = sb(X("yaC"), [128, 16, 64])
                ybb = sb(X("ybC"), [128, 16, 64])
                xs_ = sb(X("xsC"), [128, 16, 64], BF16)
                zt = sb(X("ztC"), [128, 1024])
                sq = sb(X("sqC"), [128, 1024])
                gss = sb(X("gssC"), [128, 4])
                yo = sb(X("yoC"), [128, 1024], BF16)

                def body():
                    for c in range(par, NCH, 2):
                        t0 = c * 128
                        P.dma("sp", ya[:].rearrange("p h q -> p (h q)"), yf[t0:t0 + 128, :], [], [X("yaC")])
                        P.dma("sp", ybb[:].rearrange("p h q -> p (h q)"), yb_d[t0:t0 + 128, :], [], [X("ybC")])
                        P.dma("sp", xs_[:], xs_tok[t0:t0 + 128, :].rearrange("p (h q) -> p h q", q=64), [], [X("xsC")])
                        P.dma("sp", zt[:], z_tok[t0:t0 + 128, :], [], [X("ztC")])
                        P.op("dve", "tensor_tensor", [X("yaC"), X("ybC")], [X("yaC")], out=ya[:], in0=ya[:], in1=ybb[:], op=ALU.add)
                        P.op("pool", "tensor_tensor", [X("xsC"), "dsk"], [X("ybC")], out=ybb[:], in0=xs_[:],
                             in1=dsk[:].unsqueeze(2).to_broadcast([128, 16, 64]), op=ALU.mult)
                        P.op("dve", "tensor_tensor", [X("yaC"), X("ybC")], [X("yaC")], out=ya[:], in0=ya[:], in1=ybb[:], op=ALU.add)
                        P.op("act", "activation", [X("ztC")], [X("ztC")], out=zt[:], in_=zt[:], func=AF.Silu)
                        y2 = ya[:].rearrange("p h q -> p (h q)")
                        P.op("dve", "tensor_tensor", [X("yaC"), X("ztC")], [X("yaC")], out=y2, in0=y2, in1=zt[:], op=ALU.mult)
                        P.op("pool", "tensor_tensor", [X("yaC")], [X("sqC")], out=sq[:], in0=y2, in1=y2, op=ALU.mult)
                        P.op("dve", "tensor_reduce", [X("sqC")], [X("gssC")], out=gss[:], in_=sq[:].rearrange("p (g c) -> p g c", c=256),
                             op=ALU.add, axis=AX.X)
                        P.op("dve", "tensor_scalar", [X("gssC")], [X("gssC")], out=gss[:], in0=gss[:], scalar1=1.0 / 256, scalar2=EPS,
                             op0=ALU.mult, op1=ALU.add)
                        P.op("act", "activation", [X("gssC")], [X("gssC")], out=gss[:], in_=gss[:], func=AF.Sqrt)
                        P.op("dve", "reciprocal", [X("gssC")], [X("gssC")], out=gss[:], in_=gss[:])
                        P.op("dve", "tensor_tensor", [X("yaC"), X("gssC")], [X("yaC")], out=ya[:].rearrange("p (g h) q -> p g (h q)", g=4),
                             in0=ya[:].rearrange("p (g h) q -> p g (h q)", g=4),
                             in1=gss[:].unsqueeze(2).to_broadcast([128, 4, 256]), op=ALU.mult)
                        P.op("dve", "tensor_tensor", [X("yaC"), "gn"], [X("yoC")], out=yo[:], in0=y2, in1=gn[:], op=ALU.mult)
                        P.dma("act", yssd[t0:t0 + 128, :], yo[:], [X("yoC")], [X("yssdw%d" % (c % 8))])
                return body

            P.threads([fin(0), fin(1)])
        P.emit()

        NB = LT // 8
        NBS = LSEG // 8
        P.limit = cfg.get("dmax")
        TWO_PI = 6.283185307179586
        with ExitStack() as st:
            sb = lambda n, s, d=F32: st.enter_context(nc.sbuf_tensor(n, list(s), d))
            ps = lambda n, s, d=F32: st.enter_context(nc.psum_tensor(n, list(s), d))
            identF = sb("identF", [128, 128])
            P.op("pool", "memset", [], ["identF"], identF[:], 1.0)
            P.op("pool", "affine_select", ["identF"], ["identF"], out=identF[:], in_=identF[:], pattern=[[-1, 128]],
                 compare_op=ALU.is_equal, fill=0.0, base=0, channel_multiplier=1)
            bm = sb("bmD", [128, 8, 16])
            P.op("pool", "memset", [], ["bmD"], bm[:], 1.0)
            P.op("pool", "affine_select", ["bmD"], ["bmD"], out=bm[:], in_=bm[:], pattern=[[-16, 8], [0, 16]],
                 compare_op=ALU.is_ge, fill=0.0, base=0, channel_multiplier=1)
            P.op("pool", "affine_select", ["bmD"], ["bmD"], out=bm[:], in_=bm[:], pattern=[[16, 8], [0, 16]],
                 compare_op=ALU.is_ge, fill=0.0, base=15, channel_multiplier=-1)
            PM = [sb("PM%d" % i, [128, 2, 64]) for i in range(4)]
            CM = [sb("CM%d" % i, [128, 8, 16]) for i in range(4)]
            for pr_ in range(4):
                P.op("pool", "memset", [], ["PM%d" % pr_], PM[pr_][:], 1.0)
                P.op("pool", "affine_select", ["PM%d" % pr_], ["PM%d" % pr_], out=PM[pr_][:], in_=PM[pr_][:],
                     pattern=[[-16, 2], [0, 64]], compare_op=ALU.is_ge, fill=0.0, base=-32 * pr_, channel_multiplier=1)
                P.op("pool", "affine_select", ["PM%d" % pr_], ["PM%d" % pr_], out=PM[pr_][:], in_=PM[pr_][:],
                     pattern=[[16, 2], [0, 64]], compare_op=ALU.is_ge, fill=0.0, base=32 * pr_ + 15, channel_multiplier=-1)
                P.op("pool", "memset", [], ["CM%d" % pr_], CM[pr_][:], 1.0)
                for h in range(2):
                    P.op("pool", "affine_select", ["CM%d" % pr_], ["CM%d" % pr_], out=CM[pr_][h * 64:(h + 1) * 64],
                         in_=CM[pr_][h * 64:(h + 1) * 64], pattern=[[1, 8], [0, 16]], compare_op=ALU.is_equal, fill=0.0,
                         base=-(2 * pr_ + h), channel_multiplier=0)
            KV = sb("KV", [128, 8, 9])
            P.op("pool", "iota", [], ["KV"], KV[:], pattern=[[0, 8], [1, 9]], base=0, channel_multiplier=0,
                 allow_small_or_imprecise_dtypes=True)
            BL = sb("BL", [128, NBS])
            BHs = [sb("BH%d" % i, [128, NBS]) for i in range(2)]
            P.op("pool", "iota", [], ["BL"], BL[:], pattern=[[0, NBS // 32], [1, 32]], base=0, channel_multiplier=0,
                 allow_small_or_imprecise_dtypes=True)
            for s_ in range(2):
                P.op("pool", "iota", [], ["BH%d" % s_], BHs[s_][:], pattern=[[1, NBS // 32], [0, 32]], base=s_ * (NBS // 32),
                     channel_multiplier=0, allow_small_or_imprecise_dtypes=True)
            dcol = sb("dcolD", [128, 8])
            flg = sb("flgD", [128, 1])
            P.dma("sp", dcol[:], s5_dcol, [], ["dcolD"])
            P.dma("sp", flg[:], flag, [], ["flgD"])
            sm = sb("smD", [128, 3, 8])
            bg = sb("bgD", [128, 4, 128])
            ncim = sb("ncim", [128, 128])
            sml = {n_: sb("sm_" + n_, [128, 8]) for n_ in ("dl", "xr", "xi", "numr", "den", "t1", "t2", "qr", "qi", "tb", "t0", "t32")}
            smi = sb("sm_i", [128, 8], I32)
            k9 = {n_: sb("k9_" + n_, [128, 8, 9]) for n_ in ("er", "tn", "tf", "cs", "sn", "pr", "pi")}
            k9i = sb("k9_i", [128, 8, 9], I32)
            Bb = {n_: sb("Bb_" + n_, [128, 8, 16]) for n_ in ("r", "i", "a", "b")}
            colsT = sb("colsT", [128, 3, 4])
            scr = sb("scrD", [128, 11, 512])
            RN = lambda a, b_: ["R%d" % i for i in range(a, b_)]
            X_r = scr[:, 0:2, :].rearrange("p a (b c) -> p (a b) c", c=128)
            X_i = scr[:, 2:4, :].rearrange("p a (b c) -> p (a b) c", c=128)
            Zr_t = sb("Zr_t", [128, 9, 128])
            Zi_t = sb("Zi_t", [128, 9, 128])
            xtmp = sb("xtmp", [128, 8, 128])
            W1 = sb("W1", [128, 8, 2, 4, 128], BF16)
            W2 = sb("W2", [128, 2, 9, 2, 4, 128], BF16)
            Kbd = sb("Kbd", [128, 2, 8, 128], BF16)
            ktmp = sb("ktmp", [128, 128])
            ust = sb("ust", [128, LT // 2], BF16)
            u3 = sb("uj", [128, 8, NB], BF16)
            S = sb("S_D", [128, 4, 2, NBS])
            HB = sb("HB_D", [128, 2, 4, 2, NB + 1], BF16)
            carry = sb("carryD", [128, 4, 2])
            stg = sb("stgD", [128, NBS, 8], BF16)
            gl = [sb("glD%d" % i, [128, NBS]) for i in range(3)]
            pk = ps("pkD", [128, 4, 128])
            ptl = [ps("ptD%d" % i, [128, 4, 128]) for i in range(2)]
            pS = [ps("pSD%d" % i, [128, 512]) for i in range(2)]
            po = [ps("poD%d" % i, [128, 512]) for i in range(2)]
            mskq = _rr(["dve", "pool"])
            psq = 0
            poq = 0
            T = lambda i: scr[:, i, 0:NBS]
            DST = cfg.get("dstage", 9)
            for ft in range(cfg.get("nft", 8)):
                for hf in range(2):
                    P.dma("sp", ust[:], uT[ft * 128:(ft + 1) * 128, hf * (LT // 2):(hf + 1) * (LT // 2)], ["uT"], ["ust"])
                    usv = ust[:].rearrange("p (b j) -> p j b", j=8)
                    for j in range(8):
                        P.op("act", "activation", ["ust"], ["uft"], out=u3[:, j, hf * (NB // 2):(hf + 1) * (NB // 2)], in_=usv[:, j, :], func=AF.Copy)
                for d_ in range(2):
                    P.dma("sp", sm[:], s5_small[d_, ft], [], ["smD"])
                    P.dma("sp", bg[:], s5_big[d_, ft], [], ["bgD"])
                    are, aim, lst = sm[:, 0, :], sm[:, 1, :], sm[:, 2, :]
                    bre, bim, cre, cim = bg[:, 0, :], bg[:, 1, :], bg[:, 2, :], bg[:, 3, :]
                    V = lambda n_: sml[n_][:]
                    P.op("act", "activation", ["smD"], ["sm_dl"], out=V("dl"), in_=lst, func=AF.Exp)
                    P.op("dve", "tensor_tensor", ["smD", "sm_dl"], ["sm_xr"], out=V("xr"), in0=are, in1=V("dl"), op=ALU.mult)
                    P.op("dve", "tensor_tensor", ["smD", "sm_dl"], ["sm_xi"], out=V("xi"), in0=aim, in1=V("dl"), op=ALU.mult)
                    b9 = lambda ap: ap.unsqueeze(2).to_broadcast([128, 8, 9])
                    P.op("dve", "tensor_tensor", ["KV", "sm_xr"], ["k9_er"], out=k9["er"][:], in0=KV[:], in1=b9(V("xr")), op=ALU.mult)
                    P.op("act", "activation", ["k9_er"], ["k9_er"], out=k9["er"][:], in_=k9["er"][:], func=AF.Exp)
                    P.op("dve", "tensor_tensor", ["KV", "sm_xi"], ["k9_tn"], out=k9["tn"][:], in0=KV[:], in1=b9(V("xi")), op=ALU.mult)
                    P.op("dve", "tensor_scalar", ["k9_tn"], ["k9_tn"], out=k9["tn"][:], in0=k9["tn"][:], scalar1=1.0 / TWO_PI, scalar2=None, op0=ALU.mult)
                    for (dst, off) in (("sn", 0.0), ("cs", 0.25)):
                        if off:
                            P.op("dve", "tensor_scalar", ["k9_tn"], ["k9_tn"], out=k9["tn"][:], in0=k9["tn"][:], scalar1=off, scalar2=None, op0=ALU.add)
                        P.op("dve", "tensor_copy", ["k9_tn"], ["k9_i"], out=k9i[:], in_=k9["tn"][:])
                        P.op("dve", "tensor_copy", ["k9_i"], ["k9_tf"], out=k9["tf"][:], in_=k9i[:])
                        P.op("dve", "tensor_tensor", ["k9_tn", "k9_tf"], ["k9_tf"], out=k9["tf"][:], in0=k9["tn"][:], in1=k9["tf"][:], op=ALU.subtract)
                        P.op("act", "activation", ["k9_tf"], ["k9_" + dst], out=k9[dst][:], in_=k9["tf"][:], func=AF.Sin, scale=TWO_PI)
                    P.op("dve", "tensor_tensor", ["k9_er", "k9_cs"], ["k9_pr"], out=k9["pr"][:], in0=k9["er"][:], in1=k9["cs"][:], op=ALU.mult)
                    P.op("dve", "tensor_tensor", ["k9_er", "k9_sn"], ["k9_pi"], out=k9["pi"][:], in0=k9["er"][:], in1=k9["sn"][:], op=ALU.mult)
                    pr1, pi1 = k9["pr"][:, :, 1], k9["pi"][:, :, 1]
                    P.op("dve", "tensor_scalar", ["k9_pr"], ["sm_numr"], out=V("numr"), in0=pr1, scalar1=-1.0, scalar2=None, op0=ALU.add)
                    P.op("dve", "tensor_tensor", ["smD"], ["sm_den"], out=V("den"), in0=are, in1=are, op=ALU.mult)
                    P.op("dve", "tensor_tensor", ["smD"], ["sm_t1"], out=V("t1"), in0=aim, in1=aim, op=ALU.mult)
                    P.op("dve", "tensor_tensor", ["sm_den", "sm_t1"], ["sm_den"], out=V("den"), in0=V("den"), in1=V("t1"), op=ALU.add)
                    P.op("dve", "reciprocal", ["sm_den"], ["sm_den"], out=V("den"), in_=V("den"))
                    P.op("dve", "tensor_tensor", ["sm_numr", "smD"], ["sm_t1"], out=V("t1"), in0=V("numr"), in1=are, op=ALU.mult)
                    P.op("dve", "tensor_tensor", ["k9_pi", "smD"], ["sm_t2"], out=V("t2"), in0=pi1, in1=aim, op=ALU.mult)
                    P.op("dve", "tensor_tensor", ["sm_t1", "sm_t2"], ["sm_t1"], out=V("t1"), in0=V("t1"), in1=V("t2"), op=ALU.add)
                    P.op("dve", "tensor_tensor", ["sm_t1", "sm_den"], ["sm_qr"], out=V("qr"), in0=V("t1"), in1=V("den"), op=ALU.mult)
                    P.op("dve", "tensor_tensor", ["k9_pi", "smD"], ["sm_t1"], out=V("t1"), in0=pi1, in1=are, op=ALU.mult)
                    P.op("dve", "tensor_tensor", ["sm_numr", "smD"], ["sm_t2"], out=V("t2"), in0=V("numr"), in1=aim, op=ALU.mult)
                    P.op("dve", "tensor_tensor", ["sm_t1", "sm_t2"], ["sm_t1"], out=V("t1"), in0=V("t1"), in1=V("t2"), op=ALU.subtract)
                    P.op("dve", "tensor_tensor", ["sm_t1", "sm_den"], ["sm_qi"], out=V("qi"), in0=V("t1"), in1=V("den"), op=ALU.mult)
                    b16 = lambda ap: ap.unsqueeze(2).to_broadcast([128, 8, 16])
                    g16 = lambda ap: ap.rearrange("p (g c) -> p g c", c=16)
                    TT = lambda rd, wr, out, in0, in1, op, eng="dve": P.op(eng, "tensor_tensor", rd, wr, out=out, in0=in0, in1=in1, op=op)
                    TT(["bgD", "sm_qr"], ["Bb_a"], Bb["a"][:], g16(bre), b16(V("qr")), ALU.mult)
                    TT(["bgD", "sm_qi"], ["Bb_b"], Bb["b"][:], g16(bim), b16(V("qi")), ALU.mult)
                    TT(["Bb_a", "Bb_b"], ["Bb_r"], Bb["r"][:], Bb["a"][:], Bb["b"][:], ALU.subtract)
                    TT(["bgD", "sm_qr"], ["Bb_a"], Bb["a"][:], g16(bim), b16(V("qr")), ALU.mult)
                    TT(["bgD", "sm_qi"], ["Bb_b"], Bb["b"][:], g16(bre), b16(V("qi")), ALU.mult)
                    TT(["Bb_a", "Bb_b"], ["Bb_i"], Bb["i"][:], Bb["a"][:], Bb["b"][:], ALU.add)
                    for kk in range(8):
                        prk = b16(k9["pr"][:, :, kk])
                        pik = b16(k9["pi"][:, :, kk])
                        xr_k = g16(X_r[:, kk, :])
                        xi_k = g16(X_i[:, kk, :])
                        xt_k = g16(xtmp[:, kk, :])
                        TT(["k9_pr", "Bb_r"], RN(0, 2), xr_k, Bb["r"][:], prk, ALU.mult)
                        TT(["k9_pi", "Bb_i"], ["xtmp"], xt_k, Bb["i"][:], pik, ALU.mult, "pool")
                        TT(RN(0, 2) + ["xtmp"], RN(0, 2), xr_k, xr_k, xt_k, ALU.subtract)
                        TT(["k9_pr", "Bb_i"], RN(2, 4), xi_k, Bb["i"][:], prk, ALU.mult)
                        TT(["k9_pi", "Bb_r"], ["xtmp"], xt_k, Bb["r"][:], pik, ALU.mult, "pool")
                        TT(RN(2, 4) + ["xtmp"], RN(2, 4), xi_k, xi_k, xt_k, ALU.add)
                    for kk in range(9):
                        prk = b16(k9["pr"][:, :, kk])
                        pik = b16(k9["pi"][:, :, kk])
                        zr_k = g16(Zr_t[:, kk, :])
                        zi_k = g16(Zi_t[:, kk, :])
                        xt_k = g16(xtmp[:, kk % 8, :])
                        TT(["k9_pr", "bgD"], ["Zr_t"], zr_k, g16(cre), prk, ALU.mult)
                        TT(["k9_pi", "bgD"], ["xtmp"], xt_k, g16(cim), pik, ALU.mult, "pool")
                        TT(["Zr_t", "xtmp"], ["Zr_t"], zr_k, zr_k, xt_k, ALU.subtract)
                        TT(["k9_pi", "bgD"], ["Zi_t"], zi_k, g16(cre), pik, ALU.mult)
                        TT(["k9_pr", "bgD"], ["xtmp"], xt_k, g16(cim), prk, ALU.mult, "pool")
                        TT(["Zi_t", "xtmp"], ["Zi_t"], zi_k, zi_k, xt_k, ALU.add)
                    P.op("dve", "tensor_scalar", ["bgD"], ["ncim"], out=ncim[:], in0=cim, scalar1=-1.0, scalar2=None, op0=ALU.mult)
                    for half in range(2):
                        for tl in range(4):
                            tau = half * 4 + tl
                            P.mm(pk[:, tl, :], X_r[0:64, tau, :], cre[0:64, :], True, False, RN(0, 2) + ["bgD"], ["pkD"])
                            P.mm(pk[:, tl, :], X_i[0:64, tau, :], ncim[0:64, :], False, True, RN(2, 4) + ["ncim"], ["pkD"])
                        for tl in range(4):
                            tau = half * 4 + tl
                            if tau == 0 and d_ == 0:
                                P.op("dve", "scalar_tensor_tensor", ["identF", "dcolD", "pkD"], ["ktmp"], out=ktmp[:], in0=identF[:],
                                     scalar=dcol[:, ft:ft + 1], in1=pk[:, tl, :], op0=ALU.mult, op1=ALU.add)
                                P.op("dve", "tensor_tensor", ["ktmp", "bmD"], ["Kbd"], out=Kbd[:, d_, tau, :], in0=ktmp[:],
                                     in1=bm[:].rearrange("p g c -> p (g c)"), op=ALU.mult)
                            else:
                                P.op("dve", "tensor_tensor", ["pkD", "bmD"], ["Kbd"], out=Kbd[:, d_, tau, :], in0=pk[:, tl, :],
                                     in1=bm[:].rearrange("p g c -> p (g c)"), op=ALU.mult)
                    for kk in range(8):
                        for ri in range(2):
                            src = (X_r if ri == 0 else X_i)[:, kk, :]
                            slot = (kk * 2 + ri) % 2
                            pt = ptl[slot]
                            P.tr(pt[:, 0, :], src, identF[:], (RN(0, 2) if ri == 0 else RN(2, 4)) + ["identF"], ["ptD%d" % slot])
                            for pr_ in range(4):
                                P.op("dve", "tensor_tensor", ["ptD%d" % slot, "PM%d" % pr_], ["W1"], out=W1[:, kk, ri, pr_, :],
                                     in0=pt[:, 0, :], in1=PM[pr_][:].rearrange("p a b -> p (a b)"), op=ALU.mult)
                    for kk in range(1, 9):
                        for ri in range(2):
                            for pr_ in range(4):
                                cmv = CM[pr_][:].rearrange("p g c -> p (g c)")
                                if ri == 0:
                                    P.op("pool", "tensor_tensor", ["Zr_t", "CM%d" % pr_], ["W2"], out=W2[:, d_, kk, ri, pr_, :],
                                         in0=Zr_t[:, kk, :], in1=cmv, op=ALU.mult)
                                else:
                                    P.op("dve", "scalar_tensor_tensor", ["Zi_t", "CM%d" % pr_], ["W2"], out=W2[:, d_, kk, ri, pr_, :],
                                         in0=Zi_t[:, kk, :], scalar=-1.0, in1=cmv, op0=ALU.mult, op1=ALU.mult)
                    P.op("dve", "tensor_scalar", ["sm_xi"], ["sm_tb"], out=V("tb"), in0=V("xi"), scalar1=8.0 / TWO_PI, scalar2=None, op0=ALU.mult)
                    P.op("dve", "tensor_copy", ["sm_tb"], ["sm_i"], out=smi[:], in_=V("tb"))
                    P.op("dve", "tensor_copy", ["sm_i"], ["sm_t0"], out=V("t0"), in_=smi[:])
                    P.op("dve", "tensor_tensor", ["sm_tb", "sm_t0"], ["sm_t0"], out=V("t0"), in0=V("tb"), in1=V("t0"), op=ALU.subtract)
                    P.op("dve", "tensor_scalar", ["sm_t0"], ["sm_tb"], out=V("tb"), in0=V("t0"), scalar1=32.0, scalar2=None, op0=ALU.mult)
                    P.op("dve", "tensor_copy", ["sm_tb"], ["sm_i"], out=smi[:], in_=V("tb"))
                    P.op("dve", "tensor_copy", ["sm_i"], ["sm_t32"], out=V("t32"), in_=smi[:])
                    P.op("dve", "tensor_tensor", ["sm_tb", "sm_t32"], ["sm_t32"], out=V("t32"), in0=V("tb"), in1=V("t32"), op=ALU.subtract)
                    for h in range(2):
                        hs = slice(h * 64, (h + 1) * 64)
                        P.op("dve", "tensor_copy", ["sm_t0"], ["colsT"], out=colsT[hs, 0, :], in_=sml["t0"][hs, h::2])
                        P.op("dve", "tensor_copy", ["sm_t32"], ["colsT"], out=colsT[hs, 1, :], in_=sml["t32"][hs, h::2])
                        P.op("dve", "tensor_copy", ["k9_er"], ["colsT"], out=colsT[hs, 2, :], in_=k9["er"][hs, h::2, 8])
                    if DST < 2:
                        continue
                    for ui, s_ in enumerate((0, 1) if d_ == 0 else (1, 0)):
                        blk0 = s_ * NBS
                        for pr_ in range(4):
                            for ri in range(2):
                                pb = psq % 2
                                psq += 1
                                for j in range(8):
                                    kk = (7 - j) if d_ == 0 else j
                                    P.mm(pS[pb][:, 0:NBS], W1[:, kk, ri, pr_, :], u3[:, j, blk0:blk0 + NBS], j == 0, j == 7,
                                         ["W1", "uft"], ["pSD%d" % pb])
                                P.op("act", "activation", ["pSD%d" % pb], ["S_D%d" % pr_], out=S[:, pr_, ri, :], in_=pS[pb][:, 0:NBS], func=AF.Copy)
                        if DST < 3:
                            continue
                        for pr_ in range(4):
                            tu, ti_, tf, cs, sn, T_r, T_i, g_r, g_i, m1, m2 = [T(i) for i in range(11)]
                            tii = ti_.bitcast(I32)
                            t0c, t32c, decc = colsT[:, 0, pr_:pr_ + 1], colsT[:, 1, pr_:pr_ + 1], colsT[:, 2, pr_:pr_ + 1]
                            P.op("dve", "tensor_scalar", ["BH%d" % s_, "colsT"], ["R0"], out=tu, in0=BHs[s_][:], scalar1=t32c, scalar2=None, op0=ALU.mult)
                            P.op("dve", "scalar_tensor_tensor", ["BL", "colsT", "R0"], ["R0"], out=tu, in0=BL[:], scalar=t0c, in1=tu, op0=ALU.mult, op1=ALU.add)
                            for (dst, dn, off) in ((sn, "R4", 0.0), (cs, "R3", 0.25)):
                                if off:
                                    P.op("dve", "tensor_scalar", ["R0"], ["R0"], out=tu, in0=tu, scalar1=off, scalar2=None, op0=ALU.add)
                                P.op("dve", "tensor_copy", ["R0"], ["R1"], out=tii, in_=tu)
                                P.op("dve", "tensor_copy", ["R1"], ["R2"], out=tf, in_=tii)
                                P.op("pool", "tensor_tensor", ["R0", "R2"], ["R2"], out=tf, in0=tu, in1=tf, op=ALU.subtract)
                                P.op("act", "activation", ["R2"], [dn], out=dst, in_=tf, func=AF.Sin, scale=TWO_PI)
                            S_r, S_i = S[:, pr_, 0, :], S[:, pr_, 1, :]
                            sg = 1.0 if d_ == 0 else -1.0
                            sop = ALU.add if d_ == 0 else ALU.subtract
                            sop2 = ALU.subtract if d_ == 0 else ALU.add
                            sn_ = "S_D%d" % pr_
                            TT([sn_, "R3"], ["R5"], T_r, S_r, cs, ALU.mult)
                            TT([sn_, "R4"], ["R9"], m1, S_i, sn, ALU.mult, "pool")
                            TT(["R5", "R9"], ["R5"], T_r, T_r, m1, sop)
                            TT([sn_, "R3"], ["R6"], T_i, S_i, cs, ALU.mult)
                            TT([sn_, "R4"], ["R10"], m2, S_r, sn, ALU.mult, "pool")
                            TT(["R6", "R10"], ["R6"], T_i, T_i, m2, sop2)
                            rv = (lambda ap: ap) if d_ == 0 else (lambda ap: ap[:, ::-1])
                            dbc = decc.to_broadcast([128, NBS])
                            for (src, sname, dst, dname, ri) in ((T_r, "R5", g_r, "R7", 0), (T_i, "R6", g_i, "R8", 1)):
                                init = 0.0 if ui == 0 else carry[:, pr_, ri:ri + 1]
                                P.op("dve", "tensor_tensor_scan", [sname, "colsT", "carryD"], [dname], out=rv(dst), data0=dbc, data1=rv(src),
                                     initial=init, op0=ALU.mult, op1=ALU.add)
                                if ui == 0:
                                    lastc = (NBS - 1) if d_ == 0 else 0
                                    P.op("dve", "tensor_scalar", [dname, "flgD"], ["carryD"], out=carry[:, pr_, ri:ri + 1], in0=dst[:, lastc:lastc + 1],
                                         scalar1=flg[:, 0:1], scalar2=None, op0=ALU.mult)
                            if d_ == 0:
                                o_r = HB[:, 0, pr_, 0, blk0 + 1:blk0 + NBS + 1]
                                o_i = HB[:, 0, pr_, 1, blk0 + 1:blk0 + NBS + 1]
                                sl_ = slice(0, NBS)
                            elif s_ == 1:
                                o_r = HB[:, 1, pr_, 0, NBS - 1:NB - 1]
                                o_i = HB[:, 1, pr_, 1, NBS - 1:NB - 1]
                                sl_ = slice(0, NBS)
                            else:
                                o_r = HB[:, 1, pr_, 0, 0:NBS - 1]
                                o_i = HB[:, 1, pr_, 1, 0:NBS - 1]
                                sl_ = slice(1, NBS)
                            hn = "HB%d_%d" % (d_, pr_)
                            TT(["R7", "R3"], ["R9"], m1, g_r, cs, ALU.mult)
                            TT(["R8", "R4"], ["R10"], m2, g_i, sn, ALU.mult, "pool")
                            TT(["R9", "R10"], [hn], o_r, m1[:, sl_], m2[:, sl_], sop2)
                            TT(["R8", "R3"], ["R9"], m1, g_i, cs, ALU.mult)
                            TT(["R7", "R4"], ["R10"], m2, g_r, sn, ALU.mult, "pool")
                            TT(["R9", "R10"], [hn], o_i, m1[:, sl_], m2[:, sl_], sop)
                            if ui == 1:
                                zc = 0 if d_ == 0 else NB - 1
                                fc = NBS if d_ == 0 else NBS - 1
                                P.op("dve", "memset", [], [hn], HB[:, d_, pr_, :, zc:zc + 1], 0.0)
                                P.op("dve", "tensor_scalar", [hn, "flgD"], [hn], out=HB[:, d_, pr_, :, fc:fc + 1], in0=HB[:, d_, pr_, :, fc:fc + 1],
                                     scalar1=flg[:, 0:1], scalar2=None, op0=ALU.mult)
                if DST < 4:
                    continue
                hall = ["HB%d_%d" % (a, b_) for a in range(2) for b_ in range(4)]
                for s_ in range(2):
                    blk0 = s_ * NBS
                    for i in range(8):
                        pb = poq % 2
                        poq += 1
                        mms = []
                        for j in range(0, i + 1):
                            mms.append((Kbd[:, 0, i - j, :], u3[:, j, blk0:blk0 + NBS]))
                        for j in range(i, 8):
                            mms.append((Kbd[:, 1, j - i, :], u3[:, j, blk0:blk0 + NBS]))
                        for pr_ in range(4):
                            for ri in range(2):
                                mms.append((W2[:, 0, i + 1, ri, pr_, :], HB[:, 0, pr_, ri, blk0:blk0 + NBS]))
                                mms.append((W2[:, 1, 8 - i, ri, pr_, :], HB[:, 1, pr_, ri, blk0:blk0 + NBS]))
                        for mi, (l_, r_) in enumerate(mms):
                            P.mm(po[pb][:, 0:NBS], l_, r_, mi == 0, mi == len(mms) - 1, ["Kbd", "W2", "uft"] + hall, ["poD%d" % pb])
                        v = po[pb][:, 0:NBS]
                        P.op("act", "activation", ["poD%d" % pb], ["glD0"], out=gl[0][:], in_=v, func=AF.Square)
                        P.op("dve", "tensor_scalar", ["glD0"], ["glD0"], out=gl[0][:], in0=gl[0][:], scalar1=0.044715, scalar2=1.0, op0=ALU.mult, op1=ALU.add)
                        P.op("dve", "tensor_tensor", ["glD0", "poD%d" % pb], ["glD1"], out=gl[1][:], in0=gl[0][:], in1=v, op=ALU.mult)
                        P.op("act", "activation", ["glD1"], ["glD2"], out=gl[2][:], in_=gl[1][:], func=AF.Sigmoid, scale=1.5957691216057308)
                        P.op("dve", "tensor_tensor", ["glD2", "poD%d" % pb], ["stgD"], out=stg[:, :, i], in0=gl[2][:], in1=v, op=ALU.mult)
                    P.dma("sp", y5gT[ft * 128:(ft + 1) * 128, s_ * LSEG:(s_ + 1) * LSEG], stg[:].rearrange("p b i -> p (b i)"), ["stgD"], ["y5gT"])
        if cfg.get("dump"):
            for i_, o_ in enumerate(P.ops):
                print(i_, o_.eng, getattr(o_, 'desc', 'mm/dma') if hasattr(o_, 'desc') else '-', o_.reads, o_.writes)
        P.emit()
        P.limit = None

        with ExitStack() as st:
            sb = lambda n, s, d=F32: st.enter_context(nc.sbuf_tensor(n, list(s), d))
            gA = sb("gA", [128, 16])
            gM = sb("gM", [128, 16])
            P.dma("sp", gA[:], norm_attn, [], ["gA"])
            P.dma("sp", gM[:], norm_mem, [], ["gM"])
            wst2 = [sb("wst2_%d" % i, [128, 2048]) for i in range(2)]
            wbf2 = [sb("wbf2_%d" % i, [128, 2048], BF16) for i in range(2)]
            ci = 0
            ceng = _rr(["dve", "pool"])
            for (src, dst, gn_, gname, kdim, ncol) in ((w_glu, w_glu_bf, None, None, 1024, 1024), (w_out, w_out_bf, None, None, 2048, 2048),
                                                       (w_q, w_q_bf, gA, "gA", 2048, 2048), (w_k, w_k_bf, gM, "gM", 2048, 2048),
                                                       (w_v, w_v_bf, gM, "gM", 2048, 2048), (w_o, w_o_bf, None, None, 2048, 2048)):
                for kc in range(kdim // 128):
                    b = ci % 2
                    ci += 1
                    P.dma("sp", wst2[b][:, 0:ncol], src[kc * 128:(kc + 1) * 128, :], [], ["wst2_%d" % b])
                    if gn_ is None:
                        if ceng() == "dve":
                            P.op("dve", "tensor_copy", ["wst2_%d" % b], ["wbf2_%d" % b], out=wbf2[b][:, 0:ncol], in_=wst2[b][:, 0:ncol])
                        else:
                            P.op("act", "activation", ["wst2_%d" % b], ["wbf2_%d" % b], out=wbf2[b][:, 0:ncol], in_=wst2[b][:, 0:ncol], func=AF.Copy)
                    else:
                        P.op("dve", "tensor_scalar", ["wst2_%d" % b, gname], ["wbf2_%d" % b], out=wbf2[b][:, 0:ncol], in0=wst2[b][:, 0:ncol],
                             scalar1=gn_[:, kc:kc + 1], scalar2=None, op0=ALU.mult)
                    P.dma("sp", dst[:, :, kc, :].rearrange("g p n -> p g n"), wbf2[b][:, 0:ncol].rearrange("p (g n) -> p g n", n=512),
                          ["wbf2_%d" % b], [dst.tensor.name])
        P.emit()

        with ExitStack() as st:
            sb = lambda n, s, d=F32: st.enter_context(nc.sbuf_tensor(n, list(s), d))
            ps = lambda n, s, d=F32: st.enter_context(nc.psum_tensor(n, list(s), d))
            ident = sb("identM", [128, 128], BF16)
            P.op("pool", "memset", [], ["identM"], ident[:], 1.0)
            P.op("pool", "affine_select", ["identM"], ["identM"], out=ident[:], in_=ident[:], pattern=[[-1, 128]],
                 compare_op=ALU.is_equal, fill=0.0, base=0, channel_multiplier=1)
            mt = sb("mtM", [128, 2048])
            junk = sb("junkM", [128, 2048], BF16)
            ssm = sb("ssM", [128, 1])
            mn = sb("mnM", [128, 2048], BF16)
            memT = sb("memT", [128, 16, 256], BF16)
            wck = [sb("wckM%d" % i, [128, 16, 512], BF16) for i in range(2)]
            kTs = sb("kTs", [128, 16, 256], BF16)
            vs = sb("vs", [128, 2, 2048], BF16)
            ptm = [ps("ptM%d" % i, [128, 1024], BF16) for i in range(2)]
            pmm_ = [ps("pmM%d" % i, [128, 512]) for i in range(2)]
            tq = 0
            pq = 0
            wq_ = 0
            for sg in range(2):
                for mc in range(2):
                    P.dma("sp", mt[:], mem_in[sg, mc * 128:(mc + 1) * 128, :], [], ["mtM"])
                    P.op("act", "activation", ["mtM"], ["junkM", "ssM"], out=junk[:], in_=mt[:], func=AF.Square, accum_out=ssm[:])
                    P.op("dve", "tensor_scalar", ["ssM"], ["ssM"], out=ssm[:], in0=ssm[:], scalar1=1.0 / D, scalar2=EPS, op0=ALU.mult, op1=ALU.add)
                    P.op("act", "activation", ["ssM"], ["ssM"], out=ssm[:], in_=ssm[:], func=AF.Sqrt)
                    P.op("dve", "reciprocal", ["ssM"], ["ssM"], out=ssm[:], in_=ssm[:])
                    P.op("dve", "tensor_scalar", ["mtM", "ssM"], ["mnM"], out=mn[:], in0=mt[:], scalar1=ssm[:, 0:1], scalar2=None, op0=ALU.mult)
                    for q4 in range(4):
                        pb = tq % 2
                        tq += 1
                        for j in range(4):
                            kc = q4 * 4 + j
                            P.tr(ptm[pb][:, j * 128:(j + 1) * 128], mn[:, kc * 128:(kc + 1) * 128], ident[:], ["mnM", "identM"], ["ptM%d" % pb])
                        P.op("act", "activation", ["ptM%d" % pb], ["memT"], out=memT[:, q4 * 4:(q4 + 1) * 4, mc * 128:(mc + 1) * 128],
                             in_=ptm[pb][:, 0:512].rearrange("p (j n) -> p j n", n=128), func=AF.Copy)
                for g in range(4):
                    wb = wq_ % 2
                    wq_ += 1
                    P.dma("sp", wck[wb][:], w_k_bf[g], ["w_k_bf"], ["wckM%d" % wb])
                    for mcc in range(4):
                        pb = pq % 2
                        pq += 1
                        for kc in range(16):
                            P.mm(pmm_[pb][:, 0:256], wck[wb][:, kc, mcc * 128:(mcc + 1) * 128], memT[:, kc, :], kc == 0, kc == 15,
                                 ["wckM%d" % wb, "memT"], ["pmM%d" % pb])
                        P.op("act", "activation", ["pmM%d" % pb], ["kTs"], out=kTs[:, g * 4 + mcc, :], in_=pmm_[pb][:, 0:256], func=AF.Copy)
                for g in range(4):
                    wb = wq_ % 2
                    wq_ += 1
                    P.dma("sp", wck[wb][:], w_v_bf[g], ["w_v_bf"], ["wckM%d" % wb])
                    for mc in range(2):
                        pb = pq % 2
                        pq += 1
                        for kc in range(16):
                            P.mm(pmm_[pb][:], memT[:, kc, mc * 128:(mc + 1) * 128], wck[wb][:, kc, :], kc == 0, kc == 15,
                                 ["wckM%d" % wb, "memT"], ["pmM%d" % pb])
                        P.op("dve", "tensor_copy", ["pmM%d" % pb], ["vs"], out=vs[:, mc, g * 512:(g + 1) * 512], in_=pmm_[pb][:])
                P.dma("sp", kT_d[sg], kTs[:], ["kTs"], ["kT_d"])
                P.dma("sp", v_d[sg], vs[:], ["vs"], ["v_d"])
        P.emit()

        with ExitStack() as st:
            sb = lambda n, s, d=F32: st.enter_context(nc.sbuf_tensor(n, list(s), d))
            ps = lambda n, s, d=F32: st.enter_context(nc.psum_tensor(n, list(s), d))
            wgl = sb("wgl", [128, 8, 1024], BF16)
            for g_ in range(2):
                P.dma("sp", wgl[:, :, g_ * 512:(g_ + 1) * 512], w_glu_bf[g_], ["w_glu_bf"], ["wgl"])
            onesb = sb("onesbE", [128, 128], BF16)
            P.op("pool", "memset", [], ["onesbE"], onesb[:], 1.0)
            g5 = sb("g5", [128, 8])
            P.dma("sp", g5[:], s5_ncol, [], ["g5"])
            y5 = [sb("y5E%d" % i, [128, 8, 512], BF16) for i in range(2)]
            y2 = sb("y2E", [128, 8, 512])
            sig = [sb("sigE%d" % i, [128, 512]) for i in range(2)]
            sqb = [sb("sqE%d" % i, [128, 512], BF16) for i in range(2)]
            rs = sb("rsE", [128, 512])
            pg = [ps("pgE%d" % i, [128, 512]) for i in range(2)]
            pss = ps("pssE", [128, 512])
            for s in range(NST):
                t0 = s * 512
                b = s % 2
                P.dma("sp", y5[b][:], y5gT[:, t0:t0 + 512].rearrange("(f p) t -> p f t", p=128), ["y5gT"], ["y5E%d" % b])
                for n_ in range(8):
                    q = n_ % 2
                    for kc in range(8):
                        P.mm(pg[q][:], wgl[:, kc, n_ * 128:(n_ + 1) * 128], y5[b][:, kc, :], kc == 0, kc == 7, ["wgl", "y5E%d" % b], ["pgE%d" % q])
                    P.op("act", "activation", ["pgE%d" % q], ["sigE%d" % q], out=sig[q][:], in_=pg[q][:], func=AF.Sigmoid)
                    P.op("dve", "tensor_tensor", ["y5E%d" % b, "sigE%d" % q], ["y2E"], out=y2[:, n_, :], in0=y5[b][:, n_, :], in1=sig[q][:], op=ALU.mult)
                    P.op("act", "activation", ["y2E"], ["sqE%d" % q], out=sqb[q][:], in_=y2[:, n_, :], func=AF.Square)
                    P.mm(pss[:], onesb[:], sqb[q][:], n_ == 0, n_ == 7, ["onesbE", "sqE%d" % q], ["pssE"])
                P.op("dve", "tensor_scalar", ["pssE"], ["rsE"], out=rs[:], in0=pss[:], scalar1=1.0 / 1024, scalar2=EPS, op0=ALU.mult, op1=ALU.add)
                P.op("act", "activation", ["rsE"], ["rsE"], out=rs[:], in_=rs[:], func=AF.Sqrt)
                P.op("dve", "reciprocal", ["rsE"], ["rsE"], out=rs[:], in_=rs[:])
                for n_ in range(8):
                    P.op("dve", "scalar_tensor_tensor", ["y2E", "g5", "rsE"], ["y5E%d" % b], out=y5[b][:, n_, :], in0=y2[:, n_, :],
                         scalar=g5[:, n_:n_ + 1], in1=rs[:], op0=ALU.mult, op1=ALU.mult)
                P.dma("sp", y5nT[:, t0:t0 + 512].rearrange("(f p) t -> p f t", p=128), y5[b][:], ["y5E%d" % b], ["y5nT"])
        P.emit()

        NTT = LT // 128
        with ExitStack() as st:
            sb = lambda n, s, d=F32: st.enter_context(nc.sbuf_tensor(n, list(s), d))
            ps = lambda n, s, d=F32: st.enter_context(nc.psum_tensor(n, list(s), d))
            ident = sb("identE", [128, 128], BF16)
            P.op("pool", "memset", [], ["identE"], ident[:], 1.0)
            P.op("pool", "affine_select", ["identE"], ["identE"], out=ident[:], in_=ident[:], pattern=[[-1, 128]],
                 compare_op=ALU.is_equal, fill=0.0, base=0, channel_multiplier=1)
            onesb = sb("onesb2", [128, 128], BF16)
            P.op("pool", "memset", [], ["onesb2"], onesb[:], 1.0)
            gff = sb("gff", [128, 2048])
            P.dma("sp", gff[:], nffn_rep, [], ["gff"])
            wrs = sb("wrs", [128, 16, 16])
            wrb = sb("wrb", [128, 16, 16], BF16)
            P.dma("sp", wrs[:], w_router_l, [], ["wrs"])
            P.op("dve", "tensor_copy", ["wrs"], ["wrb"], out=wrb[:], in_=wrs[:])
            zrow = sb("zrow", [128, 2048])
            zrowb = sb("zrowb", [128, 2048], BF16)
            P.op("pool", "memset", [], ["zrow"], zrow[:], 0.0)
            P.op("pool", "memset", [], ["zrowb"], zrowb[:], 0.0)
            for r_ in range(0, CAPL, 128):
                for g_ in range(8):
                    P.dma("sp", acc_l[g_][LT + r_:LT + r_ + 128, :], zrow[:, 0:256], ["zrow"], ["acc"])
                P.dma("sp", hn3_bf[LT + r_:LT + r_ + 128, :], zrowb[:], ["zrowb"], ["hn3_bf"])
            wch = [sb("wch%d" % i, [128, 16, 512], BF16) for i in range(2)]
            x1 = sb("x1", [128, 4, 2048])
            ysd = sb("ysd", [128, 4, 1024], BF16)
            yT = sb("yT", [128, 16, 512], BF16)
            hn = sb("hnE", [128, 2048], BF16)
            junk = sb("junkE", [128, 2048], BF16)
            ss = sb("ssE", [128, 1])
            hnT = sb("hnT", [128, 16, 512], BF16)
            qT = sb("qT", [128, 16, 512], BF16)
            kTs = sb("kTs2", [128, 16, 256], BF16)
            vs = sb("vs2", [128, 2, 2048], BF16)
            eT = [sb("eT%d" % i, [128, 512], BF16) for i in range(2)]
            rden = sb("rden", [128, 512])
            hT3 = sb("hT3", [128, 16, 128], BF16)
            pe_ = sb("peE", [128, 16])
            psum_ = sb("psumE", [128, 1])
            ptr = [ps("ptrE%d" % i, [128, 1024], BF16) for i in range(2)]
            pmm = [ps("pmmE%d" % i, [128, 512]) for i in range(4)]
            prt = ps("prtE", [128, 512])
            tq = [0]
            pq = [0]
            wq_ = [0]
            cpe = _rr(["act", "dve"])

            def nxt(c, n):
                v = c[0] % n
                c[0] += 1
                return v

            def rms_to(tt, out_ap, names_w, extra=None):
                P.op("act", "activation", ["x1_%d" % tt], ["junkE", "ssE"], out=junk[:], in_=x1[:, tt, :], func=AF.Square, accum_out=ss[:])
                P.op("dve", "tensor_scalar", ["ssE"], ["ssE"], out=ss[:], in0=ss[:], scalar1=1.0 / D, scalar2=EPS, op0=ALU.mult, op1=ALU.add)
                P.op("act", "activation", ["ssE"], ["ssE"], out=ss[:], in_=ss[:], func=AF.Sqrt)
                P.op("dve", "reciprocal", ["ssE"], ["ssE"], out=ss[:], in_=ss[:])
                if extra is None:
                    P.op("dve", "tensor_scalar", ["x1_%d" % tt, "ssE"], names_w, out=out_ap, in0=x1[:, tt, :], scalar1=ss[:, 0:1], scalar2=None, op0=ALU.mult)
                else:
                    P.op("dve", "scalar_tensor_tensor", ["x1_%d" % tt, "ssE", "gff"], names_w, out=out_ap, in0=x1[:, tt, :], scalar=ss[:, 0:1],
                         in1=extra, op0=ALU.mult, op1=ALU.mult)

            def transposes16(src_tile, src_name, dst_fn, dst_name):
                for q4 in range(4):
                    pb = nxt(tq, 2)
                    for j in range(4):
                        kc = q4 * 4 + j
                        P.tr(ptr[pb][:, j * 128:(j + 1) * 128], src_tile[:, kc * 128:(kc + 1) * 128], ident[:], [src_name, "identE"], ["ptrE%d" % pb])
                    ce = cpe()
                    src = ptr[pb][:, 0:512].rearrange("p (j n) -> p j n", n=128)
                    if ce == "act":
                        P.op("act", "activation", ["ptrE%d" % pb], [dst_name], out=dst_fn(q4), in_=src, func=AF.Copy)
                    else:
                        P.op("dve", "tensor_copy", ["ptrE%d" % pb], [dst_name], out=dst_fn(q4), in_=src)

            def proj_tok(w_d, wname, lhs_tile, lhs_name):
                for dch in range(4):
                    wb = nxt(wq_, 2)
                    P.dma("sp", wch[wb][:], w_d[dch], [wname], ["wch%d" % wb])
                    for tt in range(4):
                        pb = nxt(pq, 4)
                        for kc in range(16):
                            P.mm(pmm[pb][:], lhs_tile[:, kc, tt * 128:(tt + 1) * 128], wch[wb][:, kc, :], kc == 0, kc == 15,
                                 [lhs_name, "wch%d" % wb], ["pmmE%d" % pb])
                        P.op("dve", "tensor_tensor", ["x1_%d" % tt, "pmmE%d" % pb], ["x1_%d" % tt], out=x1[:, tt, dch * 512:(dch + 1) * 512],
                             in0=x1[:, tt, dch * 512:(dch + 1) * 512], in1=pmm[pb][:], op=ALU.add)

            cur_seg = -1
            for s in range(NST):
                t0 = s * 512
                sg = t0 // LSEG
                if sg != cur_seg:
                    cur_seg = sg
                    P.dma("sp", kTs[:], kT_d[sg], ["kT_d"], ["kTs2"])
                    P.dma("sp", vs[:], v_d[sg], ["v_d"], ["vs2"])
                for tt in range(4):
                    P.dma("sp", x1[:, tt, :], x_in[t0 + tt * 128:t0 + (tt + 1) * 128, :], [], ["x1_%d" % tt])
                P.dma("sp", ysd[:], yssd[t0:t0 + 512, :].rearrange("(t p) c -> p t c", p=128), ["yssd"], ["ysd"])
                P.dma("sp", yT[:, 8:16, :], y5nT[:, t0:t0 + 512].rearrange("(f p) t -> p f t", p=128), ["y5nT"], ["yT"])
                for tt in range(4):
                    for q2 in range(2):
                        pb = nxt(tq, 2)
                        for j in range(4):
                            kc = q2 * 4 + j
                            P.tr(ptr[pb][:, j * 128:(j + 1) * 128], ysd[:, tt, kc * 128:(kc + 1) * 128], ident[:], ["ysd", "identE"], ["ptrE%d" % pb])
                        P.op("act", "activation", ["ptrE%d" % pb], ["yT"], out=yT[:, q2 * 4:(q2 + 1) * 4, tt * 128:(tt + 1) * 128],
                             in_=ptr[pb][:, 0:512].rearrange("p (j n) -> p j n", n=128), func=AF.Copy)
                proj_tok(w_out_bf, "w_out_bf", yT, "yT")
                for tt in range(4):
                    rms_to(tt, hn[:], ["hnE"])
                    transposes16(hn, "hnE", lambda q4, tt=tt: hnT[:, q4 * 4:(q4 + 1) * 4, tt * 128:(tt + 1) * 128], "hnT")
                for g in range(4):
                    wb = nxt(wq_, 2)
                    P.dma("sp", wch[wb][:], w_q_bf[g], ["w_q_bf"], ["wch%d" % wb])
                    for mcc in range(4):
                        pb = nxt(pq, 4)
                        for kc in range(16):
                            P.mm(pmm[pb][:], wch[wb][:, kc, mcc * 128:(mcc + 1) * 128], hnT[:, kc, :], kc == 0, kc == 15,
                                 ["wch%d" % wb, "hnT"], ["pmmE%d" % pb])
                        P.op("act", "activation", ["pmmE%d" % pb], ["qT"], out=qT[:, g * 4 + mcc, :], in_=pmm[pb][:], func=AF.Copy)
                for h in range(4):
                    for mck in range(2):
                        pb = nxt(pq, 4)
                        for dc in range(4):
                            P.mm(pmm[pb][:], kTs[:, h * 4 + dc, mck * 128:(mck + 1) * 128], qT[:, h * 4 + dc, :], dc == 0, dc == 3,
                                 ["kTs2", "qT"], ["pmmE%d" % pb])
                        P.op("act", "activation", ["pmmE%d" % pb], ["eT%d" % mck], out=eT[mck][:], in_=pmm[pb][:], func=AF.Exp, scale=512 ** -0.5)
                    pb = nxt(pq, 4)
                    for mck in range(2):
                        P.mm(pmm[pb][:], onesb[:], eT[mck][:], mck == 0, mck == 1, ["onesb2", "eT%d" % mck], ["pmmE%d" % pb])
                    P.op("dve", "reciprocal", ["pmmE%d" % pb], ["rden"], out=rden[:], in_=pmm[pb][:])
                    for dvc in range(4):
                        pb = nxt(pq, 4)
                        for mck in range(2):
                            P.mm(pmm[pb][:], vs[:, mck, h * 512 + dvc * 128:h * 512 + (dvc + 1) * 128], eT[mck][:], mck == 0, mck == 1,
                                 ["vs2", "eT%d" % mck], ["pmmE%d" % pb])
                        P.op("dve", "tensor_tensor", ["pmmE%d" % pb, "rden"], ["hnT"], out=hnT[:, h * 4 + dvc, :], in0=pmm[pb][:], in1=rden[:], op=ALU.mult)
                proj_tok(w_o_bf, "w_o_bf", hnT, "hnT")
                for tt in range(4):
                    r0 = t0 + tt * 128
                    for g_ in range(8):
                        P.dma("sp", acc_l[g_][r0:r0 + 128, :], x1[:, tt, g_ * 256:(g_ + 1) * 256], ["x1_%d" % tt], ["acc"])
                    rms_to(tt, hn[:], ["hnE"], extra=gff[:])
                    P.dma("sp", hn3_bf[r0:r0 + 128, :], hn[:], ["hnE"], ["hn3_bf"])
                    transposes16(hn, "hnE", lambda q4: hT3[:, q4 * 4:(q4 + 1) * 4, :], "hT3")
                    for kc in range(16):
                        P.mm(prt[:, 0:16], hT3[:, kc, :], wrb[:, kc, :], kc == 0, kc == 15, ["hT3", "wrb"], ["prtE"])
                    P.op("act", "activation", ["prtE"], ["peE", "psumE"], out=pe_[:], in_=prt[:, 0:16], func=AF.Exp, accum_out=psum_[:])
                    P.op("dve", "reciprocal", ["psumE"], ["psumE"], out=psum_[:], in_=psum_[:])
                    P.op("dve", "tensor_scalar", ["peE", "psumE"], ["peE"], out=pe_[:], in0=pe_[:], scalar1=psum_[:, 0:1], scalar2=None, op0=ALU.mult)
                    P.dma("sp", probs_loc[r0:r0 + 128, :], pe_[:], ["peE"], ["probs_loc"])
        P.emit()

        GSZ = cfg.get("GSZ", 1)
        NG = GSZ * LT
        JG = NG // 128
        CAPG = NG // 8
        NCHK = CAPL // 128
        if GSZ > 1:
            P.add("pool", lambda e: e.collective_compute("AllGather", ALU.bypass, replica_groups=cfg["groups"], ins=[probs_loc], outs=[probs_all]),
                  reads=["probs_loc"], writes=["probs_all"], own_sem=P.csem)
            pall = probs_all
        else:
            pall = probs_loc
        with ExitStack() as st:
            sb = lambda n, s, d=F32: st.enter_context(nc.sbuf_tensor(n, list(s), d))
            ps = lambda n, s, d=F32: st.enter_context(nc.psum_tensor(n, list(s), d))
            PA = sb("PA", [128, JG, 16])
            cmpt = sb("cmpt", [128, JG, 16])
            P.dma("sp", PA[:], pall.rearrange("(p j) e -> p j e", j=JG), ["probs_all", "probs_loc"], ["PA"])
            onesF = sb("onesFF", [128, 128])
            P.op("pool", "memset", [], ["onesFF"], onesF[:], 1.0)
            lo, hi, mid, ge, dd, cntp = [sb("bs_" + n_, [128, 16]) for n_ in ("lo", "hi", "mid", "ge", "dd", "cntp")]
            P.op("dve", "memset", [], ["bs_lo"], lo[:], 0.0)
            P.op("dve", "memset", [], ["bs_hi"], hi[:], 1.0)
            ptot = ps("ptot", [128, 512])
            for it_ in range(32):
                P.op("dve", "tensor_tensor", ["bs_lo", "bs_hi"], ["bs_mid"], out=mid[:], in0=lo[:], in1=hi[:], op=ALU.add)
                P.op("dve", "tensor_scalar", ["bs_mid"], ["bs_mid"], out=mid[:], in0=mid[:], scalar1=0.5, scalar2=None, op0=ALU.mult)
                P.op("dve", "tensor_tensor", ["PA", "bs_mid"], ["cmpt"], out=cmpt[:], in0=PA[:], in1=mid[:].unsqueeze(1).to_broadcast([128, JG, 16]), op=ALU.is_ge)
                P.op("dve", "tensor_reduce", ["cmpt"], ["bs_cntp"], out=cntp[:], in_=cmpt[:].rearrange("p j e -> p e j"), op=ALU.add, axis=AX.X)
                P.mm(ptot[:, 0:16], onesF[:], cntp[:], True, True, ["onesFF", "bs_cntp"], ["ptot"])
                P.op("dve", "tensor_scalar", ["ptot"], ["bs_ge"], out=ge[:], in0=ptot[:, 0:16], scalar1=float(CAPG), scalar2=None, op0=ALU.is_ge)
                P.op("dve", "tensor_tensor", ["bs_mid", "bs_lo"], ["bs_dd"], out=dd[:], in0=mid[:], in1=lo[:], op=ALU.subtract)
                P.op("dve", "tensor_tensor", ["bs_dd", "bs_ge"], ["bs_dd"], out=dd[:], in0=dd[:], in1=ge[:], op=ALU.mult)
                P.op("dve", "tensor_tensor", ["bs_lo", "bs_dd"], ["bs_lo"], out=lo[:], in0=lo[:], in1=dd[:], op=ALU.add)
                P.op("dve", "tensor_tensor", ["bs_hi", "bs_mid"], ["bs_dd"], out=dd[:], in0=hi[:], in1=mid[:], op=ALU.subtract)
                P.op("dve", "tensor_tensor", ["bs_dd", "bs_ge"], ["bs_dd"], out=dd[:], in0=dd[:], in1=ge[:], op=ALU.mult)
                P.op("dve", "tensor_tensor", ["bs_mid", "bs_dd"], ["bs_hi"], out=hi[:], in0=mid[:], in1=dd[:], op=ALU.add)
            PL = sb("PL", [128, NTT, 16])
            msk = sb("msk", [128, NTT, 16])
            gat = sb("gat", [128, NTT, 16])
            mskb = sb("mskb", [128, NTT, 16], BF16)
            P.dma("sp", PL[:], probs_loc.rearrange("(t p) e -> p t e", p=128), ["probs_loc"], ["PL"])
            P.op("dve", "tensor_tensor", ["PL", "bs_lo"], ["msk"], out=msk[:], in0=PL[:], in1=lo[:].unsqueeze(1).to_broadcast([128, NTT, 16]), op=ALU.is_ge)
            P.op("dve", "tensor_tensor", ["PL", "msk"], ["gat"], out=gat[:], in0=PL[:], in1=msk[:], op=ALU.mult)
            P.op("dve", "tensor_copy", ["msk"], ["mskb"], out=mskb[:], in_=msk[:])
            triS = sb("triS", [128, 128], BF16)
            onesb = sb("onesbF", [128, 128], BF16)
            P.op("pool", "memset", [], ["onesbF"], onesb[:], 1.0)
            P.op("pool", "memset", [], ["triS"], triS[:], 1.0)
            P.op("pool", "affine_select", ["triS"], ["triS"], out=triS[:], in_=triS[:], pattern=[[1, 128]], compare_op=ALU.is_ge, fill=0.0,
                 base=-1, channel_multiplier=-1)
            posi = sb("posi", [128, NTT, 16])
            tots = sb("tots", [128, NTT, 16])
            cum = sb("cum", [128, NTT, 16])
            pps = [ps("ppsF%d" % i, [128, 512]) for i in range(2)]
            mflat = mskb[:].rearrange("p t e -> p (t e)")
            NTE = NTT * 16
            for c0 in range(0, NTE, 512):
                c1 = min(c0 + 512, NTE)
                P.mm(pps[0][:, 0:c1 - c0], triS[:], mflat[:, c0:c1], True, True, ["triS", "mskb"], ["ppsF0"])
                P.op("dve", "tensor_copy", ["ppsF0"], ["posi"], out=posi[:].rearrange("p t e -> p (t e)")[:, c0:c1], in_=pps[0][:, 0:c1 - c0])
                P.mm(pps[1][:, 0:c1 - c0], onesb[:], mflat[:, c0:c1], True, True, ["onesbF", "mskb"], ["ppsF1"])
                P.op("dve", "tensor_copy", ["ppsF1"], ["tots"], out=tots[:].rearrange("p t e -> p (t e)")[:, c0:c1], in_=pps[1][:, 0:c1 - c0])
            onecol = sb("onecol", [128, 1])
            P.op("dve", "memset", [], ["onecol"], onecol[:], 1.0)
            for e_ in range(16):
                P.op("dve", "tensor_tensor_scan", ["tots", "onecol"], ["cum"], out=cum[:, :, e_], data0=onecol[:, 0:1].to_broadcast([128, NTT]),
                     data1=tots[:, :, e_], initial=0.0, op0=ALU.mult, op1=ALU.add)
            P.op("dve", "tensor_tensor", ["cum", "tots"], ["cum"], out=cum[:], in0=cum[:], in1=tots[:], op=ALU.subtract)
            P.op("dve", "tensor_tensor", ["cum", "posi"], ["posi"], out=posi[:], in0=posi[:], in1=cum[:], op=ALU.add)
            P.op("dve", "tensor_scalar", ["posi"], ["posi"], out=posi[:], in0=posi[:], scalar1=1.0, scalar2=None, op0=ALU.add)
            P.op("dve", "tensor_tensor", ["posi", "msk"], ["posi"], out=posi[:], in0=posi[:], in1=msk[:], op=ALU.mult)
            P.op("dve", "tensor_scalar", ["posi"], ["posi"], out=posi[:], in0=posi[:], scalar1=-1.0, scalar2=None, op0=ALU.add)
            G5 = sb("G5", [128, NTT, 16, 8], BF16)
            P.op("pool", "memset", [], ["G5"], G5[:], 0.0)
            tvf = sb("tvf", [128, NTT, 16])
            P.op("pool", "iota", [], ["tvf"], tvf[:], pattern=[[0, NTT], [0, 16]], base=0, channel_multiplier=1, allow_small_or_imprecise_dtypes=True)
            P.op("dve", "tensor_copy", ["tvf", "G5"], ["G5"], out=G5[:, :, :, 0], in_=tvf[:])
            P.op("pool", "iota", ["tvf"], ["tvf"], tvf[:], pattern=[[128, NTT], [0, 16]], base=0, channel_multiplier=0, allow_small_or_imprecise_dtypes=True)
            P.op("dve", "tensor_copy", ["tvf", "G5"], ["G5"], out=G5[:, :, :, 1], in_=tvf[:])
            P.op("dve", "memset", ["G5"], ["G5"], G5[:, :, :, 5], 1.0)
            gres = sb("gres", [128, NTT, 16])
            P.op("dve", "tensor_copy", ["gat", "G5"], ["G5"], out=G5[:, :, :, 2], in_=gat[:])
            P.op("dve", "tensor_tensor", ["gat", "G5"], ["gres"], out=gres[:], in0=gat[:], in1=G5[:, :, :, 2], op=ALU.subtract)
            P.op("dve", "tensor_copy", ["gres", "G5"], ["G5"], out=G5[:, :, :, 3], in_=gres[:])
            P.op("dve", "tensor_tensor", ["gres", "G5"], ["gres"], out=gres[:], in0=gres[:], in1=G5[:, :, :, 3], op=ALU.subtract)
            P.op("dve", "tensor_copy", ["gres", "G5"], ["G5"], out=G5[:, :, :, 4], in_=gres[:])
            iotaS = sb("iotaS", [128, CAPL])
            P.op("pool", "iota", [], ["iotaS"], iotaS[:], pattern=[[1, CAPL]], base=0, channel_multiplier=0, allow_small_or_imprecise_dtypes=True)
            dmy = sb("dmy", [128, NCHK])
            P.op("pool", "iota", [], ["dmy"], dmy[:], pattern=[[128, NCHK]], base=LT, channel_multiplier=1, allow_small_or_imprecise_dtypes=True)
            oh = [sb("oh%d" % i, [128, CAPL], BF16) for i in range(2)]
            pI = [ps("pI%d" % i, [128, 64, 8]) for i in range(2)]
            rr_ = sb("rrF", [128, NCHK, 8])
            ia = sb("iaF", [128, NCHK])
            ii = sb("iiF", [128, NCHK], I32)
            gs = sb("gsF", [128, NCHK])
            ohq = 0
            for e_ in range(16):
                pb = e_ % 2
                first = True
                for t in range(NTT):
                    ob = ohq % 2
                    ohq += 1
                    P.op("dve", "tensor_scalar", ["iotaS", "posi"], ["oh%d" % ob], out=oh[ob][:], in0=iotaS[:], scalar1=posi[:, t, e_:e_ + 1],
                         scalar2=None, op0=ALU.is_equal)
                    for ch in range(NCHK):
                        P.add("pe", (lambda pb, ch, ob, t, e_, first, lastf: lambda e: e.matmul(
                            pI[pb][:, ch, :], lhsT=oh[ob][:, ch * 128:(ch + 1) * 128], rhs=G5[:, t, e_, :], start=first, stop=lastf,
                            skip_group_check=True))(pb, ch, ob, t, e_, first, (t == NTT - 1 and ch == NCHK - 1)),
                            reads=["oh%d" % ob, "G5"], writes=["pI%d" % pb], banks=["pI%d" % pb])
                        first = False
                P.op("dve", "tensor_copy", ["pI%d" % pb], ["rrF"], out=rr_[:], in_=pI[pb][:, 0:NCHK, :])
                P.op("dve", "tensor_tensor", ["rrF"], ["iaF"], out=ia[:], in0=rr_[:, :, 0], in1=rr_[:, :, 1], op=ALU.add)
                P.op("dve", "tensor_tensor", ["iaF", "dmy"], ["iaF"], out=ia[:], in0=ia[:], in1=dmy[:], op=ALU.subtract)
                P.op("dve", "tensor_tensor", ["iaF", "rrF"], ["iaF"], out=ia[:], in0=ia[:], in1=rr_[:, :, 5], op=ALU.mult)
                P.op("dve", "tensor_tensor", ["iaF", "dmy"], ["iaF"], out=ia[:], in0=ia[:], in1=dmy[:], op=ALU.add)
                P.op("dve", "tensor_copy", ["iaF"], ["iiF"], out=ii[:], in_=ia[:])
                P.op("dve", "tensor_tensor", ["rrF"], ["gsF"], out=gs[:], in0=rr_[:, :, 2], in1=rr_[:, :, 3], op=ALU.add)
                P.op("dve", "tensor_tensor", ["rrF", "gsF"], ["gsF"], out=gs[:], in0=gs[:], in1=rr_[:, :, 4], op=ALU.add)
                P.dma("sp", idx_d[e_], ii[:], ["iiF"], ["idx_d"])
                P.dma("sp", cnt_out[e_], ii[:], ["iiF"], ["cnt_out"])
                P.dma("sp", gate_d[e_], gs[:], ["gsF"], ["gate_d"])
        P.emit()

        with ExitStack() as st:
            sb = lambda n, s, d=F32: st.enter_context(nc.sbuf_tensor(n, list(s), d))
            ps = lambda n, s, d=F32: st.enter_context(nc.psum_tensor(n, list(s), d))
            ident = sb("identX", [128, 128], BF16)
            P.op("pool", "memset", [], ["identX"], ident[:], 1.0)
            P.op("pool", "affine_select", ["identX"], ["identX"], out=ident[:], in_=ident[:], pattern=[[-1, 128]],
                 compare_op=ALU.is_equal, fill=0.0, base=0, channel_multiplier=1)
            idxt = [sb("idxt%d" % i, [128, NCHK], I32) for i in range(2)]
            gtt = [sb("gtt%d" % i, [128, NCHK]) for i in range(2)]
            xe = [sb("xe%d" % i, [128, 2048], BF16) for i in range(2)]
            xeT = sb("xeT", [128, 16, CAPL], BF16)
            heT = sb("heT", [128, 16, CAPL], BF16)
            stw = sb("stw", [128, 16, 256])
            wb_ = [sb("wbX%d" % i, [128, 16, 256], BF16) for i in range(4)]
            sgl = [sb("sglX%d" % i, [128, 512]) for i in range(2)]
            yst = [sb("ystX%d" % i, [128, 256], BF16) for i in range(3)]
            ptr = [ps("ptrX%d" % i, [128, 1024], BF16) for i in range(2)]
            pg = [ps("pgX%d" % i, [128, 512]) for i in range(2)]
            pu = [ps("puX%d" % i, [128, 512]) for i in range(2)]
            py = [ps("pyX%d" % i, [128, 512]) for i in range(2)]
            cgs = [(c0, min(c0 + 512, CAPL)) for c0 in range(0, CAPL, 512)]
            cnt = {"tq": 0, "wq": 0, "gq": 0, "yq": 0, "sq": 0, "ysq": 0, "cast": 0, "xq": 0}

            def nx(k_, n):
                v = cnt[k_] % n
                cnt[k_] += 1
                return v

            def load_w(src_ap, name):
                wi = nx("wq", 4)
                P.dma("sp", stw[:], src_ap, [], ["stw"])
                ce = ("act", "dve")[nx("cast", 2)]
                if ce == "act":
                    P.op("act", "activation", ["stw"], ["wbX%d" % wi], out=wb_[wi][:], in_=stw[:], func=AF.Copy)
                else:
                    P.op("dve", "tensor_copy", ["stw"], ["wbX%d" % wi], out=wb_[wi][:], in_=stw[:])
                return wi

            prev_sc = {}
            for e_ in range(cfg.get("nexp", 16)):
                ib = e_ % 2
                P.dma("sp", idxt[ib][:], idx_d[e_], ["idx_d"], ["idxt%d" % ib])
                P.dma("sp", gtt[ib][:], gate_d[e_], ["gate_d"], ["gtt%d" % ib])
                for ch in range(NCHK):
                    xb = nx("xq", 2)
                    P.add("pool", (lambda xb, ib, ch: lambda e: e.indirect_dma_start(
                        out=xe[xb][:], out_offset=None, in_=hn3_bf[:, :],
                        in_offset=bass.IndirectOffsetOnAxis(ap=idxt[ib][:, ch:ch + 1], axis=0)))(xb, ib, ch),
                        reads=["idxt%d" % ib, "hn3_bf"], writes=["xe%d" % xb], dma=True)
                    for q4 in range(4):
                        pb = nx("tq", 2)
                        for j in range(4):
                            kc = q4 * 4 + j
                            P.tr(ptr[pb][:, j * 128:(j + 1) * 128], xe[xb][:, kc * 128:(kc + 1) * 128], ident[:], ["xe%d" % xb, "identX"], ["ptrX%d" % pb])
                        o_ap = xeT[:, q4 * 4:(q4 + 1) * 4, ch * 128:(ch + 1) * 128]
                        i_ap = ptr[pb][:, 0:512].rearrange("p (j n) -> p j n", n=128)
                        if q4 % 2 == 0:
                            P.op("act", "activation", ["ptrX%d" % pb], ["xeT"], out=o_ap, in_=i_ap, func=AF.Copy)
                        else:
                            P.op("dve", "tensor_copy", ["ptrX%d" % pb], ["xeT"], out=o_ap, in_=i_ap)
                for fg in range(8):
                    wg_i = load_w(w_gate[e_].rearrange("(kc p) f -> p kc f", p=128)[:, :, fg * 256:(fg + 1) * 256], "g")
                    wu_i = load_w(w_up[e_].rearrange("(kc p) f -> p kc f", p=128)[:, :, fg * 256:(fg + 1) * 256], "u")
                    for fc2 in range(2):
                        for (c0, c1) in cgs:
                            gb = nx("gq", 2)
                            for kc in range(16):
                                P.mm(pg[gb][:, 0:c1 - c0], wb_[wg_i][:, kc, fc2 * 128:(fc2 + 1) * 128], xeT[:, kc, c0:c1], kc == 0, kc == 15,
                                     ["wbX%d" % wg_i, "xeT"], ["pgX%d" % gb])
                            for kc in range(16):
                                P.mm(pu[gb][:, 0:c1 - c0], wb_[wu_i][:, kc, fc2 * 128:(fc2 + 1) * 128], xeT[:, kc, c0:c1], kc == 0, kc == 15,
                                     ["wbX%d" % wu_i, "xeT"], ["puX%d" % gb])
                            sb_i = nx("sq", 2)
                            P.op("act", "activation", ["pgX%d" % gb], ["sglX%d" % sb_i], out=sgl[sb_i][:, 0:c1 - c0], in_=pg[gb][:, 0:c1 - c0], func=AF.Silu)
                            P.op("dve", "tensor_tensor", ["sglX%d" % sb_i, "puX%d" % gb], ["heT"], out=heT[:, fg * 2 + fc2, c0:c1], in0=sgl[sb_i][:, 0:c1 - c0],
                                 in1=pu[gb][:, 0:c1 - c0], op=ALU.mult)
                new_sc = {}
                for dg in range(8):
                    wd_i = load_w(w_down[e_].rearrange("(kc p) f -> p kc f", p=128)[:, :, dg * 256:(dg + 1) * 256], "d")
                    for st_ in range(NCHK):
                        yb = nx("yq", 2)
                        for fc in range(16):
                            P.mm(py[yb][:, 0:256], heT[:, fc, st_ * 128:(st_ + 1) * 128], wb_[wd_i][:, fc, :], fc == 0, fc == 15,
                                 ["heT", "wbX%d" % wd_i], ["pyX%d" % yb])
                        ys = nx("ysq", 3)
                        P.op("dve", "tensor_scalar", ["pyX%d" % yb, "gtt%d" % ib], ["ystX%d" % ys], out=yst[ys][:], in0=py[yb][:, 0:256],
                             scalar1=gtt[ib][:, st_:st_ + 1], scalar2=None, op0=ALU.mult)
                        nm_ = "sc_%d_%d_%d" % (e_, dg, st_)
                        new_sc.setdefault(dg, []).append(nm_)
                        P.add("pool", (lambda ys, ib, st_, dg: lambda e: e.indirect_dma_start(
                            out=acc_l[dg][:, :], out_offset=bass.IndirectOffsetOnAxis(ap=idxt[ib][:, st_:st_ + 1], axis=0),
                            in_=yst[ys][:], in_offset=None, compute_op=ALU.add))(ys, ib, st_, dg),
                            reads=["ystX%d" % ys, "idxt%d" % ib, "acc_d"] + prev_sc.get(dg, []), writes=[nm_], dma=True)
                prev_sc = new_sc
        P.emit()

        with ExitStack() as st:
            sb = lambda n, s, d=F32: st.enter_context(nc.sbuf_tensor(n, list(s), d))
            gfin = sb("gfin", [128, 2048])
            P.dma("sp", gfin[:], nfin_rep, [], ["gfin"])
            xa = [sb("xaG%d" % i, [128, 8, 256]) for i in range(2)]
            junk = sb("junkG", [128, 2048], BF16)
            ss = [sb("ssG%d" % i, [128, 1]) for i in range(2)]
            yo_ = [sb("yoG%d" % i, [128, 2048]) for i in range(2)]
            for t in range(NTT):
                b = t % 2
                r0 = t * 128
                for g_ in range(8):
                    P.dma("sp", xa[b][:, g_, :], acc_l[g_][r0:r0 + 128, :], [], ["xaG%d" % b])
                xf = xa[b][:].rearrange("p g c -> p (g c)")
                P.op("act", "activation", ["xaG%d" % b], ["junkG", "ssG%d" % b], out=junk[:], in_=xf, func=AF.Square, accum_out=ss[b][:])
                P.op("dve", "tensor_scalar", ["ssG%d" % b], ["ssG%d" % b], out=ss[b][:], in0=ss[b][:], scalar1=1.0 / D, scalar2=EPS, op0=ALU.mult, op1=ALU.add)
                P.op("act", "activation", ["ssG%d" % b], ["ssG%d" % b], out=ss[b][:], in_=ss[b][:], func=AF.Sqrt)
                P.op("dve", "reciprocal", ["ssG%d" % b], ["ssG%d" % b], out=ss[b][:], in_=ss[b][:])
                P.op("dve", "scalar_tensor_tensor", ["xaG%d" % b, "ssG%d" % b, "gfin"], ["yoG%d" % b], out=yo_[b][:], in0=xf, scalar=ss[b][:, 0:1],
                     in1=gfin[:], op0=ALU.mult, op1=ALU.mult)
                P.dma("sp", y_out[r0:r0 + 128, :], yo_[b][:], ["yoG%d" % b], ["y_out"])
        P.emit()

        if dbg and upto not in ("all",):
            for nm_, ap_ in dbg.items():
                src = {"o_xbcT": xbcT, "o_uT": uT, "o_z": z_tok, "o_dt": dt_tok, "o_yssd": yssd, "o_yf": yf, "o_y5gT": y5gT, "o_y5nT": y5nT, "o_kT": kT_d, "o_v": v_d, "o_acc": acc_l[0], "o_probs": probs_loc, "o_hn3": hn3_bf, "o_idx": idx_d, "o_gate": gate_d}[nm_]
                P.dma("sp", ap_, src, [], [nm_])
            P.emit()
    return nc


def _rep(v):
    v = np.asarray(v, np.float32)
    return np.ascontiguousarray(np.broadcast_to(v.reshape(1, -1), (128, v.size)))


def _colpk(v):
    v = np.asarray(v, np.float32)
    return np.ascontiguousarray(v.reshape(-1, 128).T)


def _prep_weights(W):
    o = {}
    o["w_in"] = np.ascontiguousarray(W["w_in"][0])
    o["norm_mix"] = _colpk(W["norm_mix"][0])
    o["conv_w"] = np.ascontiguousarray(W["conv_w"][0].reshape(3, 16, 128).transpose(2, 1, 0))
    o["conv_b"] = _colpk(W["conv_b"][0])
    o["dtb_rep"] = _rep(W["ssd_dt_bias"][0])
    o["alog_rep"] = _rep(W["ssd_a_log"][0])
    o["ssdd_rep"] = _rep(W["ssd_d"][0])
    o["ssdn_rep"] = _rep(W["ssd_norm"][0])

    def gp(a):
        a = a.reshape(2, 8, 8, 64).transpose(0, 1, 3, 2)
        return np.concatenate([a, a], axis=2)
    are = gp(W["s5_a_re"][0])
    aim = gp(W["s5_a_im"][0])
    lst = gp(np.broadcast_to(W["s5_log_step"][0][:, :, None], (2, 64, 64)))
    o["s5_small"] = np.ascontiguousarray(np.stack([are, aim, lst], axis=3)).astype(np.float32)

    def bb(a):
        a = a.reshape(2, 8, 8, 64, 16).transpose(0, 1, 3, 2, 4).reshape(2, 8, 64, 128)
        return np.concatenate([a, a], axis=2)

    def cc(a):
        a = a.reshape(2, 8, 8, 16, 64).transpose(0, 1, 4, 2, 3).reshape(2, 8, 64, 128)
        return np.concatenate([a, a], axis=2)
    o["s5_big"] = np.ascontiguousarray(np.stack([bb(W["s5_b_re"][0]), bb(W["s5_b_im"][0]), cc(W["s5_c_re"][0]),
                                                 cc(W["s5_c_im"][0])], axis=3)).astype(np.float32)
    o["s5_dcol"] = _colpk(W["s5_d"][0])
    o["s5_ncol"] = _colpk(W["s5_norm"][0])
    o["norm_attn"] = _colpk(W["norm_attn"][0])
    o["norm_mem"] = _colpk(W["norm_mem"][0])
    for k_ in ("w_out", "w_q", "w_k", "w_v", "w_o"):
        o[k_] = np.ascontiguousarray(W[k_][0])
    o["nffn_rep"] = _rep(W["norm_ffn"][0])
    o["w_router_l"] = np.ascontiguousarray(W["w_router"][0].reshape(16, 128, 16).transpose(1, 0, 2))
    o["nfin_rep"] = _rep(W["norm_final"])
    o["w_glu"] = np.ascontiguousarray(W["s5_w_glu"][0])
    o["w_gate"] = np.ascontiguousarray(W["w_gate"][0])
    o["w_up"] = np.ascontiguousarray(W["w_up"][0])
    o["w_down"] = np.ascontiguousarray(W["w_down"][0])
    return o


_NC_CACHE = {}


def kernel(**inputs):
    W = {k_: np.asarray(v, np.float32) for k_, v in inputs.items()}
    xp, xs = W.pop("x_prompt"), W.pop("x_sample")
    mp, ms = W.pop("mem_prompt"), W.pop("mem_sample")
    LSEG = 4096
    cfg = {"LSEG": LSEG, "upto": "all", "CAPL": 1408, "GSZ": 4, "groups": [[0, 1, 2, 3], [4, 5, 6, 7]]}
    if "nc" not in _NC_CACHE:
        _NC_CACHE["nc"] = build(cfg)
    nc = _NC_CACHE["nc"]
    wl = _prep_weights(W)
    in_maps = []
    for c in range(8):
        m = dict(wl)
        if c < 4:
            m["x"] = np.ascontiguousarray(xp[c])
            m["mem"] = np.ascontiguousarray(np.stack([mp[c], mp[c]]))
            m["flag"] = np.ones((128, 1), np.float32)
        else:
            j = c - 4
            m["x"] = np.ascontiguousarray(xs[2 * j:2 * j + 2].reshape(2 * LSEG, D))
            m["mem"] = np.ascontiguousarray(ms[2 * j:2 * j + 2])
            m["flag"] = np.zeros((128, 1), np.float32)
        in_maps.append(m)
    res = run_bass_kernel_spmd(nc, in_maps, core_ids=list(range(8)))
    ys = [np.asarray(r["y"], np.float32) for r in res.results]
    try:
        import sys
        cn = np.array([[(np.asarray(r["slot_idx"])[e] < 2 * LSEG).sum() for e in range(16)] for r in res.results])
        print("[kernel] per-(core,expert) slot counts: mean %.1f std %.1f max %d (cap %d)" % (cn.mean(), cn.std(), cn.max(), cfg["CAPL"]), file=sys.stderr)
    except Exception:
        pass
    y_prompt = np.stack(ys[0:4]).reshape(4, 8192, D)
    y_sample = np.concatenate([y.reshape(2, LSEG, D) for y in ys[4:8]], axis=0)
    return (y_prompt, y_sample)
```

```python
import numpy as np
from contextlib import ExitStack
import concourse.bass as bass
import concourse.mybir as mybir
from concourse.bass_utils import run_bass_kernel_spmd

F32 = mybir.dt.float32
BF16 = mybir.dt.bfloat16
I32 = mybir.dt.int32
U32 = mybir.dt.uint32
ALU = mybir.AluOpType
AF = mybir.ActivationFunctionType
AX = mybir.AxisListType

ENGS = ("pe", "act", "dve", "pool", "sp")
NDS = 24

D = 2048
EPS = 1e-6
INC = 4112


class Op:
    __slots__ = ("eng", "fn", "reads", "writes", "dma", "sem", "val", "waits", "desc", "inc")

    def __init__(self, eng, fn, reads, writes, dma):
        self.eng = eng
        self.fn = fn
        self.reads = tuple(reads)
        self.writes = tuple(writes)
        self.dma = dma
        self.waits = {}


class Prog:
    def __init__(self, nc, stack):
        self.nc = nc
        self.ops = []
        self.last_writer = {}
        self.readers = {}
        self.cnt = {e: 0 for e in ENGS}
        self.dcnt = {e: 0 for e in ENGS}
        self.sem = {e: stack.enter_context(nc.semaphore("s_" + e)) for e in ENGS if e != "sp"}
        self.dsem = {
            e: [stack.enter_context(nc.semaphore("d_%s_%d" % (e, i))) for i in range(NDS)]
            for e in ("sp", "act", "pool")
        }
        self.waited = {e: {} for e in ENGS}
        self.semobj = {}
        self.limit = None
        self.bank_last = {}
        self.csem = stack.enter_context(nc.semaphore("s_coll"))
        self.capture = None

    def add(self, eng, fn, reads=(), writes=(), dma=False, banks=(), own_sem=None):
        if self.capture is not None:
            self.capture.append((eng, fn, tuple(reads), tuple(writes), dma, tuple(banks), own_sem))
            return None
        if self.limit is not None and len(self.ops) >= self.limit:
            return None
        op = Op(eng, fn, reads, writes, dma)
        deps = set()
        for b in op.reads:
            w = self.last_writer.get(b)
            if w is not None:
                deps.add(w)
        for b in op.writes:
            w = self.last_writer.get(b)
            if w is not None:
                deps.add(w)
            for r in self.readers.get(b, ()):
                if r.eng == eng and not r.dma and not dma:
                    continue
                deps.add(r)
        for tok in banks:
            pd = self.bank_last.get(tok)
            if pd is not None and pd.eng != eng:
                deps.add(pd)
            self.bank_last[tok] = op
        for b in op.reads:
            self.readers.setdefault(b, []).append(op)
        for b in op.writes:
            self.last_writer[b] = op
            self.readers[b] = []
        prewait = None
        op.inc = 16 if dma else 1
        if own_sem is not None:
            op.sem = own_sem
            op.val = 1
            op.inc = 1
        elif dma:
            i = self.dcnt[eng]
            self.dcnt[eng] += 1
            op.sem = self.dsem[eng][i % NDS]
            op.val = 16 * (i // NDS + 1)
            if i >= NDS:
                prewait = (op.sem, 16 * (i // NDS))
        else:
            self.cnt[eng] += 1
            op.sem = self.sem[eng]
            op.val = self.cnt[eng]
        for d in deps:
            if d is op:
                continue
            if d.eng == "pe" and eng == "pe" and not d.dma and not dma:
                continue
            k = id(d.sem)
            self.semobj[k] = d.sem
            if op.waits.get(k, 0) < d.val:
                op.waits[k] = d.val
        if prewait is not None:
            k = id(prewait[0])
            self.semobj[k] = prewait[0]
            if op.waits.get(k, 0) < prewait[1]:
                op.waits[k] = prewait[1]
        self.ops.append(op)
        return op

    @staticmethod
    def _banks(aps):
        toks = []
        for a in aps:
            sp = getattr(a, "space", None)
            if sp is not None and str(sp).endswith("PSUM"):
                toks.append(a.tensor.name)
        return toks

    def op(self, eng, method, reads, writes, *args, **kw):
        o = self.add(eng, lambda e: getattr(e, method)(*args, **kw), reads, writes,
                     banks=self._banks(list(args) + list(kw.values())))
        return o

    def dma(self, eng, out, in_, reads, writes, **kw):
        return self.add(eng, lambda e: e.dma_start(out=out, in_=in_, **kw), reads, writes, dma=True)

    def mm(self, out, lhsT, rhs, start, stop, reads, writes):
        return self.add("pe", lambda e: e.matmul(out, lhsT=lhsT, rhs=rhs, start=start, stop=stop), reads, writes,
                        banks=self._banks([out]))

    def tr(self, out, in_, ident, reads, writes):
        return self.add("pe", lambda e: e.transpose(out=out, in_=in_, identity=ident), reads, writes,
                        banks=self._banks([out]))

    def threads(self, fns):
        lists = []
        for f in fns:
            self.capture = []
            f()
            lists.append(self.capture)
        self.capture = None
        n = max(len(l) for l in lists)
        for i in range(n):
            for l in lists:
                if i < len(l):
                    self.add(*l[i])

    def emit(self):
        nc = self.nc
        ops = self.ops
        self.ops = []
        per = {e: [o for o in ops if o.eng == e] for e in ENGS}
        finals = []
        for e in ENGS:
            if e != "sp" and self.cnt[e] > 0:
                finals.append((self.sem[e], self.cnt[e]))
        for e in ("sp", "act", "pool"):
            n = self.dcnt[e]
            for j in range(min(n, NDS)):
                uses = (n - 1 - j) // NDS + 1
                finals.append((self.dsem[e][j], 16 * uses))
        waited = self.waited
        semobj = self.semobj

        def run(engname, eng):
            wd = waited[engname]
            for o in per[engname]:
                for k, v in o.waits.items():
                    if wd.get(k, 0) < v:
                        eng.wait_ge(semobj[k], v)
                        wd[k] = v
                ins = o.fn(eng)
                ins.then_inc(o.sem, o.inc)
            for s, v in finals:
                k = id(s)
                if wd.get(k, 0) < v:
                    eng.wait_ge(s, v)
                    wd[k] = v

        with nc.Block() as block:
            @block.tensor
            def _(eng):
                run("pe", eng)

            @block.scalar
            def _(eng):
                run("act", eng)

            @block.vector
            def _(eng):
                run("dve", eng)

            @block.gpsimd
            def _(eng):
                run("pool", eng)

            @block.sync
            def _(eng):
                run("sp", eng)


class K:
    pass


def _rr(lst):
    state = {"i": 0}

    def nxt():
        v = lst[state["i"] % len(lst)]
        state["i"] += 1
        return v
    return nxt


def build(cfg):
    LSEG = cfg["LSEG"]
    LT = 2 * LSEG
    NST = LT // 512
    upto = cfg.get("upto", "all")
    nc = bass.Bass("TRN2", target_bir_lowering=False)
    k = K()
    k.nc = nc
    k.cfg = cfg

    def din(name, shape, dt=F32):
        return nc.dram_tensor(name, list(shape), dt, kind="ExternalInput").ap()

    def dscr(name, shape, dt=F32):
        return nc.dram_tensor(name, list(shape), dt, kind="Internal").ap()

    def dout(name, shape, dt=F32):
        return nc.dram_tensor(name, list(shape), dt, kind="ExternalOutput").ap()

    x_in = din("x", [LT, D])
    w_in = din("w_in", [D, INC])
    norm_mix = din("norm_mix", [128, 16])
    flag = din("flag", [128, 1])

    w_in_bf = dscr("w_in_bf", [9, 128, 16, 512], BF16)
    xbcT = dscr("xbcT", [2048, LT])
    uT = dscr("uT", [1024, LT], BF16)
    z_tok = dscr("z_tok", [LT, 1024])
    dt_tok = dscr("dt_tok", [LT, 16])

    conv_w = din("conv_w", [128, 16, 3])
    conv_b = din("conv_b", [128, 16])
    dtb_rep = din("dtb_rep", [128, 32])
    alog_rep = din("alog_rep", [128, 32])
    ssdd_rep = din("ssdd_rep", [128, 16])
    ssdn_rep = din("ssdn_rep", [128, 1024])
    xs_tok = dscr("xs_tok", [LT, 1024], BF16)
    B_tok = dscr("B_tok", [LT, 512], BF16)
    BT = dscr("BT", [512, LT], BF16)
    CT = dscr("CT", [512, LT], BF16)
    yf = dscr("yf", [LT, 1024])
    yb_d = dscr("yb_d", [LT, 1024])
    yssd = dscr("yssd", [LT, 1024], BF16)
    s5_small = din("s5_small", [2, 8, 128, 3, 8])
    s5_big = din("s5_big", [2, 8, 128, 4, 128])
    s5_dcol = din("s5_dcol", [128, 8])
    y5gT = dscr("y5gT", [1024, LT], BF16)
    norm_attn = din("norm_attn", [128, 16])
    norm_mem = din("norm_mem", [128, 16])
    w_glu = din("w_glu", [1024, 1024])
    w_out = din("w_out", [2048, 2048])
    w_q = din("w_q", [2048, 2048])
    w_k = din("w_k", [2048, 2048])
    w_v = din("w_v", [2048, 2048])
    w_o = din("w_o", [2048, 2048])
    mem_in = din("mem", [2, 256, 2048])
    s5_ncol = din("s5_ncol", [128, 8])
    w_glu_bf = dscr("w_glu_bf", [2, 128, 8, 512], BF16)
    w_out_bf = dscr("w_out_bf", [4, 128, 16, 512], BF16)
    w_q_bf = dscr("w_q_bf", [4, 128, 16, 512], BF16)
    w_k_bf = dscr("w_k_bf", [4, 128, 16, 512], BF16)
    w_v_bf = dscr("w_v_bf", [4, 128, 16, 512], BF16)
    w_o_bf = dscr("w_o_bf", [4, 128, 16, 512], BF16)
    kT_d = dscr("kT_d", [2, 128, 16, 256], BF16)
    v_d = dscr("v_d", [2, 128, 2, 2048], BF16)
    y5nT = dscr("y5nT", [1024, LT], BF16)
    nffn_rep = din("nffn_rep", [128, 2048])
    w_router_l = din("w_router_l", [128, 16, 16])
    CAPL = cfg["CAPL"]
    acc_l = [dscr("accd%d" % i, [LT + CAPL, 512]) for i in range(4)]
    hn3_bf = dscr("hn3_bf", [LT + CAPL, 2048], BF16)
    idx_d = dscr("idx_d", [16, 128, CAPL // 128], I32)
    gate_d = dscr("gate_d", [16, 128, CAPL // 128])
    probs_all = dscr("probs_all", [cfg.get("GSZ", 1) * LT, 16])
    w_gate = din("w_gate", [cfg.get("nexp", 16), 2048, 2048])
    w_up = din("w_up", [cfg.get("nexp", 16), 2048, 2048])
    w_down = din("w_down", [cfg.get("nexp", 16), 2048, 2048])
    nfin_rep = din("nfin_rep", [128, 2048])
    y_out = dout("y", [LT, 2048])
    cnt_out = dout("slot_idx", [16, 128, CAPL // 128], I32)
    probs_loc = dscr("probs_loc", [LT, 16])
    dbg = {}
    if upto == "F":
        dbg["o_acc"] = dout("o_acc", [LT + CAPL, 512])
        dbg["o_idx"] = dout("o_idx", [16, 128, CAPL // 128], I32)
        dbg["o_gate"] = dout("o_gate", [16, 128, CAPL // 128])
    if upto == "E2":
        dbg["o_acc"] = dout("o_acc", [LT + CAPL, 512])
        dbg["o_probs"] = dout("o_probs", [LT, 16])
        dbg["o_hn3"] = dout("o_hn3", [LT + CAPL, 2048], BF16)
    if upto == "E1":
        dbg["o_y5nT"] = dout("o_y5nT", [1024, LT], BF16)
        dbg["o_kT"] = dout("o_kT", [2, 128, 16, 256], BF16)
        dbg["o_v"] = dout("o_v", [2, 128, 2, 2048], BF16)
    if upto == "D":
        dbg["o_y5gT"] = dout("o_y5gT", [1024, LT], BF16)
    if upto == "C":
        dbg["o_yssd"] = dout("o_yssd", [LT, 1024], BF16)
        dbg["o_yf"] = dout("o_yf", [LT, 1024])
    if upto == "A":
        dbg["o_xbcT"] = dout("o_xbcT", [2048, LT])
        dbg["o_uT"] = dout("o_uT", [1024, LT], BF16)
        dbg["o_z"] = dout("o_z", [LT, 1024])
        dbg["o_dt"] = dout("o_dt", [LT, 16])

    with ExitStack() as gst:
        P = Prog(nc, gst)
        k.P = P
        with ExitStack() as st:
            sb = lambda n, s, d=F32: st.enter_context(nc.sbuf_tensor(n, list(s), d))
            gcol = sb("gcol", [128, 16])
            P.add("sp", lambda e: e.dma_start(out=gcol[:], in_=norm_mix),
                  writes=["gcol"], dma=True)
            wst = [sb("wst%d" % i, [128, INC]) for i in range(2)]
            wbf = [sb("wbf%d" % i, [128, 9 * 512], BF16) for i in range(2)]
            for i in range(2):
                (lambda i: P.add("pool", lambda e: e.memset(wbf[i][:], 0.0), writes=["wbf%d" % i]))(i)
            engs = _rr(["dve", "act", "pool"])
            for kc in range(16):
                b = kc % 2
                P.add("sp", (lambda b, kc: lambda e: e.dma_start(out=wst[b][:], in_=w_in[kc * 128:(kc + 1) * 128, :]))(b, kc),
                      writes=["wst%d" % b], dma=True)
                def cast(b=b, kc=kc):
                    srcs = [(0, 3072, 0), (3088, 4112, 3072), (3072, 3088, 4096)]
                    for (s0, s1, d0) in srcs:
                        def f(e, s0=s0, s1=s1, d0=d0):
                            return e.tensor_scalar(out=wbf[b][:, d0:d0 + (s1 - s0)], in0=wst[b][:, s0:s1],
                                                   scalar1=gcol[:, kc:kc + 1], scalar2=None, op0=ALU.mult)
                        P.add("dve", f, reads=["wst%d" % b, "gcol"], writes=["wbf%d" % b])
                cast()
                P.add("sp", (lambda b, kc: lambda e: e.dma_start(
                    out=w_in_bf[:, :, kc, :].rearrange("g p n -> p g n"),
                    in_=wbf[b][:].rearrange("p (g n) -> p g n", n=512)))(b, kc),
                    reads=["wbf%d" % b], writes=["w_in_bf"], dma=True)
        P.emit()

        with ExitStack() as st:
            sb = lambda n, s, d=F32: st.enter_context(nc.sbuf_tensor(n, list(s), d))
            ps = lambda n, s, d=F32: st.enter_context(nc.psum_tensor(n, list(s), d))
            ident = sb("ident", [128, 128], BF16)
            P.add("pool", lambda e: e.memset(ident[:], 1.0), writes=["ident"])
            P.add("pool", lambda e: e.affine_select(out=ident[:], in_=ident[:], pattern=[[-1, 128]],
                                                    compare_op=ALU.is_equal, fill=0.0, base=0, channel_multiplier=1),
                  reads=["ident"], writes=["ident"])
            xt = [sb("xt%d" % i, [128, D]) for i in range(2)]
            junk = sb("junk", [128, D], BF16)
            ss = [sb("ss%d" % i, [128, 1]) for i in range(2)]
            xn = [sb("xn%d" % i, [128, D], BF16) for i in range(2)]
            hT = sb("hT", [128, 16, 512], BF16)
            wg = [sb("wg%d" % i, [128, 16, 512], BF16) for i in range(2)]
            ostg = [sb("ostg%d" % i, [128, 512]) for i in range(3)]
            ostb = [sb("ostb%d" % i, [128, 512], BF16) for i in range(2)]
            dstg = [sb("dstg%d" % i, [128, 16]) for i in range(2)]
            ptr = [ps("ptr%d" % i, [128, 1024], BF16) for i in range(2)]
            pmm = [ps("pmm%d" % i, [128, 512]) for i in range(4)]
            wq = _rr([0, 1])
            oq = _rr([0, 1, 2])
            obq = _rr([0, 1])
            pq = _rr([0, 1, 2, 3])
            tq = _rr([0, 1])
            cpq = _rr(["act", "dve"])
            dq = _rr([0, 1])
            it = 0
            for s in range(NST):
                t0 = s * 512
                for tt in range(4):
                    b = it % 2
                    it += 1
                    r0 = t0 + tt * 128
                    P.add("sp", (lambda b, r0: lambda e: e.dma_start(out=xt[b][:], in_=x_in[r0:r0 + 128, :]))(b, r0),
                          writes=["xt%d" % b], dma=True)
                    P.add("act", (lambda b: lambda e: e.activation(out=junk[:], in_=xt[b][:], func=AF.Square,
                                                                   accum_out=ss[b][:]))(b),
                          reads=["xt%d" % b], writes=["junk", "ss%d" % b])
                    P.add("dve", (lambda b: lambda e: e.tensor_scalar(out=ss[b][:], in0=ss[b][:], scalar1=1.0 / D,
                                                                      scalar2=EPS, op0=ALU.mult, op1=ALU.add))(b),
                          reads=["ss%d" % b], writes=["ss%d" % b])
                    P.add("act", (lambda b: lambda e: e.activation(out=ss[b][:], in_=ss[b][:], func=AF.Sqrt))(b),
                          reads=["ss%d" % b], writes=["ss%d" % b])
                    P.add("dve", (lambda b: lambda e: e.reciprocal(out=ss[b][:], in_=ss[b][:]))(b),
                          reads=["ss%d" % b], writes=["ss%d" % b])
                    P.add("dve", (lambda b: lambda e: e.tensor_scalar(out=xn[b][:], in0=xt[b][:], scalar1=ss[b][:, 0:1],
                                                                      scalar2=None, op0=ALU.mult))(b),
                          reads=["xt%d" % b, "ss%d" % b], writes=["xn%d" % b])
                    for q in range(4):
                        pb = tq()
                        for j in range(4):
                            kc = q * 4 + j
                            P.add("pe", (lambda pb, j, b, kc: lambda e: e.transpose(
                                out=ptr[pb][:, j * 128:(j + 1) * 128], in_=xn[b][:, kc * 128:(kc + 1) * 128],
                                identity=ident[:]))(pb, j, b, kc),
                                reads=["xn%d" % b, "ident"], writes=["ptr%d" % pb])
                        ce = cpq()
                        if ce == "act":
                            P.add("act", (lambda pb, q, tt: lambda e: e.activation(
                                out=hT[:, q * 4:(q + 1) * 4, tt * 128:(tt + 1) * 128],
                                in_=ptr[pb][:, 0:512].rearrange("p (j n) -> p j n", n=128), func=AF.Copy))(pb, q, tt),
                                reads=["ptr%d" % pb], writes=["hT"])
                        else:
                            P.add("dve", (lambda pb, q, tt: lambda e: e.tensor_copy(
                                out=hT[:, q * 4:(q + 1) * 4, tt * 128:(tt + 1) * 128],
                                in_=ptr[pb][:, 0:512].rearrange("p (j n) -> p j n", n=128)))(pb, q, tt),
                                reads=["ptr%d" % pb], writes=["hT"])
                for g in range(9):
                    wb = wq()
                    P.add("sp", (lambda wb, g: lambda e: e.dma_start(out=wg[wb][:], in_=w_in_bf[g]))(wb, g),
                          reads=["w_in_bf"], writes=["wg%d" % wb], dma=True)
                    if g < 2:
                        for tt in range(4):
                            pb = pq()
                            for kc in range(16):
                                P.add("pe", (lambda pb, wb, kc, tt: lambda e: e.matmul(
                                    pmm[pb][:], lhsT=hT[:, kc, tt * 128:(tt + 1) * 128], rhs=wg[wb][:, kc, :],
                                    start=(kc == 0), stop=(kc == 15)))(pb, wb, kc, tt),
                                    reads=["hT", "wg%d" % wb], writes=["pmm%d" % pb])
                            ob = oq()
                            P.add("act", (lambda ob, pb: lambda e: e.activation(out=ostg[ob][:], in_=pmm[pb][:], func=AF.Copy))(ob, pb),
                                  reads=["pmm%d" % pb], writes=["ostg%d" % ob])
                            r0 = t0 + tt * 128
                            P.add("sp", (lambda ob, r0, g: lambda e: e.dma_start(
                                out=z_tok[r0:r0 + 128, g * 512:(g + 1) * 512], in_=ostg[ob][:]))(ob, r0, g),
                                reads=["ostg%d" % ob], writes=["z_tok"], dma=True)
                    elif g < 8:
                        for mc in range(4):
                            pb = pq()
                            for kc in range(16):
                                P.add("pe", (lambda pb, wb, kc, mc: lambda e: e.matmul(
                                    pmm[pb][:], lhsT=wg[wb][:, kc, mc * 128:(mc + 1) * 128], rhs=hT[:, kc, :],
                                    start=(kc == 0), stop=(kc == 15)))(pb, wb, kc, mc),
                                    reads=["hT", "wg%d" % wb], writes=["pmm%d" % pb])
                            if g < 6:
                                ob = oq()
                                ch0 = (g - 2) * 512 + mc * 128
                                P.add("act", (lambda ob, pb: lambda e: e.activation(out=ostg[ob][:], in_=pmm[pb][:], func=AF.Copy))(ob, pb),
                                      reads=["pmm%d" % pb], writes=["ostg%d" % ob])
                                P.add("sp", (lambda ob, ch0, t0: lambda e: e.dma_start(
                                    out=xbcT[ch0:ch0 + 128, t0:t0 + 512], in_=ostg[ob][:]))(ob, ch0, t0),
                                    reads=["ostg%d" % ob], writes=["xbcT"], dma=True)
                            else:
                                ob = obq()
                                ch0 = (g - 6) * 512 + mc * 128
                                P.add("dve", (lambda ob, pb: lambda e: e.tensor_copy(out=ostb[ob][:], in_=pmm[pb][:]))(ob, pb),
                                      reads=["pmm%d" % pb], writes=["ostb%d" % ob])
                                P.add("sp", (lambda ob, ch0, t0: lambda e: e.dma_start(
                                    out=uT[ch0:ch0 + 128, t0:t0 + 512], in_=ostb[ob][:]))(ob, ch0, t0),
                                    reads=["ostb%d" % ob], writes=["uT"], dma=True)
                    else:
                        for tt in range(4):
                            pb = pq()
                            for kc in range(16):
                                P.add("pe", (lambda pb, wb, kc, tt: lambda e: e.matmul(
                                    pmm[pb][:, 0:16], lhsT=hT[:, kc, tt * 128:(tt + 1) * 128], rhs=wg[wb][:, kc, 0:16],
                                    start=(kc == 0), stop=(kc == 15)))(pb, wb, kc, tt),
                                    reads=["hT", "wg%d" % wb], writes=["pmm%d" % pb])
                            ob = dq()
                            P.add("dve", (lambda ob, pb: lambda e: e.tensor_copy(out=dstg[ob][:], in_=pmm[pb][:, 0:16]))(ob, pb),
                                  reads=["pmm%d" % pb], writes=["dstg%d" % ob])
                            r0 = t0 + tt * 128
                            P.add("sp", (lambda ob, r0: lambda e: e.dma_start(out=dt_tok[r0:r0 + 128, :], in_=dstg[ob][:]))(ob, r0),
                                  reads=["dstg%d" % ob], writes=["dt_tok"], dma=True)
        P.emit()


        with ExitStack() as st:
            sb = lambda n, s, d=F32: st.enter_context(nc.sbuf_tensor(n, list(s), d))
            ps = lambda n, s, d=F32: st.enter_context(nc.psum_tensor(n, list(s), d))
            ident = sb("identB", [128, 128], BF16)
            P.op("pool", "memset", [], ["identB"], ident[:], 1.0)
            P.op("pool", "affine_select", ["identB"], ["identB"], out=ident[:], in_=ident[:], pattern=[[-1, 128]],
                 compare_op=ALU.is_equal, fill=0.0, base=0, channel_multiplier=1)
            cw = sb("cw", [128, 16, 3])
            cb = sb("cb", [128, 16])
            flg = sb("flg", [128, 1])
            P.dma("sp", cw[:], conv_w, [], ["cw"])
            P.dma("sp", cb[:], conv_b, [], ["cb"])
            P.dma("sp", flg[:], flag, [], ["flg"])
            NB = 3
            xw = [sb("xw%d" % i, [128, 514]) for i in range(NB)]
            acc = [sb("acc%d" % i, [128, 512]) for i in range(NB)]
            sl = [sb("sl%d" % i, [128, 512], BF16) for i in range(NB)]
            xs_stg = sb("xs_stg", [128, 4, 1024], BF16)
            b_stg = sb("b_stg", [128, 4, 512], BF16)
            ptb = [ps("ptb%d" % i, [128, 1024], BF16) for i in range(2)]
            it = 0
            tq = 0
            for s in range(NST):
                t0 = s * 512
                for c in range(16):
                    b = it % NB
                    it += 1
                    lo = max(t0 - 1, 0)
                    hi = min(t0 + 513, LT)
                    if t0 == 0:
                        P.op("pool", "memset", [], ["xw%d" % b], xw[b][:, 0:1], 0.0)
                    if t0 + 512 == LT:
                        P.op("pool", "memset", [], ["xw%d" % b], xw[b][:, 513:514], 0.0)
                    P.dma("sp", xw[b][:, (lo - (t0 - 1)):(hi - (t0 - 1))], xbcT[c * 128:(c + 1) * 128, lo:hi],
                          ["xbcT"], ["xw%d" % b])
                    if t0 == LSEG:
                        P.op("dve", "tensor_scalar", ["xw%d" % b, "flg"], ["xw%d" % b], out=xw[b][:, 0:1], in0=xw[b][:, 0:1],
                             scalar1=flg[:, 0:1], scalar2=None, op0=ALU.mult)
                    if t0 + 512 == LSEG:
                        P.op("dve", "tensor_scalar", ["xw%d" % b, "flg"], ["xw%d" % b], out=xw[b][:, 513:514], in0=xw[b][:, 513:514],
                             scalar1=flg[:, 0:1], scalar2=None, op0=ALU.mult)
                    P.op("dve", "tensor_scalar", ["xw%d" % b, "cw"], ["acc%d" % b], out=acc[b][:], in0=xw[b][:, 1:513],
                         scalar1=cw[:, c, 1:2], scalar2=None, op0=ALU.mult)
                    P.op("dve", "scalar_tensor_tensor", ["xw%d" % b, "cw", "acc%d" % b], ["acc%d" % b], out=acc[b][:],
                         in0=xw[b][:, 0:512], scalar=cw[:, c, 0:1], in1=acc[b][:], op0=ALU.mult, op1=ALU.add)
                    P.op("dve", "scalar_tensor_tensor", ["xw%d" % b, "cw", "acc%d" % b], ["acc%d" % b], out=acc[b][:],
                         in0=xw[b][:, 2:514], scalar=cw[:, c, 2:3], in1=acc[b][:], op0=ALU.mult, op1=ALU.add)
                    P.op("act", "activation", ["acc%d" % b, "cb"], ["sl%d" % b], out=sl[b][:], in_=acc[b][:], func=AF.Silu,
                         bias=cb[:, c:c + 1])
                    if c >= 8:
                        dst = BT if c < 12 else CT
                        r0 = (c - 8) * 128 if c < 12 else (c - 12) * 128
                        P.dma("sp", dst[r0:r0 + 128, t0:t0 + 512], sl[b][:], ["sl%d" % b], ["BT" if c < 12 else "CT"])
                    if c < 12:
                        pb = tq % 2
                        tq += 1
                        for tt in range(4):
                            P.tr(ptb[pb][:, tt * 128:(tt + 1) * 128], sl[b][:, tt * 128:(tt + 1) * 128], ident[:],
                                 ["sl%d" % b, "identB"], ["ptb%d" % pb])
                        if c < 8:
                            P.op("act", "activation", ["ptb%d" % pb], ["xs_stg"], out=xs_stg[:, :, c * 128:(c + 1) * 128],
                                 in_=ptb[pb][:, 0:512].rearrange("p (t n) -> p t n", n=128), func=AF.Copy)
                        else:
                            P.op("act", "activation", ["ptb%d" % pb], ["b_stg"], out=b_stg[:, :, (c - 8) * 128:(c - 7) * 128],
                                 in_=ptb[pb][:, 0:512].rearrange("p (t n) -> p t n", n=128), func=AF.Copy)
                    if c == 7:
                        P.dma("sp", xs_tok[t0:t0 + 512, :].rearrange("(t p) c -> p t c", p=128), xs_stg[:], ["xs_stg"], ["xs_tok"])
                    if c == 11:
                        P.dma("sp", B_tok[t0:t0 + 512, :].rearrange("(t p) c -> p t c", p=128), b_stg[:], ["b_stg"], ["B_tok"])
        P.emit()

        NCH = LT // 128
        CPS = LSEG // 128
        with ExitStack() as st:
            sb = lambda n, s, d=F32: st.enter_context(nc.sbuf_tensor(n, list(s), d))
            ps = lambda n, s, d=F32: st.enter_context(nc.psum_tensor(n, list(s), d))
            tri = [sb("triF", [128, 128]), sb("triB", [128, 128])]
            nmk = [sb("nmkF", [128, 128]), sb("nmkB", [128, 128])]
            ones = sb("onesC", [128, 128])
            P.op("pool", "memset", [], ["onesC"], ones[:], 1.0)
            for d_ in range(2):
                pat = [[1, 128]] if d_ == 0 else [[-1, 128]]
                cm = -1 if d_ == 0 else 1
                nm = "FB"[d_]
                P.op("pool", "memset", [], ["tri" + nm], tri[d_][:], 1.0)
                P.op("pool", "affine_select", ["tri" + nm], ["tri" + nm], out=tri[d_][:], in_=tri[d_][:], pattern=pat,
                     compare_op=ALU.is_ge, fill=0.0, base=0, channel_multiplier=cm)
                P.op("pool", "memset", [], ["nmk" + nm], nmk[d_][:], 0.0)
                P.op("pool", "affine_select", ["nmk" + nm], ["nmk" + nm], out=nmk[d_][:], in_=nmk[d_][:], pattern=pat,
                     compare_op=ALU.is_ge, fill=-30000.0, base=0, channel_multiplier=cm)
            dtb = sb("dtb", [128, 32])
            aneg = sb("aneg", [128, 32])
            flg = sb("flgC", [128, 1])
            P.dma("sp", dtb[:], dtb_rep, [], ["dtb"])
            P.dma("sp", aneg[:], alog_rep, [], ["aneg"])
            P.dma("sp", flg[:], flag, [], ["flgC"])
            P.op("act", "activation", ["aneg"], ["aneg"], out=aneg[:], in_=aneg[:], func=AF.Exp)
            P.op("dve", "tensor_scalar", ["aneg"], ["aneg"], out=aneg[:], in0=aneg[:], scalar1=-1.0, scalar2=None, op0=ALU.mult)

            def ssd_dir(d_):
                nm = "FB"[d_]
                X = lambda s_: s_ + nm
                NBUF = 2
                xs = [sb(X("xs%d" % i), [128, 16, 64], BF16) for i in range(NBUF)]
                bt_ = [sb(X("btok%d" % i), [128, 512], BF16) for i in range(NBUF)]
                bT = [sb(X("bT%d" % i), [128, 4, 128], BF16) for i in range(NBUF)]
                cT = [sb(X("cT%d" % i), [128, 4, 128], BF16) for i in range(NBUF)]
                dtr = [sb(X("dtr%d" % i), [128, 16]) for i in range(NBUF)]
                dt_ = sb(X("dt_"), [128, 16])
                dtA = sb(X("dtA"), [128, 16])
                acs = sb(X("acs"), [128, 16])
                dout_ = sb(X("dout_"), [128, 16])
                cd = sb(X("cd"), [128, 16])
                rhsb = [sb(X("rhsb%d" % i), [128, 4, 128]) for i in range(2)]
                dif = [sb(X("dif%d" % i), [128, 4, 128]) for i in range(2)]
                seg = [sb(X("seg%d" % i), [128, 4, 128]) for i in range(2)]
                mT = [sb(X("mT%d" % i), [128, 4, 128], BF16) for i in range(2)]
                xdt = sb(X("xdt"), [128, 16, 64], BF16)
                xdd = [sb(X("xdd%d" % i), [128, 4, 64], BF16) for i in range(2)]
                prev = sb(X("prev"), [128, 16, 64])
                prevb = sb(X("prevb"), [128, 16, 64], BF16)
                tmp = [sb(X("tmpc%d" % i), [128, 4, 64]) for i in range(2)]
                ybuf = [sb(X("ybuf%d" % i), [128, 16, 64]) for i in range(2)]
                pA = ps(X("pA"), [128, 4, 128])
                pB = ps(X("pB"), [128, 512])
                pC = ps(X("pC"), [128, 512])
                pD = ps(X("pD"), [128, 512])
                ydst = yf if d_ == 0 else yb_d

                def body():
                    it = 0
                    last = 127 if d_ == 0 else 0
                    order = list(range(NCH)) if d_ == 0 else list(range(NCH - 1, -1, -1))
                    pbn = [X("prevb%d" % g) for g in range(4)]
                    P.op("dve", "memset", [], [X("prev")], prev[:], 0.0)
                    P.op("pool", "memset", [], pbn, prevb[:], 0.0)
                    for ci, c in enumerate(order):
                        t0 = c * 128
                        b = it % NBUF
                        it += 1
                        if ci == CPS:
                            P.op("dve", "tensor_scalar", [X("prev"), "flgC"], [X("prev")], out=prev[:], in0=prev[:], scalar1=flg[:, 0:1],
                                 scalar2=None, op0=ALU.mult)
                            P.op("dve", "tensor_scalar", pbn + ["flgC"], pbn, out=prevb[:], in0=prevb[:], scalar1=flg[:, 0:1],
                                 scalar2=None, op0=ALU.mult)
                        P.dma("sp", xs[b][:], xs_tok[t0:t0 + 128, :].rearrange("p (h q) -> p h q", q=64), ["xs_tok"], [X("xs%d" % b)])
                        P.dma("sp", bt_[b][:], B_tok[t0:t0 + 128, :], ["B_tok"], [X("btok%d" % b)])
                        P.dma("sp", bT[b][:], BT[:, t0:t0 + 128].rearrange("(g n) l -> n g l", n=128), ["BT"], [X("bT%d" % b)])
                        P.dma("sp", cT[b][:], CT[:, t0:t0 + 128].rearrange("(g n) l -> n g l", n=128), ["CT"], [X("cT%d" % b)])
                        P.dma("sp", dtr[b][:], dt_tok[t0:t0 + 128, :], ["dt_tok"], [X("dtr%d" % b)])
                        P.op("dve", "tensor_tensor", [X("dtr%d" % b), "dtb"], [X("dt_")], out=dt_[:], in0=dtr[b][:], in1=dtb[:, d_ * 16:(d_ + 1) * 16], op=ALU.add)
                        P.op("act", "activation", [X("dt_")], [X("dt_")], out=dt_[:], in_=dt_[:], func=AF.Exp)
                        P.op("act", "activation", [X("dt_")], [X("dt_")], out=dt_[:], in_=dt_[:], func=AF.Ln, bias=1.0)
                        P.op("dve", "tensor_tensor", [X("dt_"), "aneg"], [X("dtA")], out=dtA[:], in0=dt_[:], in1=aneg[:, d_ * 16:(d_ + 1) * 16], op=ALU.mult)
                        P.mm(pD[:, 0:16], tri[d_][:], dtA[:], True, True, ["tri" + nm, X("dtA")], [X("pD")])
                        P.op("dve", "tensor_copy", [X("pD")], [X("acs")], out=acs[:], in_=pD[:, 0:16])
                        P.op("act", "activation", [X("acs")], [X("dout_")], out=dout_[:], in_=acs[:], func=AF.Exp)
                        P.op("pool", "tensor_tensor", [X("xs%d" % b), X("dt_")], [X("xdt")], out=xdt[:], in0=xs[b][:],
                             in1=dt_[:].unsqueeze(2).to_broadcast([128, 16, 64]), op=ALU.mult)
                        yb_i = ci % 2
                        yn = X("ybuf%d" % yb_i)
                        for g in range(4):
                            q = g % 2
                            h0 = g * 4
                            P.op("dve", "tensor_tensor", ["tri" + nm, X("dtA")], [X("rhsb%d" % q)], out=rhsb[q][:],
                                 in0=tri[d_][:].unsqueeze(1).to_broadcast([128, 4, 128]),
                                 in1=dtA[:, h0:h0 + 4].unsqueeze(2).to_broadcast([128, 4, 128]), op=ALU.mult)
                            P.mm(pA[:].rearrange("p r l -> p (r l)"), ones[:], rhsb[q][:].rearrange("p r l -> p (r l)"), True, True,
                                 ["onesC", X("rhsb%d" % q)], [X("pA")])
                            P.op("dve", "tensor_tensor", [X("pA"), X("acs")], [X("dif%d" % q)], out=dif[q][:], in0=pA[:],
                                 in1=acs[:, h0:h0 + 4].unsqueeze(2).to_broadcast([128, 4, 128]), op=ALU.subtract)
                            P.op("act", "activation", [X("pA")], [X("cd")], out=cd[:, h0:h0 + 4], in_=pA[:, :, last], func=AF.Exp)
                            P.op("dve", "tensor_tensor", [X("dif%d" % q), "nmk" + nm], [X("dif%d" % q)], out=dif[q][:], in0=dif[q][:],
                                 in1=nmk[d_][:].unsqueeze(1).to_broadcast([128, 4, 128]), op=ALU.min)
                            P.op("act", "activation", [X("dif%d" % q)], [X("seg%d" % q)], out=seg[q][:], in_=dif[q][:], func=AF.Exp)
                            P.mm(pB[:, 0:128], bT[b][:, g, :], cT[b][:, g, :], True, True, [X("bT%d" % b), X("cT%d" % b)], [X("pBcb")])
                            P.op("dve", "tensor_tensor", [X("seg%d" % q), X("pBcb")], [X("mT%d" % q)], out=mT[q][:], in0=seg[q][:],
                                 in1=pB[:, 0:128].unsqueeze(1).to_broadcast([128, 4, 128]), op=ALU.mult)
                            for r in range(4):
                                P.mm(pB[:, 128 + r * 64:128 + (r + 1) * 64], mT[q][:, r, :], xdt[:, h0 + r, :], True, True,
                                     [X("mT%d" % q), X("xdt")], [X("pBy")])
                            P.op("pool", "tensor_tensor", [X("xdt"), X("seg%d" % q)], [X("xdd%d" % q)], out=xdd[q][:], in0=xdt[:, h0:h0 + 4, :],
                                 in1=seg[q][:, :, last:last + 1].to_broadcast([128, 4, 64]), op=ALU.mult)
                            P.mm(pC[:, 0:256], bt_[b][:, g * 128:(g + 1) * 128], xdd[q][:].rearrange("p r q -> p (r q)"), True, True,
                                 [X("btok%d" % b), X("xdd%d" % q)], [X("pCs")])
                            P.mm(pC[:, 256:512], cT[b][:, g, :], prevb[:, h0:h0 + 4, :].rearrange("p r q -> p (r q)"), True, True,
                                 [X("cT%d" % b), X("prevb%d" % g)], [X("pCo")])
                            P.op("dve", "tensor_tensor", [X("pCo"), X("dout_")], [X("tmpc%d" % q)], out=tmp[q][:],
                                 in0=pC[:, 256:512].rearrange("p (r q) -> p r q", q=64),
                                 in1=dout_[:, h0:h0 + 4].unsqueeze(2).to_broadcast([128, 4, 64]), op=ALU.mult)
                            P.op("dve", "tensor_tensor", [X("tmpc%d" % q), X("pBy")], [yn], out=ybuf[yb_i][:, h0:h0 + 4, :], in0=tmp[q][:],
                                 in1=pB[:, 128:384].rearrange("p (r q) -> p r q", q=64), op=ALU.add)
                            P.op("dve", "tensor_tensor", [X("prev"), X("cd")], [X("prev")], out=prev[:, h0:h0 + 4, :], in0=prev[:, h0:h0 + 4, :],
                                 in1=cd[:, h0:h0 + 4].unsqueeze(2).to_broadcast([128, 4, 64]), op=ALU.mult)
                            P.op("dve", "tensor_tensor", [X("prev"), X("pCs")], [X("prev")], out=prev[:, h0:h0 + 4, :], in0=prev[:, h0:h0 + 4, :],
                                 in1=pC[:, 0:256].rearrange("p (r q) -> p r q", q=64), op=ALU.add)
                            P.op("act", "activation", [X("prev")], [X("prevb%d" % g)], out=prevb[:, h0:h0 + 4, :], in_=prev[:, h0:h0 + 4, :], func=AF.Copy)
                        P.dma("act", ydst[t0:t0 + 128, :], ybuf[yb_i][:].rearrange("p h q -> p (h q)"), [yn], [X("ydst%d" % (ci % 8))])
                return body

            P.threads([ssd_dir(0), ssd_dir(1)])
        P.emit()

        with ExitStack() as st:
            sb = lambda n, s, d=F32: st.enter_context(nc.sbuf_tensor(n, list(s), d))
            dsk = sb("dsk", [128, 16])
            gn = sb("gn", [128, 1024])
            P.dma("sp", dsk[:], ssdd_rep, [], ["dsk"])
            P.dma("sp", gn[:], ssdn_rep, [], ["gn"])

            def fin(par):
                X = lambda s_: s_ + str(par)
                ya = sb(X("yaC"), [128, 16, 64])
                ybb = sb(X("ybC"), [128, 16, 64])
                xs_ = sb(X("xsC"), [128, 16, 64], BF16)
                zt = sb(X("ztC"), [128, 1024])
                sq = sb(X("sqC"), [128, 1024])
                gss = sb(X("gssC"), [128, 4])
                yo = sb(X("yoC"), [128, 1024], BF16)

                def body():
                    for c in range(par, NCH, 2):
                        t0 = c * 128
                        P.dma("sp", ya[:].rearrange("p h q -> p (h q)"), yf[t0:t0 + 128, :], [], [X("yaC")])
                        P.dma("sp", ybb[:].rearrange("p h q -> p (h q)"), yb_d[t0:t0 + 128, :], [], [X("ybC")])
                        P.dma("sp", xs_[:], xs_tok[t0:t0 + 128, :].rearrange("p (h q) -> p h q", q=64), [], [X("xsC")])
                        P.dma("sp", zt[:], z_tok[t0:t0 + 128, :], [], [X("ztC")])
                        P.op("dve", "tensor_tensor", [X("yaC"), X("ybC")], [X("yaC")], out=ya[:], in0=ya[:], in1=ybb[:], op=ALU.add)
                        P.op("pool", "tensor_tensor", [X("xsC"), "dsk"], [X("ybC")], out=ybb[:], in0=xs_[:],
                             in1=dsk[:].unsqueeze(2).to_broadcast([128, 16, 64]), op=ALU.mult)
                        P.op("dve", "tensor_tensor", [X("yaC"), X("ybC")], [X("yaC")], out=ya[:], in0=ya[:], in1=ybb[:], op=ALU.add)
                        P.op("act", "activation", [X("ztC")], [X("ztC")], out=zt[:], in_=zt[:], func=AF.Silu)
                        y2 = ya[:].rearrange("p h q -> p (h q)")
                        P.op("dve", "tensor_tensor", [X("yaC"), X("ztC")], [X("yaC")], out=y2, in0=y2, in1=zt[:], op=ALU.mult)
                        P.op("pool", "tensor_tensor", [X("yaC")], [X("sqC")], out=sq[:], in0=y2, in1=y2, op=ALU.mult)
                        P.op("dve", "tensor_reduce", [X("sqC")], [X("gssC")], out=gss[:], in_=sq[:].rearrange("p (g c) -> p g c", c=256),
                             op=ALU.add, axis=AX.X)
                        P.op("dve", "tensor_scalar", [X("gssC")], [X("gssC")], out=gss[:], in0=gss[:], scalar1=1.0 / 256, scalar2=EPS,
                             op0=ALU.mult, op1=ALU.add)
                        P.op("act", "activation", [X("gssC")], [X("gssC")], out=gss[:], in_=gss[:], func=AF.Sqrt)
                        P.op("dve", "reciprocal", [X("gssC")], [X("gssC")], out=gss[:], in_=gss[:])
                        P.op("dve", "tensor_tensor", [X("yaC"), X("gssC")], [X("yaC")], out=ya[:].rearrange("p (g h) q -> p g (h q)", g=4),
                             in0=ya[:].rearrange("p (g h) q -> p g (h q)", g=4),
                             in1=gss[:].unsqueeze(2).to_broadcast([128, 4, 256]), op=ALU.mult)
                        P.op("dve", "tensor_tensor", [X("yaC"), "gn"], [X("yoC")], out=yo[:], in0=y2, in1=gn[:], op=ALU.mult)
                        P.dma("act", yssd[t0:t0 + 128, :], yo[:], [X("yoC")], [X("yssdw%d" % (c % 8))])
                return body

            P.threads([fin(0), fin(1)])
        P.emit()

        NB = LT // 8
        NBS = LSEG // 8
        P.limit = cfg.get("dmax")
        TWO_PI = 6.283185307179586
        with ExitStack() as st:
            sb = lambda n, s, d=F32: st.enter_context(nc.sbuf_tensor(n, list(s), d))
            ps = lambda n, s, d=F32: st.enter_context(nc.psum_tensor(n, list(s), d))
            identF = sb("identF", [128, 128])
            P.op("pool", "memset", [], ["identF"], identF[:], 1.0)
            P.op("pool", "affine_select", ["identF"], ["identF"], out=identF[:], in_=identF[:], pattern=[[-1, 128]],
                 compare_op=ALU.is_equal, fill=0.0, base=0, channel_multiplier=1)
            bm = sb("bmD", [128, 8, 16])
            P.op("pool", "memset", [], ["bmD"], bm[:], 1.0)
            P.op("pool", "affine_select", ["bmD"], ["bmD"], out=bm[:], in_=bm[:], pattern=[[-16, 8], [0, 16]],
                 compare_op=ALU.is_ge, fill=0.0, base=0, channel_multiplier=1)
            P.op("pool", "affine_select", ["bmD"], ["bmD"], out=bm[:], in_=bm[:], pattern=[[16, 8], [0, 16]],
                 compare_op=ALU.is_ge, fill=0.0, base=15, channel_multiplier=-1)
            PM = [sb("PM%d" % i, [128, 2, 64]) for i in range(4)]
            CM = [sb("CM%d" % i, [128, 8, 16]) for i in range(4)]
            for pr_ in range(4):
                P.op("pool", "memset", [], ["PM%d" % pr_], PM[pr_][:], 1.0)
                P.op("pool", "affine_select", ["PM%d" % pr_], ["PM%d" % pr_], out=PM[pr_][:], in_=PM[pr_][:],
                     pattern=[[-16, 2], [0, 64]], compare_op=ALU.is_ge, fill=0.0, base=-32 * pr_, channel_multiplier=1)
                P.op("pool", "affine_select", ["PM%d" % pr_], ["PM%d" % pr_], out=PM[pr_][:], in_=PM[pr_][:],
                     pattern=[[16, 2], [0, 64]], compare_op=ALU.is_ge, fill=0.0, base=32 * pr_ + 15, channel_multiplier=-1)
                P.op("pool", "memset", [], ["CM%d" % pr_], CM[pr_][:], 1.0)
                for h in range(2):
                    P.op("pool", "affine_select", ["CM%d" % pr_], ["CM%d" % pr_], out=CM[pr_][h * 64:(h + 1) * 64],
                         in_=CM[pr_][h * 64:(h + 1) * 64], pattern=[[1, 8], [0, 16]], compare_op=ALU.is_equal, fill=0.0,
                         base=-(2 * pr_ + h), channel_multiplier=0)
            KV = sb("KV", [128, 8, 9])
            P.op("pool", "iota", [], ["KV"], KV[:], pattern=[[0, 8], [1, 9]], base=0, channel_multiplier=0,
                 allow_small_or_imprecise_dtypes=True)
            BL = sb("BL", [128, NBS])
            BHs = [sb("BH%d" % i, [128, NBS]) for i in range(2)]
            P.op("pool", "iota", [], ["BL"], BL[:], pattern=[[0, NBS // 32], [1, 32]], base=0, channel_multiplier=0,
                 allow_small_or_imprecise_dtypes=True)
            for s_ in range(2):
                P.op("pool", "iota", [], ["BH%d" % s_], BHs[s_][:], pattern=[[1, NBS // 32], [0, 32]], base=s_ * (NBS // 32),
                     channel_multiplier=0, allow_small_or_imprecise_dtypes=True)
            dcol = sb("dcolD", [128, 8])
            flg = sb("flgD", [128, 1])
            P.dma("sp", dcol[:], s5_dcol, [], ["dcolD"])
            P.dma("sp", flg[:], flag, [], ["flgD"])
            sm = sb("smD", [128, 3, 8])
            bg = sb("bgD", [128, 4, 128])
            ncim = sb("ncim", [128, 128])
            sml = {n_: sb("sm_" + n_, [128, 8]) for n_ in ("dl", "xr", "xi", "numr", "den", "t1", "t2", "qr", "qi", "tb", "t0", "t32")}
            smi = sb("sm_i", [128, 8], I32)
            k9 = {n_: sb("k9_" + n_, [128, 8, 9]) for n_ in ("er", "tn", "tf", "cs", "sn", "pr", "pi")}
            k9i = sb("k9_i", [128, 8, 9], I32)
            Bb = {n_: sb("Bb_" + n_, [128, 8, 16]) for n_ in ("r", "i", "a", "b")}
            colsT = sb("colsT", [128, 3, 4])
            scr = sb("scrD", [128, 11, 512])
            RN = lambda a, b_: ["R%d" % i for i in range(a, b_)]
            X_r = scr[:, 0:2, :].rearrange("p a (b c) -> p (a b) c", c=128)
            X_i = scr[:, 2:4, :].rearrange("p a (b c) -> p (a b) c", c=128)
            Zr_t = sb("Zr_t", [128, 9, 128])
            Zi_t = sb("Zi_t", [128, 9, 128])
            xtmp = sb("xtmp", [128, 8, 128])
            W1 = sb("W1", [128, 8, 2, 4, 128], BF16)
            W2 = sb("W2", [128, 2, 9, 2, 4, 128], BF16)
            Kbd = sb("Kbd", [128, 2, 8, 128], BF16)
            ktmp = sb("ktmp", [128, 128])
            ust = sb("ust", [128, LT // 2], BF16)
            u3 = sb("uj", [128, 8, NB], BF16)
            S = sb("S_D", [128, 4, 2, NBS])
            HB = sb("HB_D", [128, 2, 4, 2, NB + 1], BF16)
            carry = sb("carryD", [128, 4, 2])
            stg = sb("stgD", [128, NBS, 8], BF16)
            gl = [sb("glD%d" % i, [128, NBS]) for i in range(3)]
            pk = ps("pkD", [128, 4, 128])
            ptl = [ps("ptD%d" % i, [128, 4, 128]) for i in range(2)]
            pS = [ps("pSD%d" % i, [128, 512]) for i in range(2)]
            po = [ps("poD%d" % i, [128, 512]) for i in range(2)]
            mskq = _rr(["dve", "pool"])
            psq = 0
            poq = 0
            T = lambda i: scr[:, i, 0:NBS]
            DST = cfg.get("dstage", 9)
            for ft in range(cfg.get("nft", 8)):
                for hf in range(2):
                    P.dma("sp", ust[:], uT[ft * 128:(ft + 1) * 128, hf * (LT // 2):(hf + 1) * (LT // 2)], ["uT"], ["ust"])
                    usv = ust[:].rearrange("p (b j) -> p j b", j=8)
                    for j in range(8):
                        P.op("act", "activation", ["ust"], ["uft"], out=u3[:, j, hf * (NB // 2):(hf + 1) * (NB // 2)], in_=usv[:, j, :], func=AF.Copy)
                for d_ in range(2):
                    P.dma("sp", sm[:], s5_small[d_, ft], [], ["smD"])
                    P.dma("sp", bg[:], s5_big[d_, ft], [], ["bgD"])
                    are, aim, lst = sm[:, 0, :], sm[:, 1, :], sm[:, 2, :]
                    bre, bim, cre, cim = bg[:, 0, :], bg[:, 1, :], bg[:, 2, :], bg[:, 3, :]
                    V = lambda n_: sml[n_][:]
                    P.op("act", "activation", ["smD"], ["sm_dl"], out=V("dl"), in_=lst, func=AF.Exp)
                    P.op("dve", "tensor_tensor", ["smD", "sm_dl"], ["sm_xr"], out=V("xr"), in0=are, in1=V("dl"), op=ALU.mult)
                    P.op("dve", "tensor_tensor", ["smD", "sm_dl"], ["sm_xi"], out=V("xi"), in0=aim, in1=V("dl"), op=ALU.mult)
                    b9 = lambda ap: ap.unsqueeze(2).to_broadcast([128, 8, 9])
                    P.op("dve", "tensor_tensor", ["KV", "sm_xr"], ["k9_er"], out=k9["er"][:], in0=KV[:], in1=b9(V("xr")), op=ALU.mult)
                    P.op("act", "activation", ["k9_er"], ["k9_er"], out=k9["er"][:], in_=k9["er"][:], func=AF.Exp)
                    P.op("dve", "tensor_tensor", ["KV", "sm_xi"], ["k9_tn"], out=k9["tn"][:], in0=KV[:], in1=b9(V("xi")), op=ALU.mult)
                    P.op("dve", "tensor_scalar", ["k9_tn"], ["k9_tn"], out=k9["tn"][:], in0=k9["tn"][:], scalar1=1.0 / TWO_PI, scalar2=None, op0=ALU.mult)
                    for (dst, off) in (("sn", 0.0), ("cs", 0.25)):
                        if off:
                            P.op("dve", "tensor_scalar", ["k9_tn"], ["k9_tn"], out=k9["tn"][:], in0=k9["tn"][:], scalar1=off, scalar2=None, op0=ALU.add)
                        P.op("dve", "tensor_copy", ["k9_tn"], ["k9_i"], out=k9i[:], in_=k9["tn"][:])
                        P.op("dve", "tensor_copy", ["k9_i"], ["k9_tf"], out=k9["tf"][:], in_=k9i[:])
                        P.op("dve", "tensor_tensor", ["k9_tn", "k9_tf"], ["k9_tf"], out=k9["tf"][:], in0=k9["tn"][:], in1=k9["tf"][:], op=ALU.subtract)
                        P.op("act", "activation", ["k9_tf"], ["k9_" + dst], out=k9[dst][:], in_=k9["tf"][:], func=AF.Sin, scale=TWO_PI)
                    P.op("dve", "tensor_tensor", ["k9_er", "k9_cs"], ["k9_pr"], out=k9["pr"][:], in0=k9["er"][:], in1=k9["cs"][:], op=ALU.mult)
                    P.op("dve", "tensor_tensor", ["k9_er", "k9_sn"], ["k9_pi"], out=k9["pi"][:], in0=k9["er"][:], in1=k9["sn"][:], op=ALU.mult)
                    pr1, pi1 = k9["pr"][:, :, 1], k9["pi"][:, :, 1]
                    P.op("dve", "tensor_scalar", ["k9_pr"], ["sm_numr"], out=V("numr"), in0=pr1, scalar1=-1.0, scalar2=None, op0=ALU.add)
                    P.op("dve", "tensor_tensor", ["smD"], ["sm_den"], out=V("den"), in0=are, in1=are, op=ALU.mult)
                    P.op("dve", "tensor_tensor", ["smD"], ["sm_t1"], out=V("t1"), in0=aim, in1=aim, op=ALU.mult)
                    P.op("dve", "tensor_tensor", ["sm_den", "sm_t1"], ["sm_den"], out=V("den"), in0=V("den"), in1=V("t1"), op=ALU.add)
                    P.op("dve", "reciprocal", ["sm_den"], ["sm_den"], out=V("den"), in_=V("den"))
                    P.op("dve", "tensor_tensor", ["sm_numr", "smD"], ["sm_t1"], out=V("t1"), in0=V("numr"), in1=are, op=ALU.mult)
                    P.op("dve", "tensor_tensor", ["k9_pi", "smD"], ["sm_t2"], out=V("t2"), in0=pi1, in1=aim, op=ALU.mult)
                    P.op("dve", "tensor_tensor", ["sm_t1", "sm_t2"], ["sm_t1"], out=V("t1"), in0=V("t1"), in1=V("t2"), op=ALU.add)
                    P.op("dve", "tensor_tensor", ["sm_t1", "sm_den"], ["sm_qr"], out=V("qr"), in0=V("t1"), in1=V("den"), op=ALU.mult)
                    P.op("dve", "tensor_tensor", ["k9_pi", "smD"], ["sm_t1"], out=V("t1"), in0=pi1, in1=are, op=ALU.mult)
                    P.op("dve", "tensor_tensor", ["sm_numr", "smD"], ["sm_t2"], out=V("t2"), in0=V("numr"), in1=aim, op=ALU.mult)
                    P.op("dve", "tensor_tensor", ["sm_t1", "sm_t2"], ["sm_t1"], out=V("t1"), in0=V("t1"), in1=V("t2"), op=ALU.subtract)
                    P.op("dve", "tensor_tensor", ["sm_t1", "sm_den"], ["sm_qi"], out=V("qi"), in0=V("t1"), in1=V("den"), op=ALU.mult)
                    b16 = lambda ap: ap.unsqueeze(2).to_broadcast([128, 8, 16])
                    g16 = lambda ap: ap.rearrange("p (g c) -> p g c", c=16)
                    TT = lambda rd, wr, out, in0, in1, op, eng="dve": P.op(eng, "tensor_tensor", rd, wr, out=out, in0=in0, in1=in1, op=op)
                    TT(["bgD", "sm_qr"], ["Bb_a"], Bb["a"][:], g16(bre), b16(V("qr")), ALU.mult)
                    TT(["bgD", "sm_qi"], ["Bb_b"], Bb["b"][:], g16(bim), b16(V("qi")), ALU.mult)
                    TT(["Bb_a", "Bb_b"], ["Bb_r"], Bb["r"][:], Bb["a"][:], Bb["b"][:], ALU.subtract)
                    TT(["bgD", "sm_qr"], ["Bb_a"], Bb["a"][:], g16(bim), b16(V("qr")), ALU.mult)
                    TT(["bgD", "sm_qi"], ["Bb_b"], Bb["b"][:], g16(bre), b16(V("qi")), ALU.mult)
                    TT(["Bb_a", "Bb_b"], ["Bb_i"], Bb["i"][:], Bb["a"][:], Bb["b"][:], ALU.add)
                    for kk in range(8):
                        prk = b16(k9["pr"][:, :, kk])
                        pik = b16(k9["pi"][:, :, kk])
                        xr_k = g16(X_r[:, kk, :])
                        xi_k = g16(X_i[:, kk, :])
                        xt_k = g16(xtmp[:, kk, :])
                        TT(["k9_pr", "Bb_r"], RN(0, 2), xr_k, Bb["r"][:], prk, ALU.mult)
                        TT(["k9_pi", "Bb_i"], ["xtmp"], xt_k, Bb["i"][:], pik, ALU.mult, "pool")
                        TT(RN(0, 2) + ["xtmp"], RN(0, 2), xr_k, xr_k, xt_k, ALU.subtract)
                        TT(["k9_pr", "Bb_i"], RN(2, 4), xi_k, Bb["i"][:], prk, ALU.mult)
                        TT(["k9_pi", "Bb_r"], ["xtmp"], xt_k, Bb["r"][:], pik, ALU.mult, "pool")
                        TT(RN(2, 4) + ["xtmp"], RN(2, 4), xi_k, xi_k, xt_k, ALU.add)
                    for kk in range(9):
                        prk = b16(k9["pr"][:, :, kk])
                        pik = b16(k9["pi"][:, :, kk])
                        zr_k = g16(Zr_t[:, kk, :])
                        zi_k = g16(Zi_t[:, kk, :])
                        xt_k = g16(xtmp[:, kk % 8, :])
                        TT(["k9_pr", "bgD"], ["Zr_t"], zr_k, g16(cre), prk, ALU.mult)
                        TT(["k9_pi", "bgD"], ["xtmp"], xt_k, g16(cim), pik, ALU.mult, "pool")
                        TT(["Zr_t", "xtmp"], ["Zr_t"], zr_k, zr_k, xt_k, ALU.subtract)
                        TT(["k9_pi", "bgD"], ["Zi_t"], zi_k, g16(cre), pik, ALU.mult)
                        TT(["k9_pr", "bgD"], ["xtmp"], xt_k, g16(cim), prk, ALU.mult, "pool")
                        TT(["Zi_t", "xtmp"], ["Zi_t"], zi_k, zi_k, xt_k, ALU.add)
                    P.op("dve", "tensor_scalar", ["bgD"], ["ncim"], out=ncim[:], in0=cim, scalar1=-1.0, scalar2=None, op0=ALU.mult)
                    for half in range(2):
                        for tl in range(4):
                            tau = half * 4 + tl
                            P.mm(pk[:, tl, :], X_r[0:64, tau, :], cre[0:64, :], True, False, RN(0, 2) + ["bgD"], ["pkD"])
                            P.mm(pk[:, tl, :], X_i[0:64, tau, :], ncim[0:64, :], False, True, RN(2, 4) + ["ncim"], ["pkD"])
                        for tl in range(4):
                            tau = half * 4 + tl
                            if tau == 0 and d_ == 0:
                                P.op("dve", "scalar_tensor_tensor", ["identF", "dcolD", "pkD"], ["ktmp"], out=ktmp[:], in0=identF[:],
                                     scalar=dcol[:, ft:ft + 1], in1=pk[:, tl, :], op0=ALU.mult, op1=ALU.add)
                                P.op("dve", "tensor_tensor", ["ktmp", "bmD"], ["Kbd"], out=Kbd[:, d_, tau, :], in0=ktmp[:],
                                     in1=bm[:].rearrange("p g c -> p (g c)"), op=ALU.mult)
                            else:
                                P.op("dve", "tensor_tensor", ["pkD", "bmD"], ["Kbd"], out=Kbd[:, d_, tau, :], in0=pk[:, tl, :],
                                     in1=bm[:].rearrange("p g c -> p (g c)"), op=ALU.mult)
                    for kk in range(8):
                        for ri in range(2):
                            src = (X_r if ri == 0 else X_i)[:, kk, :]
                            slot = (kk * 2 + ri) % 2
                            pt = ptl[slot]
                            P.tr(pt[:, 0, :], src, identF[:], (RN(0, 2) if ri == 0 else RN(2, 4)) + ["identF"], ["ptD%d" % slot])
                            for pr_ in range(4):
                                P.op("dve", "tensor_tensor", ["ptD%d" % slot, "PM%d" % pr_], ["W1"], out=W1[:, kk, ri, pr_, :],
                                     in0=pt[:, 0, :], in1=PM[pr_][:].rearrange("p a b -> p (a b)"), op=ALU.mult)
                    for kk in range(1, 9):
                        for ri in range(2):
                            for pr_ in range(4):
                                cmv = CM[pr_][:].rearrange("p g c -> p (g c)")
                                if ri == 0:
                                    P.op("pool", "tensor_tensor", ["Zr_t", "CM%d" % pr_], ["W2"], out=W2[:, d_, kk, ri, pr_, :],
                                         in0=Zr_t[:, kk, :], in1=cmv, op=ALU.mult)
                                else:
                                    P.op("dve", "scalar_tensor_tensor", ["Zi_t", "CM%d" % pr_], ["W2"], out=W2[:, d_, kk, ri, pr_, :],
                                         in0=Zi_t[:, kk, :], scalar=-1.0, in1=cmv, op0=ALU.mult, op1=ALU.mult)
                    P.op("dve", "tensor_scalar", ["sm_xi"], ["sm_tb"], out=V("tb"), in0=V("xi"), scalar1=8.0 / TWO_PI, scalar2=None, op0=ALU.mult)
                    P.op("dve", "tensor_copy", ["sm_tb"], ["sm_i"], out=smi[:], in_=V("tb"))
                    P.op("dve", "tensor_copy", ["sm_i"], ["sm_t0"], out=V("t0"), in_=smi[:])
                    P.op("dve", "tensor_tensor", ["sm_tb", "sm_t0"], ["sm_t0"], out=V("t0"), in0=V("tb"), in1=V("t0"), op=ALU.subtract)
                    P.op("dve", "tensor_scalar", ["sm_t0"], ["sm_tb"], out=V("tb"), in0=V("t0"), scalar1=32.0, scalar2=None, op0=ALU.mult)
                    P.op("dve", "tensor_copy", ["sm_tb"], ["sm_i"], out=smi[:], in_=V("tb"))
                    P.op("dve", "tensor_copy", ["sm_i"], ["sm_t32"], out=V("t32"), in_=smi[:])
                    P.op("dve", "tensor_tensor", ["sm_tb", "sm_t32"], ["sm_t32"], out=V("t32"), in0=V("tb"), in1=V("t32"), op=ALU.subtract)
                    for h in range(2):
                        hs = slice(h * 64, (h + 1) * 64)
                        P.op("dve", "tensor_copy", ["sm_t0"], ["colsT"], out=colsT[hs, 0, :], in_=sml["t0"][hs, h::2])
                        P.op("dve", "tensor_copy", ["sm_t32"], ["colsT"], out=colsT[hs, 1, :], in_=sml["t32"][hs, h::2])
                        P.op("dve", "tensor_copy", ["k9_er"], ["colsT"], out=colsT[hs, 2, :], in_=k9["er"][hs, h::2, 8])
                    if DST < 2:
                        continue
                    for ui, s_ in enumerate((0, 1) if d_ == 0 else (1, 0)):
                        blk0 = s_ * NBS
                        for pr_ in range(4):
                            for ri in range(2):
                                pb = psq % 2
                                psq += 1
                                for j in range(8):
                                    kk = (7 - j) if d_ == 0 else j
                                    P.mm(pS[pb][:, 0:NBS], W1[:, kk, ri, pr_, :], u3[:, j, blk0:blk0 + NBS], j == 0, j == 7,
                                         ["W1", "uft"], ["pSD%d" % pb])
                                P.op("act", "activation", ["pSD%d" % pb], ["S_D%d" % pr_], out=S[:, pr_, ri, :], in_=pS[pb][:, 0:NBS], func=AF.Copy)
                        if DST < 3:
                            continue
                        for pr_ in range(4):
                            tu, ti_, tf, cs, sn, T_r, T_i, g_r, g_i, m1, m2 = [T(i) for i in range(11)]
                            tii = ti_.bitcast(I32)
                            t0c, t32c, decc = colsT[:, 0, pr_:pr_ + 1], colsT[:, 1, pr_:pr_ + 1], colsT[:, 2, pr_:pr_ + 1]
                            P.op("dve", "tensor_scalar", ["BH%d" % s_, "colsT"], ["R0"], out=tu, in0=BHs[s_][:], scalar1=t32c, scalar2=None, op0=ALU.mult)
                            P.op("dve", "scalar_tensor_tensor", ["BL", "colsT", "R0"], ["R0"], out=tu, in0=BL[:], scalar=t0c, in1=tu, op0=ALU.mult, op1=ALU.add)
                            for (dst, dn, off) in ((sn, "R4", 0.0), (cs, "R3", 0.25)):
                                if off:
                                    P.op("dve", "tensor_scalar", ["R0"], ["R0"], out=tu, in0=tu, scalar1=off, scalar2=None, op0=ALU.add)
                                P.op("dve", "tensor_copy", ["R0"], ["R1"], out=tii, in_=tu)
                                P.op("dve", "tensor_copy", ["R1"], ["R2"], out=tf, in_=tii)
                                P.op("pool", "tensor_tensor", ["R0", "R2"], ["R2"], out=tf, in0=tu, in1=tf, op=ALU.subtract)
                                P.op("act", "activation", ["R2"], [dn], out=dst, in_=tf, func=AF.Sin, scale=TWO_PI)
                            S_r, S_i = S[:, pr_, 0, :], S[:, pr_, 1, :]
                            sg = 1.0 if d_ == 0 else -1.0
                            sop = ALU.add if d_ == 0 else ALU.subtract
                            sop2 = ALU.subtract if d_ == 0 else ALU.add
                            sn_ = "S_D%d" % pr_
                            TT([sn_, "R3"], ["R5"], T_r, S_r, cs, ALU.mult)
                            TT([sn_, "R4"], ["R9"], m1, S_i, sn, ALU.mult, "pool")
                            TT(["R5", "R9"], ["R5"], T_r, T_r, m1, sop)
                            TT([sn_, "R3"], ["R6"], T_i, S_i, cs, ALU.mult)
                            TT([sn_, "R4"], ["R10"], m2, S_r, sn, ALU.mult, "pool")
                            TT(["R6", "R10"], ["R6"], T_i, T_i, m2, sop2)
                            rv = (lambda ap: ap) if d_ == 0 else (lambda ap: ap[:, ::-1])
                            dbc = decc.to_broadcast([128, NBS])
                            for (src, sname, dst, dname, ri) in ((T_r, "R5", g_r, "R7", 0), (T_i, "R6", g_i, "R8", 1)):
                                init = 0.0 if ui == 0 else carry[:, pr_, ri:ri + 1]
                                P.op("dve", "tensor_tensor_scan", [sname, "colsT", "carryD"], [dname], out=rv(dst), data0=dbc, data1=rv(src),
                                     initial=init, op0=ALU.mult, op1=ALU.add)
                                if ui == 0:
                                    lastc = (NBS - 1) if d_ == 0 else 0
                                    P.op("dve", "tensor_scalar", [dname, "flgD"], ["carryD"], out=carry[:, pr_, ri:ri + 1], in0=dst[:, lastc:lastc + 1],
                                         scalar1=flg[:, 0:1], scalar2=None, op0=ALU.mult)
                            if d_ == 0:
                                o_r = HB[:, 0, pr_, 0, blk0 + 1:blk0 + NBS + 1]
                                o_i = HB[:, 0, pr_, 1, blk0 + 1:blk0 + NBS + 1]
                                sl_ = slice(0, NBS)
                            elif s_ == 1:
                                o_r = HB[:, 1, pr_, 0, NBS - 1:NB - 1]
                                o_i = HB[:, 1, pr_, 1, NBS - 1:NB - 1]
                                sl_ = slice(0, NBS)
                            else:
                                o_r = HB[:, 1, pr_, 0, 0:NBS - 1]
                                o_i = HB[:, 1, pr_, 1, 0:NBS - 1]
                                sl_ = slice(1, NBS)
                            hn = "HB%d_%d" % (d_, pr_)
                            TT(["R7", "R3"], ["R9"], m1, g_r, cs, ALU.mult)
                            TT(["R8", "R4"], ["R10"], m2, g_i, sn, ALU.mult, "pool")
                            TT(["R9", "R10"], [hn], o_r, m1[:, sl_], m2[:, sl_], sop2)
                            TT(["R8", "R3"], ["R9"], m1, g_i, cs, ALU.mult)
                            TT(["R7", "R4"], ["R10"], m2, g_r, sn, ALU.mult, "pool")
                            TT(["R9", "R10"], [hn], o_i, m1[:, sl_], m2[:, sl_], sop)
                            if ui == 1:
                                zc = 0 if d_ == 0 else NB - 1
                                fc = NBS if d_ == 0 else NBS - 1
                                P.op("dve", "memset", [], [hn], HB[:, d_, pr_, :, zc:zc + 1], 0.0)
                                P.op("dve", "tensor_scalar", [hn, "flgD"], [hn], out=HB[:, d_, pr_, :, fc:fc + 1], in0=HB[:, d_, pr_, :, fc:fc + 1],
                                     scalar1=flg[:, 0:1], scalar2=None, op0=ALU.mult)
                if DST < 4:
                    continue
                hall = ["HB%d_%d" % (a, b_) for a in range(2) for b_ in range(4)]
                for s_ in range(2):
                    blk0 = s_ * NBS
                    for i in range(8):
                        pb = poq % 2
                        poq += 1
                        mms = []
                        for j in range(0, i + 1):
                            mms.append((Kbd[:, 0, i - j, :], u3[:, j, blk0:blk0 + NBS]))
                        for j in range(i, 8):
                            mms.append((Kbd[:, 1, j - i, :], u3[:, j, blk0:blk0 + NBS]))
                        for pr_ in range(4):
                            for ri in range(2):
                                mms.append((W2[:, 0, i + 1, ri, pr_, :], HB[:, 0, pr_, ri, blk0:blk0 + NBS]))
                                mms.append((W2[:, 1, 8 - i, ri, pr_, :], HB[:, 1, pr_, ri, blk0:blk0 + NBS]))
                        for mi, (l_, r_) in enumerate(mms):
                            P.mm(po[pb][:, 0:NBS], l_, r_, mi == 0, mi == len(mms) - 1, ["Kbd", "W2", "uft"] + hall, ["poD%d" % pb])
                        v = po[pb][:, 0:NBS]
                        P.op("act", "activation", ["poD%d" % pb], ["glD0"], out=gl[0][:], in_=v, func=AF.Square)
                        P.op("dve", "tensor_scalar", ["glD0"], ["glD0"], out=gl[0][:], in0=gl[0][:], scalar1=0.044715, scalar2=1.0, op0=ALU.mult, op1=ALU.add)
                        P.op("dve", "tensor_tensor", ["glD0", "poD%d" % pb], ["glD1"], out=gl[1][:], in0=gl[0][:], in1=v, op=ALU.mult)
                        P.op("act", "activation", ["glD1"], ["glD2"], out=gl[2][:], in_=gl[1][:], func=AF.Sigmoid, scale=1.5957691216057308)
                        P.op("dve", "tensor_tensor", ["glD2", "poD%d" % pb], ["stgD"], out=stg[:, :, i], in0=gl[2][:], in1=v, op=ALU.mult)
                    P.dma("sp", y5gT[ft * 128:(ft + 1) * 128, s_ * LSEG:(s_ + 1) * LSEG], stg[:].rearrange("p b i -> p (b i)"), ["stgD"], ["y5gT"])
        if cfg.get("dump"):
            for i_, o_ in enumerate(P.ops):
                print(i_, o_.eng, getattr(o_, 'desc', 'mm/dma') if hasattr(o_, 'desc') else '-', o_.reads, o_.writes)
        P.emit()
        P.limit = None

        with ExitStack() as st:
            sb = lambda n, s, d=F32: st.enter_context(nc.sbuf_tensor(n, list(s), d))
            gA = sb("gA", [128, 16])
            gM = sb("gM", [128, 16])
            P.dma("sp", gA[:], norm_attn, [], ["gA"])
            P.dma("sp", gM[:], norm_mem, [], ["gM"])
            wst2 = [sb("wst2_%d" % i, [128, 2048]) for i in range(2)]
            wbf2 = [sb("wbf2_%d" % i, [128, 2048], BF16) for i in range(2)]
            ci = 0
            ceng = _rr(["dve", "pool"])
            for (src, dst, gn_, gname, kdim, ncol) in ((w_glu, w_glu_bf, None, None, 1024, 1024), (w_out, w_out_bf, None, None, 2048, 2048),
                                                       (w_q, w_q_bf, gA, "gA", 2048, 2048), (w_k, w_k_bf, gM, "gM", 2048, 2048),
                                                       (w_v, w_v_bf, gM, "gM", 2048, 2048), (w_o, w_o_bf, None, None, 2048, 2048)):
                for kc in range(kdim // 128):
                    b = ci % 2
                    ci += 1
                    P.dma("sp", wst2[b][:, 0:ncol], src[kc * 128:(kc + 1) * 128, :], [], ["wst2_%d" % b])
                    if gn_ is None:
                        if ceng() == "dve":
                            P.op("dve", "tensor_copy", ["wst2_%d" % b], ["wbf2_%d" % b], out=wbf2[b][:, 0:ncol], in_=wst2[b][:, 0:ncol])
                        else:
                            P.op("act", "activation", ["wst2_%d" % b], ["wbf2_%d" % b], out=wbf2[b][:, 0:ncol], in_=wst2[b][:, 0:ncol], func=AF.Copy)
                    else:
                        P.op("dve", "tensor_scalar", ["wst2_%d" % b, gname], ["wbf2_%d" % b], out=wbf2[b][:, 0:ncol], in0=wst2[b][:, 0:ncol],
                             scalar1=gn_[:, kc:kc + 1], scalar2=None, op0=ALU.mult)
                    P.dma("sp", dst[:, :, kc, :].rearrange("g p n -> p g n"), wbf2[b][:, 0:ncol].rearrange("p (g n) -> p g n", n=512),
                          ["wbf2_%d" % b], [dst.tensor.name])
        P.emit()

        with ExitStack() as st:
            sb = lambda n, s, d=F32: st.enter_context(nc.sbuf_tensor(n, list(s), d))
            ps = lambda n, s, d=F32: st.enter_context(nc.psum_tensor(n, list(s), d))
            ident = sb("identM", [128, 128], BF16)
            P.op("pool", "memset", [], ["identM"], ident[:], 1.0)
            P.op("pool", "affine_select", ["identM"], ["identM"], out=ident[:], in_=ident[:], pattern=[[-1, 128]],
                 compare_op=ALU.is_equal, fill=0.0, base=0, channel_multiplier=1)
            mt = sb("mtM", [128, 2048])
            junk = sb("junkM", [128, 2048], BF16)
            ssm = sb("ssM", [128, 1])
            mn = sb("mnM", [128, 2048], BF16)
            memT = sb("memT", [128, 16, 256], BF16)
            wck = [sb("wckM%d" % i, [128, 16, 512], BF16) for i in range(2)]
            kTs = sb("kTs", [128, 16, 256], BF16)
            vs = sb("vs", [128, 2, 2048], BF16)
            ptm = [ps("ptM%d" % i, [128, 1024], BF16) for i in range(2)]
            pmm_ = [ps("pmM%d" % i, [128, 512]) for i in range(2)]
            tq = 0
            pq = 0
            wq_ = 0
            for sg in range(2):
                for mc in range(2):
                    P.dma("sp", mt[:], mem_in[sg, mc * 128:(mc + 1) * 128, :], [], ["mtM"])
                    P.op("act", "activation", ["mtM"], ["junkM", "ssM"], out=junk[:], in_=mt[:], func=AF.Square, accum_out=ssm[:])
                    P.op("dve", "tensor_scalar", ["ssM"], ["ssM"], out=ssm[:], in0=ssm[:], scalar1=1.0 / D, scalar2=EPS, op0=ALU.mult, op1=ALU.add)
                    P.op("act", "activation", ["ssM"], ["ssM"], out=ssm[:], in_=ssm[:], func=AF.Sqrt)
                    P.op("dve", "reciprocal", ["ssM"], ["ssM"], out=ssm[:], in_=ssm[:])
                    P.op("dve", "tensor_scalar", ["mtM", "ssM"], ["mnM"], out=mn[:], in0=mt[:], scalar1=ssm[:, 0:1], scalar2=None, op0=ALU.mult)
                    for q4 in range(4):
                        pb = tq % 2
                        tq += 1
                        for j in range(4):
                            kc = q4 * 4 + j
                            P.tr(ptm[pb][:, j * 128:(j + 1) * 128], mn[:, kc * 128:(kc + 1) * 128], ident[:], ["mnM", "identM"], ["ptM%d" % pb])
                        P.op("act", "activation", ["ptM%d" % pb], ["memT"], out=memT[:, q4 * 4:(q4 + 1) * 4, mc * 128:(mc + 1) * 128],
                             in_=ptm[pb][:, 0:512].rearrange("p (j n) -> p j n", n=128), func=AF.Copy)
                for g in range(4):
                    wb = wq_ % 2
                    wq_ += 1
                    P.dma("sp", wck[wb][:], w_k_bf[g], ["w_k_bf"], ["wckM%d" % wb])
                    for mcc in range(4):
                        pb = pq % 2
                        pq += 1
                        for kc in range(16):
                            P.mm(pmm_[pb][:, 0:256], wck[wb][:, kc, mcc * 128:(mcc + 1) * 128], memT[:, kc, :], kc == 0, kc == 15,
                                 ["wckM%d" % wb, "memT"], ["pmM%d" % pb])
                        P.op("act", "activation", ["pmM%d" % pb], ["kTs"], out=kTs[:, g * 4 + mcc, :], in_=pmm_[pb][:, 0:256], func=AF.Copy)
                for g in range(4):
                    wb = wq_ % 2
                    wq_ += 1
                    P.dma("sp", wck[wb][:], w_v_bf[g], ["w_v_bf"], ["wckM%d" % wb])
                    for mc in range(2):
                        pb = pq % 2
                        pq += 1
                        for kc in range(16):
                            P.mm(pmm_[pb][:], memT[:, kc, mc * 128:(mc + 1) * 128], wck[wb][:, kc, :], kc == 0, kc == 15,
                                 ["wckM%d" % wb, "memT"], ["pmM%d" % pb])
                        P.op("dve", "tensor_copy", ["pmM%d" % pb], ["vs"], out=vs[:, mc, g * 512:(g + 1) * 512], in_=pmm_[pb][:])
                P.dma("sp", kT_d[sg], kTs[:], ["kTs"], ["kT_d"])
                P.dma("sp", v_d[sg], vs[:], ["vs"], ["v_d"])
        P.emit()

        with ExitStack() as st:
            sb = lambda n, s, d=F32: st.enter_context(nc.sbuf_tensor(n, list(s), d))
            ps = lambda n, s, d=F32: st.enter_context(nc.psum_tensor(n, list(s), d))
            wgl = sb("wgl", [128, 8, 1024], BF16)
            for g_ in range(2):
                P.dma("sp", wgl[:, :, g_ * 512:(g_ + 1) * 512], w_glu_bf[g_], ["w_glu_bf"], ["wgl"])
            onesb = sb("onesbE", [128, 128], BF16)
            P.op("pool", "memset", [], ["onesbE"], onesb[:], 1.0)
            g5 = sb("g5", [128, 8])
            P.dma("sp", g5[:], s5_ncol, [], ["g5"])
            y5 = [sb("y5E%d" % i, [128, 8, 512], BF16) for i in range(2)]
            y2 = sb("y2E", [128, 8, 512])
            sig = [sb("sigE%d" % i, [128, 512]) for i in range(2)]
            sqb = [sb("sqE%d" % i, [128, 512], BF16) for i in range(2)]
            rs = sb("rsE", [128, 512])
            pg = [ps("pgE%d" % i, [128, 512]) for i in range(2)]
            pss = ps("pssE", [128, 512])
            for s in range(NST):
                t0 = s * 512
                b = s % 2
                P.dma("sp", y5[b][:], y5gT[:, t0:t0 + 512].rearrange("(f p) t -> p f t", p=128), ["y5gT"], ["y5E%d" % b])
                for n_ in range(8):
                    q = n_ % 2
                    for kc in range(8):
                        P.mm(pg[q][:], wgl[:, kc, n_ * 128:(n_ + 1) * 128], y5[b][:, kc, :], kc == 0, kc == 7, ["wgl", "y5E%d" % b], ["pgE%d" % q])
                    P.op("act", "activation", ["pgE%d" % q], ["sigE%d" % q], out=sig[q][:], in_=pg[q][:], func=AF.Sigmoid)
                    P.op("dve", "tensor_tensor", ["y5E%d" % b, "sigE%d" % q], ["y2E"], out=y2[:, n_, :], in0=y5[b][:, n_, :], in1=sig[q][:], op=ALU.mult)
                    P.op("act", "activation", ["y2E"], ["sqE%d" % q], out=sqb[q][:], in_=y2[:, n_, :], func=AF.Square)
                    P.mm(pss[:], onesb[:], sqb[q][:], n_ == 0, n_ == 7, ["onesbE", "sqE%d" % q], ["pssE"])
                P.op("dve", "tensor_scalar", ["pssE"], ["rsE"], out=rs[:], in0=pss[:], scalar1=1.0 / 1024, scalar2=EPS, op0=ALU.mult, op1=ALU.add)
                P.op("act", "activation", ["rsE"], ["rsE"], out=rs[:], in_=rs[:], func=AF.Sqrt)
                P.op("dve", "reciprocal", ["rsE"], ["rsE"], out=rs[:], in_=rs[:])
                for n_ in range(8):
                    P.op("dve", "scalar_tensor_tensor", ["y2E", "g5", "rsE"], ["y5E%d" % b], out=y5[b][:, n_, :], in0=y2[:, n_, :],
                         scalar=g5[:, n_:n_ + 1], in1=rs[:], op0=ALU.mult, op1=ALU.mult)
                P.dma("sp", y5nT[:, t0:t0 + 512].rearrange("(f p) t -> p f t", p=128), y5[b][:], ["y5E%d" % b], ["y5nT"])
        P.emit()

        NTT = LT // 128
        with ExitStack() as st:
            sb = lambda n, s, d=F32: st.enter_context(nc.sbuf_tensor(n, list(s), d))
            ps = lambda n, s, d=F32: st.enter_context(nc.psum_tensor(n, list(s), d))
            ident = sb("identE", [128, 128], BF16)
            P.op("pool", "memset", [], ["identE"], ident[:], 1.0)
            P.op("pool", "affine_select", ["identE"], ["identE"], out=ident[:], in_=ident[:], pattern=[[-1, 128]],
                 compare_op=ALU.is_equal, fill=0.0, base=0, channel_multiplier=1)
            onesb = sb("onesb2", [128, 128], BF16)
            P.op("pool", "memset", [], ["onesb2"], onesb[:], 1.0)
            gff = sb("gff", [128, 2048])
            P.dma("sp", gff[:], nffn_rep, [], ["gff"])
            wrs = sb("wrs", [128, 16, 16])
            wrb = sb("wrb", [128, 16, 16], BF16)
            P.dma("sp", wrs[:], w_router_l, [], ["wrs"])
            P.op("dve", "tensor_copy", ["wrs"], ["wrb"], out=wrb[:], in_=wrs[:])
            zrow = sb("zrow", [128, 2048])
            zrowb = sb("zrowb", [128, 2048], BF16)
            P.op("pool", "memset", [], ["zrow"], zrow[:], 0.0)
            P.op("pool", "memset", [], ["zrowb"], zrowb[:], 0.0)
            for r_ in range(0, CAPL, 128):
                for g_ in range(4):
                    P.dma("sp", acc_l[g_][LT + r_:LT + r_ + 128, :], zrow[:, 0:512], ["zrow"], ["acc%d" % g_])
                P.dma("sp", hn3_bf[LT + r_:LT + r_ + 128, :], zrowb[:], ["zrowb"], ["hn3_bf"])
            wch = [sb("wch%d" % i, [128, 16, 512], BF16) for i in range(2)]
            x1 = sb("x1", [128, 4, 2048])
            ysd = sb("ysd", [128, 4, 1024], BF16)
            yT = sb("yT", [128, 16, 512], BF16)
            hn = sb("hnE", [128, 2048], BF16)
            junk = sb("junkE", [128, 2048], BF16)
            ss = sb("ssE", [128, 1])
            hnT = sb("hnT", [128, 16, 512], BF16)
            qT = sb("qT", [128, 16, 512], BF16)
            kTs = sb("kTs2", [128, 16, 256], BF16)
            vs = sb("vs2", [128, 2, 2048], BF16)
            eT = [sb("eT%d" % i, [128, 512], BF16) for i in range(2)]
            rden = sb("rden", [128, 512])
            hT3 = sb("hT3", [128, 16, 128], BF16)
            pe_ = sb("peE", [128, 16])
            psum_ = sb("psumE", [128, 1])
            ptr = [ps("ptrE%d" % i, [128, 1024], BF16) for i in range(2)]
            pmm = [ps("pmmE%d" % i, [128, 512]) for i in range(4)]
            prt = ps("prtE", [128, 512])
            tq = [0]
            pq = [0]
            wq_ = [0]
            cpe = _rr(["act", "dve"])

            def nxt(c, n):
                v = c[0] % n
                c[0] += 1
                return v

            def rms_to(tt, out_ap, names_w, extra=None):
                P.op("act", "activation", ["x1_%d" % tt], ["junkE", "ssE"], out=junk[:], in_=x1[:, tt, :], func=AF.Square, accum_out=ss[:])
                P.op("dve", "tensor_scalar", ["ssE"], ["ssE"], out=ss[:], in0=ss[:], scalar1=1.0 / D, scalar2=EPS, op0=ALU.mult, op1=ALU.add)
                P.op("act", "activation", ["ssE"], ["ssE"], out=ss[:], in_=ss[:], func=AF.Sqrt)
                P.op("dve", "reciprocal", ["ssE"], ["ssE"], out=ss[:], in_=ss[:])
                if extra is None:
                    P.op("dve", "tensor_scalar", ["x1_%d" % tt, "ssE"], names_w, out=out_ap, in0=x1[:, tt, :], scalar1=ss[:, 0:1], scalar2=None, op0=ALU.mult)
                else:
                    P.op("dve", "scalar_tensor_tensor", ["x1_%d" % tt, "ssE", "gff"], names_w, out=out_ap, in0=x1[:, tt, :], scalar=ss[:, 0:1],
                         in1=extra, op0=ALU.mult, op1=ALU.mult)

            def transposes16(src_tile, src_name, dst_fn, dst_name):
                for q4 in range(4):
                    pb = nxt(tq, 2)
                    for j in range(4):
                        kc = q4 * 4 + j
                        P.tr(ptr[pb][:, j * 128:(j + 1) * 128], src_tile[:, kc * 128:(kc + 1) * 128], ident[:], [src_name, "identE"], ["ptrE%d" % pb])
                    ce = cpe()
                    src = ptr[pb][:, 0:512].rearrange("p (j n) -> p j n", n=128)
                    if ce == "act":
                        P.op("act", "activation", ["ptrE%d" % pb], [dst_name], out=dst_fn(q4), in_=src, func=AF.Copy)
                    else:
                        P.op("dve", "tensor_copy", ["ptrE%d" % pb], [dst_name], out=dst_fn(q4), in_=src)

            def proj_tok(w_d, wname, lhs_tile, lhs_name):
                for dch in range(4):
                    wb = nxt(wq_, 2)
                    P.dma("sp", wch[wb][:], w_d[dch], [wname], ["wch%d" % wb])
                    for tt in range(4):
                        pb = nxt(pq, 4)
                        for kc in range(16):
                            P.mm(pmm[pb][:], lhs_tile[:, kc, tt * 128:(tt + 1) * 128], wch[wb][:, kc, :], kc == 0, kc == 15,
                                 [lhs_name, "wch%d" % wb], ["pmmE%d" % pb])
                        P.op("dve", "tensor_tensor", ["x1_%d" % tt, "pmmE%d" % pb], ["x1_%d" % tt], out=x1[:, tt, dch * 512:(dch + 1) * 512],
                             in0=x1[:, tt, dch * 512:(dch + 1) * 512], in1=pmm[pb][:], op=ALU.add)

            cur_seg = -1
            for s in range(NST):
                t0 = s * 512
                sg = t0 // LSEG
                if sg != cur_seg:
                    cur_seg = sg
                    P.dma("sp", kTs[:], kT_d[sg], ["kT_d"], ["kTs2"])
                    P.dma("sp", vs[:], v_d[sg], ["v_d"], ["vs2"])
                for tt in range(4):
                    P.dma("sp", x1[:, tt, :], x_in[t0 + tt * 128:t0 + (tt + 1) * 128, :], [], ["x1_%d" % tt])
                P.dma("sp", ysd[:], yssd[t0:t0 + 512, :].rearrange("(t p) c -> p t c", p=128), ["yssd"], ["ysd"])
                P.dma("sp", yT[:, 8:16, :], y5nT[:, t0:t0 + 512].rearrange("(f p) t -> p f t", p=128), ["y5nT"], ["yT"])
                for tt in range(4):
                    for q2 in range(2):
                        pb = nxt(tq, 2)
                        for j in range(4):
                            kc = q2 * 4 + j
                            P.tr(ptr[pb][:, j * 128:(j + 1) * 128], ysd[:, tt, kc * 128:(kc + 1) * 128], ident[:], ["ysd", "identE"], ["ptrE%d" % pb])
                        P.op("act", "activation", ["ptrE%d" % pb], ["yT"], out=yT[:, q2 * 4:(q2 + 1) * 4, tt * 128:(tt + 1) * 128],
                             in_=ptr[pb][:, 0:512].rearrange("p (j n) -> p j n", n=128), func=AF.Copy)
                proj_tok(w_out_bf, "w_out_bf", yT, "yT")
                for tt in range(4):
                    rms_to(tt, hn[:], ["hnE"])
                    transposes16(hn, "hnE", lambda q4, tt=tt: hnT[:, q4 * 4:(q4 + 1) * 4, tt * 128:(tt + 1) * 128], "hnT")
                for g in range(4):
                    wb = nxt(wq_, 2)
                    P.dma("sp", wch[wb][:], w_q_bf[g], ["w_q_bf"], ["wch%d" % wb])
                    for mcc in range(4):
                        pb = nxt(pq, 4)
                        for kc in range(16):
                            P.mm(pmm[pb][:], wch[wb][:, kc, mcc * 128:(mcc + 1) * 128], hnT[:, kc, :], kc == 0, kc == 15,
                                 ["wch%d" % wb, "hnT"], ["pmmE%d" % pb])
                        P.op("act", "activation", ["pmmE%d" % pb], ["qT"], out=qT[:, g * 4 + mcc, :], in_=pmm[pb][:], func=AF.Copy)
                for h in range(4):
                    for mck in range(2):
                        pb = nxt(pq, 4)
                        for dc in range(4):
                            P.mm(pmm[pb][:], kTs[:, h * 4 + dc, mck * 128:(mck + 1) * 128], qT[:, h * 4 + dc, :], dc == 0, dc == 3,
                                 ["kTs2", "qT"], ["pmmE%d" % pb])
                        P.op("act", "activation", ["pmmE%d" % pb], ["eT%d" % mck], out=eT[mck][:], in_=pmm[pb][:], func=AF.Exp, scale=512 ** -0.5)
                    pb = nxt(pq, 4)
                    for mck in range(2):
                        P.mm(pmm[pb][:], onesb[:], eT[mck][:], mck == 0, mck == 1, ["onesb2", "eT%d" % mck], ["pmmE%d" % pb])
                    P.op("dve", "reciprocal", ["pmmE%d" % pb], ["rden"], out=rden[:], in_=pmm[pb][:])
                    for dvc in range(4):
                        pb = nxt(pq, 4)
                        for mck in range(2):
                            P.mm(pmm[pb][:], vs[:, mck, h * 512 + dvc * 128:h * 512 + (dvc + 1) * 128], eT[mck][:], mck == 0, mck == 1,
                                 ["vs2", "eT%d" % mck], ["pmmE%d" % pb])
                        P.op("dve", "tensor_tensor", ["pmmE%d" % pb, "rden"], ["hnT"], out=hnT[:, h * 4 + dvc, :], in0=pmm[pb][:], in1=rden[:], op=ALU.mult)
                proj_tok(w_o_bf, "w_o_bf", hnT, "hnT")
                for tt in range(4):
                    r0 = t0 + tt * 128
                    for g_ in range(4):
                        P.dma("sp", acc_l[g_][r0:r0 + 128, :], x1[:, tt, g_ * 512:(g_ + 1) * 512], ["x1_%d" % tt], ["acc%d" % g_])
                    rms_to(tt, hn[:], ["hnE"], extra=gff[:])
                    P.dma("sp", hn3_bf[r0:r0 + 128, :], hn[:], ["hnE"], ["hn3_bf"])
                    transposes16(hn, "hnE", lambda q4: hT3[:, q4 * 4:(q4 + 1) * 4, :], "hT3")
                    for kc in range(16):
                        P.mm(prt[:, 0:16], hT3[:, kc, :], wrb[:, kc, :], kc == 0, kc == 15, ["hT3", "wrb"], ["prtE"])
                    P.op("act", "activation", ["prtE"], ["peE", "psumE"], out=pe_[:], in_=prt[:, 0:16], func=AF.Exp, accum_out=psum_[:])
                    P.op("dve", "reciprocal", ["psumE"], ["psumE"], out=psum_[:], in_=psum_[:])
                    P.op("dve", "tensor_scalar", ["peE", "psumE"], ["peE"], out=pe_[:], in0=pe_[:], scalar1=psum_[:, 0:1], scalar2=None, op0=ALU.mult)
                    P.dma("sp", probs_loc[r0:r0 + 128, :], pe_[:], ["peE"], ["probs_loc"])
        P.emit()

        GSZ = cfg.get("GSZ", 1)
        NG = GSZ * LT
        JG = NG // 128
        CAPG = NG // 8
        NCHK = CAPL // 128
        if GSZ > 1:
            P.add("pool", lambda e: e.collective_compute("AllGather", ALU.bypass, replica_groups=cfg["groups"], ins=[probs_loc], outs=[probs_all]),
                  reads=["probs_loc"], writes=["probs_all"], own_sem=P.csem)
            pall = probs_all
        else:
            pall = probs_loc
        with ExitStack() as st:
            sb = lambda n, s, d=F32: st.enter_context(nc.sbuf_tensor(n, list(s), d))
            ps = lambda n, s, d=F32: st.enter_context(nc.psum_tensor(n, list(s), d))
            PA = sb("PA", [128, JG, 16])
            cmpt = sb("cmpt", [128, JG, 16])
            P.dma("sp", PA[:], pall.rearrange("(p j) e -> p j e", j=JG), ["probs_all", "probs_loc"], ["PA"])
            onesF = sb("onesFF", [128, 128])
            P.op("pool", "memset", [], ["onesFF"], onesF[:], 1.0)
            lo, hi, mid, ge, dd, cntp = [sb("bs_" + n_, [128, 16]) for n_ in ("lo", "hi", "mid", "ge", "dd", "cntp")]
            P.op("dve", "memset", [], ["bs_lo"], lo[:], 0.0)
            P.op("dve", "memset", [], ["bs_hi"], hi[:], 1.0)
            ptot = ps("ptot", [128, 512])
            for it_ in range(32):
                P.op("dve", "tensor_tensor", ["bs_lo", "bs_hi"], ["bs_mid"], out=mid[:], in0=lo[:], in1=hi[:], op=ALU.add)
                P.op("dve", "tensor_scalar", ["bs_mid"], ["bs_mid"], out=mid[:], in0=mid[:], scalar1=0.5, scalar2=None, op0=ALU.mult)
                P.op("dve", "tensor_tensor", ["PA", "bs_mid"], ["cmpt"], out=cmpt[:], in0=PA[:], in1=mid[:].unsqueeze(1).to_broadcast([128, JG, 16]), op=ALU.is_ge)
                P.op("dve", "tensor_reduce", ["cmpt"], ["bs_cntp"], out=cntp[:], in_=cmpt[:].rearrange("p j e -> p e j"), op=ALU.add, axis=AX.X)
                P.mm(ptot[:, 0:16], onesF[:], cntp[:], True, True, ["onesFF", "bs_cntp"], ["ptot"])
                P.op("dve", "tensor_scalar", ["ptot"], ["bs_ge"], out=ge[:], in0=ptot[:, 0:16], scalar1=float(CAPG), scalar2=None, op0=ALU.is_ge)
                P.op("dve", "tensor_tensor", ["bs_mid", "bs_lo"], ["bs_dd"], out=dd[:], in0=mid[:], in1=lo[:], op=ALU.subtract)
                P.op("dve", "tensor_tensor", ["bs_dd", "bs_ge"], ["bs_dd"], out=dd[:], in0=dd[:], in1=ge[:], op=ALU.mult)
                P.op("dve", "tensor_tensor", ["bs_lo", "bs_dd"], ["bs_lo"], out=lo[:], in0=lo[:], in1=dd[:], op=ALU.add)
                P.op("dve", "tensor_tensor", ["bs_hi", "bs_mid"], ["bs_dd"], out=dd[:], in0=hi[:], in1=mid[:], op=ALU.subtract)
                P.op("dve", "tensor_tensor", ["bs_dd", "bs_ge"], ["bs_dd"], out=dd[:], in0=dd[:], in1=ge[:], op=ALU.mult)
                P.op("dve", "tensor_tensor", ["bs_mid", "bs_dd"], ["bs_hi"], out=hi[:], in0=mid[:], in1=dd[:], op=ALU.add)
            PL = sb("PL", [128, NTT, 16])
            msk = sb("msk", [128, NTT, 16])
            gat = sb("gat", [128, NTT, 16])
            mskb = sb("mskb", [128, NTT, 16], BF16)
            P.dma("sp", PL[:], probs_loc.rearrange("(t p) e -> p t e", p=128), ["probs_loc"], ["PL"])
            P.op("dve", "tensor_tensor", ["PL", "bs_lo"], ["msk"], out=msk[:], in0=PL[:], in1=lo[:].unsqueeze(1).to_broadcast([128, NTT, 16]), op=ALU.is_ge)
            P.op("dve", "tensor_tensor", ["PL", "msk"], ["gat"], out=gat[:], in0=PL[:], in1=msk[:], op=ALU.mult)
            P.op("dve", "tensor_copy", ["msk"], ["mskb"], out=mskb[:], in_=msk[:])
            triS = sb("triS", [128, 128], BF16)
            onesb = sb("onesbF", [128, 128], BF16)
            P.op("pool", "memset", [], ["onesbF"], onesb[:], 1.0)
            P.op("pool", "memset", [], ["triS"], triS[:], 1.0)
            P.op("pool", "affine_select", ["triS"], ["triS"], out=triS[:], in_=triS[:], pattern=[[1, 128]], compare_op=ALU.is_ge, fill=0.0,
                 base=-1, channel_multiplier=-1)
            posi = sb("posi", [128, NTT, 16])
            tots = sb("tots", [128, NTT, 16])
            cum = sb("cum", [128, NTT, 16])
            pps = [ps("ppsF%d" % i, [128, 512]) for i in range(2)]
            mflat = mskb[:].rearrange("p t e -> p (t e)")
            NTE = NTT * 16
            for c0 in range(0, NTE, 512):
                c1 = min(c0 + 512, NTE)
                P.mm(pps[0][:, 0:c1 - c0], triS[:], mflat[:, c0:c1], True, True, ["triS", "mskb"], ["ppsF0"])
                P.op("dve", "tensor_copy", ["ppsF0"], ["posi"], out=posi[:].rearrange("p t e -> p (t e)")[:, c0:c1], in_=pps[0][:, 0:c1 - c0])
                P.mm(pps[1][:, 0:c1 - c0], onesb[:], mflat[:, c0:c1], True, True, ["onesbF", "mskb"], ["ppsF1"])
                P.op("dve", "tensor_copy", ["ppsF1"], ["tots"], out=tots[:].rearrange("p t e -> p (t e)")[:, c0:c1], in_=pps[1][:, 0:c1 - c0])
            onecol = sb("onecol", [128, 1])
            P.op("dve", "memset", [], ["onecol"], onecol[:], 1.0)
            for e_ in range(16):
                P.op("dve", "tensor_tensor_scan", ["tots", "onecol"], ["cum"], out=cum[:, :, e_], data0=onecol[:, 0:1].to_broadcast([128, NTT]),
                     data1=tots[:, :, e_], initial=0.0, op0=ALU.mult, op1=ALU.add)
            P.op("dve", "tensor_tensor", ["cum", "tots"], ["cum"], out=cum[:], in0=cum[:], in1=tots[:], op=ALU.subtract)
            P.op("dve", "tensor_tensor", ["cum", "posi"], ["posi"], out=posi[:], in0=posi[:], in1=cum[:], op=ALU.add)
            P.op("dve", "tensor_scalar", ["posi"], ["posi"], out=posi[:], in0=posi[:], scalar1=1.0, scalar2=None, op0=ALU.add)
            P.op("dve", "tensor_tensor", ["posi", "msk"], ["posi"], out=posi[:], in0=posi[:], in1=msk[:], op=ALU.mult)
            P.op("dve", "tensor_scalar", ["posi"], ["posi"], out=posi[:], in0=posi[:], scalar1=-1.0, scalar2=None, op0=ALU.add)
            G5 = sb("G5", [128, NTT, 16, 8], BF16)
            P.op("pool", "memset", [], ["G5"], G5[:], 0.0)
            tvf = sb("tvf", [128, NTT, 16])
            P.op("pool", "iota", [], ["tvf"], tvf[:], pattern=[[0, NTT], [0, 16]], base=0, channel_multiplier=1, allow_small_or_imprecise_dtypes=True)
            P.op("dve", "tensor_copy", ["tvf", "G5"], ["G5"], out=G5[:, :, :, 0], in_=tvf[:])
            P.op("pool", "iota", ["tvf"], ["tvf"], tvf[:], pattern=[[128, NTT], [0, 16]], base=0, channel_multiplier=0, allow_small_or_imprecise_dtypes=True)
            P.op("dve", "tensor_copy", ["tvf", "G5"], ["G5"], out=G5[:, :, :, 1], in_=tvf[:])
            P.op("dve", "memset", ["G5"], ["G5"], G5[:, :, :, 5], 1.0)
            gres = sb("gres", [128, NTT, 16])
            P.op("dve", "tensor_copy", ["gat", "G5"], ["G5"], out=G5[:, :, :, 2], in_=gat[:])
            P.op("dve", "tensor_tensor", ["gat", "G5"], ["gres"], out=gres[:], in0=gat[:], in1=G5[:, :, :, 2], op=ALU.subtract)
            P.op("dve", "tensor_copy", ["gres", "G5"], ["G5"], out=G5[:, :, :, 3], in_=gres[:])
            P.op("dve", "tensor_tensor", ["gres", "G5"], ["gres"], out=gres[:], in0=gres[:], in1=G5[:, :, :, 3], op=ALU.subtract)
            P.op("dve", "tensor_copy", ["gres", "G5"], ["G5"], out=G5[:, :, :, 4], in_=gres[:])
            iotaS = sb("iotaS", [128, CAPL])
            P.op("pool", "iota", [], ["iotaS"], iotaS[:], pattern=[[1, CAPL]], base=0, channel_multiplier=0, allow_small_or_imprecise_dtypes=True)
            dmy = sb("dmy", [128, NCHK])
            P.op("pool", "iota", [], ["dmy"], dmy[:], pattern=[[128, NCHK]], base=LT, channel_multiplier=1, allow_small_or_imprecise_dtypes=True)
            oh = [sb("oh%d" % i, [128, CAPL], BF16) for i in range(2)]
            pI = [ps("pI%d" % i, [128, 64, 8]) for i in range(2)]
            rr_ = sb("rrF", [128, NCHK, 8])
            ia = sb("iaF", [128, NCHK])
            ii = sb("iiF", [128, NCHK], I32)
            gs = sb("gsF", [128, NCHK])
            ohq = 0
            for e_ in range(16):
                pb = e_ % 2
                first = True
                for t in range(NTT):
                    ob = ohq % 2
                    ohq += 1
                    P.op("dve", "tensor_scalar", ["iotaS", "posi"], ["oh%d" % ob], out=oh[ob][:], in0=iotaS[:], scalar1=posi[:, t, e_:e_ + 1],
                         scalar2=None, op0=ALU.is_equal)
                    for ch in range(NCHK):
                        P.add("pe", (lambda pb, ch, ob, t, e_, first, lastf: lambda e: e.matmul(
                            pI[pb][:, ch, :], lhsT=oh[ob][:, ch * 128:(ch + 1) * 128], rhs=G5[:, t, e_, :], start=first, stop=lastf,
                            skip_group_check=True))(pb, ch, ob, t, e_, first, (t == NTT - 1 and ch == NCHK - 1)),
                            reads=["oh%d" % ob, "G5"], writes=["pI%d" % pb], banks=["pI%d" % pb])
                        first = False
                P.op("dve", "tensor_copy", ["pI%d" % pb], ["rrF"], out=rr_[:], in_=pI[pb][:, 0:NCHK, :])
                P.op("dve", "tensor_tensor", ["rrF"], ["iaF"], out=ia[:], in0=rr_[:, :, 0], in1=rr_[:, :, 1], op=ALU.add)
                P.op("dve", "tensor_tensor", ["iaF", "dmy"], ["iaF"], out=ia[:], in0=ia[:], in1=dmy[:], op=ALU.subtract)
                P.op("dve", "tensor_tensor", ["iaF", "rrF"], ["iaF"], out=ia[:], in0=ia[:], in1=rr_[:, :, 5], op=ALU.mult)
                P.op("dve", "tensor_tensor", ["iaF", "dmy"], ["iaF"], out=ia[:], in0=ia[:], in1=dmy[:], op=ALU.add)
                P.op("dve", "tensor_copy", ["iaF"], ["iiF"], out=ii[:], in_=ia[:])
                P.op("dve", "tensor_tensor", ["rrF"], ["gsF"], out=gs[:], in0=rr_[:, :, 2], in1=rr_[:, :, 3], op=ALU.add)
                P.op("dve", "tensor_tensor", ["rrF", "gsF"], ["gsF"], out=gs[:], in0=gs[:], in1=rr_[:, :, 4], op=ALU.add)
                P.dma("sp", idx_d[e_], ii[:], ["iiF"], ["idx_d"])
                P.dma("sp", cnt_out[e_], ii[:], ["iiF"], ["cnt_out"])
                P.dma("sp", gate_d[e_], gs[:], ["gsF"], ["gate_d"])
        P.emit()

        with ExitStack() as st:
            sb = lambda n, s, d=F32: st.enter_context(nc.sbuf_tensor(n, list(s), d))
            ps = lambda n, s, d=F32: st.enter_context(nc.psum_tensor(n, list(s), d))
            ident = sb("identX", [128, 128], BF16)
            P.op("pool", "memset", [], ["identX"], ident[:], 1.0)
            P.op("pool", "affine_select", ["identX"], ["identX"], out=ident[:], in_=ident[:], pattern=[[-1, 128]],
                 compare_op=ALU.is_equal, fill=0.0, base=0, channel_multiplier=1)
            idxt = [sb("idxt%d" % i, [128, NCHK], I32) for i in range(2)]
            gtt = [sb("gtt%d" % i, [128, NCHK]) for i in range(2)]
            xe = [sb("xe%d" % i, [128, 2048], BF16) for i in range(2)]
            xeT = sb("xeT", [128, 16, CAPL], BF16)
            heT = sb("heT", [128, 16, CAPL], BF16)
            stw = sb("stw", [128, 16, 256])
            wb_ = [sb("wbX%d" % i, [128, 16, 256], BF16) for i in range(4)]
            sgl = [sb("sglX%d" % i, [128, 512]) for i in range(2)]
            yst = [sb("ystX%d" % i, [128, 512], BF16) for i in range(3)]
            wbd = [sb("wbdX%d" % i, [128, 16, 512], BF16) for i in range(2)]
            ptr = [ps("ptrX%d" % i, [128, 1024], BF16) for i in range(2)]
            pg = [ps("pgX%d" % i, [128, 512]) for i in range(2)]
            pu = [ps("puX%d" % i, [128, 512]) for i in range(2)]
            py = [ps("pyX%d" % i, [128, 512]) for i in range(2)]
            cgs = [(c0, min(c0 + 512, CAPL)) for c0 in range(0, CAPL, 512)]
            cnt = {"tq": 0, "wq": 0, "gq": 0, "yq": 0, "sq": 0, "ysq": 0, "cast": 0, "xq": 0, "wdq": 0}

            def nx(k_, n):
                v = cnt[k_] % n
                cnt[k_] += 1
                return v

            def load_w(src_ap, name):
                wi = nx("wq", 4)
                P.dma("sp", stw[:], src_ap, [], ["stw"])
                ce = ("act", "dve")[nx("cast", 2)]
                if ce == "act":
                    P.op("act", "activation", ["stw"], ["wbX%d" % wi], out=wb_[wi][:], in_=stw[:], func=AF.Copy)
                else:
                    P.op("dve", "tensor_copy", ["stw"], ["wbX%d" % wi], out=wb_[wi][:], in_=stw[:])
                return wi

            prev_sc = {}
            for e_ in range(cfg.get("nexp", 16)):
                ib = e_ % 2
                P.dma("sp", idxt[ib][:], idx_d[e_], ["idx_d"], ["idxt%d" % ib])
                P.dma("sp", gtt[ib][:], gate_d[e_], ["gate_d"], ["gtt%d" % ib])
                for ch in range(NCHK):
                    xb = nx("xq", 2)
                    P.add("pool", (lambda xb, ib, ch: lambda e: e.indirect_dma_start(
                        out=xe[xb][:], out_offset=None, in_=hn3_bf[:, :],
                        in_offset=bass.IndirectOffsetOnAxis(ap=idxt[ib][:, ch:ch + 1], axis=0)))(xb, ib, ch),
                        reads=["idxt%d" % ib, "hn3_bf"], writes=["xe%d" % xb], dma=True)
                    for q4 in range(4):
                        pb = nx("tq", 2)
                        for j in range(4):
                            kc = q4 * 4 + j
                            P.tr(ptr[pb][:, j * 128:(j + 1) * 128], xe[xb][:, kc * 128:(kc + 1) * 128], ident[:], ["xe%d" % xb, "identX"], ["ptrX%d" % pb])
                        o_ap = xeT[:, q4 * 4:(q4 + 1) * 4, ch * 128:(ch + 1) * 128]
                        i_ap = ptr[pb][:, 0:512].rearrange("p (j n) -> p j n", n=128)
                        if q4 % 2 == 0:
                            P.op("act", "activation", ["ptrX%d" % pb], ["xeT"], out=o_ap, in_=i_ap, func=AF.Copy)
                        else:
                            P.op("dve", "tensor_copy", ["ptrX%d" % pb], ["xeT"], out=o_ap, in_=i_ap)
                for fg in range(8):
                    wg_i = load_w(w_gate[e_].rearrange("(kc p) f -> p kc f", p=128)[:, :, fg * 256:(fg + 1) * 256], "g")
                    wu_i = load_w(w_up[e_].rearrange("(kc p) f -> p kc f", p=128)[:, :, fg * 256:(fg + 1) * 256], "u")
                    for fc2 in range(2):
                        for (c0, c1) in cgs:
                            gb = nx("gq", 2)
                            for kc in range(16):
                                P.mm(pg[gb][:, 0:c1 - c0], wb_[wg_i][:, kc, fc2 * 128:(fc2 + 1) * 128], xeT[:, kc, c0:c1], kc == 0, kc == 15,
                                     ["wbX%d" % wg_i, "xeT"], ["pgX%d" % gb])
                            for kc in range(16):
                                P.mm(pu[gb][:, 0:c1 - c0], wb_[wu_i][:, kc, fc2 * 128:(fc2 + 1) * 128], xeT[:, kc, c0:c1], kc == 0, kc == 15,
                                     ["wbX%d" % wu_i, "xeT"], ["puX%d" % gb])
                            sb_i = nx("sq", 2)
                            P.op("act", "activation", ["pgX%d" % gb], ["sglX%d" % sb_i], out=sgl[sb_i][:, 0:c1 - c0], in_=pg[gb][:, 0:c1 - c0], func=AF.Silu)
                            P.op("dve", "tensor_tensor", ["sglX%d" % sb_i, "puX%d" % gb], ["heT"], out=heT[:, fg * 2 + fc2, c0:c1], in0=sgl[sb_i][:, 0:c1 - c0],
                                 in1=pu[gb][:, 0:c1 - c0], op=ALU.mult)
                new_sc = {}
                for dg in range(4):
                    wdi = nx("wdq", 2)
                    for hh in range(2):
                        c0_ = dg * 512 + hh * 256
                        P.dma("sp", stw[:], w_down[e_].rearrange("(kc p) f -> p kc f", p=128)[:, :, c0_:c0_ + 256], [], ["stw"])
                        if nx("cast", 2) == 0:
                            P.op("act", "activation", ["stw"], ["wbdX%d" % wdi], out=wbd[wdi][:, :, hh * 256:(hh + 1) * 256], in_=stw[:], func=AF.Copy)
                        else:
                            P.op("dve", "tensor_copy", ["stw"], ["wbdX%d" % wdi], out=wbd[wdi][:, :, hh * 256:(hh + 1) * 256], in_=stw[:])
                    for st_ in range(NCHK):
                        yb = nx("yq", 2)
                        for fc in range(16):
                            P.mm(py[yb][:], heT[:, fc, st_ * 128:(st_ + 1) * 128], wbd[wdi][:, fc, :], fc == 0, fc == 15,
                                 ["heT", "wbdX%d" % wdi], ["pyX%d" % yb])
                        ys = nx("ysq", 3)
                        P.op("dve", "tensor_scalar", ["pyX%d" % yb, "gtt%d" % ib], ["ystX%d" % ys], out=yst[ys][:], in0=py[yb][:],
                             scalar1=gtt[ib][:, st_:st_ + 1], scalar2=None, op0=ALU.mult)
                        nm_ = "sc_%d_%d_%d" % (e_, dg, st_)
                        new_sc.setdefault(dg, []).append(nm_)
                        P.add("pool", (lambda ys, ib, st_, dg: lambda e: e.indirect_dma_start(
                            out=acc_l[dg][:, :], out_offset=bass.IndirectOffsetOnAxis(ap=idxt[ib][:, st_:st_ + 1], axis=0),
                            in_=yst[ys][:], in_offset=None, compute_op=ALU.add))(ys, ib, st_, dg),
                            reads=["ystX%d" % ys, "idxt%d" % ib] + prev_sc.get(dg, []), writes=[nm_], dma=True)
                prev_sc = new_sc
        P.emit()

        with ExitStack() as st:
            sb = lambda n, s, d=F32: st.enter_context(nc.sbuf_tensor(n, list(s), d))
            gfin = sb("gfin", [128, 2048])
            P.dma("sp", gfin[:], nfin_rep, [], ["gfin"])
            xa = [sb("xaG%d" % i, [128, 4, 512]) for i in range(2)]
            junk = sb("junkG", [128, 2048], BF16)
            ss = [sb("ssG%d" % i, [128, 1]) for i in range(2)]
            yo_ = [sb("yoG%d" % i, [128, 2048]) for i in range(2)]
            for t in range(NTT):
                b = t % 2
                r0 = t * 128
                for g_ in range(4):
                    P.dma("sp", xa[b][:, g_, :], acc_l[g_][r0:r0 + 128, :], [], ["xaG%d" % b])
                xf = xa[b][:].rearrange("p g c -> p (g c)")
                P.op("act", "activation", ["xaG%d" % b], ["junkG", "ssG%d" % b], out=junk[:], in_=xf, func=AF.Square, accum_out=ss[b][:])
                P.op("dve", "tensor_scalar", ["ssG%d" % b], ["ssG%d" % b], out=ss[b][:], in0=ss[b][:], scalar1=1.0 / D, scalar2=EPS, op0=ALU.mult, op1=ALU.add)
                P.op("act", "activation", ["ssG%d" % b], ["ssG%d" % b], out=ss[b][:], in_=ss[b][:], func=AF.Sqrt)
                P.op("dve", "reciprocal", ["ssG%d" % b], ["ssG%d" % b], out=ss[b][:], in_=ss[b][:])
                P.op("dve", "scalar_tensor_tensor", ["xaG%d" % b, "ssG%d" % b, "gfin"], ["yoG%d" % b], out=yo_[b][:], in0=xf, scalar=ss[b][:, 0:1],
                     in1=gfin[:], op0=ALU.mult, op1=ALU.mult)
                P.dma("act", y_out[r0:r0 + 128, :], yo_[b][:], ["yoG%d" % b], ["y_out%d" % (t % 4)])
        P.emit()

        if dbg and upto not in ("all",):
            for nm_, ap_ in dbg.items():
                src = {"o_xbcT": xbcT, "o_uT": uT, "o_z": z_tok, "o_dt": dt_tok, "o_yssd": yssd, "o_yf": yf, "o_y5gT": y5gT, "o_y5nT": y5nT, "o_kT": kT_d, "o_v": v_d, "o_acc": acc_l[0], "o_probs": probs_loc, "o_hn3": hn3_bf, "o_idx": idx_d, "o_gate": gate_d}[nm_]
                P.dma("sp", ap_, src, [], [nm_])
            P.emit()
    return nc


def _rep(v):
    v = np.asarray(v, np.float32)
    return np.ascontiguousarray(np.broadcast_to(v.reshape(1, -1), (128, v.size)))


def _colpk(v):
    v = np.asarray(v, np.float32)
    return np.ascontiguousarray(v.reshape(-1, 128).T)


def _prep_weights(W):
    o = {}
    o["w_in"] = np.ascontiguousarray(W["w_in"][0])
    o["norm_mix"] = _colpk(W["norm_mix"][0])
    o["conv_w"] = np.ascontiguousarray(W["conv_w"][0].reshape(3, 16, 128).transpose(2, 1, 0))
    o["conv_b"] = _colpk(W["conv_b"][0])
    o["dtb_rep"] = _rep(W["ssd_dt_bias"][0])
    o["alog_rep"] = _rep(W["ssd_a_log"][0])
    o["ssdd_rep"] = _rep(W["ssd_d"][0])
    o["ssdn_rep"] = _rep(W["ssd_norm"][0])

    def gp(a):
        a = a.reshape(2, 8, 8, 64).transpose(0, 1, 3, 2)
        return np.concatenate([a, a], axis=2)
    are = gp(W["s5_a_re"][0])
    aim = gp(W["s5_a_im"][0])
    lst = gp(np.broadcast_to(W["s5_log_step"][0][:, :, None], (2, 64, 64)))
    o["s5_small"] = np.ascontiguousarray(np.stack([are, aim, lst], axis=3)).astype(np.float32)

    def bb(a):
        a = a.reshape(2, 8, 8, 64, 16).transpose(0, 1, 3, 2, 4).reshape(2, 8, 64, 128)
        return np.concatenate([a, a], axis=2)

    def cc(a):
        a = a.reshape(2, 8, 8, 16, 64).transpose(0, 1, 4, 2, 3).reshape(2, 8, 64, 128)
        return np.concatenate([a, a], axis=2)
    o["s5_big"] = np.ascontiguousarray(np.stack([bb(W["s5_b_re"][0]), bb(W["s5_b_im"][0]), cc(W["s5_c_re"][0]),
                                                 cc(W["s5_c_im"][0])], axis=3)).astype(np.float32)
    o["s5_dcol"] = _colpk(W["s5_d"][0])
    o["s5_ncol"] = _colpk(W["s5_norm"][0])
    o["norm_attn"] = _colpk(W["norm_attn"][0])
    o["norm_mem"] = _colpk(W["norm_mem"][0])
    for k_ in ("w_out", "w_q", "w_k", "w_v", "w_o"):
        o[k_] = np.ascontiguousarray(W[k_][0])
    o["nffn_rep"] = _rep(W["norm_ffn"][0])
    o["w_router_l"] = np.ascontiguousarray(W["w_router"][0].reshape(16, 128, 16).transpose(1, 0, 2))
    o["nfin_rep"] = _rep(W["norm_final"])
    o["w_glu"] = np.ascontiguousarray(W["s5_w_glu"][0])
    o["w_gate"] = np.ascontiguousarray(W["w_gate"][0])
    o["w_up"] = np.ascontiguousarray(W["w_up"][0])
    o["w_down"] = np.ascontiguousarray(W["w_down"][0])
    return o


_NC_CACHE = {}


def kernel(**inputs):
    W = {k_: np.asarray(v, np.float32) for k_, v in inputs.items()}
    xp, xs = W.pop("x_prompt"), W.pop("x_sample")
    mp, ms = W.pop("mem_prompt"), W.pop("mem_sample")
    LSEG = 4096
    cfg = {"LSEG": LSEG, "upto": "all", "CAPL": 1408, "GSZ": 4, "groups": [[0, 1, 2, 3], [4, 5, 6, 7]]}
    if "nc" not in _NC_CACHE:
        _NC_CACHE["nc"] = build(cfg)
    nc = _NC_CACHE["nc"]
    wl = _prep_weights(W)
    in_maps = []
    for c in range(8):
        m = dict(wl)
        if c < 4:
            m["x"] = np.ascontiguousarray(xp[c])
            m["mem"] = np.ascontiguousarray(np.stack([mp[c], mp[c]]))
            m["flag"] = np.ones((128, 1), np.float32)
        else:
            j = c - 4
            m["x"] = np.ascontiguousarray(xs[2 * j:2 * j + 2].reshape(2 * LSEG, D))
            m["mem"] = np.ascontiguousarray(ms[2 * j:2 * j + 2])
            m["flag"] = np.zeros((128, 1), np.float32)
        in_maps.append(m)
    res = run_bass_kernel_spmd(nc, in_maps, core_ids=list(range(8)))
    ys = [np.asarray(r["y"], np.float32) for r in res.results]
    try:
        import sys
        cn = np.array([[(np.asarray(r["slot_idx"])[e] < 2 * LSEG).sum() for e in range(16)] for r in res.results])
        print("[kernel] per-(core,expert) slot counts: mean %.1f std %.1f max %d (cap %d)" % (cn.mean(), cn.std(), cn.max(), cfg["CAPL"]), file=sys.stderr)
    except Exception:
        pass
    y_prompt = np.stack(ys[0:4]).reshape(4, 8192, D)
    y_sample = np.concatenate([y.reshape(2, LSEG, D) for y in ys[4:8]], axis=0)
    return (y_prompt, y_sample)
```

```python
import numpy as np
from contextlib import ExitStack
import concourse.bass as bass
import concourse.mybir as mybir
from concourse.bass_utils import run_bass_kernel_spmd

F32 = mybir.dt.float32
BF16 = mybir.dt.bfloat16
I32 = mybir.dt.int32
U32 = mybir.dt.uint32
ALU = mybir.AluOpType
AF = mybir.ActivationFunctionType
AX = mybir.AxisListType

ENGS = ("pe", "act", "dve", "pool", "sp")
NDS = 24

D = 2048
EPS = 1e-6
INC = 4112


class Op:
    __slots__ = ("eng", "fn", "reads", "writes", "dma", "sem", "val", "waits", "desc", "inc")

    def __init__(self, eng, fn, reads, writes, dma):
        self.eng = eng
        self.fn = fn
        self.reads = tuple(reads)
        self.writes = tuple(writes)
        self.dma = dma
        self.waits = {}


class Prog:
    def __init__(self, nc, stack):
        self.nc = nc
        self.ops = []
        self.last_writer = {}
        self.readers = {}
        self.cnt = {e: 0 for e in ENGS}
        self.dcnt = {e: 0 for e in ENGS}
        self.sem = {e: stack.enter_context(nc.semaphore("s_" + e)) for e in ENGS if e != "sp"}
        self.dsem = {
            e: [stack.enter_context(nc.semaphore("d_%s_%d" % (e, i))) for i in range(NDS)]
            for e in ("sp", "act", "pool")
        }
        self.waited = {e: {} for e in ENGS}
        self.semobj = {}
        self.limit = None
        self.bank_last = {}
        self.csem = stack.enter_context(nc.semaphore("s_coll"))
        self.capture = None
        self.sfx = ""
        self.shared = ()

    def add(self, eng, fn, reads=(), writes=(), dma=False, banks=(), own_sem=None):
        if self.capture is not None:
            if self.sfx:
                reads = [n if n in self.shared else n + self.sfx for n in reads]
                writes = [n if n in self.shared else n + self.sfx for n in writes]
            self.capture.append((eng, fn, tuple(reads), tuple(writes), dma, tuple(banks), own_sem))
            return None
        if self.limit is not None and len(self.ops) >= self.limit:
            return None
        op = Op(eng, fn, reads, writes, dma)
        deps = set()
        for b in op.reads:
            w = self.last_writer.get(b)
            if w is not None:
                deps.add(w)
        for b in op.writes:
            w = self.last_writer.get(b)
            if w is not None:
                deps.add(w)
            for r in self.readers.get(b, ()):
                if r.eng == eng and not r.dma and not dma:
                    continue
                deps.add(r)
        for tok in banks:
            pd = self.bank_last.get(tok)
            if pd is not None and pd.eng != eng:
                deps.add(pd)
            self.bank_last[tok] = op
        for b in op.reads:
            self.readers.setdefault(b, []).append(op)
        for b in op.writes:
            self.last_writer[b] = op
            self.readers[b] = []
        prewait = None
        op.inc = 16 if dma else 1
        if own_sem is not None:
            op.sem = own_sem
            op.val = 1
            op.inc = 1
        elif dma:
            i = self.dcnt[eng]
            self.dcnt[eng] += 1
            op.sem = self.dsem[eng][i % NDS]
            op.val = 16 * (i // NDS + 1)
            if i >= NDS:
                prewait = (op.sem, 16 * (i // NDS))
        else:
            self.cnt[eng] += 1
            op.sem = self.sem[eng]
            op.val = self.cnt[eng]
        for d in deps:
            if d is op:
                continue
            if d.eng == "pe" and eng == "pe" and not d.dma and not dma:
                continue
            k = id(d.sem)
            self.semobj[k] = d.sem
            if op.waits.get(k, 0) < d.val:
                op.waits[k] = d.val
        if prewait is not None:
            k = id(prewait[0])
            self.semobj[k] = prewait[0]
            if op.waits.get(k, 0) < prewait[1]:
                op.waits[k] = prewait[1]
        self.ops.append(op)
        return op

    @staticmethod
    def _banks(aps):
        toks = []
        for a in aps:
            sp = getattr(a, "space", None)
            if sp is not None and str(sp).endswith("PSUM"):
                toks.append(a.tensor.name)
        return toks

    def op(self, eng, method, reads, writes, *args, **kw):
        o = self.add(eng, lambda e: getattr(e, method)(*args, **kw), reads, writes,
                     banks=self._banks(list(args) + list(kw.values())))
        return o

    def dma(self, eng, out, in_, reads, writes, **kw):
        return self.add(eng, lambda e: e.dma_start(out=out, in_=in_, **kw), reads, writes, dma=True)

    def mm(self, out, lhsT, rhs, start, stop, reads, writes):
        return self.add("pe", lambda e: e.matmul(out, lhsT=lhsT, rhs=rhs, start=start, stop=stop), reads, writes,
                        banks=self._banks([out]))

    def tr(self, out, in_, ident, reads, writes):
        return self.add("pe", lambda e: e.transpose(out=out, in_=in_, identity=ident), reads, writes,
                        banks=self._banks([out]))

    def threads(self, fns, shared=None):
        lists = []
        for ti, f in enumerate(fns):
            self.capture = []
            if shared is not None:
                self.sfx = "#%d" % ti
                self.shared = set(shared)
            f()
            lists.append(self.capture)
        self.capture = None
        self.sfx = ""
        self.shared = ()
        n = max(len(l) for l in lists)
        for i in range(n):
            for l in lists:
                if i < len(l):
                    self.add(*l[i])

    def emit(self):
        nc = self.nc
        ops = self.ops
        self.ops = []
        per = {e: [o for o in ops if o.eng == e] for e in ENGS}
        finals = []
        for e in ENGS:
            if e != "sp" and self.cnt[e] > 0:
                finals.append((self.sem[e], self.cnt[e]))
        for e in ("sp", "act", "pool"):
            n = self.dcnt[e]
            for j in range(min(n, NDS)):
                uses = (n - 1 - j) // NDS + 1
                finals.append((self.dsem[e][j], 16 * uses))
        waited = self.waited
        semobj = self.semobj

        def run(engname, eng):
            wd = waited[engname]
            for o in per[engname]:
                for k, v in o.waits.items():
                    if wd.get(k, 0) < v:
                        eng.wait_ge(semobj[k], v)
                        wd[k] = v
                ins = o.fn(eng)
                ins.then_inc(o.sem, o.inc)
            for s, v in finals:
                k = id(s)
                if wd.get(k, 0) < v:
                    eng.wait_ge(s, v)
                    wd[k] = v

        with nc.Block() as block:
            @block.tensor
            def _(eng):
                run("pe", eng)

            @block.scalar
            def _(eng):
                run("act", eng)

            @block.vector
            def _(eng):
                run("dve", eng)

            @block.gpsimd
            def _(eng):
                run("pool", eng)

            @block.sync
            def _(eng):
                run("sp", eng)


class K:
    pass


def _rr(lst):
    state = {"i": 0}

    def nxt():
        v = lst[state["i"] % len(lst)]
        state["i"] += 1
        return v
    return nxt


def build(cfg):
    LSEG = cfg["LSEG"]
    LT = 2 * LSEG
    NST = LT // 512
    upto = cfg.get("upto", "all")
    nc = bass.Bass("TRN2", target_bir_lowering=False)
    k = K()
    k.nc = nc
    k.cfg = cfg

    def din(name, shape, dt=F32):
        return nc.dram_tensor(name, list(shape), dt, kind="ExternalInput").ap()

    def dscr(name, shape, dt=F32):
        return nc.dram_tensor(name, list(shape), dt, kind="Internal").ap()

    def dout(name, shape, dt=F32):
        return nc.dram_tensor(name, list(shape), dt, kind="ExternalOutput").ap()

    x_in = din("x", [LT, D])
    w_in = din("w_in", [D, INC])
    norm_mix = din("norm_mix", [128, 16])
    flag = din("flag", [128, 1])

    w_in_bf = dscr("w_in_bf", [9, 128, 16, 512], BF16)
    xbcT = dscr("xbcT", [2048, LT])
    uT = dscr("uT", [1024, LT], BF16)
    z_tok = dscr("z_tok", [LT, 1024])
    dt_tok = dscr("dt_tok", [LT, 16])

    conv_w = din("conv_w", [128, 16, 3])
    conv_b = din("conv_b", [128, 16])
    dtb_rep = din("dtb_rep", [128, 32])
    alog_rep = din("alog_rep", [128, 32])
    ssdd_rep = din("ssdd_rep", [128, 16])
    ssdn_rep = din("ssdn_rep", [128, 1024])
    xs_tok = dscr("xs_tok", [LT, 1024], BF16)
    B_tok = dscr("B_tok", [LT, 512], BF16)
    BT = dscr("BT", [512, LT], BF16)
    CT = dscr("CT", [512, LT], BF16)
    yf = dscr("yf", [LT, 1024])
    yb_d = dscr("yb_d", [LT, 1024])
    yssd = dscr("yssd", [LT, 1024], BF16)
    s5_small = din("s5_small", [2, 8, 128, 3, 8])
    s5_big = din("s5_big", [2, 8, 128, 4, 128])
    s5_dcol = din("s5_dcol", [128, 8])
    y5gT = dscr("y5gT", [1024, LT], BF16)
    norm_attn = din("norm_attn", [128, 16])
    norm_mem = din("norm_mem", [128, 16])
    w_glu = din("w_glu", [1024, 1024])
    w_out = din("w_out", [2048, 2048])
    w_q = din("w_q", [2048, 2048])
    w_k = din("w_k", [2048, 2048])
    w_v = din("w_v", [2048, 2048])
    w_o = din("w_o", [2048, 2048])
    mem_in = din("mem", [2, 256, 2048])
    s5_ncol = din("s5_ncol", [128, 8])
    w_glu_bf = dscr("w_glu_bf", [2, 128, 8, 512], BF16)
    w_out_bf = dscr("w_out_bf", [4, 128, 16, 512], BF16)
    w_q_bf = dscr("w_q_bf", [4, 128, 16, 512], BF16)
    w_k_bf = dscr("w_k_bf", [4, 128, 16, 512], BF16)
    w_v_bf = dscr("w_v_bf", [4, 128, 16, 512], BF16)
    w_o_bf = dscr("w_o_bf", [4, 128, 16, 512], BF16)
    kT_d = dscr("kT_d", [2, 128, 16, 256], BF16)
    v_d = dscr("v_d", [2, 128, 2, 2048], BF16)
    y5nT = dscr("y5nT", [1024, LT], BF16)
    nffn_rep = din("nffn_rep", [128, 2048])
    w_router_l = din("w_router_l", [128, 16, 16])
    CAPL = cfg["CAPL"]
    acc_l = [dscr("accd%d" % i, [LT + CAPL, 512]) for i in range(4)]
    hn3_bf = dscr("hn3_bf", [LT + CAPL, 2048], BF16)
    idx_d = dscr("idx_d", [16, 128, CAPL // 128], I32)
    gate_d = dscr("gate_d", [16, 128, CAPL // 128])
    probs_all = dscr("probs_all", [cfg.get("GSZ", 1) * LT, 16])
    w_gate = din("w_gate", [cfg.get("nexp", 16), 2048, 2048])
    w_up = din("w_up", [cfg.get("nexp", 16), 2048, 2048])
    w_down = din("w_down", [cfg.get("nexp", 16), 2048, 2048])
    nfin_rep = din("nfin_rep", [128, 2048])
    y_out = dout("y", [LT, 2048])
    cnt_out = dout("slot_idx", [16, 128, CAPL // 128], I32)
    probs_loc = dscr("probs_loc", [LT, 16])
    dbg = {}
    if upto == "F":
        dbg["o_acc"] = dout("o_acc", [LT + CAPL, 512])
        dbg["o_idx"] = dout("o_idx", [16, 128, CAPL // 128], I32)
        dbg["o_gate"] = dout("o_gate", [16, 128, CAPL // 128])
    if upto == "E2":
        dbg["o_acc"] = dout("o_acc", [LT + CAPL, 512])
        dbg["o_probs"] = dout("o_probs", [LT, 16])
        dbg["o_hn3"] = dout("o_hn3", [LT + CAPL, 2048], BF16)
    if upto == "E1":
        dbg["o_y5nT"] = dout("o_y5nT", [1024, LT], BF16)
        dbg["o_kT"] = dout("o_kT", [2, 128, 16, 256], BF16)
        dbg["o_v"] = dout("o_v", [2, 128, 2, 2048], BF16)
    if upto == "D":
        dbg["o_y5gT"] = dout("o_y5gT", [1024, LT], BF16)
    if upto == "C":
        dbg["o_yssd"] = dout("o_yssd", [LT, 1024], BF16)
        dbg["o_yf"] = dout("o_yf", [LT, 1024])
    if upto == "A":
        dbg["o_xbcT"] = dout("o_xbcT", [2048, LT])
        dbg["o_uT"] = dout("o_uT", [1024, LT], BF16)
        dbg["o_z"] = dout("o_z", [LT, 1024])
        dbg["o_dt"] = dout("o_dt", [LT, 16])

    with ExitStack() as gst:
        P = Prog(nc, gst)
        k.P = P
        with ExitStack() as st:
            sb = lambda n, s, d=F32: st.enter_context(nc.sbuf_tensor(n, list(s), d))
            gcol = sb("gcol", [128, 16])
            P.add("sp", lambda e: e.dma_start(out=gcol[:], in_=norm_mix),
                  writes=["gcol"], dma=True)
            wst = [sb("wst%d" % i, [128, INC]) for i in range(2)]
            wbf = [sb("wbf%d" % i, [128, 9 * 512], BF16) for i in range(2)]
            for i in range(2):
                (lambda i: P.add("pool", lambda e: e.memset(wbf[i][:], 0.0), writes=["wbf%d" % i]))(i)
            engs = _rr(["dve", "act", "pool"])
            for kc in range(16):
                b = kc % 2
                P.add("sp", (lambda b, kc: lambda e: e.dma_start(out=wst[b][:], in_=w_in[kc * 128:(kc + 1) * 128, :]))(b, kc),
                      writes=["wst%d" % b], dma=True)
                def cast(b=b, kc=kc):
                    srcs = [(0, 3072, 0), (3088, 4112, 3072), (3072, 3088, 4096)]
                    for (s0, s1, d0) in srcs:
                        def f(e, s0=s0, s1=s1, d0=d0):
                            return e.tensor_scalar(out=wbf[b][:, d0:d0 + (s1 - s0)], in0=wst[b][:, s0:s1],
                                                   scalar1=gcol[:, kc:kc + 1], scalar2=None, op0=ALU.mult)
                        P.add("dve", f, reads=["wst%d" % b, "gcol"], writes=["wbf%d" % b])
                cast()
                P.add("sp", (lambda b, kc: lambda e: e.dma_start(
                    out=w_in_bf[:, :, kc, :].rearrange("g p n -> p g n"),
                    in_=wbf[b][:].rearrange("p (g n) -> p g n", n=512)))(b, kc),
                    reads=["wbf%d" % b], writes=["w_in_bf"], dma=True)
        P.emit()

        with ExitStack() as st:
            sb = lambda n, s, d=F32: st.enter_context(nc.sbuf_tensor(n, list(s), d))
            ps = lambda n, s, d=F32: st.enter_context(nc.psum_tensor(n, list(s), d))
            ident = sb("ident", [128, 128], BF16)
            P.add("pool", lambda e: e.memset(ident[:], 1.0), writes=["ident"])
            P.add("pool", lambda e: e.affine_select(out=ident[:], in_=ident[:], pattern=[[-1, 128]],
                                                    compare_op=ALU.is_equal, fill=0.0, base=0, channel_multiplier=1),
                  reads=["ident"], writes=["ident"])
            ptr = [ps("ptr%d" % i, [128, 1024], BF16) for i in range(2)]
            pmm = [ps("pmm%d" % i, [128, 512]) for i in range(4)]

            def mk_thread(th):
                S = "_t%d" % th
                xt = [sb("xt%d" % i + S, [128, D]) for i in range(2)]
                junk = sb("junk" + S, [128, D], BF16)
                ss = [sb("ss%d" % i + S, [128, 1]) for i in range(2)]
                xn = [sb("xn%d" % i + S, [128, D], BF16) for i in range(2)]
                hT = sb("hT" + S, [128, 16, 512], BF16)
                wg = [sb("wg%d" % i + S, [128, 16, 512], BF16) for i in range(2)]
                ostg = [sb("ostg%d" % i + S, [128, 512]) for i in range(3)]
                ostb = [sb("ostb%d" % i + S, [128, 512], BF16) for i in range(2)]
                dstg = [sb("dstg%d" % i + S, [128, 16]) for i in range(2)]
                wq = _rr([0, 1])
                oq = _rr([0, 1, 2])
                obq = _rr([0, 1])
                pq = _rr([0, 1] if th == 0 else [2, 3])
                tq = _rr([th])
                cpq = _rr(["act", "dve"])
                dq = _rr([0, 1])

                def body():
                    it = 0
                    for s in range(th, NST, 2):
                        t0 = s * 512
                        for tt in range(4):
                            b = it % 2
                            it += 1
                            r0 = t0 + tt * 128
                            P.add("sp", (lambda b, r0: lambda e: e.dma_start(out=xt[b][:], in_=x_in[r0:r0 + 128, :]))(b, r0),
                                  writes=["xt%d" % b], dma=True)
                            P.add("act", (lambda b: lambda e: e.activation(out=junk[:], in_=xt[b][:], func=AF.Square,
                                                                           accum_out=ss[b][:]))(b),
                                  reads=["xt%d" % b], writes=["junk", "ss%d" % b])
                            P.add("dve", (lambda b: lambda e: e.tensor_scalar(out=ss[b][:], in0=ss[b][:], scalar1=1.0 / D,
                                                                              scalar2=EPS, op0=ALU.mult, op1=ALU.add))(b),
                                  reads=["ss%d" % b], writes=["ss%d" % b])
                            P.add("act", (lambda b: lambda e: e.activation(out=ss[b][:], in_=ss[b][:], func=AF.Sqrt))(b),
                                  reads=["ss%d" % b], writes=["ss%d" % b])
                            P.add("dve", (lambda b: lambda e: e.reciprocal(out=ss[b][:], in_=ss[b][:]))(b),
                                  reads=["ss%d" % b], writes=["ss%d" % b])
                            P.add("dve", (lambda b: lambda e: e.tensor_scalar(out=xn[b][:], in0=xt[b][:], scalar1=ss[b][:, 0:1],
                                                                              scalar2=None, op0=ALU.mult))(b),
                                  reads=["xt%d" % b, "ss%d" % b], writes=["xn%d" % b])
                            for q in range(4):
                                pb = tq()
                                for j in range(4):
                                    kc = q * 4 + j
                                    P.add("pe", (lambda pb, j, b, kc: lambda e: e.transpose(
                                        out=ptr[pb][:, j * 128:(j + 1) * 128], in_=xn[b][:, kc * 128:(kc + 1) * 128],
                                        identity=ident[:]))(pb, j, b, kc),
                                        reads=["xn%d" % b, "ident"], writes=["ptr%d" % pb])
                                ce = cpq()
                                if ce == "act":
                                    P.add("act", (lambda pb, q, tt: lambda e: e.activation(
                                        out=hT[:, q * 4:(q + 1) * 4, tt * 128:(tt + 1) * 128],
                                        in_=ptr[pb][:, 0:512].rearrange("p (j n) -> p j n", n=128), func=AF.Copy))(pb, q, tt),
                                        reads=["ptr%d" % pb], writes=["hT"])
                                else:
                                    P.add("dve", (lambda pb, q, tt: lambda e: e.tensor_copy(
                                        out=hT[:, q * 4:(q + 1) * 4, tt * 128:(tt + 1) * 128],
                                        in_=ptr[pb][:, 0:512].rearrange("p (j n) -> p j n", n=128)))(pb, q, tt),
                                        reads=["ptr%d" % pb], writes=["hT"])
                        for g in range(9):
                            wb = wq()
                            P.add("sp", (lambda wb, g: lambda e: e.dma_start(out=wg[wb][:], in_=w_in_bf[g]))(wb, g),
                                  reads=["w_in_bf"], writes=["wg%d" % wb], dma=True)
                            if g < 2:
                                for tt in range(4):
                                    pb = pq()
                                    for kc in range(16):
                                        P.add("pe", (lambda pb, wb, kc, tt: lambda e: e.matmul(
                                            pmm[pb][:], lhsT=hT[:, kc, tt * 128:(tt + 1) * 128], rhs=wg[wb][:, kc, :],
                                            start=(kc == 0), stop=(kc == 15)))(pb, wb, kc, tt),
                                            reads=["hT", "wg%d" % wb], writes=["pmm%d" % pb])
                                    ob = oq()
                                    P.add("act", (lambda ob, pb: lambda e: e.activation(out=ostg[ob][:], in_=pmm[pb][:], func=AF.Copy))(ob, pb),
                                          reads=["pmm%d" % pb], writes=["ostg%d" % ob])
                                    r0 = t0 + tt * 128
                                    P.add("sp", (lambda ob, r0, g: lambda e: e.dma_start(
                                        out=z_tok[r0:r0 + 128, g * 512:(g + 1) * 512], in_=ostg[ob][:]))(ob, r0, g),
                                        reads=["ostg%d" % ob], writes=["z_tok"], dma=True)
                            elif g < 8:
                                for mc in range(4):
                                    pb = pq()
                                    for kc in range(16):
                                        P.add("pe", (lambda pb, wb, kc, mc: lambda e: e.matmul(
                                            pmm[pb][:], lhsT=wg[wb][:, kc, mc * 128:(mc + 1) * 128], rhs=hT[:, kc, :],
                                            start=(kc == 0), stop=(kc == 15)))(pb, wb, kc, mc),
                                            reads=["hT", "wg%d" % wb], writes=["pmm%d" % pb])
                                    if g < 6:
                                        ob = oq()
                                        ch0 = (g - 2) * 512 + mc * 128
                                        P.add("act", (lambda ob, pb: lambda e: e.activation(out=ostg[ob][:], in_=pmm[pb][:], func=AF.Copy))(ob, pb),
                                              reads=["pmm%d" % pb], writes=["ostg%d" % ob])
                                        P.add("sp", (lambda ob, ch0, t0: lambda e: e.dma_start(
                                            out=xbcT[ch0:ch0 + 128, t0:t0 + 512], in_=ostg[ob][:]))(ob, ch0, t0),
                                            reads=["ostg%d" % ob], writes=["xbcT"], dma=True)
                                    else:
                                        ob = obq()
                                        ch0 = (g - 6) * 512 + mc * 128
                                        P.add("dve", (lambda ob, pb: lambda e: e.tensor_copy(out=ostb[ob][:], in_=pmm[pb][:]))(ob, pb),
                                              reads=["pmm%d" % pb], writes=["ostb%d" % ob])
                                        P.add("sp", (lambda ob, ch0, t0: lambda e: e.dma_start(
                                            out=uT[ch0:ch0 + 128, t0:t0 + 512], in_=ostb[ob][:]))(ob, ch0, t0),
                                            reads=["ostb%d" % ob], writes=["uT"], dma=True)
                            else:
                                for tt in range(4):
                                    pb = pq()
                                    for kc in range(16):
                                        P.add("pe", (lambda pb, wb, kc, tt: lambda e: e.matmul(
                                            pmm[pb][:, 0:16], lhsT=hT[:, kc, tt * 128:(tt + 1) * 128], rhs=wg[wb][:, kc, 0:16],
                                            start=(kc == 0), stop=(kc == 15)))(pb, wb, kc, tt),
                                            reads=["hT", "wg%d" % wb], writes=["pmm%d" % pb])
                                    ob = dq()
                                    P.add("dve", (lambda ob, pb: lambda e: e.tensor_copy(out=dstg[ob][:], in_=pmm[pb][:, 0:16]))(ob, pb),
                                          reads=["pmm%d" % pb], writes=["dstg%d" % ob])
                                    r0 = t0 + tt * 128
                                    P.add("sp", (lambda ob, r0: lambda e: e.dma_start(out=dt_tok[r0:r0 + 128, :], in_=dstg[ob][:]))(ob, r0),
                                          reads=["dstg%d" % ob], writes=["dt_tok"], dma=True)

                return body

            P.threads([mk_thread(0), mk_thread(1)], shared=("ident", "w_in_bf"))
        P.emit()


        with ExitStack() as st:
            sb = lambda n, s, d=F32: st.enter_context(nc.sbuf_tensor(n, list(s), d))
            ps = lambda n, s, d=F32: st.enter_context(nc.psum_tensor(n, list(s), d))
            ident = sb("identB", [128, 128], BF16)
            P.op("pool", "memset", [], ["identB"], ident[:], 1.0)
            P.op("pool", "affine_select", ["identB"], ["identB"], out=ident[:], in_=ident[:], pattern=[[-1, 128]],
                 compare_op=ALU.is_equal, fill=0.0, base=0, channel_multiplier=1)
            cw = sb("cw", [128, 16, 3])
            cb = sb("cb", [128, 16])
            flg = sb("flg", [128, 1])
            P.dma("sp", cw[:], conv_w, [], ["cw"])
            P.dma("sp", cb[:], conv_b, [], ["cb"])
            P.dma("sp", flg[:], flag, [], ["flg"])
            NB = 3
            xw = [sb("xw%d" % i, [128, 514]) for i in range(NB)]
            acc = [sb("acc%d" % i, [128, 512]) for i in range(NB)]
            sl = [sb("sl%d" % i, [128, 512], BF16) for i in range(NB)]
            xs_stg = sb("xs_stg", [128, 4, 1024], BF16)
            b_stg = sb("b_stg", [128, 4, 512], BF16)
            ptb = [ps("ptb%d" % i, [128, 1024], BF16) for i in range(2)]
            it = 0
            tq = 0
            for s in range(NST):
                t0 = s * 512
                for c in range(16):
                    b = it % NB
                    it += 1
                    lo = max(t0 - 1, 0)
                    hi = min(t0 + 513, LT)
                    if t0 == 0:
                        P.op("pool", "memset", [], ["xw%d" % b], xw[b][:, 0:1], 0.0)
                    if t0 + 512 == LT:
                        P.op("pool", "memset", [], ["xw%d" % b], xw[b][:, 513:514], 0.0)
                    P.dma("sp", xw[b][:, (lo - (t0 - 1)):(hi - (t0 - 1))], xbcT[c * 128:(c + 1) * 128, lo:hi],
                          ["xbcT"], ["xw%d" % b])
                    if t0 == LSEG:
                        P.op("dve", "tensor_scalar", ["xw%d" % b, "flg"], ["xw%d" % b], out=xw[b][:, 0:1], in0=xw[b][:, 0:1],
                             scalar1=flg[:, 0:1], scalar2=None, op0=ALU.mult)
                    if t0 + 512 == LSEG:
                        P.op("dve", "tensor_scalar", ["xw%d" % b, "flg"], ["xw%d" % b], out=xw[b][:, 513:514], in0=xw[b][:, 513:514],
                             scalar1=flg[:, 0:1], scalar2=None, op0=ALU.mult)
                    P.op("dve", "tensor_scalar", ["xw%d" % b, "cw"], ["acc%d" % b], out=acc[b][:], in0=xw[b][:, 1:513],
                         scalar1=cw[:, c, 1:2], scalar2=None, op0=ALU.mult)
                    P.op("dve", "scalar_tensor_tensor", ["xw%d" % b, "cw", "acc%d" % b], ["acc%d" % b], out=acc[b][:],
                         in0=xw[b][:, 0:512], scalar=cw[:, c, 0:1], in1=acc[b][:], op0=ALU.mult, op1=ALU.add)
                    P.op("dve", "scalar_tensor_tensor", ["xw%d" % b, "cw", "acc%d" % b], ["acc%d" % b], out=acc[b][:],
                         in0=xw[b][:, 2:514], scalar=cw[:, c, 2:3], in1=acc[b][:], op0=ALU.mult, op1=ALU.add)
                    P.op("act", "activation", ["acc%d" % b, "cb"], ["sl%d" % b], out=sl[b][:], in_=acc[b][:], func=AF.Silu,
                         bias=cb[:, c:c + 1])
                    if c >= 8:
                        dst = BT if c < 12 else CT
                        r0 = (c - 8) * 128 if c < 12 else (c - 12) * 128
                        P.dma("sp", dst[r0:r0 + 128, t0:t0 + 512], sl[b][:], ["sl%d" % b], ["BT" if c < 12 else "CT"])
                    if c < 12:
                        pb = tq % 2
                        tq += 1
                        for tt in range(4):
                            P.tr(ptb[pb][:, tt * 128:(tt + 1) * 128], sl[b][:, tt * 128:(tt + 1) * 128], ident[:],
                                 ["sl%d" % b, "identB"], ["ptb%d" % pb])
                        if c < 8:
                            P.op("act", "activation", ["ptb%d" % pb], ["xs_stg"], out=xs_stg[:, :, c * 128:(c + 1) * 128],
                                 in_=ptb[pb][:, 0:512].rearrange("p (t n) -> p t n", n=128), func=AF.Copy)
                        else:
                            P.op("act", "activation", ["ptb%d" % pb], ["b_stg"], out=b_stg[:, :, (c - 8) * 128:(c - 7) * 128],
                                 in_=ptb[pb][:, 0:512].rearrange("p (t n) -> p t n", n=128), func=AF.Copy)
                    if c == 7:
                        P.dma("sp", xs_tok[t0:t0 + 512, :].rearrange("(t p) c -> p t c", p=128), xs_stg[:], ["xs_stg"], ["xs_tok"])
                    if c == 11:
                        P.dma("sp", B_tok[t0:t0 + 512, :].rearrange("(t p) c -> p t c", p=128), b_stg[:], ["b_stg"], ["B_tok"])
        P.emit()

        NCH = LT // 128
        CPS = LSEG // 128
        with ExitStack() as st:
            sb = lambda n, s, d=F32: st.enter_context(nc.sbuf_tensor(n, list(s), d))
            ps = lambda n, s, d=F32: st.enter_context(nc.psum_tensor(n, list(s), d))
            tri = [sb("triF", [128, 128]), sb("triB", [128, 128])]
            nmk = [sb("nmkF", [128, 128]), sb("nmkB", [128, 128])]
            ones = sb("onesC", [128, 128])
            P.op("pool", "memset", [], ["onesC"], ones[:], 1.0)
            for d_ in range(2):
                pat = [[1, 128]] if d_ == 0 else [[-1, 128]]
                cm = -1 if d_ == 0 else 1
                nm = "FB"[d_]
                P.op("pool", "memset", [], ["tri" + nm], tri[d_][:], 1.0)
                P.op("pool", "affine_select", ["tri" + nm], ["tri" + nm], out=tri[d_][:], in_=tri[d_][:], pattern=pat,
                     compare_op=ALU.is_ge, fill=0.0, base=0, channel_multiplier=cm)
                P.op("pool", "memset", [], ["nmk" + nm], nmk[d_][:], 0.0)
                P.op("pool", "affine_select", ["nmk" + nm], ["nmk" + nm], out=nmk[d_][:], in_=nmk[d_][:], pattern=pat,
                     compare_op=ALU.is_ge, fill=-30000.0, base=0, channel_multiplier=cm)
            dtb = sb("dtb", [128, 32])
            aneg = sb("aneg", [128, 32])
            flg = sb("flgC", [128, 1])
            P.dma("sp", dtb[:], dtb_rep, [], ["dtb"])
            P.dma("sp", aneg[:], alog_rep, [], ["aneg"])
            P.dma("sp", flg[:], flag, [], ["flgC"])
            P.op("act", "activation", ["aneg"], ["aneg"], out=aneg[:], in_=aneg[:], func=AF.Exp)
            P.op("dve", "tensor_scalar", ["aneg"], ["aneg"], out=aneg[:], in0=aneg[:], scalar1=-1.0, scalar2=None, op0=ALU.mult)

            def ssd_dir(d_):
                nm = "FB"[d_]
                X = lambda s_: s_ + nm
                NBUF = 2
                xs = [sb(X("xs%d" % i), [128, 16, 64], BF16) for i in range(NBUF)]
                bt_ = [sb(X("btok%d" % i), [128, 512], BF16) for i in range(NBUF)]
                bT = [sb(X("bT%d" % i), [128, 4, 128], BF16) for i in range(NBUF)]
                cT = [sb(X("cT%d" % i), [128, 4, 128], BF16) for i in range(NBUF)]
                dtr = [sb(X("dtr%d" % i), [128, 16]) for i in range(NBUF)]
                dt_ = sb(X("dt_"), [128, 16])
                dtA = sb(X("dtA"), [128, 16])
                acs = sb(X("acs"), [128, 16])
                dout_ = sb(X("dout_"), [128, 16])
                cd = sb(X("cd"), [128, 16])
                rhsb = [sb(X("rhsb%d" % i), [128, 4, 128]) for i in range(2)]
                dif = [sb(X("dif%d" % i), [128, 4, 128]) for i in range(2)]
                seg = [sb(X("seg%d" % i), [128, 4, 128]) for i in range(2)]
                mT = [sb(X("mT%d" % i), [128, 4, 128], BF16) for i in range(2)]
                xdt = sb(X("xdt"), [128, 16, 64], BF16)
                xdd = [sb(X("xdd%d" % i), [128, 4, 64], BF16) for i in range(2)]
                prev = sb(X("prev"), [128, 16, 64])
                prevb = sb(X("prevb"), [128, 16, 64], BF16)
                tmp = [sb(X("tmpc%d" % i), [128, 4, 64]) for i in range(2)]
                ybuf = [sb(X("ybuf%d" % i), [128, 16, 64]) for i in range(2)]
                pA = ps(X("pA"), [128, 4, 128])
                pB = ps(X("pB"), [128, 512])
                pC = ps(X("pC"), [128, 512])
                pD = ps(X("pD"), [128, 512])
                ydst = yf if d_ == 0 else yb_d

                def body():
                    it = 0
                    last = 127 if d_ == 0 else 0
                    order = list(range(NCH)) if d_ == 0 else list(range(NCH - 1, -1, -1))
                    pbn = [X("prevb%d" % g) for g in range(4)]
                    P.op("dve", "memset", [], [X("prev")], prev[:], 0.0)
                    P.op("pool", "memset", [], pbn, prevb[:], 0.0)
                    for ci, c in enumerate(order):
                        t0 = c * 128
                        b = it % NBUF
                        it += 1
                        if ci == CPS:
                            P.op("dve", "tensor_scalar", [X("prev"), "flgC"], [X("prev")], out=prev[:], in0=prev[:], scalar1=flg[:, 0:1],
                                 scalar2=None, op0=ALU.mult)
                            P.op("dve", "tensor_scalar", pbn + ["flgC"], pbn, out=prevb[:], in0=prevb[:], scalar1=flg[:, 0:1],
                                 scalar2=None, op0=ALU.mult)
                        P.dma("sp", xs[b][:], xs_tok[t0:t0 + 128, :].rearrange("p (h q) -> p h q", q=64), ["xs_tok"], [X("xs%d" % b)])
                        P.dma("sp", bt_[b][:], B_tok[t0:t0 + 128, :], ["B_tok"], [X("btok%d" % b)])
                        P.dma("sp", bT[b][:], BT[:, t0:t0 + 128].rearrange("(g n) l -> n g l", n=128), ["BT"], [X("bT%d" % b)])
                        P.dma("sp", cT[b][:], CT[:, t0:t0 + 128].rearrange("(g n) l -> n g l", n=128), ["CT"], [X("cT%d" % b)])
                        P.dma("sp", dtr[b][:], dt_tok[t0:t0 + 128, :], ["dt_tok"], [X("dtr%d" % b)])
                        P.op("dve", "tensor_tensor", [X("dtr%d" % b), "dtb"], [X("dt_")], out=dt_[:], in0=dtr[b][:], in1=dtb[:, d_ * 16:(d_ + 1) * 16], op=ALU.add)
                        P.op("act", "activation", [X("dt_")], [X("dt_")], out=dt_[:], in_=dt_[:], func=AF.Exp)
                        P.op("act", "activation", [X("dt_")], [X("dt_")], out=dt_[:], in_=dt_[:], func=AF.Ln, bias=1.0)
                        P.op("dve", "tensor_tensor", [X("dt_"), "aneg"], [X("dtA")], out=dtA[:], in0=dt_[:], in1=aneg[:, d_ * 16:(d_ + 1) * 16], op=ALU.mult)
                        P.mm(pD[:, 0:16], tri[d_][:], dtA[:], True, True, ["tri" + nm, X("dtA")], [X("pD")])
                        P.op("dve", "tensor_copy", [X("pD")], [X("acs")], out=acs[:], in_=pD[:, 0:16])
                        P.op("act", "activation", [X("acs")], [X("dout_")], out=dout_[:], in_=acs[:], func=AF.Exp)
                        P.op("pool", "tensor_tensor", [X("xs%d" % b), X("dt_")], [X("xdt")], out=xdt[:], in0=xs[b][:],
                             in1=dt_[:].unsqueeze(2).to_broadcast([128, 16, 64]), op=ALU.mult)
                        yb_i = ci % 2
                        yn = X("ybuf%d" % yb_i)
                        for g in range(4):
                            q = g % 2
                            h0 = g * 4
                            P.op("dve", "tensor_tensor", ["tri" + nm, X("dtA")], [X("rhsb%d" % q)], out=rhsb[q][:],
                                 in0=tri[d_][:].unsqueeze(1).to_broadcast([128, 4, 128]),
                                 in1=dtA[:, h0:h0 + 4].unsqueeze(2).to_broadcast([128, 4, 128]), op=ALU.mult)
                            P.mm(pA[:].rearrange("p r l -> p (r l)"), ones[:], rhsb[q][:].rearrange("p r l -> p (r l)"), True, True,
                                 ["onesC", X("rhsb%d" % q)], [X("pA")])
                            P.op("dve", "tensor_tensor", [X("pA"), X("acs")], [X("dif%d" % q)], out=dif[q][:], in0=pA[:],
                                 in1=acs[:, h0:h0 + 4].unsqueeze(2).to_broadcast([128, 4, 128]), op=ALU.subtract)
                            P.op("act", "activation", [X("pA")], [X("cd")], out=cd[:, h0:h0 + 4], in_=pA[:, :, last], func=AF.Exp)
                            P.op("dve", "tensor_tensor", [X("dif%d" % q), "nmk" + nm], [X("dif%d" % q)], out=dif[q][:], in0=dif[q][:],
                                 in1=nmk[d_][:].unsqueeze(1).to_broadcast([128, 4, 128]), op=ALU.min)
                            P.op("act", "activation", [X("dif%d" % q)], [X("seg%d" % q)], out=seg[q][:], in_=dif[q][:], func=AF.Exp)
                            P.mm(pB[:, 0:128], bT[b][:, g, :], cT[b][:, g, :], True, True, [X("bT%d" % b), X("cT%d" % b)], [X("pBcb")])
                            P.op("dve", "tensor_tensor", [X("seg%d" % q), X("pBcb")], [X("mT%d" % q)], out=mT[q][:], in0=seg[q][:],
                                 in1=pB[:, 0:128].unsqueeze(1).to_broadcast([128, 4, 128]), op=ALU.mult)
                            for r in range(4):
                                P.mm(pB[:, 128 + r * 64:128 + (r + 1) * 64], mT[q][:, r, :], xdt[:, h0 + r, :], True, True,
                                     [X("mT%d" % q), X("xdt")], [X("pBy")])
                            P.op("pool", "tensor_tensor", [X("xdt"), X("seg%d" % q)], [X("xdd%d" % q)], out=xdd[q][:], in0=xdt[:, h0:h0 + 4, :],
                                 in1=seg[q][:, :, last:last + 1].to_broadcast([128, 4, 64]), op=ALU.mult)
                            P.mm(pC[:, 0:256], bt_[b][:, g * 128:(g + 1) * 128], xdd[q][:].rearrange("p r q -> p (r q)"), True, True,
                                 [X("btok%d" % b), X("xdd%d" % q)], [X("pCs")])
                            P.mm(pC[:, 256:512], cT[b][:, g, :], prevb[:, h0:h0 + 4, :].rearrange("p r q -> p (r q)"), True, True,
                                 [X("cT%d" % b), X("prevb%d" % g)], [X("pCo")])
                            P.op("dve", "tensor_tensor", [X("pCo"), X("dout_")], [X("tmpc%d" % q)], out=tmp[q][:],
                                 in0=pC[:, 256:512].rearrange("p (r q) -> p r q", q=64),
                                 in1=dout_[:, h0:h0 + 4].unsqueeze(2).to_broadcast([128, 4, 64]), op=ALU.mult)
                            P.op("dve", "tensor_tensor", [X("tmpc%d" % q), X("pBy")], [yn], out=ybuf[yb_i][:, h0:h0 + 4, :], in0=tmp[q][:],
                                 in1=pB[:, 128:384].rearrange("p (r q) -> p r q", q=64), op=ALU.add)
                            P.op("dve", "tensor_tensor", [X("prev"), X("cd")], [X("prev")], out=prev[:, h0:h0 + 4, :], in0=prev[:, h0:h0 + 4, :],
                                 in1=cd[:, h0:h0 + 4].unsqueeze(2).to_broadcast([128, 4, 64]), op=ALU.mult)
                            P.op("dve", "tensor_tensor", [X("prev"), X("pCs")], [X("prev")], out=prev[:, h0:h0 + 4, :], in0=prev[:, h0:h0 + 4, :],
                                 in1=pC[:, 0:256].rearrange("p (r q) -> p r q", q=64), op=ALU.add)
                            P.op("act", "activation", [X("prev")], [X("prevb%d" % g)], out=prevb[:, h0:h0 + 4, :], in_=prev[:, h0:h0 + 4, :], func=AF.Copy)
                        P.dma("act", ydst[t0:t0 + 128, :], ybuf[yb_i][:].rearrange("p h q -> p (h q)"), [yn], [X("ydst%d" % (ci % 8))])
                return body

            P.threads([ssd_dir(0), ssd_dir(1)])
        P.emit()

        with ExitStack() as st:
            sb = lambda n, s, d=F32: st.enter_context(nc.sbuf_tensor(n, list(s), d))
            dsk = sb("dsk", [128, 16])
            gn = sb("gn", [128, 1024])
            P.dma("sp", dsk[:], ssdd_rep, [], ["dsk"])
            P.dma("sp", gn[:], ssdn_rep, [], ["gn"])

            def fin(par):
                X = lambda s_: s_ + str(par)
                ya = sb(X("yaC"), [128, 16, 64])
                ybb = sb(X("ybC"), [128, 16, 64])
                xs_ = sb(X("xsC"), [128, 16, 64], BF16)
                zt = sb(X("ztC"), [128, 1024])
                sq = sb(X("sqC"), [128, 1024])
                gss = sb(X("gssC"), [128, 4])
                yo = sb(X("yoC"), [128, 1024], BF16)

                def body():
                    for c in range(par, NCH, 2):
                        t0 = c * 128
                        P.dma("sp", ya[:].rearrange("p h q -> p (h q)"), yf[t0:t0 + 128, :], [], [X("yaC")])
                        P.dma("sp", ybb[:].rearrange("p h q -> p (h q)"), yb_d[t0:t0 + 128, :], [], [X("ybC")])
                        P.dma("sp", xs_[:], xs_tok[t0:t0 + 128, :].rearrange("p (h q) -> p h q", q=64), [], [X("xsC")])
                        P.dma("sp", zt[:], z_tok[t0:t0 + 128, :], [], [X("ztC")])
                        P.op("dve", "tensor_tensor", [X("yaC"), X("ybC")], [X("yaC")], out=ya[:], in0=ya[:], in1=ybb[:], op=ALU.add)
                        P.op("pool", "tensor_tensor", [X("xsC"), "dsk"], [X("ybC")], out=ybb[:], in0=xs_[:],
                             in1=dsk[:].unsqueeze(2).to_broadcast([128, 16, 64]), op=ALU.mult)
                        P.op("dve", "tensor_tensor", [X("yaC"), X("ybC")], [X("yaC")], out=ya[:], in0=ya[:], in1=ybb[:], op=ALU.add)
                        P.op("act", "activation", [X("ztC")], [X("ztC")], out=zt[:], in_=zt[:], func=AF.Silu)
                        y2 = ya[:].rearrange("p h q -> p (h q)")
                        P.op("dve", "tensor_tensor", [X("yaC"), X("ztC")], [X("yaC")], out=y2, in0=y2, in1=zt[:], op=ALU.mult)
                        P.op("pool", "tensor_tensor", [X("yaC")], [X("sqC")], out=sq[:], in0=y2, in1=y2, op=ALU.mult)
                        P.op("dve", "tensor_reduce", [X("sqC")], [X("gssC")], out=gss[:], in_=sq[:].rearrange("p (g c) -> p g c", c=256),
                             op=ALU.add, axis=AX.X)
                        P.op("dve", "tensor_scalar", [X("gssC")], [X("gssC")], out=gss[:], in0=gss[:], scalar1=1.0 / 256, scalar2=EPS,
                             op0=ALU.mult, op1=ALU.add)
                        P.op("act", "activation", [X("gssC")], [X("gssC")], out=gss[:], in_=gss[:], func=AF.Sqrt)
                        P.op("dve", "reciprocal", [X("gssC")], [X("gssC")], out=gss[:], in_=gss[:])
                        P.op("dve", "tensor_tensor", [X("yaC"), X("gssC")], [X("yaC")], out=ya[:].rearrange("p (g h) q -> p g (h q)", g=4),
                             in0=ya[:].rearrange("p (g h) q -> p g (h q)", g=4),
                             in1=gss[:].unsqueeze(2).to_broadcast([128, 4, 256]), op=ALU.mult)
                        P.op("dve", "tensor_tensor", [X("yaC"), "gn"], [X("yoC")], out=yo[:], in0=y2, in1=gn[:], op=ALU.mult)
                        P.dma("act", yssd[t0:t0 + 128, :], yo[:], [X("yoC")], [X("yssdw%d" % (c % 8))])
                return body

            P.threads([fin(0), fin(1)])
        P.emit()

        NB = LT // 8
        NBS = LSEG // 8
        P.limit = cfg.get("dmax")
        TWO_PI = 6.283185307179586
        with ExitStack() as st:
            sb = lambda n, s, d=F32: st.enter_context(nc.sbuf_tensor(n, list(s), d))
            ps = lambda n, s, d=F32: st.enter_context(nc.psum_tensor(n, list(s), d))
            identF = sb("identF", [128, 128])
            P.op("pool", "memset", [], ["identF"], identF[:], 1.0)
            P.op("pool", "affine_select", ["identF"], ["identF"], out=identF[:], in_=identF[:], pattern=[[-1, 128]],
                 compare_op=ALU.is_equal, fill=0.0, base=0, channel_multiplier=1)
            bm = sb("bmD", [128, 8, 16])
            P.op("pool", "memset", [], ["bmD"], bm[:], 1.0)
            P.op("pool", "affine_select", ["bmD"], ["bmD"], out=bm[:], in_=bm[:], pattern=[[-16, 8], [0, 16]],
                 compare_op=ALU.is_ge, fill=0.0, base=0, channel_multiplier=1)
            P.op("pool", "affine_select", ["bmD"], ["bmD"], out=bm[:], in_=bm[:], pattern=[[16, 8], [0, 16]],
                 compare_op=ALU.is_ge, fill=0.0, base=15, channel_multiplier=-1)
            PM = [sb("PM%d" % i, [128, 2, 64]) for i in range(4)]
            CM = [sb("CM%d" % i, [128, 8, 16]) for i in range(4)]
            for pr_ in range(4):
                P.op("pool", "memset", [], ["PM%d" % pr_], PM[pr_][:], 1.0)
                P.op("pool", "affine_select", ["PM%d" % pr_], ["PM%d" % pr_], out=PM[pr_][:], in_=PM[pr_][:],
                     pattern=[[-16, 2], [0, 64]], compare_op=ALU.is_ge, fill=0.0, base=-32 * pr_, channel_multiplier=1)
                P.op("pool", "affine_select", ["PM%d" % pr_], ["PM%d" % pr_], out=PM[pr_][:], in_=PM[pr_][:],
                     pattern=[[16, 2], [0, 64]], compare_op=ALU.is_ge, fill=0.0, base=32 * pr_ + 15, channel_multiplier=-1)
                P.op("pool", "memset", [], ["CM%d" % pr_], CM[pr_][:], 1.0)
                for h in range(2):
                    P.op("pool", "affine_select", ["CM%d" % pr_], ["CM%d" % pr_], out=CM[pr_][h * 64:(h + 1) * 64],
                         in_=CM[pr_][h * 64:(h + 1) * 64], pattern=[[1, 8], [0, 16]], compare_op=ALU.is_equal, fill=0.0,
                         base=-(2 * pr_ + h), channel_multiplier=0)
            KV = sb("KV", [128, 8, 9])
            P.op("pool", "iota", [], ["KV"], KV[:], pattern=[[0, 8], [1, 9]], base=0, channel_multiplier=0,
                 allow_small_or_imprecise_dtypes=True)
            BL = sb("BL", [128, NBS])
            BHs = [sb("BH%d" % i, [128, NBS]) for i in range(2)]
            P.op("pool", "iota", [], ["BL"], BL[:], pattern=[[0, NBS // 32], [1, 32]], base=0, channel_multiplier=0,
                 allow_small_or_imprecise_dtypes=True)
            for s_ in range(2):
                P.op("pool", "iota", [], ["BH%d" % s_], BHs[s_][:], pattern=[[1, NBS // 32], [0, 32]], base=s_ * (NBS // 32),
                     channel_multiplier=0, allow_small_or_imprecise_dtypes=True)
            dcol = sb("dcolD", [128, 8])
            flg = sb("flgD", [128, 1])
            P.dma("sp", dcol[:], s5_dcol, [], ["dcolD"])
            P.dma("sp", flg[:], flag, [], ["flgD"])
            sm = sb("smD", [128, 3, 8])
            bg = sb("bgD", [128, 4, 128])
            ncim = sb("ncim", [128, 128])
            sml = {n_: sb("sm_" + n_, [128, 8]) for n_ in ("dl", "xr", "xi", "numr", "den", "t1", "t2", "qr", "qi", "tb", "t0", "t32")}
            smi = sb("sm_i", [128, 8], I32)
            k9 = {n_: sb("k9_" + n_, [128, 8, 9]) for n_ in ("er", "tn", "tf", "cs", "sn", "pr", "pi")}
            k9i = sb("k9_i", [128, 8, 9], I32)
            Bb = {n_: sb("Bb_" + n_, [128, 8, 16]) for n_ in ("r", "i", "a", "b")}
            colsT = sb("colsT", [128, 3, 4])
            scr = sb("scrD", [128, 11, 512])
            RN = lambda a, b_: ["R%d" % i for i in range(a, b_)]
            X_r = scr[:, 0:2, :].rearrange("p a (b c) -> p (a b) c", c=128)
            X_i = scr[:, 2:4, :].rearrange("p a (b c) -> p (a b) c", c=128)
            Zr_t = sb("Zr_t", [128, 9, 128])
            Zi_t = sb("Zi_t", [128, 9, 128])
            xtmp = sb("xtmp", [128, 8, 128])
            W1 = sb("W1", [128, 8, 2, 4, 128], BF16)
            W2 = sb("W2", [128, 2, 9, 2, 4, 128], BF16)
            Kbd = sb("Kbd", [128, 2, 8, 128], BF16)
            ktmp = sb("ktmp", [128, 128])
            ust = sb("ust", [128, LT // 2], BF16)
            u3 = sb("uj", [128, 8, NB], BF16)
            S = sb("S_D", [128, 4, 2, NBS])
            HB = sb("HB_D", [128, 2, 4, 2, NB + 1], BF16)
            carry = sb("carryD", [128, 4, 2])
            stg = sb("stgD", [128, NBS, 8], BF16)
            gl = [sb("glD%d" % i, [128, NBS]) for i in range(3)]
            pk = ps("pkD", [128, 4, 128])
            ptl = [ps("ptD%d" % i, [128, 4, 128]) for i in range(2)]
            pS = [ps("pSD%d" % i, [128, 512]) for i in range(2)]
            po = [ps("poD%d" % i, [128, 512]) for i in range(2)]
            mskq = _rr(["dve", "pool"])
            psq = 0
            poq = 0
            T = lambda i: scr[:, i, 0:NBS]
            DST = cfg.get("dstage", 9)
            for ft in range(cfg.get("nft", 8)):
                for hf in range(2):
                    P.dma("sp", ust[:], uT[ft * 128:(ft + 1) * 128, hf * (LT // 2):(hf + 1) * (LT // 2)], ["uT"], ["ust"])
                    usv = ust[:].rearrange("p (b j) -> p j b", j=8)
                    for j in range(8):
                        P.op("act", "activation", ["ust"], ["uft"], out=u3[:, j, hf * (NB // 2):(hf + 1) * (NB // 2)], in_=usv[:, j, :], func=AF.Copy)
                for d_ in range(2):
                    P.dma("sp", sm[:], s5_small[d_, ft], [], ["smD"])
                    P.dma("sp", bg[:], s5_big[d_, ft], [], ["bgD"])
                    are, aim, lst = sm[:, 0, :], sm[:, 1, :], sm[:, 2, :]
                    bre, bim, cre, cim = bg[:, 0, :], bg[:, 1, :], bg[:, 2, :], bg[:, 3, :]
                    V = lambda n_: sml[n_][:]
                    P.op("act", "activation", ["smD"], ["sm_dl"], out=V("dl"), in_=lst, func=AF.Exp)
                    P.op("dve", "tensor_tensor", ["smD", "sm_dl"], ["sm_xr"], out=V("xr"), in0=are, in1=V("dl"), op=ALU.mult)
                    P.op("dve", "tensor_tensor", ["smD", "sm_dl"], ["sm_xi"], out=V("xi"), in0=aim, in1=V("dl"), op=ALU.mult)
                    b9 = lambda ap: ap.unsqueeze(2).to_broadcast([128, 8, 9])
                    P.op("dve", "tensor_tensor", ["KV", "sm_xr"], ["k9_er"], out=k9["er"][:], in0=KV[:], in1=b9(V("xr")), op=ALU.mult)
                    P.op("act", "activation", ["k9_er"], ["k9_er"], out=k9["er"][:], in_=k9["er"][:], func=AF.Exp)
                    P.op("dve", "tensor_tensor", ["KV", "sm_xi"], ["k9_tn"], out=k9["tn"][:], in0=KV[:], in1=b9(V("xi")), op=ALU.mult)
                    P.op("dve", "tensor_scalar", ["k9_tn"], ["k9_tn"], out=k9["tn"][:], in0=k9["tn"][:], scalar1=1.0 / TWO_PI, scalar2=None, op0=ALU.mult)
                    for (dst, off) in (("sn", 0.0), ("cs", 0.25)):
                        if off:
                            P.op("dve", "tensor_scalar", ["k9_tn"], ["k9_tn"], out=k9["tn"][:], in0=k9["tn"][:], scalar1=off, scalar2=None, op0=ALU.add)
                        P.op("dve", "tensor_copy", ["k9_tn"], ["k9_i"], out=k9i[:], in_=k9["tn"][:])
                        P.op("dve", "tensor_copy", ["k9_i"], ["k9_tf"], out=k9["tf"][:], in_=k9i[:])
                        P.op("dve", "tensor_tensor", ["k9_tn", "k9_tf"], ["k9_tf"], out=k9["tf"][:], in0=k9["tn"][:], in1=k9["tf"][:], op=ALU.subtract)
                        P.op("act", "activation", ["k9_tf"], ["k9_" + dst], out=k9[dst][:], in_=k9["tf"][:], func=AF.Sin, scale=TWO_PI)
                    P.op("dve", "tensor_tensor", ["k9_er", "k9_cs"], ["k9_pr"], out=k9["pr"][:], in0=k9["er"][:], in1=k9["cs"][:], op=ALU.mult)
                    P.op("dve", "tensor_tensor", ["k9_er", "k9_sn"], ["k9_pi"], out=k9["pi"][:], in0=k9["er"][:], in1=k9["sn"][:], op=ALU.mult)
                    pr1, pi1 = k9["pr"][:, :, 1], k9["pi"][:, :, 1]
                    P.op("dve", "tensor_scalar", ["k9_pr"], ["sm_numr"], out=V("numr"), in0=pr1, scalar1=-1.0, scalar2=None, op0=ALU.add)
                    P.op("dve", "tensor_tensor", ["smD"], ["sm_den"], out=V("den"), in0=are, in1=are, op=ALU.mult)
                    P.op("dve", "tensor_tensor", ["smD"], ["sm_t1"], out=V("t1"), in0=aim, in1=aim, op=ALU.mult)
                    P.op("dve", "tensor_tensor", ["sm_den", "sm_t1"], ["sm_den"], out=V("den"), in0=V("den"), in1=V("t1"), op=ALU.add)
                    P.op("dve", "reciprocal", ["sm_den"], ["sm_den"], out=V("den"), in_=V("den"))
                    P.op("dve", "tensor_tensor", ["sm_numr", "smD"], ["sm_t1"], out=V("t1"), in0=V("numr"), in1=are, op=ALU.mult)
                    P.op("dve", "tensor_tensor", ["k9_pi", "smD"], ["sm_t2"], out=V("t2"), in0=pi1, in1=aim, op=ALU.mult)
                    P.op("dve", "tensor_tensor", ["sm_t1", "sm_t2"], ["sm_t1"], out=V("t1"), in0=V("t1"), in1=V("t2"), op=ALU.add)
                    P.op("dve", "tensor_tensor", ["sm_t1", "sm_den"], ["sm_qr"], out=V("qr"), in0=V("t1"), in1=V("den"), op=ALU.mult)
                    P.op("dve", "tensor_tensor", ["k9_pi", "smD"], ["sm_t1"], out=V("t1"), in0=pi1, in1=are, op=ALU.mult)
                    P.op("dve", "tensor_tensor", ["sm_numr", "smD"], ["sm_t2"], out=V("t2"), in0=V("numr"), in1=aim, op=ALU.mult)
                    P.op("dve", "tensor_tensor", ["sm_t1", "sm_t2"], ["sm_t1"], out=V("t1"), in0=V("t1"), in1=V("t2"), op=ALU.subtract)
                    P.op("dve", "tensor_tensor", ["sm_t1", "sm_den"], ["sm_qi"], out=V("qi"), in0=V("t1"), in1=V("den"), op=ALU.mult)
                    b16 = lambda ap: ap.unsqueeze(2).to_broadcast([128, 8, 16])
                    g16 = lambda ap: ap.rearrange("p (g c) -> p g c", c=16)
                    TT = lambda rd, wr, out, in0, in1, op, eng="dve": P.op(eng, "tensor_tensor", rd, wr, out=out, in0=in0, in1=in1, op=op)
                    TT(["bgD", "sm_qr"], ["Bb_a"], Bb["a"][:], g16(bre), b16(V("qr")), ALU.mult)
                    TT(["bgD", "sm_qi"], ["Bb_b"], Bb["b"][:], g16(bim), b16(V("qi")), ALU.mult)
                    TT(["Bb_a", "Bb_b"], ["Bb_r"], Bb["r"][:], Bb["a"][:], Bb["b"][:], ALU.subtract)
                    TT(["bgD", "sm_qr"], ["Bb_a"], Bb["a"][:], g16(bim), b16(V("qr")), ALU.mult)
                    TT(["bgD", "sm_qi"], ["Bb_b"], Bb["b"][:], g16(bre), b16(V("qi")), ALU.mult)
                    TT(["Bb_a", "Bb_b"], ["Bb_i"], Bb["i"][:], Bb["a"][:], Bb["b"][:], ALU.add)
                    for kk in range(8):
                        prk = b16(k9["pr"][:, :, kk])
                        pik = b16(k9["pi"][:, :, kk])
                        xr_k = g16(X_r[:, kk, :])
                        xi_k = g16(X_i[:, kk, :])
                        xt_k = g16(xtmp[:, kk, :])
                        TT(["k9_pr", "Bb_r"], RN(0, 2), xr_k, Bb["r"][:], prk, ALU.mult)
                        TT(["k9_pi", "Bb_i"], ["xtmp"], xt_k, Bb["i"][:], pik, ALU.mult, "pool")
                        TT(RN(0, 2) + ["xtmp"], RN(0, 2), xr_k, xr_k, xt_k, ALU.subtract)
                        TT(["k9_pr", "Bb_i"], RN(2, 4), xi_k, Bb["i"][:], prk, ALU.mult)
                        TT(["k9_pi", "Bb_r"], ["xtmp"], xt_k, Bb["r"][:], pik, ALU.mult, "pool")
                        TT(RN(2, 4) + ["xtmp"], RN(2, 4), xi_k, xi_k, xt_k, ALU.add)
                    for kk in range(9):
                        prk = b16(k9["pr"][:, :, kk])
                        pik = b16(k9["pi"][:, :, kk])
                        zr_k = g16(Zr_t[:, kk, :])
                        zi_k = g16(Zi_t[:, kk, :])
                        xt_k = g16(xtmp[:, kk % 8, :])
                        TT(["k9_pr", "bgD"], ["Zr_t"], zr_k, g16(cre), prk, ALU.mult)
                        TT(["k9_pi", "bgD"], ["xtmp"], xt_k, g16(cim), pik, ALU.mult, "pool")
                        TT(["Zr_t", "xtmp"], ["Zr_t"], zr_k, zr_k, xt_k, ALU.subtract)
                        TT(["k9_pi", "bgD"], ["Zi_t"], zi_k, g16(cre), pik, ALU.mult)
                        TT(["k9_pr", "bgD"], ["xtmp"], xt_k, g16(cim), prk, ALU.mult, "pool")
                        TT(["Zi_t", "xtmp"], ["Zi_t"], zi_k, zi_k, xt_k, ALU.add)
                    P.op("dve", "tensor_scalar", ["bgD"], ["ncim"], out=ncim[:], in0=cim, scalar1=-1.0, scalar2=None, op0=ALU.mult)
                    for half in range(2):
                        for tl in range(4):
                            tau = half * 4 + tl
                            P.mm(pk[:, tl, :], X_r[0:64, tau, :], cre[0:64, :], True, False, RN(0, 2) + ["bgD"], ["pkD"])
                            P.mm(pk[:, tl, :], X_i[0:64, tau, :], ncim[0:64, :], False, True, RN(2, 4) + ["ncim"], ["pkD"])
                        for tl in range(4):
                            tau = half * 4 + tl
                            if tau == 0 and d_ == 0:
                                P.op("dve", "scalar_tensor_tensor", ["identF", "dcolD", "pkD"], ["ktmp"], out=ktmp[:], in0=identF[:],
                                     scalar=dcol[:, ft:ft + 1], in1=pk[:, tl, :], op0=ALU.mult, op1=ALU.add)
                                P.op("dve", "tensor_tensor", ["ktmp", "bmD"], ["Kbd"], out=Kbd[:, d_, tau, :], in0=ktmp[:],
                                     in1=bm[:].rearrange("p g c -> p (g c)"), op=ALU.mult)
                            else:
                                P.op("dve", "tensor_tensor", ["pkD", "bmD"], ["Kbd"], out=Kbd[:, d_, tau, :], in0=pk[:, tl, :],
                                     in1=bm[:].rearrange("p g c -> p (g c)"), op=ALU.mult)
                    for kk in range(8):
                        for ri in range(2):
                            src = (X_r if ri == 0 else X_i)[:, kk, :]
                            slot = (kk * 2 + ri) % 2
                            pt = ptl[slot]
                            P.tr(pt[:, 0, :], src, identF[:], (RN(0, 2) if ri == 0 else RN(2, 4)) + ["identF"], ["ptD%d" % slot])
                            for pr_ in range(4):
                                P.op("dve", "tensor_tensor", ["ptD%d" % slot, "PM%d" % pr_], ["W1"], out=W1[:, kk, ri, pr_, :],
                                     in0=pt[:, 0, :], in1=PM[pr_][:].rearrange("p a b -> p (a b)"), op=ALU.mult)
                    for kk in range(1, 9):
                        for ri in range(2):
                            for pr_ in range(4):
                                cmv = CM[pr_][:].rearrange("p g c -> p (g c)")
                                if ri == 0:
                                    P.op("pool", "tensor_tensor", ["Zr_t", "CM%d" % pr_], ["W2"], out=W2[:, d_, kk, ri, pr_, :],
                                         in0=Zr_t[:, kk, :], in1=cmv, op=ALU.mult)
                                else:
                                    P.op("dve", "scalar_tensor_tensor", ["Zi_t", "CM%d" % pr_], ["W2"], out=W2[:, d_, kk, ri, pr_, :],
                                         in0=Zi_t[:, kk, :], scalar=-1.0, in1=cmv, op0=ALU.mult, op1=ALU.mult)
                    P.op("dve", "tensor_scalar", ["sm_xi"], ["sm_tb"], out=V("tb"), in0=V("xi"), scalar1=8.0 / TWO_PI, scalar2=None, op0=ALU.mult)
                    P.op("dve", "tensor_copy", ["sm_tb"], ["sm_i"], out=smi[:], in_=V("tb"))
                    P.op("dve", "tensor_copy", ["sm_i"], ["sm_t0"], out=V("t0"), in_=smi[:])
                    P.op("dve", "tensor_tensor", ["sm_tb", "sm_t0"], ["sm_t0"], out=V("t0"), in0=V("tb"), in1=V("t0"), op=ALU.subtract)
                    P.op("dve", "tensor_scalar", ["sm_t0"], ["sm_tb"], out=V("tb"), in0=V("t0"), scalar1=32.0, scalar2=None, op0=ALU.mult)
                    P.op("dve", "tensor_copy", ["sm_tb"], ["sm_i"], out=smi[:], in_=V("tb"))
                    P.op("dve", "tensor_copy", ["sm_i"], ["sm_t32"], out=V("t32"), in_=smi[:])
                    P.op("dve", "tensor_tensor", ["sm_tb", "sm_t32"], ["sm_t32"], out=V("t32"), in0=V("tb"), in1=V("t32"), op=ALU.subtract)
                    for h in range(2):
                        hs = slice(h * 64, (h + 1) * 64)
                        P.op("dve", "tensor_copy", ["sm_t0"], ["colsT"], out=colsT[hs, 0, :], in_=sml["t0"][hs, h::2])
                        P.op("dve", "tensor_copy", ["sm_t32"], ["colsT"], out=colsT[hs, 1, :], in_=sml["t32"][hs, h::2])
                        P.op("dve", "tensor_copy", ["k9_er"], ["colsT"], out=colsT[hs, 2, :], in_=k9["er"][hs, h::2, 8])
                    if DST < 2:
                        continue
                    for ui, s_ in enumerate((0, 1) if d_ == 0 else (1, 0)):
                        blk0 = s_ * NBS
                        for pr_ in range(4):
                            for ri in range(2):
                                pb = psq % 2
                                psq += 1
                                for j in range(8):
                                    kk = (7 - j) if d_ == 0 else j
                                    P.mm(pS[pb][:, 0:NBS], W1[:, kk, ri, pr_, :], u3[:, j, blk0:blk0 + NBS], j == 0, j == 7,
                                         ["W1", "uft"], ["pSD%d" % pb])
                                P.op("act", "activation", ["pSD%d" % pb], ["S_D%d" % pr_], out=S[:, pr_, ri, :], in_=pS[pb][:, 0:NBS], func=AF.Copy)
                        if DST < 3:
                            continue
                        for pr_ in range(4):
                            tu, ti_, tf, cs, sn, T_r, T_i, g_r, g_i, m1, m2 = [T(i) for i in range(11)]
                            tii = ti_.bitcast(I32)
                            t0c, t32c, decc = colsT[:, 0, pr_:pr_ + 1], colsT[:, 1, pr_:pr_ + 1], colsT[:, 2, pr_:pr_ + 1]
                            P.op("dve", "tensor_scalar", ["BH%d" % s_, "colsT"], ["R0"], out=tu, in0=BHs[s_][:], scalar1=t32c, scalar2=None, op0=ALU.mult)
                            P.op("dve", "scalar_tensor_tensor", ["BL", "colsT", "R0"], ["R0"], out=tu, in0=BL[:], scalar=t0c, in1=tu, op0=ALU.mult, op1=ALU.add)
                            for (dst, dn, off) in ((sn, "R4", 0.0), (cs, "R3", 0.25)):
                                if off:
                                    P.op("dve", "tensor_scalar", ["R0"], ["R0"], out=tu, in0=tu, scalar1=off, scalar2=None, op0=ALU.add)
                                P.op("dve", "tensor_copy", ["R0"], ["R1"], out=tii, in_=tu)
                                P.op("dve", "tensor_copy", ["R1"], ["R2"], out=tf, in_=tii)
                                P.op("pool", "tensor_tensor", ["R0", "R2"], ["R2"], out=tf, in0=tu, in1=tf, op=ALU.subtract)
                                P.op("act", "activation", ["R2"], [dn], out=dst, in_=tf, func=AF.Sin, scale=TWO_PI)
                            S_r, S_i = S[:, pr_, 0, :], S[:, pr_, 1, :]
                            sg = 1.0 if d_ == 0 else -1.0
                            sop = ALU.add if d_ == 0 else ALU.subtract
                            sop2 = ALU.subtract if d_ == 0 else ALU.add
                            sn_ = "S_D%d" % pr_
                            TT([sn_, "R3"], ["R5"], T_r, S_r, cs, ALU.mult)
                            TT([sn_, "R4"], ["R9"], m1, S_i, sn, ALU.mult, "pool")
                            TT(["R5", "R9"], ["R5"], T_r, T_r, m1, sop)
                            TT([sn_, "R3"], ["R6"], T_i, S_i, cs, ALU.mult)
                            TT([sn_, "R4"], ["R10"], m2, S_r, sn, ALU.mult, "pool")
                            TT(["R6", "R10"], ["R6"], T_i, T_i, m2, sop2)
                            rv = (lambda ap: ap) if d_ == 0 else (lambda ap: ap[:, ::-1])
                            dbc = decc.to_broadcast([128, NBS])
                            for (src, sname, dst, dname, ri) in ((T_r, "R5", g_r, "R7", 0), (T_i, "R6", g_i, "R8", 1)):
                                init = 0.0 if ui == 0 else carry[:, pr_, ri:ri + 1]
                                P.op("dve", "tensor_tensor_scan", [sname, "colsT", "carryD"], [dname], out=rv(dst), data0=dbc, data1=rv(src),
                                     initial=init, op0=ALU.mult, op1=ALU.add)
                                if ui == 0:
                                    lastc = (NBS - 1) if d_ == 0 else 0
                                    P.op("dve", "tensor_scalar", [dname, "flgD"], ["carryD"], out=carry[:, pr_, ri:ri + 1], in0=dst[:, lastc:lastc + 1],
                                         scalar1=flg[:, 0:1], scalar2=None, op0=ALU.mult)
                            if d_ == 0:
                                o_r = HB[:, 0, pr_, 0, blk0 + 1:blk0 + NBS + 1]
                                o_i = HB[:, 0, pr_, 1, blk0 + 1:blk0 + NBS + 1]
                                sl_ = slice(0, NBS)
                            elif s_ == 1:
                                o_r = HB[:, 1, pr_, 0, NBS - 1:NB - 1]
                                o_i = HB[:, 1, pr_, 1, NBS - 1:NB - 1]
                                sl_ = slice(0, NBS)
                            else:
                                o_r = HB[:, 1, pr_, 0, 0:NBS - 1]
                                o_i = HB[:, 1, pr_, 1, 0:NBS - 1]
                                sl_ = slice(1, NBS)
                            hn = "HB%d_%d" % (d_, pr_)
                            TT(["R7", "R3"], ["R9"], m1, g_r, cs, ALU.mult)
                            TT(["R8", "R4"], ["R10"], m2, g_i, sn, ALU.mult, "pool")
                            TT(["R9", "R10"], [hn], o_r, m1[:, sl_], m2[:, sl_], sop2)
                            TT(["R8", "R3"], ["R9"], m1, g_i, cs, ALU.mult)
                            TT(["R7", "R4"], ["R10"], m2, g_r, sn, ALU.mult, "pool")
                            TT(["R9", "R10"], [hn], o_i, m1[:, sl_], m2[:, sl_], sop)
                            if ui == 1:
                                zc = 0 if d_ == 0 else NB - 1
                                fc = NBS if d_ == 0 else NBS - 1
                                P.op("dve", "memset", [], [hn], HB[:, d_, pr_, :, zc:zc + 1], 0.0)
                                P.op("dve", "tensor_scalar", [hn, "flgD"], [hn], out=HB[:, d_, pr_, :, fc:fc + 1], in0=HB[:, d_, pr_, :, fc:fc + 1],
                                     scalar1=flg[:, 0:1], scalar2=None, op0=ALU.mult)
                if DST < 4:
                    continue
                hall = ["HB%d_%d" % (a, b_) for a in range(2) for b_ in range(4)]
                for s_ in range(2):
                    blk0 = s_ * NBS
                    for i in range(8):
                        pb = poq % 2
                        poq += 1
                        mms = []
                        for j in range(0, i + 1):
                            mms.append((Kbd[:, 0, i - j, :], u3[:, j, blk0:blk0 + NBS]))
                        for j in range(i, 8):
                            mms.append((Kbd[:, 1, j - i, :], u3[:, j, blk0:blk0 + NBS]))
                        for pr_ in range(4):
                            for ri in range(2):
                                mms.append((W2[:, 0, i + 1, ri, pr_, :], HB[:, 0, pr_, ri, blk0:blk0 + NBS]))
                                mms.append((W2[:, 1, 8 - i, ri, pr_, :], HB[:, 1, pr_, ri, blk0:blk0 + NBS]))
                        for mi, (l_, r_) in enumerate(mms):
                            P.mm(po[pb][:, 0:NBS], l_, r_, mi == 0, mi == len(mms) - 1, ["Kbd", "W2", "uft"] + hall, ["poD%d" % pb])
                        v = po[pb][:, 0:NBS]
                        P.op("act", "activation", ["poD%d" % pb], ["glD0"], out=gl[0][:], in_=v, func=AF.Square)
                        P.op("dve", "tensor_scalar", ["glD0"], ["glD0"], out=gl[0][:], in0=gl[0][:], scalar1=0.044715, scalar2=1.0, op0=ALU.mult, op1=ALU.add)
                        P.op("dve", "tensor_tensor", ["glD0", "poD%d" % pb], ["glD1"], out=gl[1][:], in0=gl[0][:], in1=v, op=ALU.mult)
                        P.op("act", "activation", ["glD1"], ["glD2"], out=gl[2][:], in_=gl[1][:], func=AF.Sigmoid, scale=1.5957691216057308)
                        P.op("dve", "tensor_tensor", ["glD2", "poD%d" % pb], ["stgD"], out=stg[:, :, i], in0=gl[2][:], in1=v, op=ALU.mult)
                    P.dma("sp", y5gT[ft * 128:(ft + 1) * 128, s_ * LSEG:(s_ + 1) * LSEG], stg[:].rearrange("p b i -> p (b i)"), ["stgD"], ["y5gT"])
        if cfg.get("dump"):
            for i_, o_ in enumerate(P.ops):
                print(i_, o_.eng, getattr(o_, 'desc', 'mm/dma') if hasattr(o_, 'desc') else '-', o_.reads, o_.writes)
        P.emit()
        P.limit = None

        with ExitStack() as st:
            sb = lambda n, s, d=F32: st.enter_context(nc.sbuf_tensor(n, list(s), d))
            gA = sb("gA", [128, 16])
            gM = sb("gM", [128, 16])
            P.dma("sp", gA[:], norm_attn, [], ["gA"])
            P.dma("sp", gM[:], norm_mem, [], ["gM"])
            wst2 = [sb("wst2_%d" % i, [128, 2048]) for i in range(2)]
            wbf2 = [sb("wbf2_%d" % i, [128, 2048], BF16) for i in range(2)]
            ci = 0
            ceng = _rr(["dve", "pool"])
            for (src, dst, gn_, gname, kdim, ncol) in ((w_glu, w_glu_bf, None, None, 1024, 1024), (w_out, w_out_bf, None, None, 2048, 2048),
                                                       (w_q, w_q_bf, gA, "gA", 2048, 2048), (w_k, w_k_bf, gM, "gM", 2048, 2048),
                                                       (w_v, w_v_bf, gM, "gM", 2048, 2048), (w_o, w_o_bf, None, None, 2048, 2048)):
                for kc in range(kdim // 128):
                    b = ci % 2
                    ci += 1
                    P.dma("sp", wst2[b][:, 0:ncol], src[kc * 128:(kc + 1) * 128, :], [], ["wst2_%d" % b])
                    if gn_ is None:
                        if ceng() == "dve":
                            P.op("dve", "tensor_copy", ["wst2_%d" % b], ["wbf2_%d" % b], out=wbf2[b][:, 0:ncol], in_=wst2[b][:, 0:ncol])
                        else:
                            P.op("act", "activation", ["wst2_%d" % b], ["wbf2_%d" % b], out=wbf2[b][:, 0:ncol], in_=wst2[b][:, 0:ncol], func=AF.Copy)
                    else:
                        P.op("dve", "tensor_scalar", ["wst2_%d" % b, gname], ["wbf2_%d" % b], out=wbf2[b][:, 0:ncol], in0=wst2[b][:, 0:ncol],
                             scalar1=gn_[:, kc:kc + 1], scalar2=None, op0=ALU.mult)
                    P.dma("sp", dst[:, :, kc, :].rearrange("g p n -> p g n"), wbf2[b][:, 0:ncol].rearrange("p (g n) -> p g n", n=512),
                          ["wbf2_%d" % b], [dst.tensor.name])
        P.emit()

        with ExitStack() as st:
            sb = lambda n, s, d=F32: st.enter_context(nc.sbuf_tensor(n, list(s), d))
            ps = lambda n, s, d=F32: st.enter_context(nc.psum_tensor(n, list(s), d))
            ident = sb("identM", [128, 128], BF16)
            P.op("pool", "memset", [], ["identM"], ident[:], 1.0)
            P.op("pool", "affine_select", ["identM"], ["identM"], out=ident[:], in_=ident[:], pattern=[[-1, 128]],
                 compare_op=ALU.is_equal, fill=0.0, base=0, channel_multiplier=1)
            mt = sb("mtM", [128, 2048])
            junk = sb("junkM", [128, 2048], BF16)
            ssm = sb("ssM", [128, 1])
            mn = sb("mnM", [128, 2048], BF16)
            memT = sb("memT", [128, 16, 256], BF16)
            wck = [sb("wckM%d" % i, [128, 16, 512], BF16) for i in range(2)]
            kTs = sb("kTs", [128, 16, 256], BF16)
            vs = sb("vs", [128, 2, 2048], BF16)
            ptm = [ps("ptM%d" % i, [128, 1024], BF16) for i in range(2)]
            pmm_ = [ps("pmM%d" % i, [128, 512]) for i in range(2)]
            tq = 0
            pq = 0
            wq_ = 0
            for sg in range(2):
                for mc in range(2):
                    P.dma("sp", mt[:], mem_in[sg, mc * 128:(mc + 1) * 128, :], [], ["mtM"])
                    P.op("act", "activation", ["mtM"], ["junkM", "ssM"], out=junk[:], in_=mt[:], func=AF.Square, accum_out=ssm[:])
                    P.op("dve", "tensor_scalar", ["ssM"], ["ssM"], out=ssm[:], in0=ssm[:], scalar1=1.0 / D, scalar2=EPS, op0=ALU.mult, op1=ALU.add)
                    P.op("act", "activation", ["ssM"], ["ssM"], out=ssm[:], in_=ssm[:], func=AF.Sqrt)
                    P.op("dve", "reciprocal", ["ssM"], ["ssM"], out=ssm[:], in_=ssm[:])
                    P.op("dve", "tensor_scalar", ["mtM", "ssM"], ["mnM"], out=mn[:], in0=mt[:], scalar1=ssm[:, 0:1], scalar2=None, op0=ALU.mult)
                    for q4 in range(4):
                        pb = tq % 2
                        tq += 1
                        for j in range(4):
                            kc = q4 * 4 + j
                            P.tr(ptm[pb][:, j * 128:(j + 1) * 128], mn[:, kc * 128:(kc + 1) * 128], ident[:], ["mnM", "identM"], ["ptM%d" % pb])
                        P.op("act", "activation", ["ptM%d" % pb], ["memT"], out=memT[:, q4 * 4:(q4 + 1) * 4, mc * 128:(mc + 1) * 128],
                             in_=ptm[pb][:, 0:512].rearrange("p (j n) -> p j n", n=128), func=AF.Copy)
                for g in range(4):
                    wb = wq_ % 2
                    wq_ += 1
                    P.dma("sp", wck[wb][:], w_k_bf[g], ["w_k_bf"], ["wckM%d" % wb])
                    for mcc in range(4):
                        pb = pq % 2
                        pq += 1
                        for kc in range(16):
                            P.mm(pmm_[pb][:, 0:256], wck[wb][:, kc, mcc * 128:(mcc + 1) * 128], memT[:, kc, :], kc == 0, kc == 15,
                                 ["wckM%d" % wb, "memT"], ["pmM%d" % pb])
                        P.op("act", "activation", ["pmM%d" % pb], ["kTs"], out=kTs[:, g * 4 + mcc, :], in_=pmm_[pb][:, 0:256], func=AF.Copy)
                for g in range(4):
                    wb = wq_ % 2
                    wq_ += 1
                    P.dma("sp", wck[wb][:], w_v_bf[g], ["w_v_bf"], ["wckM%d" % wb])
                    for mc in range(2):
                        pb = pq % 2
                        pq += 1
                        for kc in range(16):
                            P.mm(pmm_[pb][:], memT[:, kc, mc * 128:(mc + 1) * 128], wck[wb][:, kc, :], kc == 0, kc == 15,
                                 ["wckM%d" % wb, "memT"], ["pmM%d" % pb])
                        P.op("dve", "tensor_copy", ["pmM%d" % pb], ["vs"], out=vs[:, mc, g * 512:(g + 1) * 512], in_=pmm_[pb][:])
                P.dma("sp", kT_d[sg], kTs[:], ["kTs"], ["kT_d"])
                P.dma("sp", v_d[sg], vs[:], ["vs"], ["v_d"])
        P.emit()

        with ExitStack() as st:
            sb = lambda n, s, d=F32: st.enter_context(nc.sbuf_tensor(n, list(s), d))
            ps = lambda n, s, d=F32: st.enter_context(nc.psum_tensor(n, list(s), d))
            wgl = sb("wgl", [128, 8, 1024], BF16)
            for g_ in range(2):
                P.dma("sp", wgl[:, :, g_ * 512:(g_ + 1) * 512], w_glu_bf[g_], ["w_glu_bf"], ["wgl"])
            onesb = sb("onesbE", [128, 128], BF16)
            P.op("pool", "memset", [], ["onesbE"], onesb[:], 1.0)
            g5 = sb("g5", [128, 8])
            P.dma("sp", g5[:], s5_ncol, [], ["g5"])
            y5 = [sb("y5E%d" % i, [128, 8, 512], BF16) for i in range(2)]
            y2 = sb("y2E", [128, 8, 512])
            sig = [sb("sigE%d" % i, [128, 512]) for i in range(2)]
            sqb = [sb("sqE%d" % i, [128, 512], BF16) for i in range(2)]
            rs = sb("rsE", [128, 512])
            pg = [ps("pgE%d" % i, [128, 512]) for i in range(2)]
            pss = ps("pssE", [128, 512])
            for s in range(NST):
                t0 = s * 512
                b = s % 2
                P.dma("sp", y5[b][:], y5gT[:, t0:t0 + 512].rearrange("(f p) t -> p f t", p=128), ["y5gT"], ["y5E%d" % b])
                for n_ in range(8):
                    q = n_ % 2
                    for kc in range(8):
                        P.mm(pg[q][:], wgl[:, kc, n_ * 128:(n_ + 1) * 128], y5[b][:, kc, :], kc == 0, kc == 7, ["wgl", "y5E%d" % b], ["pgE%d" % q])
                    P.op("act", "activation", ["pgE%d" % q], ["sigE%d" % q], out=sig[q][:], in_=pg[q][:], func=AF.Sigmoid)
                    P.op("dve", "tensor_tensor", ["y5E%d" % b, "sigE%d" % q], ["y2E"], out=y2[:, n_, :], in0=y5[b][:, n_, :], in1=sig[q][:], op=ALU.mult)
                    P.op("act", "activation", ["y2E"], ["sqE%d" % q], out=sqb[q][:], in_=y2[:, n_, :], func=AF.Square)
                    P.mm(pss[:], onesb[:], sqb[q][:], n_ == 0, n_ == 7, ["onesbE", "sqE%d" % q], ["pssE"])
                P.op("dve", "tensor_scalar", ["pssE"], ["rsE"], out=rs[:], in0=pss[:], scalar1=1.0 / 1024, scalar2=EPS, op0=ALU.mult, op1=ALU.add)
                P.op("act", "activation", ["rsE"], ["rsE"], out=rs[:], in_=rs[:], func=AF.Sqrt)
                P.op("dve", "reciprocal", ["rsE"], ["rsE"], out=rs[:], in_=rs[:])
                for n_ in range(8):
                    P.op("dve", "scalar_tensor_tensor", ["y2E", "g5", "rsE"], ["y5E%d" % b], out=y5[b][:, n_, :], in0=y2[:, n_, :],
                         scalar=g5[:, n_:n_ + 1], in1=rs[:], op0=ALU.mult, op1=ALU.mult)
                P.dma("sp", y5nT[:, t0:t0 + 512].rearrange("(f p) t -> p f t", p=128), y5[b][:], ["y5E%d" % b], ["y5nT"])
        P.emit()

        NTT = LT // 128
        with ExitStack() as st:
            sb = lambda n, s, d=F32: st.enter_context(nc.sbuf_tensor(n, list(s), d))
            ps = lambda n, s, d=F32: st.enter_context(nc.psum_tensor(n, list(s), d))
            ident = sb("identE", [128, 128], BF16)
            P.op("pool", "memset", [], ["identE"], ident[:], 1.0)
            P.op("pool", "affine_select", ["identE"], ["identE"], out=ident[:], in_=ident[:], pattern=[[-1, 128]],
                 compare_op=ALU.is_equal, fill=0.0, base=0, channel_multiplier=1)
            onesb = sb("onesb2", [128, 128], BF16)
            P.op("pool", "memset", [], ["onesb2"], onesb[:], 1.0)
            gff = sb("gff", [128, 2048])
            P.dma("sp", gff[:], nffn_rep, [], ["gff"])
            wrs = sb("wrs", [128, 16, 16])
            wrb = sb("wrb", [128, 16, 16], BF16)
            P.dma("sp", wrs[:], w_router_l, [], ["wrs"])
            P.op("dve", "tensor_copy", ["wrs"], ["wrb"], out=wrb[:], in_=wrs[:])
            zrow = sb("zrow", [128, 2048])
            zrowb = sb("zrowb", [128, 2048], BF16)
            P.op("pool", "memset", [], ["zrow"], zrow[:], 0.0)
            P.op("pool", "memset", [], ["zrowb"], zrowb[:], 0.0)
            for r_ in range(0, CAPL, 128):
                for g_ in range(4):
                    P.dma("sp", acc_l[g_][LT + r_:LT + r_ + 128, :], zrow[:, 0:512], ["zrow"], ["acc%d" % g_])
                P.dma("sp", hn3_bf[LT + r_:LT + r_ + 128, :], zrowb[:], ["zrowb"], ["hn3_bf"])
            wch = [sb("wch%d" % i, [128, 16, 512], BF16) for i in range(2)]
            x1 = sb("x1", [128, 4, 2048])
            ysd = sb("ysd", [128, 4, 1024], BF16)
            yT = sb("yT", [128, 16, 512], BF16)
            hn = sb("hnE", [128, 2048], BF16)
            junk = sb("junkE", [128, 2048], BF16)
            ss = sb("ssE", [128, 1])
            hnT = sb("hnT", [128, 16, 512], BF16)
            qT = sb("qT", [128, 16, 512], BF16)
            kTs = sb("kTs2", [128, 16, 256], BF16)
            vs = sb("vs2", [128, 2, 2048], BF16)
            eT = [sb("eT%d" % i, [128, 512], BF16) for i in range(2)]
            rden = sb("rden", [128, 512])
            hT3 = sb("hT3", [128, 16, 128], BF16)
            pe_ = sb("peE", [128, 16])
            psum_ = sb("psumE", [128, 1])
            ptr = [ps("ptrE%d" % i, [128, 1024], BF16) for i in range(2)]
            pmm = [ps("pmmE%d" % i, [128, 512]) for i in range(4)]
            prt = ps("prtE", [128, 512])
            tq = [0]
            pq = [0]
            wq_ = [0]
            cpe = _rr(["act", "dve"])

            def nxt(c, n):
                v = c[0] % n
                c[0] += 1
                return v

            def rms_to(tt, out_ap, names_w, extra=None):
                P.op("act", "activation", ["x1_%d" % tt], ["junkE", "ssE"], out=junk[:], in_=x1[:, tt, :], func=AF.Square, accum_out=ss[:])
                P.op("dve", "tensor_scalar", ["ssE"], ["ssE"], out=ss[:], in0=ss[:], scalar1=1.0 / D, scalar2=EPS, op0=ALU.mult, op1=ALU.add)
                P.op("act", "activation", ["ssE"], ["ssE"], out=ss[:], in_=ss[:], func=AF.Sqrt)
                P.op("dve", "reciprocal", ["ssE"], ["ssE"], out=ss[:], in_=ss[:])
                if extra is None:
                    P.op("dve", "tensor_scalar", ["x1_%d" % tt, "ssE"], names_w, out=out_ap, in0=x1[:, tt, :], scalar1=ss[:, 0:1], scalar2=None, op0=ALU.mult)
                else:
                    P.op("dve", "scalar_tensor_tensor", ["x1_%d" % tt, "ssE", "gff"], names_w, out=out_ap, in0=x1[:, tt, :], scalar=ss[:, 0:1],
                         in1=extra, op0=ALU.mult, op1=ALU.mult)

            def transposes16(src_tile, src_name, dst_fn, dst_name):
                for q4 in range(4):
                    pb = nxt(tq, 2)
                    for j in range(4):
                        kc = q4 * 4 + j
                        P.tr(ptr[pb][:, j * 128:(j + 1) * 128], src_tile[:, kc * 128:(kc + 1) * 128], ident[:], [src_name, "identE"], ["ptrE%d" % pb])
                    ce = cpe()
                    src = ptr[pb][:, 0:512].rearrange("p (j n) -> p j n", n=128)
                    if ce == "act":
                        P.op("act", "activation", ["ptrE%d" % pb], [dst_name], out=dst_fn(q4), in_=src, func=AF.Copy)
                    else:
                        P.op("dve", "tensor_copy", ["ptrE%d" % pb], [dst_name], out=dst_fn(q4), in_=src)

            def proj_tok(w_d, wname, lhs_tile, lhs_name):
                for dch in range(4):
                    wb = nxt(wq_, 2)
                    P.dma("sp", wch[wb][:], w_d[dch], [wname], ["wch%d" % wb])
                    for tt in range(4):
                        pb = nxt(pq, 4)
                        for kc in range(16):
                            P.mm(pmm[pb][:], lhs_tile[:, kc, tt * 128:(tt + 1) * 128], wch[wb][:, kc, :], kc == 0, kc == 15,
                                 [lhs_name, "wch%d" % wb], ["pmmE%d" % pb])
                        P.op("dve", "tensor_tensor", ["x1_%d" % tt, "pmmE%d" % pb], ["x1_%d" % tt], out=x1[:, tt, dch * 512:(dch + 1) * 512],
                             in0=x1[:, tt, dch * 512:(dch + 1) * 512], in1=pmm[pb][:], op=ALU.add)

            cur_seg = -1
            for s in range(NST):
                t0 = s * 512
                sg = t0 // LSEG
                if sg != cur_seg:
                    cur_seg = sg
                    P.dma("sp", kTs[:], kT_d[sg], ["kT_d"], ["kTs2"])
                    P.dma("sp", vs[:], v_d[sg], ["v_d"], ["vs2"])
                for tt in range(4):
                    P.dma("sp", x1[:, tt, :], x_in[t0 + tt * 128:t0 + (tt + 1) * 128, :], [], ["x1_%d" % tt])
                P.dma("sp", ysd[:], yssd[t0:t0 + 512, :].rearrange("(t p) c -> p t c", p=128), ["yssd"], ["ysd"])
                P.dma("sp", yT[:, 8:16, :], y5nT[:, t0:t0 + 512].rearrange("(f p) t -> p f t", p=128), ["y5nT"], ["yT"])
                for tt in range(4):
                    for q2 in range(2):
                        pb = nxt(tq, 2)
                        for j in range(4):
                            kc = q2 * 4 + j
                            P.tr(ptr[pb][:, j * 128:(j + 1) * 128], ysd[:, tt, kc * 128:(kc + 1) * 128], ident[:], ["ysd", "identE"], ["ptrE%d" % pb])
                        P.op("act", "activation", ["ptrE%d" % pb], ["yT"], out=yT[:, q2 * 4:(q2 + 1) * 4, tt * 128:(tt + 1) * 128],
                             in_=ptr[pb][:, 0:512].rearrange("p (j n) -> p j n", n=128), func=AF.Copy)
                proj_tok(w_out_bf, "w_out_bf", yT, "yT")
                for tt in range(4):
                    rms_to(tt, hn[:], ["hnE"])
                    transposes16(hn, "hnE", lambda q4, tt=tt: hnT[:, q4 * 4:(q4 + 1) * 4, tt * 128:(tt + 1) * 128], "hnT")
                for g in range(4):
                    wb = nxt(wq_, 2)
                    P.dma("sp", wch[wb][:], w_q_bf[g], ["w_q_bf"], ["wch%d" % wb])
                    for mcc in range(4):
                        pb = nxt(pq, 4)
                        for kc in range(16):
                            P.mm(pmm[pb][:], wch[wb][:, kc, mcc * 128:(mcc + 1) * 128], hnT[:, kc, :], kc == 0, kc == 15,
                                 ["wch%d" % wb, "hnT"], ["pmmE%d" % pb])
                        P.op("act", "activation", ["pmmE%d" % pb], ["qT"], out=qT[:, g * 4 + mcc, :], in_=pmm[pb][:], func=AF.Copy)
                for h in range(4):
                    for mck in range(2):
                        pb = nxt(pq, 4)
                        for dc in range(4):
                            P.mm(pmm[pb][:], kTs[:, h * 4 + dc, mck * 128:(mck + 1) * 128], qT[:, h * 4 + dc, :], dc == 0, dc == 3,
                                 ["kTs2", "qT"], ["pmmE%d" % pb])
                        P.op("act", "activation", ["pmmE%d" % pb], ["eT%d" % mck], out=eT[mck][:], in_=pmm[pb][:], func=AF.Exp, scale=512 ** -0.5)
                    pb = nxt(pq, 4)
                    for mck in range(2):
                        P.mm(pmm[pb][:], onesb[:], eT[mck][:], mck == 0, mck == 1, ["onesb2", "eT%d" % mck], ["pmmE%d" % pb])
                    P.op("dve", "reciprocal", ["pmmE%d" % pb], ["rden"], out=rden[:], in_=pmm[pb][:])
                    for dvc in range(4):
                        pb = nxt(pq, 4)
                        for mck in range(2):
                            P.mm(pmm[pb][:], vs[:, mck, h * 512 + dvc * 128:h * 512 + (dvc + 1) * 128], eT[mck][:], mck == 0, mck == 1,
                                 ["vs2", "eT%d" % mck], ["pmmE%d" % pb])
                        P.op("dve", "tensor_tensor", ["pmmE%d" % pb, "rden"], ["hnT"], out=hnT[:, h * 4 + dvc, :], in0=pmm[pb][:], in1=rden[:], op=ALU.mult)
                proj_tok(w_o_bf, "w_o_bf", hnT, "hnT")
                for tt in range(4):
                    r0 = t0 + tt * 128
                    for g_ in range(4):
                        P.dma("sp", acc_l[g_][r0:r0 + 128, :], x1[:, tt, g_ * 512:(g_ + 1) * 512], ["x1_%d" % tt], ["acc%d" % g_])
                    rms_to(tt, hn[:], ["hnE"], extra=gff[:])
                    P.dma("sp", hn3_bf[r0:r0 + 128, :], hn[:], ["hnE"], ["hn3_bf"])
                    transposes16(hn, "hnE", lambda q4: hT3[:, q4 * 4:(q4 + 1) * 4, :], "hT3")
                    for kc in range(16):
                        P.mm(prt[:, 0:16], hT3[:, kc, :], wrb[:, kc, :], kc == 0, kc == 15, ["hT3", "wrb"], ["prtE"])
                    P.op("act", "activation", ["prtE"], ["peE", "psumE"], out=pe_[:], in_=prt[:, 0:16], func=AF.Exp, accum_out=psum_[:])
                    P.op("dve", "reciprocal", ["psumE"], ["psumE"], out=psum_[:], in_=psum_[:])
                    P.op("dve", "tensor_scalar", ["peE", "psumE"], ["peE"], out=pe_[:], in0=pe_[:], scalar1=psum_[:, 0:1], scalar2=None, op0=ALU.mult)
                    P.dma("sp", probs_loc[r0:r0 + 128, :], pe_[:], ["peE"], ["probs_loc"])
        P.emit()

        GSZ = cfg.get("GSZ", 1)
        NG = GSZ * LT
        JG = NG // 128
        CAPG = NG // 8
        NCHK = CAPL // 128
        if GSZ > 1:
            P.add("pool", lambda e: e.collective_compute("AllGather", ALU.bypass, replica_groups=cfg["groups"], ins=[probs_loc], outs=[probs_all]),
                  reads=["probs_loc"], writes=["probs_all"], own_sem=P.csem)
            pall = probs_all
        else:
            pall = probs_loc
        with ExitStack() as st:
            sb = lambda n, s, d=F32: st.enter_context(nc.sbuf_tensor(n, list(s), d))
            ps = lambda n, s, d=F32: st.enter_context(nc.psum_tensor(n, list(s), d))
            PA = sb("PA", [128, JG, 16])
            cmpt = sb("cmpt", [128, JG, 16])
            P.dma("sp", PA[:], pall.rearrange("(p j) e -> p j e", j=JG), ["probs_all", "probs_loc"], ["PA"])
            onesF = sb("onesFF", [128, 128])
            P.op("pool", "memset", [], ["onesFF"], onesF[:], 1.0)
            lo, hi, mid, ge, dd, cntp = [sb("bs_" + n_, [128, 16]) for n_ in ("lo", "hi", "mid", "ge", "dd", "cntp")]
            P.op("dve", "memset", [], ["bs_lo"], lo[:], 0.0)
            P.op("dve", "memset", [], ["bs_hi"], hi[:], 1.0)
            ptot = ps("ptot", [128, 512])
            for it_ in range(32):
                P.op("dve", "tensor_tensor", ["bs_lo", "bs_hi"], ["bs_mid"], out=mid[:], in0=lo[:], in1=hi[:], op=ALU.add)
                P.op("dve", "tensor_scalar", ["bs_mid"], ["bs_mid"], out=mid[:], in0=mid[:], scalar1=0.5, scalar2=None, op0=ALU.mult)
                P.op("dve", "tensor_tensor", ["PA", "bs_mid"], ["cmpt"], out=cmpt[:], in0=PA[:], in1=mid[:].unsqueeze(1).to_broadcast([128, JG, 16]), op=ALU.is_ge)
                P.op("dve", "tensor_reduce", ["cmpt"], ["bs_cntp"], out=cntp[:], in_=cmpt[:].rearrange("p j e -> p e j"), op=ALU.add, axis=AX.X)
                P.mm(ptot[:, 0:16], onesF[:], cntp[:], True, True, ["onesFF", "bs_cntp"], ["ptot"])
                P.op("dve", "tensor_scalar", ["ptot"], ["bs_ge"], out=ge[:], in0=ptot[:, 0:16], scalar1=float(CAPG), scalar2=None, op0=ALU.is_ge)
                P.op("dve", "tensor_tensor", ["bs_mid", "bs_lo"], ["bs_dd"], out=dd[:], in0=mid[:], in1=lo[:], op=ALU.subtract)
                P.op("dve", "tensor_tensor", ["bs_dd", "bs_ge"], ["bs_dd"], out=dd[:], in0=dd[:], in1=ge[:], op=ALU.mult)
                P.op("dve", "tensor_tensor", ["bs_lo", "bs_dd"], ["bs_lo"], out=lo[:], in0=lo[:], in1=dd[:], op=ALU.add)
                P.op("dve", "tensor_tensor", ["bs_hi", "bs_mid"], ["bs_dd"], out=dd[:], in0=hi[:], in1=mid[:], op=ALU.subtract)
                P.op("dve", "tensor_tensor", ["bs_dd", "bs_ge"], ["bs_dd"], out=dd[:], in0=dd[:], in1=ge[:], op=ALU.mult)
                P.op("dve", "tensor_tensor", ["bs_mid", "bs_dd"], ["bs_hi"], out=hi[:], in0=mid[:], in1=dd[:], op=ALU.add)
            PL = sb("PL", [128, NTT, 16])
            msk = sb("msk", [128, NTT, 16])
            gat = sb("gat", [128, NTT, 16])
            mskb = sb("mskb", [128, NTT, 16], BF16)
            P.dma("sp", PL[:], probs_loc.rearrange("(t p) e -> p t e", p=128), ["probs_loc"], ["PL"])
            P.op("dve", "tensor_tensor", ["PL", "bs_lo"], ["msk"], out=msk[:], in0=PL[:], in1=lo[:].unsqueeze(1).to_broadcast([128, NTT, 16]), op=ALU.is_ge)
            P.op("dve", "tensor_tensor", ["PL", "msk"], ["gat"], out=gat[:], in0=PL[:], in1=msk[:], op=ALU.mult)
            P.op("dve", "tensor_copy", ["msk"], ["mskb"], out=mskb[:], in_=msk[:])
            triS = sb("triS", [128, 128], BF16)
            onesb = sb("onesbF", [128, 128], BF16)
            P.op("pool", "memset", [], ["onesbF"], onesb[:], 1.0)
            P.op("pool", "memset", [], ["triS"], triS[:], 1.0)
            P.op("pool", "affine_select", ["triS"], ["triS"], out=triS[:], in_=triS[:], pattern=[[1, 128]], compare_op=ALU.is_ge, fill=0.0,
                 base=-1, channel_multiplier=-1)
            posi = sb("posi", [128, NTT, 16])
            tots = sb("tots", [128, NTT, 16])
            cum = sb("cum", [128, NTT, 16])
            pps = [ps("ppsF%d" % i, [128, 512]) for i in range(2)]
            mflat = mskb[:].rearrange("p t e -> p (t e)")
            NTE = NTT * 16
            for c0 in range(0, NTE, 512):
                c1 = min(c0 + 512, NTE)
                P.mm(pps[0][:, 0:c1 - c0], triS[:], mflat[:, c0:c1], True, True, ["triS", "mskb"], ["ppsF0"])
                P.op("dve", "tensor_copy", ["ppsF0"], ["posi"], out=posi[:].rearrange("p t e -> p (t e)")[:, c0:c1], in_=pps[0][:, 0:c1 - c0])
                P.mm(pps[1][:, 0:c1 - c0], onesb[:], mflat[:, c0:c1], True, True, ["onesbF", "mskb"], ["ppsF1"])
                P.op("dve", "tensor_copy", ["ppsF1"], ["tots"], out=tots[:].rearrange("p t e -> p (t e)")[:, c0:c1], in_=pps[1][:, 0:c1 - c0])
            onecol = sb("onecol", [128, 1])
            P.op("dve", "memset", [], ["onecol"], onecol[:], 1.0)
            for e_ in range(16):
                P.op("dve", "tensor_tensor_scan", ["tots", "onecol"], ["cum"], out=cum[:, :, e_], data0=onecol[:, 0:1].to_broadcast([128, NTT]),
                     data1=tots[:, :, e_], initial=0.0, op0=ALU.mult, op1=ALU.add)
            P.op("dve", "tensor_tensor", ["cum", "tots"], ["cum"], out=cum[:], in0=cum[:], in1=tots[:], op=ALU.subtract)
            P.op("dve", "tensor_tensor", ["cum", "posi"], ["posi"], out=posi[:], in0=posi[:], in1=cum[:], op=ALU.add)
            P.op("dve", "tensor_scalar", ["posi"], ["posi"], out=posi[:], in0=posi[:], scalar1=1.0, scalar2=None, op0=ALU.add)
            P.op("dve", "tensor_tensor", ["posi", "msk"], ["posi"], out=posi[:], in0=posi[:], in1=msk[:], op=ALU.mult)
            P.op("dve", "tensor_scalar", ["posi"], ["posi"], out=posi[:], in0=posi[:], scalar1=-1.0, scalar2=None, op0=ALU.add)
            G5 = sb("G5", [128, NTT, 16, 8], BF16)
            P.op("pool", "memset", [], ["G5"], G5[:], 0.0)
            tvf = sb("tvf", [128, NTT, 16])
            P.op("pool", "iota", [], ["tvf"], tvf[:], pattern=[[0, NTT], [0, 16]], base=0, channel_multiplier=1, allow_small_or_imprecise_dtypes=True)
            P.op("dve", "tensor_copy", ["tvf", "G5"], ["G5"], out=G5[:, :, :, 0], in_=tvf[:])
            P.op("pool", "iota", ["tvf"], ["tvf"], tvf[:], pattern=[[128, NTT], [0, 16]], base=0, channel_multiplier=0, allow_small_or_imprecise_dtypes=True)
            P.op("dve", "tensor_copy", ["tvf", "G5"], ["G5"], out=G5[:, :, :, 1], in_=tvf[:])
            P.op("dve", "memset", ["G5"], ["G5"], G5[:, :, :, 5], 1.0)
            gres = sb("gres", [128, NTT, 16])
            P.op("dve", "tensor_copy", ["gat", "G5"], ["G5"], out=G5[:, :, :, 2], in_=gat[:])
            P.op("dve", "tensor_tensor", ["gat", "G5"], ["gres"], out=gres[:], in0=gat[:], in1=G5[:, :, :, 2], op=ALU.subtract)
            P.op("dve", "tensor_copy", ["gres", "G5"], ["G5"], out=G5[:, :, :, 3], in_=gres[:])
            P.op("dve", "tensor_tensor", ["gres", "G5"], ["gres"], out=gres[:], in0=gres[:], in1=G5[:, :, :, 3], op=ALU.subtract)
            P.op("dve", "tensor_copy", ["gres", "G5"], ["G5"], out=G5[:, :, :, 4], in_=gres[:])
            iotaS = sb("iotaS", [128, CAPL])
            P.op("pool", "iota", [], ["iotaS"], iotaS[:], pattern=[[1, CAPL]], base=0, channel_multiplier=0, allow_small_or_imprecise_dtypes=True)
            dmy = sb("dmy", [128, NCHK])
            P.op("pool", "iota", [], ["dmy"], dmy[:], pattern=[[128, NCHK]], base=LT, channel_multiplier=1, allow_small_or_imprecise_dtypes=True)
            oh = [sb("oh%d" % i, [128, CAPL], BF16) for i in range(2)]
            pI = [ps("pI%d" % i, [128, 64, 8]) for i in range(2)]
            rr_ = sb("rrF", [128, NCHK, 8])
            ia = sb("iaF", [128, NCHK])
            ii = sb("iiF", [128, NCHK], I32)
            gs = sb("gsF", [128, NCHK])
            ohq = 0
            for e_ in range(16):
                pb = e_ % 2
                first = True
                for t in range(NTT):
                    ob = ohq % 2
                    ohq += 1
                    P.op("dve", "tensor_scalar", ["iotaS", "posi"], ["oh%d" % ob], out=oh[ob][:], in0=iotaS[:], scalar1=posi[:, t, e_:e_ + 1],
                         scalar2=None, op0=ALU.is_equal)
                    for ch in range(NCHK):
                        P.add("pe", (lambda pb, ch, ob, t, e_, first, lastf: lambda e: e.matmul(
                            pI[pb][:, ch, :], lhsT=oh[ob][:, ch * 128:(ch + 1) * 128], rhs=G5[:, t, e_, :], start=first, stop=lastf,
                            skip_group_check=True))(pb, ch, ob, t, e_, first, (t == NTT - 1 and ch == NCHK - 1)),
                            reads=["oh%d" % ob, "G5"], writes=["pI%d" % pb], banks=["pI%d" % pb])
                        first = False
                P.op("dve", "tensor_copy", ["pI%d" % pb], ["rrF"], out=rr_[:], in_=pI[pb][:, 0:NCHK, :])
                P.op("dve", "tensor_tensor", ["rrF"], ["iaF"], out=ia[:], in0=rr_[:, :, 0], in1=rr_[:, :, 1], op=ALU.add)
                P.op("dve", "tensor_tensor", ["iaF", "dmy"], ["iaF"], out=ia[:], in0=ia[:], in1=dmy[:], op=ALU.subtract)
                P.op("dve", "tensor_tensor", ["iaF", "rrF"], ["iaF"], out=ia[:], in0=ia[:], in1=rr_[:, :, 5], op=ALU.mult)
                P.op("dve", "tensor_tensor", ["iaF", "dmy"], ["iaF"], out=ia[:], in0=ia[:], in1=dmy[:], op=ALU.add)
                P.op("dve", "tensor_copy", ["iaF"], ["iiF"], out=ii[:], in_=ia[:])
                P.op("dve", "tensor_tensor", ["rrF"], ["gsF"], out=gs[:], in0=rr_[:, :, 2], in1=rr_[:, :, 3], op=ALU.add)
                P.op("dve", "tensor_tensor", ["rrF", "gsF"], ["gsF"], out=gs[:], in0=gs[:], in1=rr_[:, :, 4], op=ALU.add)
                P.dma("sp", idx_d[e_], ii[:], ["iiF"], ["idx_d"])
                P.dma("sp", cnt_out[e_], ii[:], ["iiF"], ["cnt_out"])
                P.dma("sp", gate_d[e_], gs[:], ["gsF"], ["gate_d"])
        P.emit()

        with ExitStack() as st:
            sb = lambda n, s, d=F32: st.enter_context(nc.sbuf_tensor(n, list(s), d))
            ps = lambda n, s, d=F32: st.enter_context(nc.psum_tensor(n, list(s), d))
            ident = sb("identX", [128, 128], BF16)
            P.op("pool", "memset", [], ["identX"], ident[:], 1.0)
            P.op("pool", "affine_select", ["identX"], ["identX"], out=ident[:], in_=ident[:], pattern=[[-1, 128]],
                 compare_op=ALU.is_equal, fill=0.0, base=0, channel_multiplier=1)
            idxt = [sb("idxt%d" % i, [128, NCHK], I32) for i in range(2)]
            gtt = [sb("gtt%d" % i, [128, NCHK]) for i in range(2)]
            xe = [sb("xe%d" % i, [128, 2048], BF16) for i in range(2)]
            xeT = sb("xeT", [128, 16, CAPL], BF16)
            heT = sb("heT", [128, 16, CAPL], BF16)
            stw = sb("stw", [128, 16, 256])
            wb_ = [sb("wbX%d" % i, [128, 16, 256], BF16) for i in range(4)]
            sgl = [sb("sglX%d" % i, [128, 512]) for i in range(2)]
            yst = [sb("ystX%d" % i, [128, 512], BF16) for i in range(3)]
            wbd = [sb("wbdX%d" % i, [128, 16, 512], BF16) for i in range(2)]
            ptr = [ps("ptrX%d" % i, [128, 1024], BF16) for i in range(2)]
            pg = [ps("pgX%d" % i, [128, 512]) for i in range(2)]
            pu = [ps("puX%d" % i, [128, 512]) for i in range(2)]
            py = [ps("pyX%d" % i, [128, 512]) for i in range(2)]
            cgs = [(c0, min(c0 + 512, CAPL)) for c0 in range(0, CAPL, 512)]
            cnt = {"tq": 0, "wq": 0, "gq": 0, "yq": 0, "sq": 0, "ysq": 0, "cast": 0, "xq": 0, "wdq": 0}

            def nx(k_, n):
                v = cnt[k_] % n
                cnt[k_] += 1
                return v

            def load_w(src_ap, name):
                wi = nx("wq", 4)
                P.dma("sp", stw[:], src_ap, [], ["stw"])
                ce = ("act", "dve")[nx("cast", 2)]
                if ce == "act":
                    P.op("act", "activation", ["stw"], ["wbX%d" % wi], out=wb_[wi][:], in_=stw[:], func=AF.Copy)
                else:
                    P.op("dve", "tensor_copy", ["stw"], ["wbX%d" % wi], out=wb_[wi][:], in_=stw[:])
                return wi

            prev_sc = {}
            for e_ in range(cfg.get("nexp", 16)):
                ib = e_ % 2
                P.dma("sp", idxt[ib][:], idx_d[e_], ["idx_d"], ["idxt%d" % ib])
                P.dma("sp", gtt[ib][:], gate_d[e_], ["gate_d"], ["gtt%d" % ib])
                for ch in range(NCHK):
                    xb = nx("xq", 2)
                    P.add("pool", (lambda xb, ib, ch: lambda e: e.indirect_dma_start(
                        out=xe[xb][:], out_offset=None, in_=hn3_bf[:, :],
                        in_offset=bass.IndirectOffsetOnAxis(ap=idxt[ib][:, ch:ch + 1], axis=0)))(xb, ib, ch),
                        reads=["idxt%d" % ib, "hn3_bf"], writes=["xe%d" % xb], dma=True)
                    for q4 in range(4):
                        pb = nx("tq", 2)
                        for j in range(4):
                            kc = q4 * 4 + j
                            P.tr(ptr[pb][:, j * 128:(j + 1) * 128], xe[xb][:, kc * 128:(kc + 1) * 128], ident[:], ["xe%d" % xb, "identX"], ["ptrX%d" % pb])
                        o_ap = xeT[:, q4 * 4:(q4 + 1) * 4, ch * 128:(ch + 1) * 128]
                        i_ap = ptr[pb][:, 0:512].rearrange("p (j n) -> p j n", n=128)
                        if q4 % 2 == 0:
                            P.op("act", "activation", ["ptrX%d" % pb], ["xeT"], out=o_ap, in_=i_ap, func=AF.Copy)
                        else:
                            P.op("dve", "tensor_copy", ["ptrX%d" % pb], ["xeT"], out=o_ap, in_=i_ap)
                for fg in range(8):
                    wg_i = load_w(w_gate[e_].rearrange("(kc p) f -> p kc f", p=128)[:, :, fg * 256:(fg + 1) * 256], "g")
                    wu_i = load_w(w_up[e_].rearrange("(kc p) f -> p kc f", p=128)[:, :, fg * 256:(fg + 1) * 256], "u")
                    for fc2 in range(2):
                        for (c0, c1) in cgs:
                            gb = nx("gq", 2)
                            for kc in range(16):
                                P.mm(pg[gb][:, 0:c1 - c0], wb_[wg_i][:, kc, fc2 * 128:(fc2 + 1) * 128], xeT[:, kc, c0:c1], kc == 0, kc == 15,
                                     ["wbX%d" % wg_i, "xeT"], ["pgX%d" % gb])
                            for kc in range(16):
                                P.mm(pu[gb][:, 0:c1 - c0], wb_[wu_i][:, kc, fc2 * 128:(fc2 + 1) * 128], xeT[:, kc, c0:c1], kc == 0, kc == 15,
                                     ["wbX%d" % wu_i, "xeT"], ["puX%d" % gb])
                            sb_i = nx("sq", 2)
                            P.op("act", "activation", ["pgX%d" % gb], ["sglX%d" % sb_i], out=sgl[sb_i][:, 0:c1 - c0], in_=pg[gb][:, 0:c1 - c0], func=AF.Silu)
                            P.op("dve", "tensor_tensor", ["sglX%d" % sb_i, "puX%d" % gb], ["heT"], out=heT[:, fg * 2 + fc2, c0:c1], in0=sgl[sb_i][:, 0:c1 - c0],
                                 in1=pu[gb][:, 0:c1 - c0], op=ALU.mult)
                new_sc = {}
                for dg in range(4):
                    wdi = nx("wdq", 2)
                    for hh in range(2):
                        c0_ = dg * 512 + hh * 256
                        P.dma("sp", stw[:], w_down[e_].rearrange("(kc p) f -> p kc f", p=128)[:, :, c0_:c0_ + 256], [], ["stw"])
                        if nx("cast", 2) == 0:
                            P.op("act", "activation", ["stw"], ["wbdX%d" % wdi], out=wbd[wdi][:, :, hh * 256:(hh + 1) * 256], in_=stw[:], func=AF.Copy)
                        else:
                            P.op("dve", "tensor_copy", ["stw"], ["wbdX%d" % wdi], out=wbd[wdi][:, :, hh * 256:(hh + 1) * 256], in_=stw[:])
                    for st_ in range(NCHK):
                        yb = nx("yq", 2)
                        for fc in range(16):
                            P.mm(py[yb][:], heT[:, fc, st_ * 128:(st_ + 1) * 128], wbd[wdi][:, fc, :], fc == 0, fc == 15,
                                 ["heT", "wbdX%d" % wdi], ["pyX%d" % yb])
                        ys = nx("ysq", 3)
                        P.op("dve", "tensor_scalar", ["pyX%d" % yb, "gtt%d" % ib], ["ystX%d" % ys], out=yst[ys][:], in0=py[yb][:],
                             scalar1=gtt[ib][:, st_:st_ + 1], scalar2=None, op0=ALU.mult)
                        nm_ = "sc_%d_%d_%d" % (e_, dg, st_)
                        new_sc.setdefault(dg, []).append(nm_)
                        P.add("pool", (lambda ys, ib, st_, dg: lambda e: e.indirect_dma_start(
                            out=acc_l[dg][:, :], out_offset=bass.IndirectOffsetOnAxis(ap=idxt[ib][:, st_:st_ + 1], axis=0),
                            in_=yst[ys][:], in_offset=None, compute_op=ALU.add))(ys, ib, st_, dg),
                            reads=["ystX%d" % ys, "idxt%d" % ib] + prev_sc.get(dg, []), writes=[nm_], dma=True)
                prev_sc = new_sc
        P.emit()

        with ExitStack() as st:
            sb = lambda n, s, d=F32: st.enter_context(nc.sbuf_tensor(n, list(s), d))
            gfin = sb("gfin", [128, 2048])
            P.dma("sp", gfin[:], nfin_rep, [], ["gfin"])
            xa = [sb("xaG%d" % i, [128, 4, 512]) for i in range(2)]
            junk = sb("junkG", [128, 2048], BF16)
            ss = [sb("ssG%d" % i, [128, 1]) for i in range(2)]
            yo_ = [sb("yoG%d" % i, [128, 2048]) for i in range(2)]
            for t in range(NTT):
                b = t % 2
                r0 = t * 128
                for g_ in range(4):
                    P.dma("sp", xa[b][:, g_, :], acc_l[g_][r0:r0 + 128, :], [], ["xaG%d" % b])
                xf = xa[b][:].rearrange("p g c -> p (g c)")
                P.op("act", "activation", ["xaG%d" % b], ["junkG", "ssG%d" % b], out=junk[:], in_=xf, func=AF.Square, accum_out=ss[b][:])
                P.op("dve", "tensor_scalar", ["ssG%d" % b], ["ssG%d" % b], out=ss[b][:], in0=ss[b][:], scalar1=1.0 / D, scalar2=EPS, op0=ALU.mult, op1=ALU.add)
                P.op("act", "activation", ["ssG%d" % b], ["ssG%d" % b], out=ss[b][:], in_=ss[b][:], func=AF.Sqrt)
                P.op("dve", "reciprocal", ["ssG%d" % b], ["ssG%d" % b], out=ss[b][:], in_=ss[b][:])
                P.op("dve", "scalar_tensor_tensor", ["xaG%d" % b, "ssG%d" % b, "gfin"], ["yoG%d" % b], out=yo_[b][:], in0=xf, scalar=ss[b][:, 0:1],
                     in1=gfin[:], op0=ALU.mult, op1=ALU.mult)
                P.dma("act", y_out[r0:r0 + 128, :], yo_[b][:], ["yoG%d" % b], ["y_out%d" % (t % 4)])
        P.emit()

        if dbg and upto not in ("all",):
            for nm_, ap_ in dbg.items():
                src = {"o_xbcT": xbcT, "o_uT": uT, "o_z": z_tok, "o_dt": dt_tok, "o_yssd": yssd, "o_yf": yf, "o_y5gT": y5gT, "o_y5nT": y5nT, "o_kT": kT_d, "o_v": v_d, "o_acc": acc_l[0], "o_probs": probs_loc, "o_hn3": hn3_bf, "o_idx": idx_d, "o_gate": gate_d}[nm_]
                P.dma("sp", ap_, src, [], [nm_])
            P.emit()
    return nc


def _rep(v):
    v = np.asarray(v, np.float32)
    return np.ascontiguousarray(np.broadcast_to(v.reshape(1, -1), (128, v.size)))


def _colpk(v):
    v = np.asarray(v, np.float32)
    return np.ascontiguousarray(v.reshape(-1, 128).T)


def _prep_weights(W):
    o = {}
    o["w_in"] = np.ascontiguousarray(W["w_in"][0])
    o["norm_mix"] = _colpk(W["norm_mix"][0])
    o["conv_w"] = np.ascontiguousarray(W["conv_w"][0].reshape(3, 16, 128).transpose(2, 1, 0))
    o["conv_b"] = _colpk(W["conv_b"][0])
    o["dtb_rep"] = _rep(W["ssd_dt_bias"][0])
    o["alog_rep"] = _rep(W["ssd_a_log"][0])
    o["ssdd_rep"] = _rep(W["ssd_d"][0])
    o["ssdn_rep"] = _rep(W["ssd_norm"][0])

    def gp(a):
        a = a.reshape(2, 8, 8, 64).transpose(0, 1, 3, 2)
        return np.concatenate([a, a], axis=2)
    are = gp(W["s5_a_re"][0])
    aim = gp(W["s5_a_im"][0])
    lst = gp(np.broadcast_to(W["s5_log_step"][0][:, :, None], (2, 64, 64)))
    o["s5_small"] = np.ascontiguousarray(np.stack([are, aim, lst], axis=3)).astype(np.float32)

    def bb(a):
        a = a.reshape(2, 8, 8, 64, 16).transpose(0, 1, 3, 2, 4).reshape(2, 8, 64, 128)
        return np.concatenate([a, a], axis=2)

    def cc(a):
        a = a.reshape(2, 8, 8, 16, 64).transpose(0, 1, 4, 2, 3).reshape(2, 8, 64, 128)
        return np.concatenate([a, a], axis=2)
    o["s5_big"] = np.ascontiguousarray(np.stack([bb(W["s5_b_re"][0]), bb(W["s5_b_im"][0]), cc(W["s5_c_re"][0]),
                                                 cc(W["s5_c_im"][0])], axis=3)).astype(np.float32)
    o["s5_dcol"] = _colpk(W["s5_d"][0])
    o["s5_ncol"] = _colpk(W["s5_norm"][0])
    o["norm_attn"] = _colpk(W["norm_attn"][0])
    o["norm_mem"] = _colpk(W["norm_mem"][0])
    for k_ in ("w_out", "w_q", "w_k", "w_v", "w_o"):
        o[k_] = np.ascontiguousarray(W[k_][0])
    o["nffn_rep"] = _rep(W["norm_ffn"][0])
    o["w_router_l"] = np.ascontiguousarray(W["w_router"][0].reshape(16, 128, 16).transpose(1, 0, 2))
    o["nfin_rep"] = _rep(W["norm_final"])
    o["w_glu"] = np.ascontiguousarray(W["s5_w_glu"][0])
    o["w_gate"] = np.ascontiguousarray(W["w_gate"][0])
    o["w_up"] = np.ascontiguousarray(W["w_up"][0])
    o["w_down"] = np.ascontiguousarray(W["w_down"][0])
    return o


_NC_CACHE = {}


def kernel(**inputs):
    W = {k_: np.asarray(v, np.float32) for k_, v in inputs.items()}
    xp, xs = W.pop("x_prompt"), W.pop("x_sample")
    mp, ms = W.pop("mem_prompt"), W.pop("mem_sample")
    LSEG = 4096
    cfg = {"LSEG": LSEG, "upto": "all", "CAPL": 1408, "GSZ": 4, "groups": [[0, 1, 2, 3], [4, 5, 6, 7]]}
    if "nc" not in _NC_CACHE:
        _NC_CACHE["nc"] = build(cfg)
    nc = _NC_CACHE["nc"]
    wl = _prep_weights(W)
    in_maps = []
    for c in range(8):
        m = dict(wl)
        if c < 4:
            m["x"] = np.ascontiguousarray(xp[c])
            m["mem"] = np.ascontiguousarray(np.stack([mp[c], mp[c]]))
            m["flag"] = np.ones((128, 1), np.float32)
        else:
            j = c - 4
            m["x"] = np.ascontiguousarray(xs[2 * j:2 * j + 2].reshape(2 * LSEG, D))
            m["mem"] = np.ascontiguousarray(ms[2 * j:2 * j + 2])
            m["flag"] = np.zeros((128, 1), np.float32)
        in_maps.append(m)
    res = run_bass_kernel_spmd(nc, in_maps, core_ids=list(range(8)))
    ys = [np.asarray(r["y"], np.float32) for r in res.results]
    try:
        import sys
        cn = np.array([[(np.asarray(r["slot_idx"])[e] < 2 * LSEG).sum() for e in range(16)] for r in res.results])
        print("[kernel] per-(core,expert) slot counts: mean %.1f std %.1f max %d (cap %d)" % (cn.mean(), cn.std(), cn.max(), cfg["CAPL"]), file=sys.stderr)
    except Exception:
        pass
    y_prompt = np.stack(ys[0:4]).reshape(4, 8192, D)
    y_sample = np.concatenate([y.reshape(2, LSEG, D) for y in ys[4:8]], axis=0)
    return (y_prompt, y_sample)
```
